# Optimizing a Trainium2 kernel written in Bass

```python
import math
import jax
import jax.numpy as jnp
from jax import lax
import numpy as np

D_MODEL = 2048
BATCH = 4
SEQ = 4096
DEPTH = 1

GRID_W = 64
CTX_LEN = 256
D_SSM = 2048
SSM_HEAD_DIM = 64
SSM_HEADS = D_SSM // SSM_HEAD_DIM
SSM_GROUPS = 4
SSM_HPG = SSM_HEADS // SSM_GROUPS
SSM_STATE = 128
SSM_CONV = 3
SSD_CHUNK = 128
D_XBC = D_SSM + 2 * SSM_GROUPS * SSM_STATE
D_SSM_IN = D_SSM + D_XBC + 2 * SSM_HEADS
D_SC = 2048
SC_CONV = 3
D_MIX = D_SSM + D_SC
D_IN_PROJ = D_SSM_IN + 3 * D_SC
N_EXPERTS = 16
EC_CAPACITY_FACTOR = 2
EXPERT_FF = 1024
N_MOD = 6
EPS = 1e-6

kernel_name = 'hybrid_ssd_shortconv_ec_dit_layer'


def rmsnorm(x, w):
    xf = x.astype(jnp.float32)
    xf = xf * lax.rsqrt(jnp.mean(xf * xf, axis=-1, keepdims=True) + EPS)
    return (xf * w.astype(jnp.float32)).astype(x.dtype)


def group_rmsnorm(y, w, groups):
    shp = y.shape
    yf = y.astype(jnp.float32).reshape(*shp[:-1], groups, shp[-1] // groups)
    yf = yf * lax.rsqrt(jnp.mean(yf * yf, axis=-1, keepdims=True) + EPS)
    return yf.reshape(shp) * w.astype(jnp.float32)


def modulate(h, shift, scale):
    return h * (1 + scale) + shift


def _dwconv(u, w, axis):
    k = w.shape[0]
    half = k // 2
    n = u.shape[axis]
    pad = [(0, 0)] * u.ndim
    pad[axis] = (half, half)
    up = jnp.pad(u, pad)
    return sum(lax.slice_in_dim(up, j, j + n, axis=axis) * w[j] for j in range(k))


def dwconv_ctx(u, w):
    return _dwconv(u, w, axis=1)


def dwconv_grid(u, w, rows):
    b, n, ch = u.shape
    g = u.reshape(b, rows, GRID_W, ch)
    return _dwconv(g, w, axis=2).reshape(b, n, ch)


def segsum(a):
    t = a.shape[-1]
    cs = jnp.cumsum(a, axis=-1)
    diff = cs[..., :, None] - cs[..., None, :]
    mask = jnp.tril(jnp.ones((t, t), dtype=bool))
    return jnp.where(mask, diff, -jnp.inf)


def ssd_chunked(xs, dt, a_neg, bm, cm, init):
    b, l, g, r, p = xs.shape
    n = bm.shape[-1]
    q = SSD_CHUNK
    nc = l // q
    xdt = (xs * dt[..., None]).reshape(b, nc, q, g, r, p)
    la = jnp.moveaxis((dt * a_neg).reshape(b, nc, q, g, r), 2, -1)
    la_cum = jnp.cumsum(la, axis=-1)
    bc = bm.reshape(b, nc, q, g, n)
    cc = cm.reshape(b, nc, q, g, n)
    decay_in = jnp.exp(segsum(la))
    cb = jnp.einsum('bcign,bcjgn->bcgij', cc, bc)
    y_diag = jnp.einsum('bcgij,bcgrij,bcjgrp->bcigrp', cb, decay_in, xdt)
    decay_to_end = jnp.exp(la_cum[..., -1:] - la_cum)
    chunk_states = jnp.einsum('bcjgn,bcgrj,bcjgrp->bcgrpn', bc, decay_to_end, xdt)
    states = jnp.concatenate([init[:, None], chunk_states], axis=1)
    chunk_decay = jnp.pad(la_cum[..., -1], ((0, 0), (1, 0), (0, 0), (0, 0)))
    decay_chunks = jnp.exp(segsum(jnp.moveaxis(chunk_decay, 1, -1)))
    states = jnp.einsum('bgrzc,bcgrpn->bzgrpn', decay_chunks, states)
    y_off = jnp.einsum('bcign,bcgrpn,bcgri->bcigrp', cc, states[:, :-1], jnp.exp(la_cum))
    y = (y_diag + y_off).reshape(b, l, g, r, p)
    return y, states[:, -1]


def scan_direction(xs, dt, a_neg, bm, cm, n_ctx, reverse):
    def orient(t):
        return jnp.flip(t, axis=1) if reverse else t
    b = xs.shape[0]
    state = jnp.zeros((b, SSM_GROUPS, SSM_HPG, SSM_HEAD_DIM, SSM_STATE), jnp.float32)
    parts = []
    for seg in (slice(0, n_ctx), slice(n_ctx, None)):
        y, state = ssd_chunked(orient(xs[:, seg]), orient(dt[:, seg]), a_neg,
                               orient(bm[:, seg]), orient(cm[:, seg]), state)
        parts.append(orient(y))
    return jnp.concatenate(parts, axis=1)


def ssd_branch(u, n_ctx, rows, conv_w, conv_b, a_log, dt_bias, d_skip, norm_w):
    dtype = u.dtype
    z = u[..., :D_SSM]
    xbc = u[..., D_SSM:D_SSM + D_XBC]
    dt_raw = u[..., D_SSM + D_XBC:]
    xbc = jnp.concatenate([dwconv_ctx(xbc[:, :n_ctx], conv_w),
                           dwconv_grid(xbc[:, n_ctx:], conv_w, rows)], axis=1)
    xbc = jax.nn.silu(xbc + conv_b).astype(jnp.float32)
    b, l, _ = xbc.shape
    gn = SSM_GROUPS * SSM_STATE
    xs = xbc[..., :D_SSM].reshape(b, l, SSM_GROUPS, SSM_HPG, SSM_HEAD_DIM)
    bm = xbc[..., D_SSM:D_SSM + gn].reshape(b, l, SSM_GROUPS, SSM_STATE)
    cm = xbc[..., D_SSM + gn:].reshape(b, l, SSM_GROUPS, SSM_STATE)
    dt = jax.nn.softplus(dt_raw.astype(jnp.float32).reshape(b, l, 2, SSM_GROUPS, SSM_HPG)
                         + dt_bias.astype(jnp.float32).reshape(2, SSM_GROUPS, SSM_HPG))
    a_neg = -jnp.exp(a_log.astype(jnp.float32)).reshape(2, SSM_GROUPS, SSM_HPG)
    y = d_skip.astype(jnp.float32).reshape(SSM_GROUPS, SSM_HPG, 1) * xs
    for direction, reverse in ((0, False), (1, True)):
        y = y + scan_direction(xs, dt[:, :, direction], a_neg[direction], bm, cm, n_ctx, reverse)
    y = y.reshape(b, l, D_SSM) * jax.nn.silu(z.astype(jnp.float32))
    return group_rmsnorm(y, norm_w, SSM_GROUPS).astype(dtype)


def short_conv_branch(u, n_ctx, rows, conv_w, norm_w):
    bg, cg, hv = jnp.split(u, 3, axis=-1)
    v = cg * hv
    v = jnp.concatenate([dwconv_ctx(v[:, :n_ctx], conv_w),
                         dwconv_grid(v[:, n_ctx:], conv_w, rows)], axis=1)
    return rmsnorm(bg * v, norm_w)


def expert_choice_ffn(h, w_router, w_gate, w_up, w_down):
    b, n, d = h.shape
    cap = EC_CAPACITY_FACTOR * n // N_EXPERTS
    aff = jax.nn.softmax(jnp.einsum('bnd,de->bne', h, w_router).astype(jnp.float32), axis=-1)
    gates, idx = lax.top_k(jnp.swapaxes(aff, 1, 2), cap)
    xe = jax.vmap(lambda hb, ib: hb[ib])(h, idx)
    hid = jax.nn.silu(jnp.einsum('becd,edf->becf', xe, w_gate)) * jnp.einsum('becd,edf->becf', xe, w_up)
    ye = jnp.einsum('becf,efd->becd', hid, w_down) * gates[..., None].astype(h.dtype)
    def combine(yb, ib):
        return jnp.zeros((n, d), h.dtype).at[ib.reshape(-1)].add(yb.reshape(-1, d))
    return jax.vmap(combine)(ye, idx)


def setup_inputs(seed: int = 0) -> dict:
    key = jax.random.key(seed)
    ks = jax.random.split(key, 24)
    f32 = jnp.float32
    L = DEPTH

    def nrm(k, shape, s):
        return s * jax.random.normal(k, shape, f32)

    def gain(k, shape):
        return 1.0 + 0.05 * jax.random.normal(k, shape, f32)

    dt0 = jnp.exp(jax.random.uniform(ks[11], (L, 2 * SSM_HEADS), f32, math.log(1e-3), math.log(1e-1)))
    return {
        'x': jax.random.normal(ks[0], (BATCH, SEQ, D_MODEL), f32),
        'c': jax.random.normal(ks[1], (BATCH, D_MODEL), f32),
        'ctx': jax.random.normal(ks[2], (BATCH, CTX_LEN, D_MODEL), f32),
        'c_ctx': jax.random.normal(ks[3], (D_MODEL,), f32),
        'w_mod': nrm(ks[4], (L, D_MODEL, N_MOD * D_MODEL), 0.5 * D_MODEL ** -0.5),
        'b_mod': nrm(ks[5], (L, N_MOD * D_MODEL), 0.02),
        'norm_mix_w': gain(ks[6], (L, D_MODEL)),
        'w_in': nrm(ks[7], (L, D_MODEL, D_IN_PROJ), D_MODEL ** -0.5),
        'ssm_conv_w': nrm(ks[8], (L, SSM_CONV, D_XBC), SSM_CONV ** -0.5),
        'ssm_conv_b': nrm(ks[9], (L, D_XBC), 0.02),
        'ssm_a_log': jnp.log(jax.random.uniform(ks[10], (L, 2 * SSM_HEADS), f32, 1.0, 16.0)),
        'ssm_dt_bias': dt0 + jnp.log(-jnp.expm1(-dt0)),
        'ssm_d': gain(ks[12], (L, SSM_HEADS)),
        'ssm_norm_w': gain(ks[13], (L, D_SSM)),
        'sc_conv_w': nrm(ks[14], (L, SC_CONV, D_SC), SC_CONV ** -0.5),
        'sc_norm_w': gain(ks[15], (L, D_SC)),
        'w_out': nrm(ks[16], (L, D_MIX, D_MODEL), D_MIX ** -0.5),
        'norm_ffn_w': gain(ks[17], (L, D_MODEL)),
        'w_router': nrm(ks[18], (L, D_MODEL, N_EXPERTS), D_MODEL ** -0.5),
        'w_gate': nrm(ks[19], (L, N_EXPERTS, D_MODEL, EXPERT_FF), D_MODEL ** -0.5),
        'w_up': nrm(ks[20], (L, N_EXPERTS, D_MODEL, EXPERT_FF), D_MODEL ** -0.5),
        'w_down': nrm(ks[21], (L, N_EXPERTS, EXPERT_FF, D_MODEL), EXPERT_FF ** -0.5),
        'final_norm_w': gain(ks[22], (D_MODEL,)),
    }


def reference(x, c, ctx, c_ctx, w_mod, b_mod, norm_mix_w, w_in, ssm_conv_w, ssm_conv_b, ssm_a_log,
              ssm_dt_bias, ssm_d, ssm_norm_w, sc_conv_w, sc_norm_w, w_out, norm_ffn_w, w_router,
              w_gate, w_up, w_down, final_norm_w):
    n_ctx = ctx.shape[1]
    rows = x.shape[1] // GRID_W
    h_lat, h_ctx = x, ctx
    for layer in range(DEPTH):
        mod_lat = jnp.einsum('bd,de->be', jax.nn.silu(c), w_mod[layer]) + b_mod[layer]
        mod_ctx = jax.nn.silu(c_ctx) @ w_mod[layer] + b_mod[layer]
        sh1_l, sc1_l, g1_l, sh2_l, sc2_l, g2_l = jnp.split(mod_lat[:, None, :], N_MOD, axis=-1)
        sh1_c, sc1_c, g1_c, sh2_c, sc2_c, g2_c = jnp.split(mod_ctx[None, None, :], N_MOD, axis=-1)

        a_ctx = modulate(rmsnorm(h_ctx, norm_mix_w[layer]), sh1_c, sc1_c)
        a_lat = modulate(rmsnorm(h_lat, norm_mix_w[layer]), sh1_l, sc1_l)
        u = jnp.einsum('bld,de->ble', jnp.concatenate([a_ctx, a_lat], axis=1), w_in[layer])
        y_ssm = ssd_branch(u[..., :D_SSM_IN], n_ctx, rows, ssm_conv_w[layer], ssm_conv_b[layer],
                           ssm_a_log[layer], ssm_dt_bias[layer], ssm_d[layer], ssm_norm_w[layer])
        y_sc = short_conv_branch(u[..., D_SSM_IN:], n_ctx, rows, sc_conv_w[layer], sc_norm_w[layer])
        mix = jnp.einsum('ble,ed->bld', jnp.concatenate([y_ssm, y_sc], axis=-1), w_out[layer])
        h_lat = h_lat + g1_l * mix[:, n_ctx:]

        if layer + 1 < DEPTH:
            h_ctx = h_ctx + g1_c * mix[:, :n_ctx]
            f_ctx = modulate(rmsnorm(h_ctx, norm_ffn_w[layer]), sh2_c, sc2_c)
            h_ctx = h_ctx + g2_c * expert_choice_ffn(f_ctx, w_router[layer], w_gate[layer],
                                                     w_up[layer], w_down[layer])

        f_lat = modulate(rmsnorm(h_lat, norm_ffn_w[layer]), sh2_l, sc2_l)
        h_lat = h_lat + g2_l * expert_choice_ffn(f_lat, w_router[layer], w_gate[layer],
                                                 w_up[layer], w_down[layer])
    return rmsnorm(h_lat, final_norm_w)
```

```python
import numpy as np
from contextlib import ExitStack
import concourse.bass as bass
import concourse.mybir as mybir
from concourse.bass_utils import run_bass_kernel_spmd

F32 = mybir.dt.float32
BF16 = mybir.dt.bfloat16
I32 = mybir.dt.int32
U32 = mybir.dt.uint32
U8 = mybir.dt.uint8
AF = mybir.ActivationFunctionType
ALU = mybir.AluOpType

ENGS = ("pe", "act", "dve", "pool", "sp")
DEBUG = False
LIMIT = 99
STOPAT = None
MARKS = {}
NG = 4
SCAN = 2
NCORES = 4
EPS = 1e-6
NT = 34
NTL = 32
NEG = -30000.0


class Dep:
    __slots__ = ("w", "r", "excl")

    def __init__(self, excl=False):
        self.w = None
        self.r = []
        self.excl = excl


class Instr:
    __slots__ = ("eng", "fn", "dma", "deps", "sig", "sem", "val")

    def __init__(self, eng, fn, dma):
        self.eng = eng
        self.fn = fn
        self.dma = dma
        self.deps = set()
        self.sig = False
        self.sem = None
        self.val = 0


def _nop_fn(eng):
    return eng.nop()


class Prog:
    def __init__(self, nc, n_dma_sems=12):
        self.nc = nc
        self.lists = {e: [] for e in ENGS}
        self.last = {e: None for e in ENGS}
        self.n_dma_sems = n_dma_sems
        self.dma_count = {e: 0 for e in ENGS}
        self.dma_ring = {e: [None] * n_dma_sems for e in ENGS}

    def emit(self, eng, fn, reads=(), writes=(), dma=False, extra=()):
        ins = Instr(eng, fn, dma)
        self.count = getattr(self, "count", 0) + 1
        if STOPAT is not None and self.count > STOPAT and not getattr(self, "in_finish", False):
            return ins
        for d in reads:
            if d.w is not None:
                ins.deps.add(d.w)
            if d.excl:
                for r in d.r:
                    if r.eng != eng:
                        ins.deps.add(r)
            if not dma:
                d.r = [r for r in d.r if r.dma or r.eng != eng]
            d.r.append(ins)
        for d in writes:
            if d.w is not None:
                ins.deps.add(d.w)
            for r in d.r:
                ins.deps.add(r)
            d.w = ins
            d.r = []
        for p in extra:
            if p is not None:
                ins.deps.add(p)
        ins.deps.discard(ins)
        if dma:
            k = self.dma_count[eng]
            self.dma_count[eng] = k + 1
            slot = k % self.n_dma_sems
            prev = self.dma_ring[eng][slot]
            if prev is not None:
                ins.deps.add(prev)
            self.dma_ring[eng][slot] = ins
            ins.sem = (eng, slot)
        self.lists[eng].append(ins)
        self.last[eng] = ins
        return ins

    def barrier(self):
        ext = [self.last[e] for e in ENGS if self.last[e] is not None]
        for e in ENGS:
            for p in self.dma_ring[e]:
                if p is not None:
                    ext.append(p)
        b = self.emit("dve", lambda eng: eng.engine_nop(), extra=ext)
        for e in ENGS:
            if e != "dve":
                self.emit(e, _nop_fn, extra=[b])
        return b

    def finalize(self, stack):
        nc = self.nc
        for e in ENGS:
            for ins in self.lists[e]:
                for p in ins.deps:
                    if p.eng == "pe" and ins.eng == "pe" and not p.dma and not ins.dma:
                        continue
                    p.sig = True
        esem = {e: stack.enter_context(nc.semaphore("s_" + e)) for e in ENGS}
        dsem = {}
        for e in ENGS:
            for s in range(min(self.n_dma_sems, self.dma_count[e])):
                dsem[(e, s)] = stack.enter_context(nc.semaphore("d_%s_%d" % (e, s)))
        dcount = {k: 0 for k in dsem}
        for e in ENGS:
            c = 0
            for ins in self.lists[e]:
                if ins.dma:
                    dcount[ins.sem] += 16
                    ins.val = dcount[ins.sem]
                    ins.sig = True
                elif ins.sig:
                    c += 1
                    ins.val = c
                    ins.sem = e
        block = stack.enter_context(nc.Block())
        engobj = {"pe": "tensor", "act": "scalar", "dve": "vector", "pool": "gpsimd", "sp": "sync"}

        def make_body(e):
            def body(eng):
                known = {}
                for ins in self.lists[e]:
                    for p in ins.deps:
                        if p.eng == "pe" and e == "pe" and not p.dma and not ins.dma:
                            continue
                        if p.dma:
                            sem = dsem[p.sem]
                            key = ("d",) + p.sem
                        else:
                            sem = esem[p.eng]
                            key = ("e", p.eng)
                        if known.get(key, 0) >= p.val:
                            continue
                        known[key] = p.val
                        eng.wait_ge(sem, p.val)
                    r = ins.fn(eng)
                    if ins.dma:
                        r.then_inc(dsem[ins.sem], 16)
                    elif ins.sig:
                        r.then_inc(esem[e], 1)
            return body

        for e in ENGS:
            if self.lists[e]:
                getattr(block, engobj[e])(make_body(e))


class Arena:
    def __init__(self, ap_u8, nbytes):
        self.a = ap_u8
        self.n = nbytes
        self.top = 0

    def alloc(self, shape, dt):
        esz = {F32: 4, BF16: 2, I32: 4, U32: 4}[dt]
        free = 1
        for s in shape[1:]:
            free *= s
        nb = (free * esz + 63) // 64 * 64
        assert self.top + nb <= self.n, ("SBUF arena overflow", self.top, nb, self.n)
        v = self.a[0:shape[0], self.top:self.top + free * esz].bitcast(dt)
        self.top += nb
        if len(shape) == 3:
            v = v.rearrange("p (a b) -> p a b", b=shape[2])
        return v


class T:
    def __init__(self, ap, nsub=0):
        self.ap = ap
        self.d = Dep()
        self.ds = [Dep() for _ in range(nsub)]


def build_program():
    nc = bass.Bass("TRN2", target_bir_lowering=False)
    st = ExitStack()
    P = Prog(nc)

    def finish():
        P.in_finish = True
        P.barrier()
        P.finalize(st)
        st.close()
        return nc

    def din(name, shape, dt=F32):
        return nc.dram_tensor(name, list(shape), dt, kind="ExternalInput").ap()

    def dscr(name, shape, dt):
        return nc.dram_tensor(name, list(shape), dt, kind="ExternalOutput" if DEBUG else "Internal").ap()

    xin = din("xin", [NT, 128, 2048])
    cvec = din("cvec", [128, 16, 2])
    w_mod = din("w_mod", [128, 16, 12288])
    b_mod = din("b_mod", [1, 12288])
    nmix = din("nmix", [1, 2048])
    nffn = din("nffn", [1, 2048])
    nfin = din("nfin", [1, 2048])
    wg_ssm = din("wg_ssm", [4, 128, 16, 1296]) if LIMIT >= 2 else None
    wg_sc = din("wg_sc", [4, 128, 16, 1536]) if LIMIT >= 3 else None
    cw_ssm = din("cw_ssm", [4, 128, 6, 3])
    cb_ssm = din("cb_ssm", [4, 128, 6])
    cw_sc = din("cw_sc", [4, 128, 4, 3])
    scw = din("scw", [4, 128, 4])
    dtb = din("dtb", [4, 1, 16])
    alog = din("alog", [4, 1, 16])
    dsk = din("dsk", [4, 1, 512])
    ssm_nw = din("ssm_nw", [4, 1, 512])
    w_out = din("w_out", [2, 128, 32, 1024]) if LIMIT >= 4 else None
    w_rt = din("w_rt", [128, 16, 16])
    if LIMIT >= 6:
        w_gate = din("w_gate", [16, 2, 128, 16, 512])
        w_up = din("w_up", [16, 2, 128, 16, 512])
        w_down = din("w_down", [16, 2, 128, 8, 1024])
    c_ident = din("c_ident", [128, 128])
    c_tri = din("c_tri", [2, 128, 128])
    c_mask = din("c_mask", [2, 128, 512])
    out = nc.dram_tensor("out", [NTL, 128, 2048], F32, kind="ExternalOutput").ap()

    mod_d = T(dscr("mod_d", [2, 12288], F32))
    aT_d = T(dscr("aT_d", [9, 128, 16, 512], BF16), 9)
    z_d = T(dscr("z_d", [4, NTL, 128, 512], BF16), 4 * NTL)
    yT_d = T(dscr("yT_d", [NTL, 128, 32, 128], BF16), NTL)
    hacc_h = [T(dscr("hacc_d%d" % i, [NTL * 128, 1024], F32), NTL) for i in range(2)]
    f_d = T(dscr("f_d", [NTL * 128, 2048], BF16))
    dbg = {}
    if DEBUG:
        dbg["h1"] = nc.dram_tensor("dbg_h1", [NTL * 128, 2048], F32, kind="ExternalOutput").ap()
        dbg["aff"] = nc.dram_tensor("dbg_aff", [16, 4096], F32, kind="ExternalOutput").ap()
        dbg["vals"] = nc.dram_tensor("dbg_vals", [16, 512], F32, kind="ExternalOutput").ap()
        dbg["idx"] = nc.dram_tensor("dbg_idx", [16, 512], U32, kind="ExternalOutput").ap()
        dbg["rsc"] = nc.dram_tensor("dbg_rsc", [128, NTL], F32, kind="ExternalOutput").ap()
        dbg["dt"] = nc.dram_tensor("dbg_dt", [4, 128, NT, 16], F32, kind="ExternalOutput").ap()
        dbg["xs"] = nc.dram_tensor("dbg_xs", [4, 128, NT, 512], BF16, kind="ExternalOutput").ap()

    sb_bytes = (int(nc.sbuf_bytes_remaining) - 2048) // 256 * 256
    arena_t = st.enter_context(nc.sbuf_tensor("arena", [128, sb_bytes], U8))
    AR = Arena(arena_t[:, :], sb_bytes)
    banks = [st.enter_context(nc.psum_tensor("bank%d" % i, [128, 512], F32)) for i in range(8)]
    PS = [T(b[:, :]) for b in banks]
    for p_ in PS:
        p_.d.excl = True

    def psbf(i):
        return banks[i][:, :].bitcast(BF16)

    def MM(out, lhsT, rhs, start, stop, rd, wr):
        return P.emit("pe", lambda e: e.matmul(out, lhsT=lhsT, rhs=rhs, start=start, stop=stop), rd, wr)

    def TR(out, in_, ident, rd, wr):
        return P.emit("pe", lambda e: e.transpose(out, in_, ident), rd, wr)

    def ACT(out, in_, func, rd, wr, bias=None, scale=None, accum=None):
        kw = {}
        if bias is not None:
            kw["bias"] = bias
        if scale is not None:
            kw["scale"] = scale
        if accum is not None:
            kw["accum_out"] = accum
        return P.emit("act", lambda e: e.activation(out=out, in_=in_, func=func, **kw), rd, wr)

    def TT(eng, out, in0, in1, op, rd, wr):
        return P.emit(eng, lambda e: e.tensor_tensor(out=out, in0=in0, in1=in1, op=op), rd, wr)

    def TS(eng, out, in0, s1, s2, op0, op1, rd, wr):
        if s2 is None:
            return P.emit(eng, lambda e: e.tensor_scalar(out=out, in0=in0, scalar1=s1, scalar2=None, op0=op0), rd, wr)
        return P.emit(eng, lambda e: e.tensor_scalar(out=out, in0=in0, scalar1=s1, scalar2=s2, op0=op0, op1=op1), rd, wr)

    def STT(eng, out, in0, scalar, in1, op0, op1, rd, wr):
        return P.emit(eng, lambda e: e.scalar_tensor_tensor(out=out, in0=in0, scalar=scalar, in1=in1, op0=op0, op1=op1), rd, wr)

    def CP(eng, out, in_, rd, wr):
        if eng == "act":
            return P.emit("act", lambda e: e.copy(out=out, in_=in_), rd, wr)
        return P.emit(eng, lambda e: e.tensor_copy(out=out, in_=in_), rd, wr)

    def MEMSET(eng, ap, val, wr):
        return P.emit(eng, lambda e: e.memset(ap, val), (), wr)

    def RECIP(out, in_, rd, wr):
        return P.emit("dve", lambda e: e.reciprocal(out=out, in_=in_), rd, wr)

    def DMA(q, out, in_, rd, wr):
        return P.emit(q, lambda e: e.dma_start(out=out, in_=in_), rd, wr, dma=True)

    def rstd_from_ss(ss_ap, std_ap, r_ap, n, dss, dstd, dr):
        ACT(std_ap, ss_ap, AF.Sqrt, [dss], [dstd], bias=epsc.ap[0:ss_ap.shape[0], :], scale=1.0 / n)
        RECIP(r_ap, std_ap, [dstd], [dr])

    ident32 = T(AR.alloc([128, 128], F32))
    identbf = T(AR.alloc([128, 128], BF16))
    ones32 = T(AR.alloc([128, 128], F32))
    onesbf = T(AR.alloc([128, 8], BF16))
    epsc = T(AR.alloc([128, 1], F32))
    DMA("sp", ident32.ap, c_ident, [], [ident32.d])
    DMA("pool", identbf.ap, c_ident, [], [identbf.d])
    MEMSET("dve", ones32.ap, 1.0, [ones32.d])
    MEMSET("dve", onesbf.ap, 1.0, [onesbf.d])
    MEMSET("dve", epsc.ap, EPS, [epsc.d])
    persist_mark = AR.top

    cv = T(AR.alloc([128, 16, 2], F32))
    cs_ = T(AR.alloc([128, 16, 2], F32))
    DMA("sp", cv.ap, cvec, [], [cv.d])
    ACT(cs_.ap, cv.ap, AF.Silu, [cv.d], [cs_.d])
    wch = [T(AR.alloc([128, 16, 512], F32)) for _ in range(3)]
    bch = [T(AR.alloc([2, 512], F32)) for _ in range(2)]
    mch = [T(AR.alloc([2, 512], F32)) for _ in range(2)]
    for n in range(24):
        w = wch[n % 3]
        cols = slice(n * 512, (n + 1) * 512)
        DMA("sp", w.ap[:, 0:8, :], w_mod[:, 0:8, cols], [], [w.d])
        DMA("act", w.ap[:, 8:16, :], w_mod[:, 8:16, cols], [], [w.d])
        bb = bch[n % 2]
        DMA("sp", bb.ap, b_mod[0:1, cols].partition_broadcast(2), [], [bb.d])
        pb = PS[n % 2]
        for kc in range(16):
            MM(pb.ap[0:2, :], cs_.ap[:, kc, :], w.ap[:, kc, :], kc == 0, kc == 15, [cs_.d, w.d], [pb.d])
        m = mch[n % 2]
        TT("dve", m.ap, pb.ap[0:2, :], bb.ap, ALU.add, [pb.d, bb.d], [m.d])
        if n // 4 in (1, 4):
            TS("dve", m.ap, m.ap, 1.0, None, ALU.add, None, [m.d], [m.d])
        DMA("sp", mod_d.ap[:, cols], m.ap, [m.d], [mod_d.d])
    P.barrier()
    if LIMIT == 0:
        return finish()
    AR.top = persist_mark

    def load_mod_bc(dst, row, seg, q="sp"):
        DMA(q, dst.ap, mod_d.ap[row:row + 1, seg * 2048:(seg + 1) * 2048].partition_broadcast(128), [mod_d.d], [dst.d])

    def load_vec_bc(dst, src, q="sp"):
        DMA(q, dst.ap, src.partition_broadcast(128), [], [dst.d])

    A1 = [T(AR.alloc([128, 2048], F32)) for _ in range(2)]
    B1 = [T(AR.alloc([128, 2048], F32)) for _ in range(2)]
    nmb = T(AR.alloc([128, 2048], F32))
    load_vec_bc(nmb, nmix)
    for r in range(2):
        load_mod_bc(A1[r], r, 1)
        load_mod_bc(B1[r], r, 0)
        TT("pool", A1[r].ap, A1[r].ap, nmb.ap, ALU.mult, [A1[r].d, nmb.d], [A1[r].d])
    xt = [T(AR.alloc([128, 2048], F32)) for _ in range(2)]
    tmp = [T(AR.alloc([128, 2048], F32)) for _ in range(2)]
    abf = [T(AR.alloc([128, 2048], BF16)) for _ in range(2)]
    junk = T(AR.alloc([128, 2048], BF16))
    ss1 = T(AR.alloc([128, NT], F32), NT)
    sd1 = T(AR.alloc([128, NT], F32), NT)
    rs1 = T(AR.alloc([128, NT], F32), NT)
    aTb = [T(AR.alloc([128, 16, 512], BF16)) for _ in range(2)]
    MEMSET("dve", ss1.ap, 0.0, ss1.ds)
    for tb in range(9):
        tiles = [0, 1] if tb == 0 else [2 + 4 * (tb - 1) + i for i in range(4)]
        ab = aTb[tb % 2]
        for j, t in enumerate(tiles):
            r = 1 if t < 2 else 0
            x_ = xt[t % 2]
            DMA("sp", x_.ap, xin[t], [], [x_.d])
            ACT(junk.ap, x_.ap, AF.Square, [x_.d], [junk.d, ss1.ds[t]], accum=ss1.ap[:, t:t + 1])
            rstd_from_ss(ss1.ap[:, t:t + 1], sd1.ap[:, t:t + 1], rs1.ap[:, t:t + 1], 2048.0, ss1.ds[t], sd1.ds[t], rs1.ds[t])
            tm = tmp[t % 2]
            STT("dve", tm.ap, x_.ap, rs1.ap[:, t:t + 1], A1[r].ap, ALU.mult, ALU.mult, [x_.d, rs1.ds[t], A1[r].d], [tm.d])
            a_ = abf[t % 2]
            TT("pool", a_.ap, tm.ap, B1[r].ap, ALU.add, [tm.d, B1[r].d], [a_.d])
            for q in range(4):
                pb = PS[2 + (q % 2)]
                pv = psbf(2 + (q % 2))
                for i in range(4):
                    kc = 4 * q + i
                    TR(pv[:, i * 128:(i + 1) * 128], a_.ap[:, kc * 128:(kc + 1) * 128], identbf.ap, [a_.d, identbf.d], [pb.d])
                CP("act" if q % 2 == 0 else "dve", ab.ap[:, 4 * q:4 * q + 4, j * 128:(j + 1) * 128],
                   pv[:, 0:512].rearrange("p (a b) -> p a b", b=128), [pb.d], [ab.d])
        ntok = 128 * len(tiles)
        DMA("sp", aT_d.ap[tb][:, :, 0:ntok], ab.ap[:, :, 0:ntok], [ab.d], [aT_d.ds[tb]])
    P.barrier()
    if LIMIT == 1:
        return finish()
    AR.top = persist_mark

    tri = [T(AR.alloc([128, 128], F32)) for _ in range(2)]
    maskn = [T(AR.alloc([128, 512], BF16)) for _ in range(2)]
    for d_ in range(2):
        DMA("sp", tri[d_].ap, c_tri[d_], [], [tri[d_].d])
        DMA("pool", maskn[d_].ap, c_mask[d_], [], [maskn[d_].d])
    xs = T(AR.alloc([128, NT, 512], BF16), NT)
    Btok = T(AR.alloc([128, NT, 128], BF16), NT)
    BT = T(AR.alloc([128, NT * 128], BF16), NT)
    CT = T(AR.alloc([128, NT * 128], BF16), NT)
    dtr = T(AR.alloc([128, NT, 16], F32))
    dtv = T(AR.alloc([128, NT, 16], F32))
    lav = T(AR.alloc([128, NT, 16], F32))
    cwt = T(AR.alloc([128, 6, 3], F32))
    cbt = T(AR.alloc([128, 6], F32))
    dtbb = T(AR.alloc([128, 16], F32))
    aneg = T(AR.alloc([128, 16], F32))
    Dbc = T(AR.alloc([128, 512], F32))
    nwb = T(AR.alloc([128, 512], F32))
    p2_mark = AR.top

    for g in range(NG):
        DMA("sp", cwt.ap, cw_ssm[g], [], [cwt.d])
        DMA("sp", cbt.ap, cb_ssm[g], [], [cbt.d])
        load_vec_bc(dtbb, dtb[g])
        load_vec_bc(aneg, alog[g])
        load_vec_bc(Dbc, dsk[g])
        load_vec_bc(nwb, ssm_nw[g])
        ACT(aneg.ap, aneg.ap, AF.Exp, [aneg.d], [aneg.d])
        TS("dve", aneg.ap, aneg.ap, -1.0, None, ALU.mult, None, [aneg.d], [aneg.d])
        wg = T(AR.alloc([128, 16, 1296], BF16))
        for k2 in range(8):
            DMA("pool", wg.ap[:, 2 * k2:2 * k2 + 2, :], wg_ssm[g][:, 2 * k2:2 * k2 + 2, :], [], [wg.d])
        aTl = [T(AR.alloc([128, 16, 512], BF16)) for _ in range(2)]
        acc = [T(AR.alloc([128, 512], F32)) for _ in range(2)]
        xTt = [[T(AR.alloc([128, 512], BF16)) for _ in range(4)] for _ in range(2)]
        zst = [T(AR.alloc([128, 512], BF16)) for _ in range(2)]
        fmi = 0
        zi = 0
        MARKS.setdefault('A', P.count)
        for tb in range(9):
            ntok = 256 if tb == 0 else 512
            W = 256 if tb == 0 else 64
            tiles = [0, 1] if tb == 0 else [2 + 4 * (tb - 1) + i for i in range(4)]
            tok0 = tiles[0] * 128
            if tb == 1:
                MARKS.setdefault('C', P.count)
            if tb == 2:
                MARKS.setdefault('D', P.count)
            al = aTl[tb % 2]
            DMA("sp", al.ap[:, :, 0:ntok], aT_d.ap[tb][:, :, 0:ntok], [aT_d.ds[tb]], [al.d])
            xTb = xTt[tb % 2]
            for oc in range(6):
                pb = PS[fmi % 2]
                fmi += 1
                for kc in range(16):
                    MM(pb.ap[:, 0:ntok], wg.ap[:, kc, oc * 128:(oc + 1) * 128], al.ap[:, kc, 0:ntok], kc == 0, kc == 15,
                       [wg.d, al.d], [pb.d])
                ac = acc[oc % 2]
                ACT(ac.ap[:, 0:ntok], pb.ap[:, 0:ntok], AF.Identity, [pb.d, cwt.d, cbt.d], [ac.d],
                    bias=cbt.ap[:, oc:oc + 1], scale=cwt.ap[:, oc, 1:2])
                u3 = pb.ap[:, 0:ntok].rearrange("p (r w) -> p r w", w=W)
                a3 = ac.ap[:, 0:ntok].rearrange("p (r w) -> p r w", w=W)
                STT("dve", a3[:, :, 1:W], u3[:, :, 0:W - 1], cwt.ap[:, oc, 0:1], a3[:, :, 1:W], ALU.mult, ALU.add,
                    [pb.d, cwt.d, ac.d], [ac.d])
                STT("dve", a3[:, :, 0:W - 1], u3[:, :, 1:W], cwt.ap[:, oc, 2:3], a3[:, :, 0:W - 1], ALU.mult, ALU.add,
                    [pb.d, cwt.d, ac.d], [ac.d])
                if oc < 4:
                    ACT(xTb[oc].ap[:, 0:ntok], ac.ap[:, 0:ntok], AF.Silu, [ac.d], [xTb[oc].d])
                elif oc == 4:
                    ACT(BT.ap[:, tok0:tok0 + ntok], ac.ap[:, 0:ntok], AF.Silu, [ac.d], [BT.ds[t] for t in tiles])
                else:
                    ACT(CT.ap[:, tok0:tok0 + ntok], ac.ap[:, 0:ntok], AF.Silu, [ac.d], [CT.ds[t] for t in tiles])
            MARKS.setdefault('B', P.count)
            for j, t in enumerate(tiles):
                pb = PS[2 + (t % 2)]
                pv = psbf(2 + (t % 2))
                for oc in range(4):
                    TR(pv[:, oc * 128:(oc + 1) * 128], xTb[oc].ap[:, j * 128:(j + 1) * 128], identbf.ap,
                       [xTb[oc].d, identbf.d], [pb.d])
                TR(psbf(7)[:, 0:128], BT.ap[:, t * 128:(t + 1) * 128], identbf.ap, [BT.ds[t], identbf.d], [PS[7].d])
                CP("dve", xs.ap[:, t, :], pv[:, 0:512], [pb.d], [xs.ds[t]])
                CP("act", Btok.ap[:, t, :], psbf(7)[:, 0:128], [PS[7].d], [Btok.ds[t]])
                if t >= 2:
                    pz = PS[4 + (zi % 2)]
                    zz = zst[zi % 2]
                    zi += 1
                    for kc in range(16):
                        MM(pz.ap, al.ap[:, kc, j * 128:(j + 1) * 128], wg.ap[:, kc, 768:1280], kc == 0, kc == 15,
                           [al.d, wg.d], [pz.d])
                    ACT(zz.ap, pz.ap, AF.Silu, [pz.d], [zz.d])
                    DMA("sp", z_d.ap[g, t - 2], zz.ap, [zz.d], [z_d.ds[g * NTL + t - 2]])
                pd = PS[6]
                for kc in range(16):
                    MM(pd.ap[:, 0:16], al.ap[:, kc, j * 128:(j + 1) * 128], wg.ap[:, kc, 1280:1296], kc == 0, kc == 15,
                       [al.d, wg.d], [pd.d])
                TT("dve", dtr.ap[:, t, :], pd.ap[:, 0:16], dtbb.ap, ALU.add, [pd.d, dtbb.d], [dtr.d])
        MARKS.setdefault('E', P.count)
        ACT(dtv.ap, dtr.ap, AF.Exp, [dtr.d], [dtv.d])
        ACT(dtv.ap, dtv.ap, AF.Ln, [dtv.d], [dtv.d], bias=ones32.ap[:, 0:1])
        TT("dve", lav.ap, dtv.ap, aneg.ap.unsqueeze(1).to_broadcast([128, NT, 16]), ALU.mult, [dtv.d, aneg.d], [lav.d])
        if DEBUG:
            DMA("sp", dbg["dt"][g], dtv.ap, [dtv.d], [])
            DMA("sp", dbg["xs"][g], xs.ap, xs.ds, [])
        P.barrier()
        AR.top = p2_mark

        yacc = T(AR.alloc([128, NTL, 512], BF16), NTL)
        S32 = T(AR.alloc([128, 512], F32))
        Sbf = T(AR.alloc([128, 512], BF16))
        rc = [T(AR.alloc([128, 8, 128], F32)) for _ in range(2)]
        Lall = [T(AR.alloc([128, 8, 128], BF16), 8) for _ in range(2)]
        Mall = [T(AR.alloc([128, 8, 128], BF16), 8) for _ in range(2)]
        cbT = [T(AR.alloc([128, 128], BF16)) for _ in range(2)]
        smal = [T(AR.alloc([128, 32], F32)) for _ in range(2)]
        xw = [T(AR.alloc([128, 512], BF16)) for _ in range(2)]
        tA = [T(AR.alloc([128, 512], F32)) for _ in range(2)]
        tB = [T(AR.alloc([128, 512], F32)) for _ in range(2)]
        zt = [T(AR.alloc([128, 512], BF16)) for _ in range(2)]
        ynb = [T(AR.alloc([128, 512], BF16)) for _ in range(2)]
        yTo = [T(AR.alloc([128, 4, 128], BF16)) for _ in range(2)]
        gst = [T(AR.alloc([128, 4], F32)) for _ in range(2)]
        junk2 = T(AR.alloc([128, 512], BF16))
        it = 0
        for d_ in range(SCAN):
            order = list(range(NT)) if d_ == 0 else [1, 0] + list(range(NT - 1, 1, -1))
            last = 127 if d_ == 0 else 0
            MEMSET("dve", S32.ap, 0.0, [S32.d])
            MEMSET("pool", Sbf.ap, 0.0, [Sbf.d])
            for c in order:
                lat = c >= 2
                k = it % 2
                it += 1
                la_c = lav.ap[:, c, d_ * 8:(d_ + 1) * 8]
                dt_c = dtv.ap[:, c, d_ * 8:(d_ + 1) * 8]
                sm = smal[k]
                pS = PS[7]
                MM(pS.ap[:, 0:8], tri[d_].ap, la_c, True, True, [tri[d_].d, lav.d], [pS.d])
                MM(pS.ap[:, 8:16], ones32.ap, la_c, True, True, [ones32.d, lav.d], [pS.d])
                TT("dve", rc[k].ap, tri[d_].ap.unsqueeze(1).to_broadcast([128, 8, 128]),
                   la_c.unsqueeze(2).to_broadcast([128, 8, 128]), ALU.mult, [tri[d_].d, lav.d], [rc[k].d])
                Rb = [PS[0 + 2 * k], PS[1 + 2 * k]]
                for hf in range(2):
                    MM(Rb[hf].ap, ones32.ap, rc[k].ap[:, 4 * hf:4 * hf + 4, :].rearrange("p a b -> p (a b)"), True, False,
                       [ones32.d, rc[k].d], [Rb[hf].d])
                    MM(Rb[hf].ap, identbf.ap, maskn[d_].ap, False, True, [identbf.d, maskn[d_].d], [Rb[hf].d])
                ACT(sm.ap[:, 0:8], pS.ap[:, 0:8], AF.Identity, [pS.d], [sm.d], scale=-1.0)
                ACT(sm.ap[:, 8:24], pS.ap[:, 0:16], AF.Exp, [pS.d], [sm.d])
                if lat:
                    MM(pS.ap[:, 128:256], BT.ap[:, c * 128:(c + 1) * 128], CT.ap[:, c * 128:(c + 1) * 128], True, True,
                       [BT.ds[c], CT.ds[c]], [pS.d])
                    CP("act", cbT[k].ap, pS.ap[:, 128:256], [pS.d], [cbT[k].d])
                La = Lall[k]
                for h in range(8):
                    ACT(La.ap[:, h, :], Rb[h // 4].ap[:, (h % 4) * 128:(h % 4 + 1) * 128], AF.Exp, [Rb[h // 4].d, sm.d],
                        [La.ds[h]], bias=sm.ap[:, h:h + 1])
                TT("dve", sm.ap[:, 24:32], La.ap[:, :, last], dt_c, ALU.mult, La.ds + [dtv.d], [sm.d])
                TT("pool", xw[k].ap.rearrange("p (h d) -> p h d", d=64), xs.ap[:, c, :].rearrange("p (h d) -> p h d", d=64),
                   sm.ap[:, 24:32].unsqueeze(2).to_broadcast([128, 8, 64]), ALU.mult, [xs.ds[c], sm.d], [xw[k].d])
                if lat:
                    Ma = Mall[k]
                    for h in range(8):
                        STT("dve", Ma.ap[:, h, :], La.ap[:, h, :], dt_c[:, h:h + 1], cbT[k].ap, ALU.mult, ALU.mult,
                            [La.ds[h], dtv.d, cbT[k].d], [Ma.ds[h]])
                    for h in range(8):
                        MM(PS[4].ap[:, h * 64:(h + 1) * 64], Ma.ap[:, h, :], xs.ap[:, c, h * 64:(h + 1) * 64], True, True,
                           [Ma.ds[h], xs.ds[c]], [PS[4].d])
                    MM(PS[5].ap, CT.ap[:, c * 128:(c + 1) * 128], Sbf.ap, True, True, [CT.ds[c], Sbf.d], [PS[5].d])
                MM(PS[6].ap, Btok.ap[:, c, :], xw[k].ap, True, True, [Btok.ds[c], xw[k].d], [PS[6].d])
                if lat:
                    TT("dve", tB[k].ap.rearrange("p (h d) -> p h d", d=64), PS[5].ap.rearrange("p (h d) -> p h d", d=64),
                       sm.ap[:, 8:16].unsqueeze(2).to_broadcast([128, 8, 64]), ALU.mult, [PS[5].d, sm.d], [tB[k].d])
                    TT("dve", tB[k].ap, tB[k].ap, PS[4].ap, ALU.add, [tB[k].d, PS[4].d], [tB[k].d])
                TT("dve", S32.ap.rearrange("p (h d) -> p h d", d=64), S32.ap.rearrange("p (h d) -> p h d", d=64),
                   sm.ap[:, 16:24].unsqueeze(2).to_broadcast([128, 8, 64]), ALU.mult, [S32.d, sm.d], [S32.d])
                TT("dve", S32.ap, S32.ap, PS[6].ap, ALU.add, [S32.d, PS[6].d], [S32.d])
                CP("act", Sbf.ap, S32.ap, [S32.d], [Sbf.d])
                if lat and d_ == 0:
                    TT("pool", tA[k].ap, xs.ap[:, c, :], Dbc.ap, ALU.mult, [xs.ds[c], Dbc.d], [tA[k].d])
                    TT("pool", yacc.ap[:, c - 2, :], tA[k].ap, tB[k].ap, ALU.add, [tA[k].d, tB[k].d], [yacc.ds[c - 2]])
                if lat and d_ == 1:
                    DMA("sp", zt[k].ap, z_d.ap[g, c - 2], [z_d.ds[g * NTL + c - 2]], [zt[k].d])
                    TT("pool", tA[k].ap, tB[k].ap, yacc.ap[:, c - 2, :], ALU.add, [tB[k].d, yacc.ds[c - 2]], [tA[k].d])
                    TT("pool", tA[k].ap, tA[k].ap, zt[k].ap, ALU.mult, [tA[k].d, zt[k].d], [tA[k].d])
                    gs = gst[k]
                    MEMSET("pool", gs.ap[:, 0:1], 0.0, [gs.d])
                    ACT(junk2.ap, tA[k].ap, AF.Square, [tA[k].d], [junk2.d, gs.d], accum=gs.ap[:, 0:1])
                    rstd_from_ss(gs.ap[:, 0:1], gs.ap[:, 1:2], gs.ap[:, 2:3], 512.0, gs.d, gs.d, gs.d)
                    STT("dve", ynb[k].ap, tA[k].ap, gs.ap[:, 2:3], nwb.ap, ALU.mult, ALU.mult, [tA[k].d, gs.d, nwb.d], [ynb[k].d])
                    pv = psbf(7)
                    for q in range(4):
                        TR(pv[:, 512 + q * 128:512 + (q + 1) * 128], ynb[k].ap[:, q * 128:(q + 1) * 128], identbf.ap,
                           [ynb[k].d, identbf.d], [PS[7].d])
                    CP("act", yTo[k].ap, pv[:, 512:1024].rearrange("p (a b) -> p a b", b=128), [PS[7].d], [yTo[k].d])
                    DMA("sp", yT_d.ap[c - 2][:, g * 4:(g + 1) * 4, :], yTo[k].ap, [yTo[k].d], [yT_d.ds[c - 2]])
        P.barrier()
        AR.top = p2_mark
    AR.top = persist_mark
    if LIMIT == 2:
        return finish()

    ss_sc = T(AR.alloc([128, NTL], F32))
    rs_sc = T(AR.alloc([128, NTL], F32))
    sd_sc = T(AR.alloc([128, NTL], F32))
    MEMSET("dve", ss_sc.ap, 0.0, [ss_sc.d])
    p3_mark = AR.top
    cwc = T(AR.alloc([128, 4, 3], F32))
    scwt = T(AR.alloc([128, 4], F32))
    wsc = T(AR.alloc([128, 16, 1536], BF16))
    aTl = [T(AR.alloc([128, 16, 512], BF16)) for _ in range(2)]
    cgs = [T(AR.alloc([128, 512], F32)) for _ in range(2)]
    vv = [T(AR.alloc([128, 512], F32)) for _ in range(2)]
    acc = [T(AR.alloc([128, 512], F32)) for _ in range(2)]
    q2 = [T(AR.alloc([128, 512], BF16)) for _ in range(4)]
    qw = [T(AR.alloc([128, 512], BF16)) for _ in range(4)]
    it = 0
    for jb in range(4):
        DMA("sp", cwc.ap, cw_sc[jb], [], [cwc.d])
        DMA("sp", scwt.ap, scw[jb], [], [scwt.d])
        for k2 in range(8):
            DMA("pool", wsc.ap[:, 2 * k2:2 * k2 + 2, :], wg_sc[jb][:, 2 * k2:2 * k2 + 2, :], [], [wsc.d])
        for tb in range(1, 9):
            t0 = 4 * (tb - 1)
            al = aTl[tb % 2]
            DMA("sp", al.ap, aT_d.ap[tb], [aT_d.ds[tb]], [al.d])
            for cc in range(4):
                k = it % 2
                it += 1
                pbg, pcg, phv = PS[0 + 3 * k], PS[1 + 3 * k], PS[2 + 3 * k]
                for (pb, off) in ((pbg, 0), (pcg, 512), (phv, 1024)):
                    for kc in range(16):
                        MM(pb.ap, wsc.ap[:, kc, off + cc * 128:off + (cc + 1) * 128], al.ap[:, kc, :], kc == 0, kc == 15,
                           [wsc.d, al.d], [pb.d])
                CP("act", cgs[k].ap, pcg.ap, [pcg.d], [cgs[k].d])
                TT("dve", vv[k].ap, cgs[k].ap, phv.ap, ALU.mult, [cgs[k].d, phv.d], [vv[k].d])
                ACT(acc[k].ap, vv[k].ap, AF.Identity, [vv[k].d, cwc.d], [acc[k].d], scale=cwc.ap[:, cc, 1:2])
                v3 = vv[k].ap.rearrange("p (r w) -> p r w", w=64)
                a3 = acc[k].ap.rearrange("p (r w) -> p r w", w=64)
                STT("dve", a3[:, :, 1:64], v3[:, :, 0:63], cwc.ap[:, cc, 0:1], a3[:, :, 1:64], ALU.mult, ALU.add,
                    [vv[k].d, cwc.d, acc[k].d], [acc[k].d])
                STT("dve", a3[:, :, 0:63], v3[:, :, 1:64], cwc.ap[:, cc, 2:3], a3[:, :, 0:63], ALU.mult, ALU.add,
                    [vv[k].d, cwc.d, acc[k].d], [acc[k].d])
                TT("dve", acc[k].ap, acc[k].ap, pbg.ap, ALU.mult, [acc[k].d, pbg.d], [acc[k].d])
                ACT(q2[cc].ap, acc[k].ap, AF.Square, [acc[k].d], [q2[cc].d])
                TS("pool", qw[cc].ap, acc[k].ap, scwt.ap[:, cc:cc + 1], None, ALU.mult, None, [acc[k].d, scwt.d], [qw[cc].d])
                DMA("sp", yT_d.ap[t0:t0 + 4, :, 16 + jb * 4 + cc, :].rearrange("t p k -> p t k"),
                    qw[cc].ap.rearrange("p (t k) -> p t k", k=128), [qw[cc].d], [yT_d.ds[t0 + i] for i in range(4)])
            pss = PS[6 + (tb % 2)]
            for i in range(4):
                for cc in range(4):
                    MM(pss.ap[:, i:i + 1], q2[cc].ap[:, i * 128:(i + 1) * 128], onesbf.ap[:, 0:1], cc == 0, cc == 3,
                       [q2[cc].d, onesbf.d], [pss.d])
            TT("dve", ss_sc.ap[:, t0:t0 + 4], ss_sc.ap[:, t0:t0 + 4], pss.ap[:, 0:4], ALU.add, [ss_sc.d, pss.d], [ss_sc.d])
    rstd_from_ss(ss_sc.ap, sd_sc.ap, rs_sc.ap, 2048.0, ss_sc.d, sd_sc.d, rs_sc.d)
    if DEBUG:
        DMA("sp", dbg["rsc"], rs_sc.ap, [rs_sc.d], [])
    P.barrier()
    if LIMIT == 3:
        return finish()
    AR.top = p3_mark

    g1b = T(AR.alloc([128, 2048], F32))
    load_mod_bc(g1b, 0, 2)
    wo = T(AR.alloc([128, 32, 1024], BF16))
    yt = [T(AR.alloc([128, 32, 128], BF16)) for _ in range(2)]
    xh = [T(AR.alloc([128, 1024], F32)) for _ in range(2)]
    t1 = [T(AR.alloc([128, 512], F32)) for _ in range(2)]
    ht = [T(AR.alloc([128, 1024], F32)) for _ in range(2)]
    it = 0
    for hf in range(2):
        for k4 in range(8):
            DMA("pool", wo.ap[:, 4 * k4:4 * k4 + 4, :], w_out[hf][:, 4 * k4:4 * k4 + 4, :], [], [wo.d])
        for t in range(NTL):
            y_ = yt[t % 2]
            DMA("sp", y_.ap, yT_d.ap[t], [yT_d.ds[t]], [y_.d])
            x_ = xh[t % 2]
            DMA("sp", x_.ap, xin[t + 2][:, hf * 1024:(hf + 1) * 1024], [], [x_.d])
            h_ = ht[t % 2]
            for q in range(2):
                k = it % 2
                it += 1
                pm, pc = PS[0 + 2 * k], PS[1 + 2 * k]
                cols = slice(q * 512, (q + 1) * 512)
                for kc in range(16):
                    MM(pm.ap, y_.ap[:, kc, :], wo.ap[:, kc, cols], kc == 0, kc == 15, [y_.d, wo.d], [pm.d])
                for kc in range(16, 32):
                    MM(pc.ap, y_.ap[:, kc, :], wo.ap[:, kc, cols], kc == 16, kc == 31, [y_.d, wo.d], [pc.d])
                ACT(t1[k].ap, pc.ap, AF.Identity, [pc.d, rs_sc.d], [t1[k].d], scale=rs_sc.ap[:, t:t + 1])
                TT("dve", t1[k].ap, t1[k].ap, pm.ap, ALU.add, [t1[k].d, pm.d], [t1[k].d])
                gcols = slice(hf * 1024 + q * 512, hf * 1024 + (q + 1) * 512)
                TT("pool", t1[k].ap, t1[k].ap, g1b.ap[:, gcols], ALU.mult, [t1[k].d, g1b.d], [t1[k].d])
                TT("pool", h_.ap[:, cols], t1[k].ap, x_.ap[:, cols], ALU.add, [t1[k].d, x_.d], [h_.d])
            DMA("sp", hacc_h[hf].ap[t * 128:(t + 1) * 128, :], h_.ap, [h_.d], [hacc_h[hf].ds[t]])
    P.barrier()
    if LIMIT == 4:
        return finish()
    AR.top = persist_mark

    idxc = T(AR.alloc([128, 4, 16], I32))
    gatc = T(AR.alloc([128, 4, 16], F32))
    p5_mark = AR.top
    A2 = T(AR.alloc([128, 2048], F32))
    B2 = T(AR.alloc([128, 2048], F32))
    nfb = T(AR.alloc([128, 2048], F32))
    load_vec_bc(nfb, nffn)
    load_mod_bc(A2, 0, 4)
    load_mod_bc(B2, 0, 3)
    TT("pool", A2.ap, A2.ap, nfb.ap, ALU.mult, [A2.d, nfb.d], [A2.d])
    wr = T(AR.alloc([128, 16, 16], F32))
    DMA("sp", wr.ap, w_rt, [], [wr.d])
    affT = T(AR.alloc([16, 4096], F32))
    hx = [T(AR.alloc([128, 2048], F32)) for _ in range(2)]
    ff = [T(AR.alloc([128, 2048], F32)) for _ in range(2)]
    fb = [T(AR.alloc([128, 2048], BF16)) for _ in range(2)]
    fT = [T(AR.alloc([128, 16, 128], F32)) for _ in range(2)]
    st5 = [T(AR.alloc([128, 8], F32)) for _ in range(2)]
    ex = [T(AR.alloc([128, 16], F32)) for _ in range(2)]
    junk5 = T(AR.alloc([128, 2048], BF16))
    for t in range(NTL):
        k = t % 2
        h_ = hx[k]
        for i2 in range(2):
            DMA("sp", h_.ap[:, i2 * 1024:(i2 + 1) * 1024], hacc_h[i2].ap[t * 128:(t + 1) * 128, :], [hacc_h[i2].ds[t]], [h_.d])
        if DEBUG:
            DMA("sp", dbg["h1"][t * 128:(t + 1) * 128, :], h_.ap, [h_.d], [])
        s5 = st5[k]
        MEMSET("pool", s5.ap, 0.0, [s5.d])
        ACT(junk5.ap, h_.ap, AF.Square, [h_.d], [junk5.d, s5.d], accum=s5.ap[:, 0:1])
        rstd_from_ss(s5.ap[:, 0:1], s5.ap[:, 1:2], s5.ap[:, 2:3], 2048.0, s5.d, s5.d, s5.d)
        f_ = ff[k]
        STT("dve", f_.ap, h_.ap, s5.ap[:, 2:3], A2.ap, ALU.mult, ALU.mult, [h_.d, s5.d, A2.d], [f_.d])
        TT("pool", f_.ap, f_.ap, B2.ap, ALU.add, [f_.d, B2.d], [f_.d])
        ACT(fb[k].ap, f_.ap, AF.Copy, [f_.d], [fb[k].d])
        DMA("sp", f_d.ap[t * 128:(t + 1) * 128, :], fb[k].ap, [fb[k].d], [f_d.d])
        for q in range(4):
            pb = PS[q % 4]
            for i in range(4):
                kc = 4 * q + i
                TR(pb.ap[:, i * 128:(i + 1) * 128], f_.ap[:, kc * 128:(kc + 1) * 128], ident32.ap, [f_.d, ident32.d], [pb.d])
            CP("act" if q % 2 == 0 else "dve", fT[k].ap[:, 4 * q:4 * q + 4, :], pb.ap.rearrange("p (a b) -> p a b", b=128),
               [pb.d], [fT[k].d])
        pl = PS[4 + k]
        for kc in range(16):
            MM(pl.ap[:, 0:16], fT[k].ap[:, kc, :], wr.ap[:, kc, :], kc == 0, kc == 15, [fT[k].d, wr.d], [pl.d])
        P.emit("dve", (lambda o, i: (lambda e: e.reduce_max(out=o, in_=i, axis=mybir.AxisListType.X)))(s5.ap[:, 3:4], pl.ap[:, 0:16]),
               [pl.d], [s5.d])
        TS("dve", s5.ap[:, 4:5], s5.ap[:, 3:4], -1.0, None, ALU.mult, None, [s5.d], [s5.d])
        ACT(ex[k].ap, pl.ap[:, 0:16], AF.Exp, [pl.d, s5.d], [ex[k].d, s5.d], bias=s5.ap[:, 4:5], accum=s5.ap[:, 5:6])
        RECIP(s5.ap[:, 6:7], s5.ap[:, 5:6], [s5.d], [s5.d])
        TS("dve", ex[k].ap, ex[k].ap, s5.ap[:, 6:7], None, ALU.mult, None, [ex[k].d, s5.d], [ex[k].d])
        pa = PS[6 + k]
        TR(pa.ap[0:16, 0:128], ex[k].ap, ident32.ap, [ex[k].d, ident32.d], [pa.d])
        CP("act", affT.ap[:, t * 128:(t + 1) * 128], pa.ap[0:16, 0:128], [pa.d], [affT.d])
    if DEBUG:
        DMA("sp", dbg["aff"], affT.ap, [affT.d], [])
    vals = T(AR.alloc([16, 512], F32))
    idxu = T(AR.alloc([16, 512], U32))
    idxf = T(AR.alloc([16, 512], F32))
    for r in range(64):
        v8 = vals.ap[:, r * 8:(r + 1) * 8]
        i8 = idxu.ap[:, r * 8:(r + 1) * 8]
        P.emit("dve", (lambda o, i: (lambda e: e.max(out=o, in_=i)))(v8, affT.ap), [affT.d], [vals.d])
        P.emit("dve", (lambda o, m, i: (lambda e: e.max_index(out=o, in_max=m, in_values=i)))(i8, v8, affT.ap),
               [affT.d, vals.d], [idxu.d])
        P.emit("dve", (lambda o, m, i: (lambda e: e.match_replace(out=o, in_to_replace=m, in_values=i, imm_value=-1.0)))(affT.ap, v8, affT.ap),
               [vals.d, idxu.d], [affT.d])
    CP("dve", idxf.ap, idxu.ap, [idxu.d], [idxf.d])
    if DEBUG:
        DMA("sp", dbg["vals"], vals.ap, [vals.d], [])
        DMA("sp", dbg["idx"], idxu.ap, [idxu.d], [])
    for s in range(4):
        pa = PS[s % 2]
        TR(pa.ap[:, 0:16], idxf.ap[:, s * 128:(s + 1) * 128], ident32.ap[0:16, 0:16], [idxf.d, ident32.d], [pa.d])
        TR(pa.ap[:, 16:32], vals.ap[:, s * 128:(s + 1) * 128], ident32.ap[0:16, 0:16], [vals.d, ident32.d], [pa.d])
        CP("dve", idxc.ap[:, s, :], pa.ap[:, 0:16], [pa.d], [idxc.d])
        CP("act", gatc.ap[:, s, :], pa.ap[:, 16:32], [pa.d], [gatc.d])
    P.barrier()
    if LIMIT == 5:
        return finish()
    AR.top = p5_mark

    g2b = T(AR.alloc([128, 2048], F32))
    load_mod_bc(g2b, 0, 5)
    NW = 5
    wbuf = [T(AR.alloc([128, 8192], BF16)) for _ in range(NW)]
    xg = [T(AR.alloc([128, 2048], BF16)) for _ in range(8)]
    xeT = [T(AR.alloc([128, 16, 512], BF16)) for _ in range(2)]
    hidT = [T(AR.alloc([128, 8, 512], BF16), 8) for _ in range(2)]
    silt = [T(AR.alloc([128, 512], BF16)) for _ in range(2)]
    yet = [T(AR.alloc([128, 512], F32)) for _ in range(2)]
    yo = [T(AR.alloc([128, 1024], F32)) for _ in range(2)]
    hall = Dep()
    srcs_all = []
    for e_ in range(16):
        srcs_all += [(w_gate[e_, 0], 0), (w_up[e_, 0], 0), (w_gate[e_, 1], 0), (w_up[e_, 1], 0),
                     (w_down[e_, 0], 1), (w_down[e_, 1], 1)]
    piece_view = {}
    state6 = {"issued": 0}

    def ensure_issued(upto):
        while state6["issued"] < min(upto, len(srcs_all)):
            i = state6["issued"]
            src, kind = srcs_all[i]
            wb = wbuf[i % NW]
            if kind == 0:
                dst = wb.ap.rearrange("p (a b) -> p a b", b=512)
                DMA("pool", dst[:, 0:8, :], src[:, 0:8, :], [], [wb.d])
                DMA("pool", dst[:, 8:16, :], src[:, 8:16, :], [], [wb.d])
            else:
                dst = wb.ap.rearrange("p (a b) -> p a b", b=1024)
                DMA("pool", dst[:, 0:4, :], src[:, 0:4, :], [], [wb.d])
                DMA("pool", dst[:, 4:8, :], src[:, 4:8, :], [], [wb.d])
            piece_view[i] = (wb, dst)
            state6["issued"] = i + 1

    def gather_issue(e_):
        for s in range(4):
            xg_ = xg[(e_ % 2) * 4 + s]
            P.emit("pool", (lambda o, i, ix: (lambda e: e.indirect_dma_start(
                out=o, out_offset=None, in_=i, in_offset=bass.IndirectOffsetOnAxis(ap=ix, axis=0))))(
                    xg_.ap, f_d.ap, idxc.ap[:, s, e_:e_ + 1]), [f_d.d, idxc.d], [xg_.d], dma=True)

    yi = 0
    pgi = 0
    gather_issue(0)
    ensure_issued(NW - 1)
    for e_ in range(16):
        xe = xeT[e_ % 2]
        for s in range(4):
            xg_ = xg[(e_ % 2) * 4 + s]
            for q in range(4):
                pv = psbf(6 + (q % 2))
                pb = PS[6 + (q % 2)]
                for i in range(4):
                    kc = 4 * q + i
                    TR(pv[:, i * 128:(i + 1) * 128], xg_.ap[:, kc * 128:(kc + 1) * 128], identbf.ap, [xg_.d, identbf.d], [pb.d])
                CP("act" if q % 2 == 0 else "dve", xe.ap[:, 4 * q:4 * q + 4, s * 128:(s + 1) * 128],
                   pv[:, 0:512].rearrange("p (a b) -> p a b", b=128), [pb.d], [xe.d])
        if e_ + 1 < 16:
            gather_issue(e_ + 1)
        hT = hidT[e_ % 2]
        p0 = 6 * e_
        for fcu in range(8):
            if fcu % 4 == 0:
                ensure_issued(p0 + 2 * (fcu // 4) + NW)
            wgp, wgv = piece_view[p0 + 0 + 2 * (fcu // 4)]
            wup, wuv = piece_view[p0 + 1 + 2 * (fcu // 4)]
            off = (fcu % 4) * 128
            k = pgi % 2
            pgi += 1
            pg, pu = PS[0 + 2 * k], PS[1 + 2 * k]
            for kc in range(16):
                MM(pg.ap, wgv[:, kc, off:off + 128], xe.ap[:, kc, :], kc == 0, kc == 15, [wgp.d, xe.d], [pg.d])
            for kc in range(16):
                MM(pu.ap, wuv[:, kc, off:off + 128], xe.ap[:, kc, :], kc == 0, kc == 15, [wup.d, xe.d], [pu.d])
            ACT(silt[k].ap, pg.ap, AF.Silu, [pg.d], [silt[k].d])
            TT("dve", hT.ap[:, fcu, :], silt[k].ap, pu.ap, ALU.mult, [silt[k].d, pu.d], [hT.ds[fcu]])
        for dh in range(2):
            ensure_issued(p0 + 4 + dh + NW)
            wdp, wdv = piece_view[p0 + 4 + dh]
            for s in range(4):
                yo_ = yo[yi % 2]
                yi += 1
                for dq in range(2):
                    k = pgi % 2
                    pgi += 1
                    py = PS[4 + k]
                    for fc in range(8):
                        MM(py.ap, hT.ap[:, fc, s * 128:(s + 1) * 128], wdv[:, fc, dq * 512:(dq + 1) * 512], fc == 0, fc == 7,
                           [hT.ds[fc], wdp.d], [py.d])
                    ACT(yet[k].ap, py.ap, AF.Identity, [py.d, gatc.d], [yet[k].d], scale=gatc.ap[:, s, e_:e_ + 1])
                    gc = slice(dh * 1024 + dq * 512, dh * 1024 + (dq + 1) * 512)
                    TT("dve", yo_.ap[:, dq * 512:(dq + 1) * 512], yet[k].ap, g2b.ap[:, gc], ALU.mult,
                       [yet[k].d, g2b.d], [yo_.d])
                P.emit("pool", (lambda o, i, ix: (lambda e: e.indirect_dma_start(
                    out=o, out_offset=bass.IndirectOffsetOnAxis(ap=ix, axis=0), in_=i, in_offset=None, compute_op=ALU.add)))(
                        hacc_h[dh].ap, yo_.ap, idxc.ap[:, s, e_:e_ + 1]),
                    [yo_.d, idxc.d], [hall], dma=True)
    P.barrier()
    if LIMIT == 6:
        return finish()
    AR.top = persist_mark

    fnb = T(AR.alloc([128, 2048], F32))
    load_vec_bc(fnb, nfin)
    hx = [T(AR.alloc([128, 2048], F32)) for _ in range(2)]
    ob = [T(AR.alloc([128, 2048], F32)) for _ in range(2)]
    st7 = [T(AR.alloc([128, 4], F32)) for _ in range(2)]
    junk7 = T(AR.alloc([128, 2048], BF16))
    for t in range(NTL):
        k = t % 2
        for i2 in range(2):
            DMA("sp", hx[k].ap[:, i2 * 1024:(i2 + 1) * 1024], hacc_h[i2].ap[t * 128:(t + 1) * 128, :], [hall], [hx[k].d])
        s7 = st7[k]
        MEMSET("pool", s7.ap, 0.0, [s7.d])
        ACT(junk7.ap, hx[k].ap, AF.Square, [hx[k].d], [junk7.d, s7.d], accum=s7.ap[:, 0:1])
        rstd_from_ss(s7.ap[:, 0:1], s7.ap[:, 1:2], s7.ap[:, 2:3], 2048.0, s7.d, s7.d, s7.d)
        STT("dve", ob[k].ap, hx[k].ap, s7.ap[:, 2:3], fnb.ap, ALU.mult, ALU.mult, [hx[k].d, s7.d, fnb.d], [ob[k].d])
        DMA("sp", out[t], ob[k].ap, [ob[k].d], [])
    P.barrier()
    P.finalize(st)
    st.close()
    return nc


def _kc_p(w):
    K, N = w.shape
    return np.ascontiguousarray(w.reshape(K // 128, 128, N).transpose(1, 0, 2))


def _prep_shared(c_ctx, w_mod, b_mod, norm_mix_w, w_in, ssm_conv_w, ssm_conv_b, ssm_a_log, ssm_dt_bias, ssm_d,
                 ssm_norm_w, sc_conv_w, sc_norm_w, w_out, norm_ffn_w, w_router, w_gate, w_up, w_down, final_norm_w):
    f = np.float32
    sh = {}
    sh["w_mod"] = np.ascontiguousarray(w_mod[0].reshape(128, 16, 12288))
    sh["b_mod"] = np.ascontiguousarray(b_mod[0].reshape(1, 12288))
    sh["nmix"] = norm_mix_w[0].reshape(1, 2048)
    sh["nffn"] = norm_ffn_w[0].reshape(1, 2048)
    sh["nfin"] = final_norm_w.reshape(1, 2048)
    win = w_in[0]
    wg, cw, cb = [], [], []
    for g in range(4):
        cols = np.concatenate([2048 + g * 512 + np.arange(512), 4096 + g * 128 + np.arange(128),
                               4608 + g * 128 + np.arange(128), g * 512 + np.arange(512),
                               5120 + g * 8 + np.arange(8), 5152 + g * 8 + np.arange(8)])
        wg.append(_kc_p(win[:, cols]))
        ch = np.concatenate([g * 512 + np.arange(512), 2048 + g * 128 + np.arange(128), 2560 + g * 128 + np.arange(128)])
        cw.append(ssm_conv_w[0][:, ch].T.reshape(6, 128, 3).transpose(1, 0, 2))
        cb.append(ssm_conv_b[0][ch].reshape(6, 128).T)
    sh["wg_ssm"] = np.ascontiguousarray(np.stack(wg)).astype(f)
    sh["cw_ssm"] = np.ascontiguousarray(np.stack(cw)).astype(f)
    sh["cb_ssm"] = np.ascontiguousarray(np.stack(cb)).astype(f)
    wsc, cws, scw = [], [], []
    for j in range(4):
        ch = j * 512 + np.arange(512)
        cols = np.concatenate([5184 + ch, 7232 + ch, 9280 + ch])
        wsc.append(_kc_p(win[:, cols]))
        cws.append(sc_conv_w[0][:, ch].T.reshape(4, 128, 3).transpose(1, 0, 2))
        scw.append(sc_norm_w[0][ch].reshape(4, 128).T)
    sh["wg_sc"] = np.ascontiguousarray(np.stack(wsc)).astype(f)
    sh["cw_sc"] = np.ascontiguousarray(np.stack(cws)).astype(f)
    sh["scw"] = np.ascontiguousarray(np.stack(scw)).astype(f)
    hsel = np.stack([np.concatenate([g * 8 + np.arange(8), 32 + g * 8 + np.arange(8)]) for g in range(4)])
    sh["dtb"] = np.ascontiguousarray(ssm_dt_bias[0][hsel].reshape(4, 1, 16))
    sh["alog"] = np.ascontiguousarray(ssm_a_log[0][hsel].reshape(4, 1, 16))
    sh["dsk"] = np.ascontiguousarray(np.repeat(ssm_d[0].reshape(4, 8), 64, axis=1).reshape(4, 1, 512))
    sh["ssm_nw"] = np.ascontiguousarray(ssm_norm_w[0].reshape(4, 1, 512))
    wo = _kc_p(w_out[0])
    sh["w_out"] = np.ascontiguousarray(np.stack([wo[:, :, 0:1024], wo[:, :, 1024:2048]]))
    sh["w_rt"] = _kc_p(w_router[0])
    wgt = w_gate[0].reshape(16, 16, 128, 2, 512).transpose(0, 3, 2, 1, 4)
    wup = w_up[0].reshape(16, 16, 128, 2, 512).transpose(0, 3, 2, 1, 4)
    wdn = w_down[0].reshape(16, 8, 128, 2, 1024).transpose(0, 3, 2, 1, 4)
    sh["w_gate"] = np.ascontiguousarray(wgt)
    sh["w_up"] = np.ascontiguousarray(wup)
    sh["w_down"] = np.ascontiguousarray(wdn)
    sh["c_ident"] = np.eye(128, dtype=f)
    k = np.arange(128)[:, None]
    i = np.arange(128)[None, :]
    sh["c_tri"] = np.stack([(k <= i), (k >= i)]).astype(f)
    mf = np.where(i < k, NEG, 0.0).astype(f)
    mb = np.where(i > k, NEG, 0.0).astype(f)
    sh["c_mask"] = np.stack([np.tile(mf, (1, 4)), np.tile(mb, (1, 4))]).astype(f)
    return sh


_CACHE = {}


def kernel(x, c, ctx, c_ctx, w_mod, b_mod, norm_mix_w, w_in, ssm_conv_w, ssm_conv_b, ssm_a_log, ssm_dt_bias, ssm_d,
           ssm_norm_w, sc_conv_w, sc_norm_w, w_out, norm_ffn_w, w_router, w_gate, w_up, w_down, final_norm_w):
    args = [np.asarray(a, dtype=np.float32) for a in (
        c_ctx, w_mod, b_mod, norm_mix_w, w_in, ssm_conv_w, ssm_conv_b, ssm_a_log, ssm_dt_bias, ssm_d, ssm_norm_w,
        sc_conv_w, sc_norm_w, w_out, norm_ffn_w, w_router, w_gate, w_up, w_down, final_norm_w)]
    x = np.asarray(x, dtype=np.float32)
    c = np.asarray(c, dtype=np.float32)
    ctx = np.asarray(ctx, dtype=np.float32)
    sh = _prep_shared(*args)
    if "nc" not in _CACHE:
        _CACHE["nc"] = build_program()
    nc = _CACHE["nc"]
    in_maps = []
    for core in range(NCORES):
        b = core % 4
        m = dict(sh)
        m["xin"] = np.ascontiguousarray(np.concatenate([ctx[b], x[b]], axis=0).reshape(NT, 128, 2048))
        m["cvec"] = np.ascontiguousarray(np.stack([c[b], args[0]], axis=-1).reshape(128, 16, 2))
        in_maps.append(m)
    res = run_bass_kernel_spmd(nc, in_maps, core_ids=list(range(NCORES)))
    _CACHE["res"] = res
    outs = [res.results[b % NCORES]["out"].reshape(4096, 2048) for b in range(4)]
    return np.stack(outs).astype(np.float32)
```

```python
import numpy as np
from contextlib import ExitStack
import concourse.bass as bass
import concourse.mybir as mybir
from concourse.bass_utils import run_bass_kernel_spmd

F32 = mybir.dt.float32
BF16 = mybir.dt.bfloat16
I32 = mybir.dt.int32
U32 = mybir.dt.uint32
U8 = mybir.dt.uint8
AF = mybir.ActivationFunctionType
ALU = mybir.AluOpType

ENGS = ("pe", "act", "dve", "pool", "sp")
DEBUG = False
LIMIT = 99
STOPAT = None
MARKS = {}
NG = 4
SCAN = 2
NCORES = 4
EPS = 1e-6
NT = 34
NTL = 32
NEG = -30000.0


class Dep:
    __slots__ = ("w", "r", "excl")

    def __init__(self, excl=False):
        self.w = None
        self.r = []
        self.excl = excl


class Instr:
    __slots__ = ("eng", "fn", "dma", "deps", "sig", "sem", "val")

    def __init__(self, eng, fn, dma):
        self.eng = eng
        self.fn = fn
        self.dma = dma
        self.deps = set()
        self.sig = False
        self.sem = None
        self.val = 0


def _nop_fn(eng):
    return eng.nop()


class Prog:
    def __init__(self, nc, n_dma_sems=12):
        self.nc = nc
        self.lists = {e: [] for e in ENGS}
        self.last = {e: None for e in ENGS}
        self.n_dma_sems = n_dma_sems
        self.dma_count = {e: 0 for e in ENGS}
        self.dma_ring = {e: [None] * n_dma_sems for e in ENGS}

    def emit(self, eng, fn, reads=(), writes=(), dma=False, extra=()):
        ins = Instr(eng, fn, dma)
        self.count = getattr(self, "count", 0) + 1
        if STOPAT is not None and self.count > STOPAT and not getattr(self, "in_finish", False):
            return ins
        for d in reads:
            if d.w is not None:
                ins.deps.add(d.w)
            if d.excl:
                for r in d.r:
                    if r.eng != eng:
                        ins.deps.add(r)
            if not dma:
                d.r = [r for r in d.r if r.dma or r.eng != eng]
            d.r.append(ins)
        for d in writes:
            if d.w is not None:
                ins.deps.add(d.w)
            for r in d.r:
                ins.deps.add(r)
            d.w = ins
            d.r = []
        for p in extra:
            if p is not None:
                ins.deps.add(p)
        ins.deps.discard(ins)
        if dma:
            k = self.dma_count[eng]
            self.dma_count[eng] = k + 1
            slot = k % self.n_dma_sems
            prev = self.dma_ring[eng][slot]
            if prev is not None:
                ins.deps.add(prev)
            self.dma_ring[eng][slot] = ins
            ins.sem = (eng, slot)
        self.lists[eng].append(ins)
        self.last[eng] = ins
        return ins

    def barrier(self):
        ext = [self.last[e] for e in ENGS if self.last[e] is not None]
        for e in ENGS:
            for p in self.dma_ring[e]:
                if p is not None:
                    ext.append(p)
        b = self.emit("dve", lambda eng: eng.engine_nop(), extra=ext)
        for e in ENGS:
            if e != "dve":
                self.emit(e, _nop_fn, extra=[b])
        return b

    def finalize(self, stack):
        nc = self.nc
        for e in ENGS:
            for ins in self.lists[e]:
                for p in ins.deps:
                    if p.eng == "pe" and ins.eng == "pe" and not p.dma and not ins.dma:
                        continue
                    p.sig = True
        esem = {e: stack.enter_context(nc.semaphore("s_" + e)) for e in ENGS}
        dsem = {}
        for e in ENGS:
            for s in range(min(self.n_dma_sems, self.dma_count[e])):
                dsem[(e, s)] = stack.enter_context(nc.semaphore("d_%s_%d" % (e, s)))
        dcount = {k: 0 for k in dsem}
        for e in ENGS:
            c = 0
            for ins in self.lists[e]:
                if ins.dma:
                    dcount[ins.sem] += 16
                    ins.val = dcount[ins.sem]
                    ins.sig = True
                elif ins.sig:
                    c += 1
                    ins.val = c
                    ins.sem = e
        block = stack.enter_context(nc.Block())
        engobj = {"pe": "tensor", "act": "scalar", "dve": "vector", "pool": "gpsimd", "sp": "sync"}

        def make_body(e):
            def body(eng):
                known = {}
                for ins in self.lists[e]:
                    for p in ins.deps:
                        if p.eng == "pe" and e == "pe" and not p.dma and not ins.dma:
                            continue
                        if p.dma:
                            sem = dsem[p.sem]
                            key = ("d",) + p.sem
                        else:
                            sem = esem[p.eng]
                            key = ("e", p.eng)
                        if known.get(key, 0) >= p.val:
                            continue
                        known[key] = p.val
                        eng.wait_ge(sem, p.val)
                    r = ins.fn(eng)
                    if ins.dma:
                        r.then_inc(dsem[ins.sem], 16)
                    elif ins.sig:
                        r.then_inc(esem[e], 1)
            return body

        for e in ENGS:
            if self.lists[e]:
                getattr(block, engobj[e])(make_body(e))


class Arena:
    def __init__(self, ap_u8, nbytes):
        self.a = ap_u8
        self.n = nbytes
        self.top = 0

    def alloc(self, shape, dt):
        esz = {F32: 4, BF16: 2, I32: 4, U32: 4}[dt]
        free = 1
        for s in shape[1:]:
            free *= s
        nb = (free * esz + 63) // 64 * 64
        assert self.top + nb <= self.n, ("SBUF arena overflow", self.top, nb, self.n)
        v = self.a[0:shape[0], self.top:self.top + free * esz].bitcast(dt)
        self.top += nb
        if len(shape) == 3:
            v = v.rearrange("p (a b) -> p a b", b=shape[2])
        return v


class T:
    def __init__(self, ap, nsub=0):
        self.ap = ap
        self.d = Dep()
        self.ds = [Dep() for _ in range(nsub)]


def build_program():
    nc = bass.Bass("TRN2", target_bir_lowering=False)
    st = ExitStack()
    P = Prog(nc)

    def finish():
        P.in_finish = True
        P.barrier()
        P.finalize(st)
        st.close()
        return nc

    def din(name, shape, dt=F32):
        return nc.dram_tensor(name, list(shape), dt, kind="ExternalInput").ap()

    def dscr(name, shape, dt):
        return nc.dram_tensor(name, list(shape), dt, kind="ExternalOutput" if DEBUG else "Internal").ap()

    xin = din("xin", [NT, 128, 2048])
    cvec = din("cvec", [128, 16, 2])
    w_mod = din("w_mod", [128, 16, 12288])
    b_mod = din("b_mod", [1, 12288])
    nmix = din("nmix", [1, 2048])
    nffn = din("nffn", [1, 2048])
    nfin = din("nfin", [1, 2048])
    wg_ssm = din("wg_ssm", [4, 128, 16, 1296]) if LIMIT >= 2 else None
    wg_sc = din("wg_sc", [4, 128, 16, 1536]) if LIMIT >= 3 else None
    cw_ssm = din("cw_ssm", [4, 128, 6, 3])
    cb_ssm = din("cb_ssm", [4, 128, 6])
    cw_sc = din("cw_sc", [4, 128, 4, 3])
    scw = din("scw", [4, 128, 4])
    dtb = din("dtb", [4, 1, 16])
    alog = din("alog", [4, 1, 16])
    dsk = din("dsk", [4, 1, 512])
    ssm_nw = din("ssm_nw", [4, 1, 512])
    w_out = din("w_out", [2, 128, 32, 1024]) if LIMIT >= 4 else None
    w_rt = din("w_rt", [128, 16, 16])
    if LIMIT >= 6:
        w_gate = din("w_gate", [16, 2, 128, 16, 512])
        w_up = din("w_up", [16, 2, 128, 16, 512])
        w_down = din("w_down", [16, 2, 128, 8, 1024])
    c_ident = din("c_ident", [128, 128])
    c_tri = din("c_tri", [2, 128, 128])
    c_mask = din("c_mask", [2, 128, 512])
    out = nc.dram_tensor("out", [NTL, 128, 2048], F32, kind="ExternalOutput").ap()

    mod_d = T(dscr("mod_d", [2, 12288], F32))
    aT_d = T(dscr("aT_d", [9, 128, 16, 512], BF16), 9)
    z_d = T(dscr("z_d", [4, NTL, 128, 512], BF16), 4 * NTL)
    yT_d = T(dscr("yT_d", [NTL, 128, 32, 128], BF16), NTL)
    hacc_h = [T(dscr("hacc_d%d" % i, [NTL * 128, 1024], F32), NTL) for i in range(2)]
    f_d = T(dscr("f_d", [NTL * 128, 2048], BF16))
    dbg = {}
    if DEBUG:
        dbg["h1"] = nc.dram_tensor("dbg_h1", [NTL * 128, 2048], F32, kind="ExternalOutput").ap()
        dbg["aff"] = nc.dram_tensor("dbg_aff", [16, 4096], F32, kind="ExternalOutput").ap()
        dbg["vals"] = nc.dram_tensor("dbg_vals", [16, 512], F32, kind="ExternalOutput").ap()
        dbg["idx"] = nc.dram_tensor("dbg_idx", [16, 512], U32, kind="ExternalOutput").ap()
        dbg["rsc"] = nc.dram_tensor("dbg_rsc", [128, NTL], F32, kind="ExternalOutput").ap()
        dbg["dt"] = nc.dram_tensor("dbg_dt", [4, 128, NT, 16], F32, kind="ExternalOutput").ap()
        dbg["xs"] = nc.dram_tensor("dbg_xs", [4, 128, NT, 512], BF16, kind="ExternalOutput").ap()

    sb_bytes = (int(nc.sbuf_bytes_remaining) - 2048) // 256 * 256
    arena_t = st.enter_context(nc.sbuf_tensor("arena", [128, sb_bytes], U8))
    AR = Arena(arena_t[:, :], sb_bytes)
    banks = [st.enter_context(nc.psum_tensor("bank%d" % i, [128, 512], F32)) for i in range(8)]
    PS = [T(b[:, :]) for b in banks]
    for p_ in PS:
        p_.d.excl = True

    def psbf(i):
        return banks[i][:, :].bitcast(BF16)

    def MM(out, lhsT, rhs, start, stop, rd, wr):
        return P.emit("pe", lambda e: e.matmul(out, lhsT=lhsT, rhs=rhs, start=start, stop=stop), rd, wr)

    def TR(out, in_, ident, rd, wr):
        return P.emit("pe", lambda e: e.transpose(out, in_, ident), rd, wr)

    def ACT(out, in_, func, rd, wr, bias=None, scale=None, accum=None):
        kw = {}
        if bias is not None:
            kw["bias"] = bias
        if scale is not None:
            kw["scale"] = scale
        if accum is not None:
            kw["accum_out"] = accum
        return P.emit("act", lambda e: e.activation(out=out, in_=in_, func=func, **kw), rd, wr)

    def TT(eng, out, in0, in1, op, rd, wr):
        return P.emit(eng, lambda e: e.tensor_tensor(out=out, in0=in0, in1=in1, op=op), rd, wr)

    def TS(eng, out, in0, s1, s2, op0, op1, rd, wr):
        if s2 is None:
            return P.emit(eng, lambda e: e.tensor_scalar(out=out, in0=in0, scalar1=s1, scalar2=None, op0=op0), rd, wr)
        return P.emit(eng, lambda e: e.tensor_scalar(out=out, in0=in0, scalar1=s1, scalar2=s2, op0=op0, op1=op1), rd, wr)

    def STT(eng, out, in0, scalar, in1, op0, op1, rd, wr):
        return P.emit(eng, lambda e: e.scalar_tensor_tensor(out=out, in0=in0, scalar=scalar, in1=in1, op0=op0, op1=op1), rd, wr)

    def CP(eng, out, in_, rd, wr):
        if eng == "act":
            return P.emit("act", lambda e: e.copy(out=out, in_=in_), rd, wr)
        return P.emit(eng, lambda e: e.tensor_copy(out=out, in_=in_), rd, wr)

    def MEMSET(eng, ap, val, wr):
        return P.emit(eng, lambda e: e.memset(ap, val), (), wr)

    def RECIP(out, in_, rd, wr):
        return P.emit("dve", lambda e: e.reciprocal(out=out, in_=in_), rd, wr)

    def DMA(q, out, in_, rd, wr):
        return P.emit(q, lambda e: e.dma_start(out=out, in_=in_), rd, wr, dma=True)

    def rstd_from_ss(ss_ap, std_ap, r_ap, n, dss, dstd, dr):
        ACT(std_ap, ss_ap, AF.Sqrt, [dss], [dstd], bias=epsc.ap[0:ss_ap.shape[0], :], scale=1.0 / n)
        RECIP(r_ap, std_ap, [dstd], [dr])

    ident32 = T(AR.alloc([128, 128], F32))
    identbf = T(AR.alloc([128, 128], BF16))
    ones32 = T(AR.alloc([128, 128], F32))
    onesbf = T(AR.alloc([128, 8], BF16))
    epsc = T(AR.alloc([128, 1], F32))
    DMA("sp", ident32.ap, c_ident, [], [ident32.d])
    DMA("pool", identbf.ap, c_ident, [], [identbf.d])
    MEMSET("dve", ones32.ap, 1.0, [ones32.d])
    MEMSET("dve", onesbf.ap, 1.0, [onesbf.d])
    MEMSET("dve", epsc.ap, EPS, [epsc.d])
    persist_mark = AR.top

    cv = T(AR.alloc([128, 16, 2], F32))
    cs_ = T(AR.alloc([128, 16, 2], F32))
    DMA("sp", cv.ap, cvec, [], [cv.d])
    ACT(cs_.ap, cv.ap, AF.Silu, [cv.d], [cs_.d])
    wch = [T(AR.alloc([128, 16, 512], F32)) for _ in range(3)]
    bch = [T(AR.alloc([2, 512], F32)) for _ in range(2)]
    mch = [T(AR.alloc([2, 512], F32)) for _ in range(2)]
    for n in range(24):
        w = wch[n % 3]
        cols = slice(n * 512, (n + 1) * 512)
        DMA("sp", w.ap[:, 0:8, :], w_mod[:, 0:8, cols], [], [w.d])
        DMA("act", w.ap[:, 8:16, :], w_mod[:, 8:16, cols], [], [w.d])
        bb = bch[n % 2]
        DMA("sp", bb.ap, b_mod[0:1, cols].partition_broadcast(2), [], [bb.d])
        pb = PS[n % 2]
        for kc in range(16):
            MM(pb.ap[0:2, :], cs_.ap[:, kc, :], w.ap[:, kc, :], kc == 0, kc == 15, [cs_.d, w.d], [pb.d])
        m = mch[n % 2]
        TT("dve", m.ap, pb.ap[0:2, :], bb.ap, ALU.add, [pb.d, bb.d], [m.d])
        if n // 4 in (1, 4):
            TS("dve", m.ap, m.ap, 1.0, None, ALU.add, None, [m.d], [m.d])
        DMA("sp", mod_d.ap[:, cols], m.ap, [m.d], [mod_d.d])
    P.barrier()
    if LIMIT == 0:
        return finish()
    AR.top = persist_mark

    def load_mod_bc(dst, row, seg, q="sp"):
        DMA(q, dst.ap, mod_d.ap[row:row + 1, seg * 2048:(seg + 1) * 2048].partition_broadcast(128), [mod_d.d], [dst.d])

    def load_vec_bc(dst, src, q="sp"):
        DMA(q, dst.ap, src.partition_broadcast(128), [], [dst.d])

    A1 = [T(AR.alloc([128, 2048], F32)) for _ in range(2)]
    B1 = [T(AR.alloc([128, 2048], F32)) for _ in range(2)]
    nmb = T(AR.alloc([128, 2048], F32))
    load_vec_bc(nmb, nmix)
    for r in range(2):
        load_mod_bc(A1[r], r, 1)
        load_mod_bc(B1[r], r, 0)
        TT("pool", A1[r].ap, A1[r].ap, nmb.ap, ALU.mult, [A1[r].d, nmb.d], [A1[r].d])
    xt = [T(AR.alloc([128, 2048], F32)) for _ in range(2)]
    tmp = [T(AR.alloc([128, 2048], F32)) for _ in range(2)]
    abf = [T(AR.alloc([128, 2048], BF16)) for _ in range(2)]
    junk = T(AR.alloc([128, 2048], BF16))
    ss1 = T(AR.alloc([128, NT], F32), NT)
    sd1 = T(AR.alloc([128, NT], F32), NT)
    rs1 = T(AR.alloc([128, NT], F32), NT)
    aTb = [T(AR.alloc([128, 16, 512], BF16)) for _ in range(2)]
    MEMSET("dve", ss1.ap, 0.0, ss1.ds)
    order1 = []
    for tb in range(9):
        tiles = [0, 1] if tb == 0 else [2 + 4 * (tb - 1) + i for i in range(4)]
        for j, t in enumerate(tiles):
            order1.append((tb, j, t, j == len(tiles) - 1, 128 * len(tiles)))

    def p1_load(t):
        DMA("sp", xt[t % 2].ap, xin[t], [], [xt[t % 2].d])

    def p1_stats(t):
        x_ = xt[t % 2]
        ACT(junk.ap, x_.ap, AF.Square, [x_.d], [junk.d, ss1.ds[t]], accum=ss1.ap[:, t:t + 1])
        rstd_from_ss(ss1.ap[:, t:t + 1], sd1.ap[:, t:t + 1], rs1.ap[:, t:t + 1], 2048.0, ss1.ds[t], sd1.ds[t], rs1.ds[t])

    p1_load(0)
    p1_stats(0)
    for i1, (tb, j, t, lastj, ntok) in enumerate(order1):
        ab = aTb[tb % 2]
        r = 1 if t < 2 else 0
        x_ = xt[t % 2]
        tm = tmp[t % 2]
        STT("dve", tm.ap, x_.ap, rs1.ap[:, t:t + 1], A1[r].ap, ALU.mult, ALU.mult, [x_.d, rs1.ds[t], A1[r].d], [tm.d])
        if i1 + 1 < len(order1):
            p1_load(order1[i1 + 1][2])
            p1_stats(order1[i1 + 1][2])
        a_ = abf[t % 2]
        TT("pool", a_.ap, tm.ap, B1[r].ap, ALU.add, [tm.d, B1[r].d], [a_.d])
        for q in range(4):
            pb = PS[2 + (q % 2)]
            pv = psbf(2 + (q % 2))
            for i in range(4):
                kc = 4 * q + i
                TR(pv[:, i * 128:(i + 1) * 128], a_.ap[:, kc * 128:(kc + 1) * 128], identbf.ap, [a_.d, identbf.d], [pb.d])
            CP("act" if q % 2 == 0 else "dve", ab.ap[:, 4 * q:4 * q + 4, j * 128:(j + 1) * 128],
               pv[:, 0:512].rearrange("p (a b) -> p a b", b=128), [pb.d], [ab.d])
        if lastj:
            DMA("sp", aT_d.ap[tb][:, :, 0:ntok], ab.ap[:, :, 0:ntok], [ab.d], [aT_d.ds[tb]])
    P.barrier()
    if LIMIT == 1:
        return finish()
    AR.top = persist_mark

    tri = [T(AR.alloc([128, 128], F32)) for _ in range(2)]
    maskn = [T(AR.alloc([128, 512], BF16)) for _ in range(2)]
    for d_ in range(2):
        DMA("sp", tri[d_].ap, c_tri[d_], [], [tri[d_].d])
        DMA("pool", maskn[d_].ap, c_mask[d_], [], [maskn[d_].d])
    xs = T(AR.alloc([128, NT, 512], BF16), NT)
    Btok = T(AR.alloc([128, NT, 128], BF16), NT)
    BT = T(AR.alloc([128, NT * 128], BF16), NT)
    CT = T(AR.alloc([128, NT * 128], BF16), NT)
    dtr = T(AR.alloc([128, NT, 16], F32))
    dtv = T(AR.alloc([128, NT, 16], F32))
    lav = T(AR.alloc([128, NT, 16], F32))
    cwt = T(AR.alloc([128, 6, 3], F32))
    cbt = T(AR.alloc([128, 6], F32))
    dtbb = T(AR.alloc([128, 16], F32))
    aneg = T(AR.alloc([128, 16], F32))
    Dbc = T(AR.alloc([128, 512], F32))
    nwb = T(AR.alloc([128, 512], F32))
    p2_mark = AR.top

    for g in range(NG):
        DMA("sp", cwt.ap, cw_ssm[g], [], [cwt.d])
        DMA("sp", cbt.ap, cb_ssm[g], [], [cbt.d])
        load_vec_bc(dtbb, dtb[g])
        load_vec_bc(aneg, alog[g])
        load_vec_bc(Dbc, dsk[g])
        load_vec_bc(nwb, ssm_nw[g])
        ACT(aneg.ap, aneg.ap, AF.Exp, [aneg.d], [aneg.d])
        TS("dve", aneg.ap, aneg.ap, -1.0, None, ALU.mult, None, [aneg.d], [aneg.d])
        wg = T(AR.alloc([128, 16, 1296], BF16))
        for k2 in range(8):
            DMA("pool", wg.ap[:, 2 * k2:2 * k2 + 2, :], wg_ssm[g][:, 2 * k2:2 * k2 + 2, :], [], [wg.d])
        aTl = [T(AR.alloc([128, 16, 512], BF16)) for _ in range(2)]
        acc = [T(AR.alloc([128, 512], F32)) for _ in range(2)]
        xTt = [[T(AR.alloc([128, 512], BF16)) for _ in range(4)] for _ in range(2)]
        zst = [T(AR.alloc([128, 512], BF16)) for _ in range(2)]
        fmi = 0
        zi = 0
        MARKS.setdefault('A', P.count)
        for tb in range(9):
            ntok = 256 if tb == 0 else 512
            W = 256 if tb == 0 else 64
            tiles = [0, 1] if tb == 0 else [2 + 4 * (tb - 1) + i for i in range(4)]
            tok0 = tiles[0] * 128
            if tb == 1:
                MARKS.setdefault('C', P.count)
            if tb == 2:
                MARKS.setdefault('D', P.count)
            al = aTl[tb % 2]
            if tb == 0:
                DMA("sp", al.ap[:, :, 0:ntok], aT_d.ap[tb][:, :, 0:ntok], [aT_d.ds[tb]], [al.d])
            if tb + 1 < 9:
                DMA("sp", aTl[(tb + 1) % 2].ap, aT_d.ap[tb + 1], [aT_d.ds[tb + 1]], [aTl[(tb + 1) % 2].d])
            xTb = xTt[tb % 2]
            for oc in range(6):
                pb = PS[fmi % 2]
                fmi += 1
                for kc in range(16):
                    MM(pb.ap[:, 0:ntok], wg.ap[:, kc, oc * 128:(oc + 1) * 128], al.ap[:, kc, 0:ntok], kc == 0, kc == 15,
                       [wg.d, al.d], [pb.d])
                ac = acc[oc % 2]
                ACT(ac.ap[:, 0:ntok], pb.ap[:, 0:ntok], AF.Identity, [pb.d, cwt.d, cbt.d], [ac.d],
                    bias=cbt.ap[:, oc:oc + 1], scale=cwt.ap[:, oc, 1:2])
                u3 = pb.ap[:, 0:ntok].rearrange("p (r w) -> p r w", w=W)
                a3 = ac.ap[:, 0:ntok].rearrange("p (r w) -> p r w", w=W)
                STT("dve", a3[:, :, 1:W], u3[:, :, 0:W - 1], cwt.ap[:, oc, 0:1], a3[:, :, 1:W], ALU.mult, ALU.add,
                    [pb.d, cwt.d, ac.d], [ac.d])
                STT("dve", a3[:, :, 0:W - 1], u3[:, :, 1:W], cwt.ap[:, oc, 2:3], a3[:, :, 0:W - 1], ALU.mult, ALU.add,
                    [pb.d, cwt.d, ac.d], [ac.d])
                if oc < 4:
                    ACT(xTb[oc].ap[:, 0:ntok], ac.ap[:, 0:ntok], AF.Silu, [ac.d], [xTb[oc].d])
                elif oc == 4:
                    ACT(BT.ap[:, tok0:tok0 + ntok], ac.ap[:, 0:ntok], AF.Silu, [ac.d], [BT.ds[t] for t in tiles])
                else:
                    ACT(CT.ap[:, tok0:tok0 + ntok], ac.ap[:, 0:ntok], AF.Silu, [ac.d], [CT.ds[t] for t in tiles])
            MARKS.setdefault('B', P.count)
            for j, t in enumerate(tiles):
                pb = PS[2 + (t % 2)]
                pv = psbf(2 + (t % 2))
                for oc in range(4):
                    TR(pv[:, oc * 128:(oc + 1) * 128], xTb[oc].ap[:, j * 128:(j + 1) * 128], identbf.ap,
                       [xTb[oc].d, identbf.d], [pb.d])
                TR(psbf(7)[:, 0:128], BT.ap[:, t * 128:(t + 1) * 128], identbf.ap, [BT.ds[t], identbf.d], [PS[7].d])
                CP("dve", xs.ap[:, t, :], pv[:, 0:512], [pb.d], [xs.ds[t]])
                CP("act", Btok.ap[:, t, :], psbf(7)[:, 0:128], [PS[7].d], [Btok.ds[t]])
                if t >= 2:
                    pz = PS[4 + (zi % 2)]
                    zz = zst[zi % 2]
                    zi += 1
                    for kc in range(16):
                        MM(pz.ap, al.ap[:, kc, j * 128:(j + 1) * 128], wg.ap[:, kc, 768:1280], kc == 0, kc == 15,
                           [al.d, wg.d], [pz.d])
                    ACT(zz.ap, pz.ap, AF.Silu, [pz.d], [zz.d])
                    DMA("sp", z_d.ap[g, t - 2], zz.ap, [zz.d], [z_d.ds[g * NTL + t - 2]])
                pd = PS[6]
                for kc in range(16):
                    MM(pd.ap[:, 0:16], al.ap[:, kc, j * 128:(j + 1) * 128], wg.ap[:, kc, 1280:1296], kc == 0, kc == 15,
                       [al.d, wg.d], [pd.d])
                TT("dve", dtr.ap[:, t, :], pd.ap[:, 0:16], dtbb.ap, ALU.add, [pd.d, dtbb.d], [dtr.d])
        MARKS.setdefault('E', P.count)
        ACT(dtv.ap, dtr.ap, AF.Exp, [dtr.d], [dtv.d])
        ACT(dtv.ap, dtv.ap, AF.Ln, [dtv.d], [dtv.d], bias=ones32.ap[:, 0:1])
        TT("dve", lav.ap, dtv.ap, aneg.ap.unsqueeze(1).to_broadcast([128, NT, 16]), ALU.mult, [dtv.d, aneg.d], [lav.d])
        if DEBUG:
            DMA("sp", dbg["dt"][g], dtv.ap, [dtv.d], [])
            DMA("sp", dbg["xs"][g], xs.ap, xs.ds, [])
        P.barrier()
        AR.top = p2_mark

        yacc = T(AR.alloc([128, NTL, 512], BF16), NTL)
        S32 = T(AR.alloc([128, 512], F32))
        Sbf = T(AR.alloc([128, 512], BF16))
        rc = [T(AR.alloc([128, 8, 128], F32)) for _ in range(2)]
        Lall = [T(AR.alloc([128, 8, 128], BF16), 8) for _ in range(2)]
        Mall = [T(AR.alloc([128, 8, 128], BF16), 8) for _ in range(2)]
        cbT = [T(AR.alloc([128, 128], BF16)) for _ in range(2)]
        smal = [T(AR.alloc([128, 32], F32)) for _ in range(2)]
        xw = [T(AR.alloc([128, 512], BF16)) for _ in range(2)]
        tA = [T(AR.alloc([128, 512], F32)) for _ in range(2)]
        tB = [T(AR.alloc([128, 512], F32)) for _ in range(2)]
        zt = [T(AR.alloc([128, 512], BF16)) for _ in range(2)]
        ynb = [T(AR.alloc([128, 512], BF16)) for _ in range(2)]
        yTo = [T(AR.alloc([128, 4, 128], BF16)) for _ in range(2)]
        gst = [T(AR.alloc([128, 4], F32)) for _ in range(2)]
        junk2 = T(AR.alloc([128, 512], BF16))
        def stageA(d_, c, k):
            lat = c >= 2
            last = 127 if d_ == 0 else 0
            la_c = lav.ap[:, c, d_ * 8:(d_ + 1) * 8]
            dt_c = dtv.ap[:, c, d_ * 8:(d_ + 1) * 8]
            sm = smal[k]
            pS = PS[7]
            MM(pS.ap[:, 0:8], tri[d_].ap, la_c, True, True, [tri[d_].d, lav.d], [pS.d])
            MM(pS.ap[:, 8:16], ones32.ap, la_c, True, True, [ones32.d, lav.d], [pS.d])
            TT("dve", rc[k].ap, tri[d_].ap.unsqueeze(1).to_broadcast([128, 8, 128]),
               la_c.unsqueeze(2).to_broadcast([128, 8, 128]), ALU.mult, [tri[d_].d, lav.d], [rc[k].d])
            Rb = [PS[0], PS[1]]
            for hf in range(2):
                MM(Rb[hf].ap, ones32.ap, rc[k].ap[:, 4 * hf:4 * hf + 4, :].rearrange("p a b -> p (a b)"), True, False,
                   [ones32.d, rc[k].d], [Rb[hf].d])
                MM(Rb[hf].ap, identbf.ap, maskn[d_].ap, False, True, [identbf.d, maskn[d_].d], [Rb[hf].d])
            ACT(sm.ap[:, 0:8], pS.ap[:, 0:8], AF.Identity, [pS.d], [sm.d], scale=-1.0)
            ACT(sm.ap[:, 8:24], pS.ap[:, 0:16], AF.Exp, [pS.d], [sm.d])
            if lat:
                MM(pS.ap[:, 128:256], BT.ap[:, c * 128:(c + 1) * 128], CT.ap[:, c * 128:(c + 1) * 128], True, True,
                   [BT.ds[c], CT.ds[c]], [pS.d])
                CP("act", cbT[k].ap, pS.ap[:, 128:256], [pS.d], [cbT[k].d])
            La = Lall[k]
            for h in range(8):
                ACT(La.ap[:, h, :], Rb[h // 4].ap[:, (h % 4) * 128:(h % 4 + 1) * 128], AF.Exp, [Rb[h // 4].d, sm.d],
                    [La.ds[h]], bias=sm.ap[:, h:h + 1])
            TT("dve", sm.ap[:, 24:32], La.ap[:, :, last], dt_c, ALU.mult, La.ds + [dtv.d], [sm.d])
            TT("pool", xw[k].ap.rearrange("p (h d) -> p h d", d=64), xs.ap[:, c, :].rearrange("p (h d) -> p h d", d=64),
               sm.ap[:, 24:32].unsqueeze(2).to_broadcast([128, 8, 64]), ALU.mult, [xs.ds[c], sm.d], [xw[k].d])
            if lat:
                Ma = Mall[k]
                for h in range(8):
                    STT("dve", Ma.ap[:, h, :], La.ap[:, h, :], dt_c[:, h:h + 1], cbT[k].ap, ALU.mult, ALU.mult,
                        [La.ds[h], dtv.d, cbT[k].d], [Ma.ds[h]])
                for h in range(8):
                    MM(PS[2 + k].ap[:, h * 64:(h + 1) * 64], Ma.ap[:, h, :], xs.ap[:, c, h * 64:(h + 1) * 64], True, True,
                       [Ma.ds[h], xs.ds[c]], [PS[2 + k].d])
            MM(PS[4 + k].ap, Btok.ap[:, c, :], xw[k].ap, True, True, [Btok.ds[c], xw[k].d], [PS[4 + k].d])

        def stageB(d_, c, k):
            lat = c >= 2
            sm = smal[k]
            Pd, Ps_, Po = PS[2 + k], PS[4 + k], PS[6]
            if lat:
                MM(Po.ap, CT.ap[:, c * 128:(c + 1) * 128], Sbf.ap, True, True, [CT.ds[c], Sbf.d], [Po.d])
                TT("dve", tB[k].ap.rearrange("p (h d) -> p h d", d=64), Po.ap.rearrange("p (h d) -> p h d", d=64),
                   sm.ap[:, 8:16].unsqueeze(2).to_broadcast([128, 8, 64]), ALU.mult, [Po.d, sm.d], [tB[k].d])
                TT("dve", tB[k].ap, tB[k].ap, Pd.ap, ALU.add, [tB[k].d, Pd.d], [tB[k].d])
            TT("dve", S32.ap.rearrange("p (h d) -> p h d", d=64), S32.ap.rearrange("p (h d) -> p h d", d=64),
               sm.ap[:, 16:24].unsqueeze(2).to_broadcast([128, 8, 64]), ALU.mult, [S32.d, sm.d], [S32.d])
            TT("dve", S32.ap, S32.ap, Ps_.ap, ALU.add, [S32.d, Ps_.d], [S32.d])
            CP("act", Sbf.ap, S32.ap, [S32.d], [Sbf.d])
            if lat and d_ == 0:
                TT("pool", tA[k].ap, xs.ap[:, c, :], Dbc.ap, ALU.mult, [xs.ds[c], Dbc.d], [tA[k].d])
                TT("pool", yacc.ap[:, c - 2, :], tA[k].ap, tB[k].ap, ALU.add, [tA[k].d, tB[k].d], [yacc.ds[c - 2]])
            if lat and d_ == 1:
                DMA("sp", zt[k].ap, z_d.ap[g, c - 2], [z_d.ds[g * NTL + c - 2]], [zt[k].d])
                TT("pool", tA[k].ap, tB[k].ap, yacc.ap[:, c - 2, :], ALU.add, [tB[k].d, yacc.ds[c - 2]], [tA[k].d])
                TT("pool", tA[k].ap, tA[k].ap, zt[k].ap, ALU.mult, [tA[k].d, zt[k].d], [tA[k].d])
                gs = gst[k]
                MEMSET("pool", gs.ap[:, 0:1], 0.0, [gs.d])
                ACT(junk2.ap, tA[k].ap, AF.Square, [tA[k].d], [junk2.d, gs.d], accum=gs.ap[:, 0:1])
                rstd_from_ss(gs.ap[:, 0:1], gs.ap[:, 1:2], gs.ap[:, 2:3], 512.0, gs.d, gs.d, gs.d)
                STT("dve", ynb[k].ap, tA[k].ap, gs.ap[:, 2:3], nwb.ap, ALU.mult, ALU.mult, [tA[k].d, gs.d, nwb.d], [ynb[k].d])
                pv = psbf(7)
                for q in range(4):
                    TR(pv[:, 512 + q * 128:512 + (q + 1) * 128], ynb[k].ap[:, q * 128:(q + 1) * 128], identbf.ap,
                       [ynb[k].d, identbf.d], [PS[7].d])
                CP("act", yTo[k].ap, pv[:, 512:1024].rearrange("p (a b) -> p a b", b=128), [PS[7].d], [yTo[k].d])
                DMA("sp", yT_d.ap[c - 2][:, g * 4:(g + 1) * 4, :], yTo[k].ap, [yTo[k].d], [yT_d.ds[c - 2]])

        items = []
        for d_ in range(SCAN):
            order = list(range(NT)) if d_ == 0 else [1, 0] + list(range(NT - 1, 1, -1))
            items += [(d_, c, ci == 0) for ci, c in enumerate(order)]
        if items:
            stageA(items[0][0], items[0][1], 0)
        for i_, (d_, c, first) in enumerate(items):
            if i_ + 1 < len(items):
                stageA(items[i_ + 1][0], items[i_ + 1][1], (i_ + 1) % 2)
            if first:
                MEMSET("dve", S32.ap, 0.0, [S32.d])
                MEMSET("pool", Sbf.ap, 0.0, [Sbf.d])
            stageB(d_, c, i_ % 2)
        P.barrier()
        AR.top = p2_mark
    AR.top = persist_mark
    if LIMIT == 2:
        return finish()

    ss_sc = T(AR.alloc([128, NTL], F32))
    rs_sc = T(AR.alloc([128, NTL], F32))
    sd_sc = T(AR.alloc([128, NTL], F32))
    MEMSET("dve", ss_sc.ap, 0.0, [ss_sc.d])
    p3_mark = AR.top
    wscs = [T(AR.alloc([128, 16, 1536], BF16)) for _ in range(2)]
    cwcs = [T(AR.alloc([128, 4, 3], F32)) for _ in range(2)]
    scwts = [T(AR.alloc([128, 4], F32)) for _ in range(2)]

    def p3_wload(jb):
        DMA("sp", cwcs[jb % 2].ap, cw_sc[jb], [], [cwcs[jb % 2].d])
        DMA("sp", scwts[jb % 2].ap, scw[jb], [], [scwts[jb % 2].d])
        for k2 in range(8):
            DMA("pool", wscs[jb % 2].ap[:, 2 * k2:2 * k2 + 2, :], wg_sc[jb][:, 2 * k2:2 * k2 + 2, :], [], [wscs[jb % 2].d])
    aTl = [T(AR.alloc([128, 16, 512], BF16)) for _ in range(2)]
    cgs = [T(AR.alloc([128, 512], F32)) for _ in range(2)]
    vv = [T(AR.alloc([128, 512], F32)) for _ in range(2)]
    acc = [T(AR.alloc([128, 512], F32)) for _ in range(2)]
    q2 = [T(AR.alloc([128, 512], BF16)) for _ in range(4)]
    qw = [T(AR.alloc([128, 512], BF16)) for _ in range(4)]
    it = 0
    p3_wload(0)
    p3i = 0
    DMA("sp", aTl[0].ap, aT_d.ap[1], [aT_d.ds[1]], [aTl[0].d])
    for jb in range(4):
        wsc, cwc, scwt = wscs[jb % 2], cwcs[jb % 2], scwts[jb % 2]
        if jb + 1 < 4:
            p3_wload(jb + 1)
        for tb in range(1, 9):
            t0 = 4 * (tb - 1)
            al = aTl[p3i % 2]
            p3i += 1
            if p3i < 32:
                ntb = (p3i % 8) + 1
                DMA("sp", aTl[p3i % 2].ap, aT_d.ap[ntb], [aT_d.ds[ntb]], [aTl[p3i % 2].d])
            for cc in range(4):
                k = it % 2
                it += 1
                pbg, pcg, phv = PS[0 + 3 * k], PS[1 + 3 * k], PS[2 + 3 * k]
                for (pb, off) in ((pbg, 0), (pcg, 512), (phv, 1024)):
                    for kc in range(16):
                        MM(pb.ap, wsc.ap[:, kc, off + cc * 128:off + (cc + 1) * 128], al.ap[:, kc, :], kc == 0, kc == 15,
                           [wsc.d, al.d], [pb.d])
                CP("act", cgs[k].ap, pcg.ap, [pcg.d], [cgs[k].d])
                TT("dve", vv[k].ap, cgs[k].ap, phv.ap, ALU.mult, [cgs[k].d, phv.d], [vv[k].d])
                ACT(acc[k].ap, vv[k].ap, AF.Identity, [vv[k].d, cwc.d], [acc[k].d], scale=cwc.ap[:, cc, 1:2])
                v3 = vv[k].ap.rearrange("p (r w) -> p r w", w=64)
                a3 = acc[k].ap.rearrange("p (r w) -> p r w", w=64)
                STT("dve", a3[:, :, 1:64], v3[:, :, 0:63], cwc.ap[:, cc, 0:1], a3[:, :, 1:64], ALU.mult, ALU.add,
                    [vv[k].d, cwc.d, acc[k].d], [acc[k].d])
                STT("dve", a3[:, :, 0:63], v3[:, :, 1:64], cwc.ap[:, cc, 2:3], a3[:, :, 0:63], ALU.mult, ALU.add,
                    [vv[k].d, cwc.d, acc[k].d], [acc[k].d])
                TT("dve", acc[k].ap, acc[k].ap, pbg.ap, ALU.mult, [acc[k].d, pbg.d], [acc[k].d])
                ACT(q2[cc].ap, acc[k].ap, AF.Square, [acc[k].d], [q2[cc].d])
                TS("pool", qw[cc].ap, acc[k].ap, scwt.ap[:, cc:cc + 1], None, ALU.mult, None, [acc[k].d, scwt.d], [qw[cc].d])
                DMA("sp", yT_d.ap[t0:t0 + 4, :, 16 + jb * 4 + cc, :].rearrange("t p k -> p t k"),
                    qw[cc].ap.rearrange("p (t k) -> p t k", k=128), [qw[cc].d], [yT_d.ds[t0 + i] for i in range(4)])
            pss = PS[6 + (tb % 2)]
            for i in range(4):
                for cc in range(4):
                    MM(pss.ap[:, i:i + 1], q2[cc].ap[:, i * 128:(i + 1) * 128], onesbf.ap[:, 0:1], cc == 0, cc == 3,
                       [q2[cc].d, onesbf.d], [pss.d])
            TT("dve", ss_sc.ap[:, t0:t0 + 4], ss_sc.ap[:, t0:t0 + 4], pss.ap[:, 0:4], ALU.add, [ss_sc.d, pss.d], [ss_sc.d])
    rstd_from_ss(ss_sc.ap, sd_sc.ap, rs_sc.ap, 2048.0, ss_sc.d, sd_sc.d, rs_sc.d)
    if DEBUG:
        DMA("sp", dbg["rsc"], rs_sc.ap, [rs_sc.d], [])
    P.barrier()
    if LIMIT == 3:
        return finish()
    AR.top = p3_mark

    g1b = T(AR.alloc([128, 2048], F32))
    load_mod_bc(g1b, 0, 2)
    wos = [T(AR.alloc([128, 32, 1024], BF16)) for _ in range(2)]
    yt = [T(AR.alloc([128, 32, 128], BF16)) for _ in range(2)]
    xh = [T(AR.alloc([128, 1024], F32)) for _ in range(2)]
    t1 = [T(AR.alloc([128, 512], F32)) for _ in range(2)]
    ht = [T(AR.alloc([128, 1024], F32)) for _ in range(2)]
    it = 0
    for hf in range(2):
        for k4 in range(8):
            DMA("pool", wos[hf].ap[:, 4 * k4:4 * k4 + 4, :], w_out[hf][:, 4 * k4:4 * k4 + 4, :], [], [wos[hf].d])

    def p4_load(i4):
        hf_, t_ = i4 // NTL, i4 % NTL
        DMA("sp", yt[t_ % 2].ap, yT_d.ap[t_], [yT_d.ds[t_]], [yt[t_ % 2].d])
        DMA("sp", xh[t_ % 2].ap, xin[t_ + 2][:, hf_ * 1024:(hf_ + 1) * 1024], [], [xh[t_ % 2].d])

    p4_load(0)
    for hf in range(2):
        wo = wos[hf]
        for t in range(NTL):
            y_ = yt[t % 2]
            x_ = xh[t % 2]
            if hf * NTL + t + 1 < 2 * NTL:
                p4_load(hf * NTL + t + 1)
            h_ = ht[t % 2]
            for q in range(2):
                k = it % 2
                it += 1
                pm, pc = PS[0 + 2 * k], PS[1 + 2 * k]
                cols = slice(q * 512, (q + 1) * 512)
                for kc in range(16):
                    MM(pm.ap, y_.ap[:, kc, :], wo.ap[:, kc, cols], kc == 0, kc == 15, [y_.d, wo.d], [pm.d])
                for kc in range(16, 32):
                    MM(pc.ap, y_.ap[:, kc, :], wo.ap[:, kc, cols], kc == 16, kc == 31, [y_.d, wo.d], [pc.d])
                ACT(t1[k].ap, pc.ap, AF.Identity, [pc.d, rs_sc.d], [t1[k].d], scale=rs_sc.ap[:, t:t + 1])
                TT("dve", t1[k].ap, t1[k].ap, pm.ap, ALU.add, [t1[k].d, pm.d], [t1[k].d])
                gcols = slice(hf * 1024 + q * 512, hf * 1024 + (q + 1) * 512)
                TT("pool", t1[k].ap, t1[k].ap, g1b.ap[:, gcols], ALU.mult, [t1[k].d, g1b.d], [t1[k].d])
                TT("pool", h_.ap[:, cols], t1[k].ap, x_.ap[:, cols], ALU.add, [t1[k].d, x_.d], [h_.d])
            DMA("sp", hacc_h[hf].ap[t * 128:(t + 1) * 128, :], h_.ap, [h_.d], [hacc_h[hf].ds[t]])
    P.barrier()
    if LIMIT == 4:
        return finish()
    AR.top = persist_mark

    idxc = T(AR.alloc([128, 4, 16], I32))
    gatc = T(AR.alloc([128, 4, 16], F32))
    p5_mark = AR.top
    A2 = T(AR.alloc([128, 2048], F32))
    B2 = T(AR.alloc([128, 2048], F32))
    nfb = T(AR.alloc([128, 2048], F32))
    load_vec_bc(nfb, nffn)
    load_mod_bc(A2, 0, 4)
    load_mod_bc(B2, 0, 3)
    TT("pool", A2.ap, A2.ap, nfb.ap, ALU.mult, [A2.d, nfb.d], [A2.d])
    wr = T(AR.alloc([128, 16, 16], F32))
    DMA("sp", wr.ap, w_rt, [], [wr.d])
    affT = T(AR.alloc([16, 4096], F32))
    hx = [T(AR.alloc([128, 2048], F32)) for _ in range(2)]
    ff = [T(AR.alloc([128, 2048], F32)) for _ in range(2)]
    fb = [T(AR.alloc([128, 2048], BF16)) for _ in range(2)]
    fT = [T(AR.alloc([128, 16, 128], F32)) for _ in range(2)]
    st5 = [T(AR.alloc([128, 8], F32)) for _ in range(2)]
    ex = [T(AR.alloc([128, 16], F32)) for _ in range(2)]
    junk5 = T(AR.alloc([128, 2048], BF16))
    def p5_load(t_):
        for i2 in range(2):
            DMA("sp", hx[t_ % 2].ap[:, i2 * 1024:(i2 + 1) * 1024], hacc_h[i2].ap[t_ * 128:(t_ + 1) * 128, :],
                [hacc_h[i2].ds[t_]], [hx[t_ % 2].d])

    p5_load(0)
    for t in range(NTL):
        k = t % 2
        h_ = hx[k]
        if t + 1 < NTL:
            p5_load(t + 1)
        if DEBUG:
            DMA("sp", dbg["h1"][t * 128:(t + 1) * 128, :], h_.ap, [h_.d], [])
        s5 = st5[k]
        MEMSET("pool", s5.ap, 0.0, [s5.d])
        ACT(junk5.ap, h_.ap, AF.Square, [h_.d], [junk5.d, s5.d], accum=s5.ap[:, 0:1])
        rstd_from_ss(s5.ap[:, 0:1], s5.ap[:, 1:2], s5.ap[:, 2:3], 2048.0, s5.d, s5.d, s5.d)
        f_ = ff[k]
        STT("dve", f_.ap, h_.ap, s5.ap[:, 2:3], A2.ap, ALU.mult, ALU.mult, [h_.d, s5.d, A2.d], [f_.d])
        TT("pool", f_.ap, f_.ap, B2.ap, ALU.add, [f_.d, B2.d], [f_.d])
        ACT(fb[k].ap, f_.ap, AF.Copy, [f_.d], [fb[k].d])
        DMA("sp", f_d.ap[t * 128:(t + 1) * 128, :], fb[k].ap, [fb[k].d], [f_d.d])
        for q in range(4):
            pb = PS[q % 4]
            for i in range(4):
                kc = 4 * q + i
                TR(pb.ap[:, i * 128:(i + 1) * 128], f_.ap[:, kc * 128:(kc + 1) * 128], ident32.ap, [f_.d, ident32.d], [pb.d])
            CP("act" if q % 2 == 0 else "dve", fT[k].ap[:, 4 * q:4 * q + 4, :], pb.ap.rearrange("p (a b) -> p a b", b=128),
               [pb.d], [fT[k].d])
        pl = PS[4 + k]
        for kc in range(16):
            MM(pl.ap[:, 0:16], fT[k].ap[:, kc, :], wr.ap[:, kc, :], kc == 0, kc == 15, [fT[k].d, wr.d], [pl.d])
        P.emit("dve", (lambda o, i: (lambda e: e.reduce_max(out=o, in_=i, axis=mybir.AxisListType.X)))(s5.ap[:, 3:4], pl.ap[:, 0:16]),
               [pl.d], [s5.d])
        TS("dve", s5.ap[:, 4:5], s5.ap[:, 3:4], -1.0, None, ALU.mult, None, [s5.d], [s5.d])
        ACT(ex[k].ap, pl.ap[:, 0:16], AF.Exp, [pl.d, s5.d], [ex[k].d, s5.d], bias=s5.ap[:, 4:5], accum=s5.ap[:, 5:6])
        RECIP(s5.ap[:, 6:7], s5.ap[:, 5:6], [s5.d], [s5.d])
        TS("dve", ex[k].ap, ex[k].ap, s5.ap[:, 6:7], None, ALU.mult, None, [ex[k].d, s5.d], [ex[k].d])
        pa = PS[6 + k]
        TR(pa.ap[0:16, 0:128], ex[k].ap, ident32.ap, [ex[k].d, ident32.d], [pa.d])
        CP("act", affT.ap[:, t * 128:(t + 1) * 128], pa.ap[0:16, 0:128], [pa.d], [affT.d])
    if DEBUG:
        DMA("sp", dbg["aff"], affT.ap, [affT.d], [])
    vals = T(AR.alloc([16, 512], F32))
    idxu = T(AR.alloc([16, 512], U32))
    idxf = T(AR.alloc([16, 512], F32))
    for r in range(64):
        v8 = vals.ap[:, r * 8:(r + 1) * 8]
        i8 = idxu.ap[:, r * 8:(r + 1) * 8]
        P.emit("dve", (lambda o, i: (lambda e: e.max(out=o, in_=i)))(v8, affT.ap), [affT.d], [vals.d])
        P.emit("dve", (lambda o, m, i: (lambda e: e.max_index(out=o, in_max=m, in_values=i)))(i8, v8, affT.ap),
               [affT.d, vals.d], [idxu.d])
        P.emit("dve", (lambda o, m, i: (lambda e: e.match_replace(out=o, in_to_replace=m, in_values=i, imm_value=-1.0)))(affT.ap, v8, affT.ap),
               [vals.d, idxu.d], [affT.d])
    CP("dve", idxf.ap, idxu.ap, [idxu.d], [idxf.d])
    if DEBUG:
        DMA("sp", dbg["vals"], vals.ap, [vals.d], [])
        DMA("sp", dbg["idx"], idxu.ap, [idxu.d], [])
    for s in range(4):
        pa = PS[s % 2]
        TR(pa.ap[:, 0:16], idxf.ap[:, s * 128:(s + 1) * 128], ident32.ap[0:16, 0:16], [idxf.d, ident32.d], [pa.d])
        TR(pa.ap[:, 16:32], vals.ap[:, s * 128:(s + 1) * 128], ident32.ap[0:16, 0:16], [vals.d, ident32.d], [pa.d])
        CP("dve", idxc.ap[:, s, :], pa.ap[:, 0:16], [pa.d], [idxc.d])
        CP("act", gatc.ap[:, s, :], pa.ap[:, 16:32], [pa.d], [gatc.d])
    P.barrier()
    if LIMIT == 5:
        return finish()
    AR.top = p5_mark

    g2b = T(AR.alloc([128, 2048], F32))
    load_mod_bc(g2b, 0, 5)
    NW = 5
    wbuf = [T(AR.alloc([128, 8192], BF16)) for _ in range(NW)]
    xg = [T(AR.alloc([128, 2048], BF16)) for _ in range(8)]
    xeT = [T(AR.alloc([128, 16, 512], BF16)) for _ in range(2)]
    hidT = [T(AR.alloc([128, 8, 512], BF16), 8) for _ in range(2)]
    silt = [T(AR.alloc([128, 512], BF16)) for _ in range(2)]
    yet = [T(AR.alloc([128, 512], F32)) for _ in range(2)]
    yo = [T(AR.alloc([128, 1024], F32)) for _ in range(2)]
    hall = Dep()
    srcs_all = []
    for e_ in range(16):
        srcs_all += [(w_gate[e_, 0], 0), (w_up[e_, 0], 0), (w_gate[e_, 1], 0), (w_up[e_, 1], 0),
                     (w_down[e_, 0], 1), (w_down[e_, 1], 1)]
    piece_view = {}
    state6 = {"issued": 0}

    def ensure_issued(upto):
        while state6["issued"] < min(upto, len(srcs_all)):
            i = state6["issued"]
            src, kind = srcs_all[i]
            wb = wbuf[i % NW]
            if kind == 0:
                dst = wb.ap.rearrange("p (a b) -> p a b", b=512)
                DMA("pool", dst[:, 0:8, :], src[:, 0:8, :], [], [wb.d])
                DMA("pool", dst[:, 8:16, :], src[:, 8:16, :], [], [wb.d])
            else:
                dst = wb.ap.rearrange("p (a b) -> p a b", b=1024)
                DMA("pool", dst[:, 0:4, :], src[:, 0:4, :], [], [wb.d])
                DMA("pool", dst[:, 4:8, :], src[:, 4:8, :], [], [wb.d])
            piece_view[i] = (wb, dst)
            state6["issued"] = i + 1

    def gather_issue(e_):
        for s in range(4):
            xg_ = xg[(e_ % 2) * 4 + s]
            P.emit("pool", (lambda o, i, ix: (lambda e: e.indirect_dma_start(
                out=o, out_offset=None, in_=i, in_offset=bass.IndirectOffsetOnAxis(ap=ix, axis=0))))(
                    xg_.ap, f_d.ap, idxc.ap[:, s, e_:e_ + 1]), [f_d.d, idxc.d], [xg_.d], dma=True)

    yi = 0
    pgi = 0
    gather_issue(0)
    ensure_issued(NW - 1)
    for e_ in range(16):
        xe = xeT[e_ % 2]
        for s in range(4):
            xg_ = xg[(e_ % 2) * 4 + s]
            for q in range(4):
                pv = psbf(6 + (q % 2))
                pb = PS[6 + (q % 2)]
                for i in range(4):
                    kc = 4 * q + i
                    TR(pv[:, i * 128:(i + 1) * 128], xg_.ap[:, kc * 128:(kc + 1) * 128], identbf.ap, [xg_.d, identbf.d], [pb.d])
                CP("act" if q % 2 == 0 else "dve", xe.ap[:, 4 * q:4 * q + 4, s * 128:(s + 1) * 128],
                   pv[:, 0:512].rearrange("p (a b) -> p a b", b=128), [pb.d], [xe.d])
        if e_ + 1 < 16:
            gather_issue(e_ + 1)
        hT = hidT[e_ % 2]
        p0 = 6 * e_
        for fcu in range(8):
            if fcu % 4 == 0:
                ensure_issued(p0 + 2 * (fcu // 4) + NW)
            wgp, wgv = piece_view[p0 + 0 + 2 * (fcu // 4)]
            wup, wuv = piece_view[p0 + 1 + 2 * (fcu // 4)]
            off = (fcu % 4) * 128
            k = pgi % 2
            pgi += 1
            pg, pu = PS[0 + 2 * k], PS[1 + 2 * k]
            for kc in range(16):
                MM(pg.ap, wgv[:, kc, off:off + 128], xe.ap[:, kc, :], kc == 0, kc == 15, [wgp.d, xe.d], [pg.d])
            for kc in range(16):
                MM(pu.ap, wuv[:, kc, off:off + 128], xe.ap[:, kc, :], kc == 0, kc == 15, [wup.d, xe.d], [pu.d])
            ACT(silt[k].ap, pg.ap, AF.Silu, [pg.d], [silt[k].d])
            TT("dve", hT.ap[:, fcu, :], silt[k].ap, pu.ap, ALU.mult, [silt[k].d, pu.d], [hT.ds[fcu]])
        for dh in range(2):
            ensure_issued(p0 + 4 + dh + NW)
            wdp, wdv = piece_view[p0 + 4 + dh]
            for s in range(4):
                yo_ = yo[yi % 2]
                yi += 1
                for dq in range(2):
                    k = pgi % 2
                    pgi += 1
                    py = PS[4 + k]
                    for fc in range(8):
                        MM(py.ap, hT.ap[:, fc, s * 128:(s + 1) * 128], wdv[:, fc, dq * 512:(dq + 1) * 512], fc == 0, fc == 7,
                           [hT.ds[fc], wdp.d], [py.d])
                    ACT(yet[k].ap, py.ap, AF.Identity, [py.d, gatc.d], [yet[k].d], scale=gatc.ap[:, s, e_:e_ + 1])
                    gc = slice(dh * 1024 + dq * 512, dh * 1024 + (dq + 1) * 512)
                    TT("dve", yo_.ap[:, dq * 512:(dq + 1) * 512], yet[k].ap, g2b.ap[:, gc], ALU.mult,
                       [yet[k].d, g2b.d], [yo_.d])
                P.emit("pool", (lambda o, i, ix: (lambda e: e.indirect_dma_start(
                    out=o, out_offset=bass.IndirectOffsetOnAxis(ap=ix, axis=0), in_=i, in_offset=None, compute_op=ALU.add)))(
                        hacc_h[dh].ap, yo_.ap, idxc.ap[:, s, e_:e_ + 1]),
                    [yo_.d, idxc.d], [hall], dma=True)
    P.barrier()
    if LIMIT == 6:
        return finish()
    AR.top = persist_mark

    fnb = T(AR.alloc([128, 2048], F32))
    load_vec_bc(fnb, nfin)
    hx = [T(AR.alloc([128, 2048], F32)) for _ in range(2)]
    ob = [T(AR.alloc([128, 2048], F32)) for _ in range(2)]
    st7 = [T(AR.alloc([128, 4], F32)) for _ in range(2)]
    junk7 = T(AR.alloc([128, 2048], BF16))
    def p7_load(t_):
        for i2 in range(2):
            DMA("sp", hx[t_ % 2].ap[:, i2 * 1024:(i2 + 1) * 1024], hacc_h[i2].ap[t_ * 128:(t_ + 1) * 128, :], [hall], [hx[t_ % 2].d])

    p7_load(0)
    for t in range(NTL):
        k = t % 2
        if t + 1 < NTL:
            p7_load(t + 1)
        s7 = st7[k]
        MEMSET("pool", s7.ap, 0.0, [s7.d])
        ACT(junk7.ap, hx[k].ap, AF.Square, [hx[k].d], [junk7.d, s7.d], accum=s7.ap[:, 0:1])
        rstd_from_ss(s7.ap[:, 0:1], s7.ap[:, 1:2], s7.ap[:, 2:3], 2048.0, s7.d, s7.d, s7.d)
        STT("dve", ob[k].ap, hx[k].ap, s7.ap[:, 2:3], fnb.ap, ALU.mult, ALU.mult, [hx[k].d, s7.d, fnb.d], [ob[k].d])
        DMA("sp", out[t], ob[k].ap, [ob[k].d], [])
    P.barrier()
    P.finalize(st)
    st.close()
    return nc


def _kc_p(w):
    K, N = w.shape
    return np.ascontiguousarray(w.reshape(K // 128, 128, N).transpose(1, 0, 2))


def _prep_shared(c_ctx, w_mod, b_mod, norm_mix_w, w_in, ssm_conv_w, ssm_conv_b, ssm_a_log, ssm_dt_bias, ssm_d,
                 ssm_norm_w, sc_conv_w, sc_norm_w, w_out, norm_ffn_w, w_router, w_gate, w_up, w_down, final_norm_w):
    f = np.float32
    sh = {}
    sh["w_mod"] = np.ascontiguousarray(w_mod[0].reshape(128, 16, 12288))
    sh["b_mod"] = np.ascontiguousarray(b_mod[0].reshape(1, 12288))
    sh["nmix"] = norm_mix_w[0].reshape(1, 2048)
    sh["nffn"] = norm_ffn_w[0].reshape(1, 2048)
    sh["nfin"] = final_norm_w.reshape(1, 2048)
    win = w_in[0]
    wg, cw, cb = [], [], []
    for g in range(4):
        cols = np.concatenate([2048 + g * 512 + np.arange(512), 4096 + g * 128 + np.arange(128),
                               4608 + g * 128 + np.arange(128), g * 512 + np.arange(512),
                               5120 + g * 8 + np.arange(8), 5152 + g * 8 + np.arange(8)])
        wg.append(_kc_p(win[:, cols]))
        ch = np.concatenate([g * 512 + np.arange(512), 2048 + g * 128 + np.arange(128), 2560 + g * 128 + np.arange(128)])
        cw.append(ssm_conv_w[0][:, ch].T.reshape(6, 128, 3).transpose(1, 0, 2))
        cb.append(ssm_conv_b[0][ch].reshape(6, 128).T)
    sh["wg_ssm"] = np.ascontiguousarray(np.stack(wg)).astype(f)
    sh["cw_ssm"] = np.ascontiguousarray(np.stack(cw)).astype(f)
    sh["cb_ssm"] = np.ascontiguousarray(np.stack(cb)).astype(f)
    wsc, cws, scw = [], [], []
    for j in range(4):
        ch = j * 512 + np.arange(512)
        cols = np.concatenate([5184 + ch, 7232 + ch, 9280 + ch])
        wsc.append(_kc_p(win[:, cols]))
        cws.append(sc_conv_w[0][:, ch].T.reshape(4, 128, 3).transpose(1, 0, 2))
        scw.append(sc_norm_w[0][ch].reshape(4, 128).T)
    sh["wg_sc"] = np.ascontiguousarray(np.stack(wsc)).astype(f)
    sh["cw_sc"] = np.ascontiguousarray(np.stack(cws)).astype(f)
    sh["scw"] = np.ascontiguousarray(np.stack(scw)).astype(f)
    hsel = np.stack([np.concatenate([g * 8 + np.arange(8), 32 + g * 8 + np.arange(8)]) for g in range(4)])
    sh["dtb"] = np.ascontiguousarray(ssm_dt_bias[0][hsel].reshape(4, 1, 16))
    sh["alog"] = np.ascontiguousarray(ssm_a_log[0][hsel].reshape(4, 1, 16))
    sh["dsk"] = np.ascontiguousarray(np.repeat(ssm_d[0].reshape(4, 8), 64, axis=1).reshape(4, 1, 512))
    sh["ssm_nw"] = np.ascontiguousarray(ssm_norm_w[0].reshape(4, 1, 512))
    wo = _kc_p(w_out[0])
    sh["w_out"] = np.ascontiguousarray(np.stack([wo[:, :, 0:1024], wo[:, :, 1024:2048]]))
    sh["w_rt"] = _kc_p(w_router[0])
    wgt = w_gate[0].reshape(16, 16, 128, 2, 512).transpose(0, 3, 2, 1, 4)
    wup = w_up[0].reshape(16, 16, 128, 2, 512).transpose(0, 3, 2, 1, 4)
    wdn = w_down[0].reshape(16, 8, 128, 2, 1024).transpose(0, 3, 2, 1, 4)
    sh["w_gate"] = np.ascontiguousarray(wgt)
    sh["w_up"] = np.ascontiguousarray(wup)
    sh["w_down"] = np.ascontiguousarray(wdn)
    sh["c_ident"] = np.eye(128, dtype=f)
    k = np.arange(128)[:, None]
    i = np.arange(128)[None, :]
    sh["c_tri"] = np.stack([(k <= i), (k >= i)]).astype(f)
    mf = np.where(i < k, NEG, 0.0).astype(f)
    mb = np.where(i > k, NEG, 0.0).astype(f)
    sh["c_mask"] = np.stack([np.tile(mf, (1, 4)), np.tile(mb, (1, 4))]).astype(f)
    return sh


_CACHE = {}


def kernel(x, c, ctx, c_ctx, w_mod, b_mod, norm_mix_w, w_in, ssm_conv_w, ssm_conv_b, ssm_a_log, ssm_dt_bias, ssm_d,
           ssm_norm_w, sc_conv_w, sc_norm_w, w_out, norm_ffn_w, w_router, w_gate, w_up, w_down, final_norm_w):
    args = [np.asarray(a, dtype=np.float32) for a in (
        c_ctx, w_mod, b_mod, norm_mix_w, w_in, ssm_conv_w, ssm_conv_b, ssm_a_log, ssm_dt_bias, ssm_d, ssm_norm_w,
        sc_conv_w, sc_norm_w, w_out, norm_ffn_w, w_router, w_gate, w_up, w_down, final_norm_w)]
    x = np.asarray(x, dtype=np.float32)
    c = np.asarray(c, dtype=np.float32)
    ctx = np.asarray(ctx, dtype=np.float32)
    sh = _prep_shared(*args)
    if "nc" not in _CACHE:
        _CACHE["nc"] = build_program()
    nc = _CACHE["nc"]
    in_maps = []
    for core in range(NCORES):
        b = core % 4
        m = dict(sh)
        m["xin"] = np.ascontiguousarray(np.concatenate([ctx[b], x[b]], axis=0).reshape(NT, 128, 2048))
        m["cvec"] = np.ascontiguousarray(np.stack([c[b], args[0]], axis=-1).reshape(128, 16, 2))
        in_maps.append(m)
    res = run_bass_kernel_spmd(nc, in_maps, core_ids=list(range(NCORES)))
    _CACHE["res"] = res
    outs = [res.results[b % NCORES]["out"].reshape(4096, 2048) for b in range(4)]
    return np.stack(outs).astype(np.float32)
```

```python
import numpy as np
from contextlib import ExitStack
import concourse.bass as bass
import concourse.mybir as mybir
from concourse.bass_utils import run_bass_kernel_spmd

F32 = mybir.dt.float32
BF16 = mybir.dt.bfloat16
I32 = mybir.dt.int32
U32 = mybir.dt.uint32
U8 = mybir.dt.uint8
AF = mybir.ActivationFunctionType
ALU = mybir.AluOpType

ENGS = ("pe", "act", "dve", "pool", "sp")
DEBUG = False
LIMIT = 99
STOPAT = None
MARKS = {}
NG = 4
SCAN = 2
NCORES = 4
EPS = 1e-6
NT = 34
NTL = 32
NEG = -30000.0


class Dep:
    __slots__ = ("w", "r", "excl")

    def __init__(self, excl=False):
        self.w = None
        self.r = []
        self.excl = excl


class Instr:
    __slots__ = ("eng", "fn", "dma", "deps", "sig", "sem", "val")

    def __init__(self, eng, fn, dma):
        self.eng = eng
        self.fn = fn
        self.dma = dma
        self.deps = set()
        self.sig = False
        self.sem = None
        self.val = 0


def _nop_fn(eng):
    return eng.nop()


class Prog:
    def __init__(self, nc, n_dma_sems=12):
        self.nc = nc
        self.lists = {e: [] for e in ENGS}
        self.last = {e: None for e in ENGS}
        self.n_dma_sems = n_dma_sems
        self.dma_count = {e: 0 for e in ENGS}
        self.dma_ring = {e: [None] * n_dma_sems for e in ENGS}

    def emit(self, eng, fn, reads=(), writes=(), dma=False, extra=()):
        ins = Instr(eng, fn, dma)
        self.count = getattr(self, "count", 0) + 1
        if STOPAT is not None and self.count > STOPAT and not getattr(self, "in_finish", False):
            return ins
        for d in reads:
            if d.w is not None:
                ins.deps.add(d.w)
            if d.excl:
                for r in d.r:
                    if r.eng != eng:
                        ins.deps.add(r)
            if not dma:
                d.r = [r for r in d.r if r.dma or r.eng != eng]
            d.r.append(ins)
        for d in writes:
            if d.w is not None:
                ins.deps.add(d.w)
            for r in d.r:
                ins.deps.add(r)
            d.w = ins
            d.r = []
        for p in extra:
            if p is not None:
                ins.deps.add(p)
        ins.deps.discard(ins)
        if dma:
            k = self.dma_count[eng]
            self.dma_count[eng] = k + 1
            slot = k % self.n_dma_sems
            prev = self.dma_ring[eng][slot]
            if prev is not None:
                ins.deps.add(prev)
            self.dma_ring[eng][slot] = ins
            ins.sem = (eng, slot)
        self.lists[eng].append(ins)
        self.last[eng] = ins
        return ins

    def barrier(self):
        ext = [self.last[e] for e in ENGS if self.last[e] is not None]
        for e in ENGS:
            for p in self.dma_ring[e]:
                if p is not None:
                    ext.append(p)
        b = self.emit("dve", lambda eng: eng.engine_nop(), extra=ext)
        for e in ENGS:
            if e != "dve":
                self.emit(e, _nop_fn, extra=[b])
        return b

    def finalize(self, stack):
        nc = self.nc
        for e in ENGS:
            for ins in self.lists[e]:
                for p in ins.deps:
                    if p.eng == "pe" and ins.eng == "pe" and not p.dma and not ins.dma:
                        continue
                    p.sig = True
        esem = {e: stack.enter_context(nc.semaphore("s_" + e)) for e in ENGS}
        dsem = {}
        for e in ENGS:
            for s in range(min(self.n_dma_sems, self.dma_count[e])):
                dsem[(e, s)] = stack.enter_context(nc.semaphore("d_%s_%d" % (e, s)))
        dcount = {k: 0 for k in dsem}
        for e in ENGS:
            c = 0
            for ins in self.lists[e]:
                if ins.dma:
                    dcount[ins.sem] += 16
                    ins.val = dcount[ins.sem]
                    ins.sig = True
                elif ins.sig:
                    c += 1
                    ins.val = c
                    ins.sem = e
        block = stack.enter_context(nc.Block())
        engobj = {"pe": "tensor", "act": "scalar", "dve": "vector", "pool": "gpsimd", "sp": "sync"}

        def make_body(e):
            def body(eng):
                known = {}
                for ins in self.lists[e]:
                    for p in ins.deps:
                        if p.eng == "pe" and e == "pe" and not p.dma and not ins.dma:
                            continue
                        if p.dma:
                            sem = dsem[p.sem]
                            key = ("d",) + p.sem
                        else:
                            sem = esem[p.eng]
                            key = ("e", p.eng)
                        if known.get(key, 0) >= p.val:
                            continue
                        known[key] = p.val
                        eng.wait_ge(sem, p.val)
                    r = ins.fn(eng)
                    if ins.dma:
                        r.then_inc(dsem[ins.sem], 16)
                    elif ins.sig:
                        r.then_inc(esem[e], 1)
            return body

        for e in ENGS:
            if self.lists[e]:
                getattr(block, engobj[e])(make_body(e))


class Arena:
    def __init__(self, ap_u8, nbytes):
        self.a = ap_u8
        self.n = nbytes
        self.top = 0

    def alloc(self, shape, dt):
        esz = {F32: 4, BF16: 2, I32: 4, U32: 4}[dt]
        free = 1
        for s in shape[1:]:
            free *= s
        nb = (free * esz + 63) // 64 * 64
        assert self.top + nb <= self.n, ("SBUF arena overflow", self.top, nb, self.n)
        v = self.a[0:shape[0], self.top:self.top + free * esz].bitcast(dt)
        self.top += nb
        if len(shape) == 3:
            v = v.rearrange("p (a b) -> p a b", b=shape[2])
        return v


class T:
    def __init__(self, ap, nsub=0):
        self.ap = ap
        self.d = Dep()
        self.ds = [Dep() for _ in range(nsub)]


def build_program():
    nc = bass.Bass("TRN2", target_bir_lowering=False)
    st = ExitStack()
    P = Prog(nc)

    def finish():
        P.in_finish = True
        P.barrier()
        P.finalize(st)
        st.close()
        return nc

    def din(name, shape, dt=F32):
        return nc.dram_tensor(name, list(shape), dt, kind="ExternalInput").ap()

    def dscr(name, shape, dt):
        return nc.dram_tensor(name, list(shape), dt, kind="ExternalOutput" if DEBUG else "Internal").ap()

    xin = din("xin", [NT, 128, 2048])
    cvec = din("cvec", [128, 16, 2])
    w_mod = din("w_mod", [128, 16, 12288])
    b_mod = din("b_mod", [1, 12288])
    nmix = din("nmix", [1, 2048])
    nffn = din("nffn", [1, 2048])
    nfin = din("nfin", [1, 2048])
    wg_ssm = din("wg_ssm", [4, 128, 16, 1296]) if LIMIT >= 2 else None
    wg_sc = din("wg_sc", [4, 128, 16, 1536]) if LIMIT >= 3 else None
    cw_ssm = din("cw_ssm", [4, 128, 6, 3])
    cb_ssm = din("cb_ssm", [4, 128, 6])
    cw_sc = din("cw_sc", [4, 128, 4, 3])
    scw = din("scw", [4, 128, 4])
    dtb = din("dtb", [4, 1, 16])
    alog = din("alog", [4, 1, 16])
    dsk = din("dsk", [4, 1, 512])
    ssm_nw = din("ssm_nw", [4, 1, 512])
    w_out = din("w_out", [2, 128, 32, 1024]) if LIMIT >= 4 else None
    w_rt = din("w_rt", [128, 16, 16])
    if LIMIT >= 6:
        w_gate = din("w_gate", [16, 2, 128, 16, 512])
        w_up = din("w_up", [16, 2, 128, 16, 512])
        w_down = din("w_down", [16, 2, 128, 8, 1024])
    c_ident = din("c_ident", [128, 128])
    c_tri = din("c_tri", [2, 128, 128])
    c_mask = din("c_mask", [2, 128, 512])
    out = nc.dram_tensor("out", [NTL, 128, 2048], F32, kind="ExternalOutput").ap()

    mod_d = T(dscr("mod_d", [2, 12288], F32))
    aT_d = T(dscr("aT_d", [9, 128, 16, 512], BF16), 9)
    z_d = T(dscr("z_d", [4, NTL, 128, 512], BF16), 4 * NTL)
    yT_d = T(dscr("yT_d", [NTL, 128, 32, 128], BF16), NTL)
    hacc_h = [T(dscr("hacc_d%d" % i, [NTL * 128, 1024], F32), NTL) for i in range(2)]
    f_d = T(dscr("f_d", [NTL * 128, 2048], BF16))
    dbg = {}
    if DEBUG:
        dbg["h1"] = nc.dram_tensor("dbg_h1", [NTL * 128, 2048], F32, kind="ExternalOutput").ap()
        dbg["aff"] = nc.dram_tensor("dbg_aff", [16, 4096], F32, kind="ExternalOutput").ap()
        dbg["vals"] = nc.dram_tensor("dbg_vals", [16, 512], F32, kind="ExternalOutput").ap()
        dbg["idx"] = nc.dram_tensor("dbg_idx", [16, 512], U32, kind="ExternalOutput").ap()
        dbg["rsc"] = nc.dram_tensor("dbg_rsc", [128, NTL], F32, kind="ExternalOutput").ap()
        dbg["dt"] = nc.dram_tensor("dbg_dt", [4, 128, NT, 16], F32, kind="ExternalOutput").ap()
        dbg["xs"] = nc.dram_tensor("dbg_xs", [4, 128, NT, 512], BF16, kind="ExternalOutput").ap()

    sb_bytes = (int(nc.sbuf_bytes_remaining) - 2048) // 256 * 256
    arena_t = st.enter_context(nc.sbuf_tensor("arena", [128, sb_bytes], U8))
    AR = Arena(arena_t[:, :], sb_bytes)
    banks = [st.enter_context(nc.psum_tensor("bank%d" % i, [128, 512], F32)) for i in range(8)]
    PS = [T(b[:, :]) for b in banks]
    for p_ in PS:
        p_.d.excl = True

    def psbf(i):
        return banks[i][:, :].bitcast(BF16)

    def MM(out, lhsT, rhs, start, stop, rd, wr):
        return P.emit("pe", lambda e: e.matmul(out, lhsT=lhsT, rhs=rhs, start=start, stop=stop), rd, wr)

    def TR(out, in_, ident, rd, wr):
        return P.emit("pe", lambda e: e.transpose(out, in_, ident), rd, wr)

    def ACT(out, in_, func, rd, wr, bias=None, scale=None, accum=None):
        kw = {}
        if bias is not None:
            kw["bias"] = bias
        if scale is not None:
            kw["scale"] = scale
        if accum is not None:
            kw["accum_out"] = accum
        return P.emit("act", lambda e: e.activation(out=out, in_=in_, func=func, **kw), rd, wr)

    def TT(eng, out, in0, in1, op, rd, wr):
        return P.emit(eng, lambda e: e.tensor_tensor(out=out, in0=in0, in1=in1, op=op), rd, wr)

    def TS(eng, out, in0, s1, s2, op0, op1, rd, wr):
        if s2 is None:
            return P.emit(eng, lambda e: e.tensor_scalar(out=out, in0=in0, scalar1=s1, scalar2=None, op0=op0), rd, wr)
        return P.emit(eng, lambda e: e.tensor_scalar(out=out, in0=in0, scalar1=s1, scalar2=s2, op0=op0, op1=op1), rd, wr)

    def STT(eng, out, in0, scalar, in1, op0, op1, rd, wr):
        return P.emit(eng, lambda e: e.scalar_tensor_tensor(out=out, in0=in0, scalar=scalar, in1=in1, op0=op0, op1=op1), rd, wr)

    def CP(eng, out, in_, rd, wr):
        if eng == "act":
            return P.emit("act", lambda e: e.copy(out=out, in_=in_), rd, wr)
        return P.emit(eng, lambda e: e.tensor_copy(out=out, in_=in_), rd, wr)

    def MEMSET(eng, ap, val, wr):
        return P.emit(eng, lambda e: e.memset(ap, val), (), wr)

    def RECIP(out, in_, rd, wr):
        return P.emit("dve", lambda e: e.reciprocal(out=out, in_=in_), rd, wr)

    def DMA(q, out, in_, rd, wr):
        return P.emit(q, lambda e: e.dma_start(out=out, in_=in_), rd, wr, dma=True)

    def rstd_from_ss(ss_ap, std_ap, r_ap, n, dss, dstd, dr):
        ACT(std_ap, ss_ap, AF.Sqrt, [dss], [dstd], bias=epsc.ap[0:ss_ap.shape[0], :], scale=1.0 / n)
        RECIP(r_ap, std_ap, [dstd], [dr])

    ident32 = T(AR.alloc([128, 128], F32))
    identbf = T(AR.alloc([128, 128], BF16))
    ones32 = T(AR.alloc([128, 128], F32))
    onesbf = T(AR.alloc([128, 8], BF16))
    epsc = T(AR.alloc([128, 1], F32))
    DMA("sp", ident32.ap, c_ident, [], [ident32.d])
    DMA("pool", identbf.ap, c_ident, [], [identbf.d])
    MEMSET("dve", ones32.ap, 1.0, [ones32.d])
    MEMSET("dve", onesbf.ap, 1.0, [onesbf.d])
    MEMSET("dve", epsc.ap, EPS, [epsc.d])
    persist_mark = AR.top

    cv = T(AR.alloc([128, 16, 2], F32))
    cs_ = T(AR.alloc([128, 16, 2], F32))
    DMA("sp", cv.ap, cvec, [], [cv.d])
    ACT(cs_.ap, cv.ap, AF.Silu, [cv.d], [cs_.d])
    wch = [T(AR.alloc([128, 16, 512], F32)) for _ in range(3)]
    bch = [T(AR.alloc([2, 512], F32)) for _ in range(2)]
    mch = [T(AR.alloc([2, 512], F32)) for _ in range(2)]
    for n in range(24):
        w = wch[n % 3]
        cols = slice(n * 512, (n + 1) * 512)
        DMA("sp", w.ap[:, 0:8, :], w_mod[:, 0:8, cols], [], [w.d])
        DMA("act", w.ap[:, 8:16, :], w_mod[:, 8:16, cols], [], [w.d])
        bb = bch[n % 2]
        DMA("sp", bb.ap, b_mod[0:1, cols].partition_broadcast(2), [], [bb.d])
        pb = PS[n % 2]
        for kc in range(16):
            MM(pb.ap[0:2, :], cs_.ap[:, kc, :], w.ap[:, kc, :], kc == 0, kc == 15, [cs_.d, w.d], [pb.d])
        m = mch[n % 2]
        TT("dve", m.ap, pb.ap[0:2, :], bb.ap, ALU.add, [pb.d, bb.d], [m.d])
        if n // 4 in (1, 4):
            TS("dve", m.ap, m.ap, 1.0, None, ALU.add, None, [m.d], [m.d])
        DMA("sp", mod_d.ap[:, cols], m.ap, [m.d], [mod_d.d])
    P.barrier()
    if LIMIT == 0:
        return finish()
    AR.top = persist_mark

    def load_mod_bc(dst, row, seg, q="sp"):
        DMA(q, dst.ap, mod_d.ap[row:row + 1, seg * 2048:(seg + 1) * 2048].partition_broadcast(128), [mod_d.d], [dst.d])

    def load_vec_bc(dst, src, q="sp"):
        DMA(q, dst.ap, src.partition_broadcast(128), [], [dst.d])

    A1 = [T(AR.alloc([128, 2048], F32)) for _ in range(2)]
    B1 = [T(AR.alloc([128, 2048], F32)) for _ in range(2)]
    nmb = T(AR.alloc([128, 2048], F32))
    load_vec_bc(nmb, nmix)
    for r in range(2):
        load_mod_bc(A1[r], r, 1)
        load_mod_bc(B1[r], r, 0)
        TT("pool", A1[r].ap, A1[r].ap, nmb.ap, ALU.mult, [A1[r].d, nmb.d], [A1[r].d])
    xt = [T(AR.alloc([128, 2048], F32)) for _ in range(2)]
    tmp = [T(AR.alloc([128, 2048], F32)) for _ in range(2)]
    abf = [T(AR.alloc([128, 2048], BF16)) for _ in range(2)]
    junk = T(AR.alloc([128, 2048], BF16))
    ss1 = T(AR.alloc([128, NT], F32), NT)
    sd1 = T(AR.alloc([128, NT], F32), NT)
    rs1 = T(AR.alloc([128, NT], F32), NT)
    aTb = [T(AR.alloc([128, 16, 512], BF16)) for _ in range(2)]
    MEMSET("dve", ss1.ap, 0.0, ss1.ds)
    order1 = []
    for tb in range(9):
        tiles = [0, 1] if tb == 0 else [2 + 4 * (tb - 1) + i for i in range(4)]
        for j, t in enumerate(tiles):
            order1.append((tb, j, t, j == len(tiles) - 1, 128 * len(tiles)))

    def p1_load(t):
        DMA("sp", xt[t % 2].ap, xin[t], [], [xt[t % 2].d])

    def p1_stats(t):
        x_ = xt[t % 2]
        ACT(junk.ap, x_.ap, AF.Square, [x_.d], [junk.d, ss1.ds[t]], accum=ss1.ap[:, t:t + 1])
        rstd_from_ss(ss1.ap[:, t:t + 1], sd1.ap[:, t:t + 1], rs1.ap[:, t:t + 1], 2048.0, ss1.ds[t], sd1.ds[t], rs1.ds[t])

    p1_load(0)
    p1_stats(0)
    for i1, (tb, j, t, lastj, ntok) in enumerate(order1):
        ab = aTb[tb % 2]
        r = 1 if t < 2 else 0
        x_ = xt[t % 2]
        tm = tmp[t % 2]
        STT("dve", tm.ap, x_.ap, rs1.ap[:, t:t + 1], A1[r].ap, ALU.mult, ALU.mult, [x_.d, rs1.ds[t], A1[r].d], [tm.d])
        if i1 + 1 < len(order1):
            p1_load(order1[i1 + 1][2])
            p1_stats(order1[i1 + 1][2])
        a_ = abf[t % 2]
        TT("pool", a_.ap, tm.ap, B1[r].ap, ALU.add, [tm.d, B1[r].d], [a_.d])
        for q in range(4):
            pb = PS[2 + (q % 2)]
            pv = psbf(2 + (q % 2))
            for i in range(4):
                kc = 4 * q + i
                TR(pv[:, i * 128:(i + 1) * 128], a_.ap[:, kc * 128:(kc + 1) * 128], identbf.ap, [a_.d, identbf.d], [pb.d])
            CP("act" if q % 2 == 0 else "dve", ab.ap[:, 4 * q:4 * q + 4, j * 128:(j + 1) * 128],
               pv[:, 0:512].rearrange("p (a b) -> p a b", b=128), [pb.d], [ab.d])
        if lastj:
            DMA("sp", aT_d.ap[tb][:, :, 0:ntok], ab.ap[:, :, 0:ntok], [ab.d], [aT_d.ds[tb]])
    P.barrier()
    if LIMIT == 1:
        return finish()
    AR.top = persist_mark

    tri = [T(AR.alloc([128, 128], F32)) for _ in range(2)]
    maskn = [T(AR.alloc([128, 512], BF16)) for _ in range(2)]
    for d_ in range(2):
        DMA("sp", tri[d_].ap, c_tri[d_], [], [tri[d_].d])
        DMA("pool", maskn[d_].ap, c_mask[d_], [], [maskn[d_].d])
    xs = T(AR.alloc([128, NT, 512], BF16), NT)
    Btok = T(AR.alloc([128, NT, 128], BF16), NT)
    BT = T(AR.alloc([128, NT * 128], BF16), NT)
    CT = T(AR.alloc([128, NT * 128], BF16), NT)
    dtr = T(AR.alloc([128, NT, 16], F32))
    dtv = T(AR.alloc([128, NT, 16], F32))
    lav = T(AR.alloc([128, NT, 16], F32))
    cwt = T(AR.alloc([128, 6, 3], F32))
    cbt = T(AR.alloc([128, 6], F32))
    dtbb = T(AR.alloc([128, 16], F32))
    aneg = T(AR.alloc([128, 16], F32))
    Dbc = T(AR.alloc([128, 512], F32))
    nwb = T(AR.alloc([128, 512], F32))
    p2_mark = AR.top

    for g in range(NG):
        DMA("sp", cwt.ap, cw_ssm[g], [], [cwt.d])
        DMA("sp", cbt.ap, cb_ssm[g], [], [cbt.d])
        load_vec_bc(dtbb, dtb[g])
        load_vec_bc(aneg, alog[g])
        load_vec_bc(Dbc, dsk[g])
        load_vec_bc(nwb, ssm_nw[g])
        ACT(aneg.ap, aneg.ap, AF.Exp, [aneg.d], [aneg.d])
        TS("dve", aneg.ap, aneg.ap, -1.0, None, ALU.mult, None, [aneg.d], [aneg.d])
        wg = T(AR.alloc([128, 16, 1296], BF16))
        for k2 in range(8):
            DMA("pool", wg.ap[:, 2 * k2:2 * k2 + 2, :], wg_ssm[g][:, 2 * k2:2 * k2 + 2, :], [], [wg.d])
        aTl = [T(AR.alloc([128, 16, 512], BF16)) for _ in range(2)]
        acc = [T(AR.alloc([128, 512], F32)) for _ in range(2)]
        xTt = [[T(AR.alloc([128, 512], BF16)) for _ in range(4)] for _ in range(2)]
        zst = [T(AR.alloc([128, 512], BF16)) for _ in range(2)]
        fmi = 0
        zi = 0
        MARKS.setdefault('A', P.count)
        for tb in range(9):
            ntok = 256 if tb == 0 else 512
            W = 256 if tb == 0 else 64
            tiles = [0, 1] if tb == 0 else [2 + 4 * (tb - 1) + i for i in range(4)]
            tok0 = tiles[0] * 128
            if tb == 1:
                MARKS.setdefault('C', P.count)
            if tb == 2:
                MARKS.setdefault('D', P.count)
            al = aTl[tb % 2]
            if tb == 0:
                DMA("sp", al.ap[:, :, 0:ntok], aT_d.ap[tb][:, :, 0:ntok], [aT_d.ds[tb]], [al.d])
            if tb + 1 < 9:
                DMA("sp", aTl[(tb + 1) % 2].ap, aT_d.ap[tb + 1], [aT_d.ds[tb + 1]], [aTl[(tb + 1) % 2].d])
            xTb = xTt[tb % 2]
            for oc in range(6):
                pb = PS[fmi % 2]
                fmi += 1
                for kc in range(16):
                    MM(pb.ap[:, 0:ntok], wg.ap[:, kc, oc * 128:(oc + 1) * 128], al.ap[:, kc, 0:ntok], kc == 0, kc == 15,
                       [wg.d, al.d], [pb.d])
                ac = acc[oc % 2]
                ACT(ac.ap[:, 0:ntok], pb.ap[:, 0:ntok], AF.Identity, [pb.d, cwt.d, cbt.d], [ac.d],
                    bias=cbt.ap[:, oc:oc + 1], scale=cwt.ap[:, oc, 1:2])
                u3 = pb.ap[:, 0:ntok].rearrange("p (r w) -> p r w", w=W)
                a3 = ac.ap[:, 0:ntok].rearrange("p (r w) -> p r w", w=W)
                STT("dve", a3[:, :, 1:W], u3[:, :, 0:W - 1], cwt.ap[:, oc, 0:1], a3[:, :, 1:W], ALU.mult, ALU.add,
                    [pb.d, cwt.d, ac.d], [ac.d])
                STT("dve", a3[:, :, 0:W - 1], u3[:, :, 1:W], cwt.ap[:, oc, 2:3], a3[:, :, 0:W - 1], ALU.mult, ALU.add,
                    [pb.d, cwt.d, ac.d], [ac.d])
                if oc < 4:
                    ACT(xTb[oc].ap[:, 0:ntok], ac.ap[:, 0:ntok], AF.Silu, [ac.d], [xTb[oc].d])
                elif oc == 4:
                    ACT(BT.ap[:, tok0:tok0 + ntok], ac.ap[:, 0:ntok], AF.Silu, [ac.d], [BT.ds[t] for t in tiles])
                else:
                    ACT(CT.ap[:, tok0:tok0 + ntok], ac.ap[:, 0:ntok], AF.Silu, [ac.d], [CT.ds[t] for t in tiles])
            MARKS.setdefault('B', P.count)
            for j, t in enumerate(tiles):
                pb = PS[2 + (t % 2)]
                pv = psbf(2 + (t % 2))
                for oc in range(4):
                    TR(pv[:, oc * 128:(oc + 1) * 128], xTb[oc].ap[:, j * 128:(j + 1) * 128], identbf.ap,
                       [xTb[oc].d, identbf.d], [pb.d])
                TR(psbf(7)[:, 0:128], BT.ap[:, t * 128:(t + 1) * 128], identbf.ap, [BT.ds[t], identbf.d], [PS[7].d])
                CP("dve", xs.ap[:, t, :], pv[:, 0:512], [pb.d], [xs.ds[t]])
                CP("act", Btok.ap[:, t, :], psbf(7)[:, 0:128], [PS[7].d], [Btok.ds[t]])
                if t >= 2:
                    pz = PS[4 + (zi % 2)]
                    zz = zst[zi % 2]
                    zi += 1
                    for kc in range(16):
                        MM(pz.ap, al.ap[:, kc, j * 128:(j + 1) * 128], wg.ap[:, kc, 768:1280], kc == 0, kc == 15,
                           [al.d, wg.d], [pz.d])
                    ACT(zz.ap, pz.ap, AF.Silu, [pz.d], [zz.d])
                    DMA("sp", z_d.ap[g, t - 2], zz.ap, [zz.d], [z_d.ds[g * NTL + t - 2]])
                pd = PS[6]
                for kc in range(16):
                    MM(pd.ap[:, 0:16], al.ap[:, kc, j * 128:(j + 1) * 128], wg.ap[:, kc, 1280:1296], kc == 0, kc == 15,
                       [al.d, wg.d], [pd.d])
                TT("dve", dtr.ap[:, t, :], pd.ap[:, 0:16], dtbb.ap, ALU.add, [pd.d, dtbb.d], [dtr.d])
        MARKS.setdefault('E', P.count)
        ACT(dtv.ap, dtr.ap, AF.Exp, [dtr.d], [dtv.d])
        ACT(dtv.ap, dtv.ap, AF.Ln, [dtv.d], [dtv.d], bias=ones32.ap[:, 0:1])
        TT("dve", lav.ap, dtv.ap, aneg.ap.unsqueeze(1).to_broadcast([128, NT, 16]), ALU.mult, [dtv.d, aneg.d], [lav.d])
        if DEBUG:
            DMA("sp", dbg["dt"][g], dtv.ap, [dtv.d], [])
            DMA("sp", dbg["xs"][g], xs.ap, xs.ds, [])
        P.barrier()
        AR.top = p2_mark

        yacc = T(AR.alloc([128, NTL, 512], BF16), NTL)
        S32 = T(AR.alloc([128, 512], F32))
        Sbf = T(AR.alloc([128, 512], BF16))
        rc = [T(AR.alloc([128, 8, 128], F32)) for _ in range(2)]
        Lall = [T(AR.alloc([128, 8, 128], BF16), 8) for _ in range(2)]
        Mall = [T(AR.alloc([128, 8, 128], BF16), 8) for _ in range(2)]
        cbT = [T(AR.alloc([128, 128], BF16)) for _ in range(2)]
        smal = [T(AR.alloc([128, 32], F32)) for _ in range(2)]
        xw = [T(AR.alloc([128, 512], BF16)) for _ in range(2)]
        tA = [T(AR.alloc([128, 512], F32)) for _ in range(2)]
        tB = [T(AR.alloc([128, 512], F32)) for _ in range(2)]
        zt = [T(AR.alloc([128, 512], BF16)) for _ in range(2)]
        ynb = [T(AR.alloc([128, 512], BF16)) for _ in range(2)]
        yTo = [T(AR.alloc([128, 4, 128], BF16)) for _ in range(2)]
        gst = [T(AR.alloc([128, 4], F32)) for _ in range(2)]
        junk2 = T(AR.alloc([128, 512], BF16))
        ssg = T(AR.alloc([128, NTL], F32), NTL)
        sdg = T(AR.alloc([128, NTL], F32))
        rsg = T(AR.alloc([128, NTL], F32))
        MEMSET("pool", ssg.ap, 0.0, ssg.ds)
        def stageA(d_, c, k):
            lat = c >= 2
            last = 127 if d_ == 0 else 0
            la_c = lav.ap[:, c, d_ * 8:(d_ + 1) * 8]
            dt_c = dtv.ap[:, c, d_ * 8:(d_ + 1) * 8]
            sm = smal[k]
            pS = PS[7]
            MM(pS.ap[:, 0:8], tri[d_].ap, la_c, True, True, [tri[d_].d, lav.d], [pS.d])
            MM(pS.ap[:, 8:16], ones32.ap, la_c, True, True, [ones32.d, lav.d], [pS.d])
            TT("dve", rc[k].ap, tri[d_].ap.unsqueeze(1).to_broadcast([128, 8, 128]),
               la_c.unsqueeze(2).to_broadcast([128, 8, 128]), ALU.mult, [tri[d_].d, lav.d], [rc[k].d])
            Rb = [PS[0], PS[1]]
            for hf in range(2):
                MM(Rb[hf].ap, ones32.ap, rc[k].ap[:, 4 * hf:4 * hf + 4, :].rearrange("p a b -> p (a b)"), True, False,
                   [ones32.d, rc[k].d], [Rb[hf].d])
                MM(Rb[hf].ap, identbf.ap, maskn[d_].ap, False, True, [identbf.d, maskn[d_].d], [Rb[hf].d])
            ACT(sm.ap[:, 0:8], pS.ap[:, 0:8], AF.Identity, [pS.d], [sm.d], scale=-1.0)
            ACT(sm.ap[:, 8:24], pS.ap[:, 0:16], AF.Exp, [pS.d], [sm.d])
            if lat:
                MM(pS.ap[:, 128:256], BT.ap[:, c * 128:(c + 1) * 128], CT.ap[:, c * 128:(c + 1) * 128], True, True,
                   [BT.ds[c], CT.ds[c]], [pS.d])
                CP("act", cbT[k].ap, pS.ap[:, 128:256], [pS.d], [cbT[k].d])
            La = Lall[k]
            for h in range(8):
                ACT(La.ap[:, h, :], Rb[h // 4].ap[:, (h % 4) * 128:(h % 4 + 1) * 128], AF.Exp, [Rb[h // 4].d, sm.d],
                    [La.ds[h]], bias=sm.ap[:, h:h + 1])
            TT("dve", sm.ap[:, 24:32], La.ap[:, :, last], dt_c, ALU.mult, La.ds + [dtv.d], [sm.d])
            TT("pool", xw[k].ap.rearrange("p (h d) -> p h d", d=64), xs.ap[:, c, :].rearrange("p (h d) -> p h d", d=64),
               sm.ap[:, 24:32].unsqueeze(2).to_broadcast([128, 8, 64]), ALU.mult, [xs.ds[c], sm.d], [xw[k].d])
            if lat:
                Ma = Mall[k]
                for h in range(8):
                    STT("dve", Ma.ap[:, h, :], La.ap[:, h, :], dt_c[:, h:h + 1], cbT[k].ap, ALU.mult, ALU.mult,
                        [La.ds[h], dtv.d, cbT[k].d], [Ma.ds[h]])
                for h in range(8):
                    MM(PS[2 + k].ap[:, h * 64:(h + 1) * 64], Ma.ap[:, h, :], xs.ap[:, c, h * 64:(h + 1) * 64], True, True,
                       [Ma.ds[h], xs.ds[c]], [PS[2 + k].d])
            MM(PS[4 + k].ap, Btok.ap[:, c, :], xw[k].ap, True, True, [Btok.ds[c], xw[k].d], [PS[4 + k].d])

        def stageB(d_, c, k):
            lat = c >= 2
            sm = smal[k]
            Pd, Ps_, Po = PS[2 + k], PS[4 + k], PS[6]
            if lat:
                MM(Po.ap, CT.ap[:, c * 128:(c + 1) * 128], Sbf.ap, True, True, [CT.ds[c], Sbf.d], [Po.d])
                TT("dve", tB[k].ap.rearrange("p (h d) -> p h d", d=64), Po.ap.rearrange("p (h d) -> p h d", d=64),
                   sm.ap[:, 8:16].unsqueeze(2).to_broadcast([128, 8, 64]), ALU.mult, [Po.d, sm.d], [tB[k].d])
                TT("dve", tB[k].ap, tB[k].ap, Pd.ap, ALU.add, [tB[k].d, Pd.d], [tB[k].d])
            TT("dve", S32.ap.rearrange("p (h d) -> p h d", d=64), S32.ap.rearrange("p (h d) -> p h d", d=64),
               sm.ap[:, 16:24].unsqueeze(2).to_broadcast([128, 8, 64]), ALU.mult, [S32.d, sm.d], [S32.d])
            TT("dve", S32.ap, S32.ap, Ps_.ap, ALU.add, [S32.d, Ps_.d], [S32.d])
            CP("act", Sbf.ap, S32.ap, [S32.d], [Sbf.d])
            if lat and d_ == 0:
                TT("pool", tA[k].ap, xs.ap[:, c, :], Dbc.ap, ALU.mult, [xs.ds[c], Dbc.d], [tA[k].d])
                TT("pool", yacc.ap[:, c - 2, :], tA[k].ap, tB[k].ap, ALU.add, [tA[k].d, tB[k].d], [yacc.ds[c - 2]])
            if lat and d_ == 1:
                DMA("sp", zt[k].ap, z_d.ap[g, c - 2], [z_d.ds[g * NTL + c - 2]], [zt[k].d])
                TT("pool", tA[k].ap, tB[k].ap, yacc.ap[:, c - 2, :], ALU.add, [tB[k].d, yacc.ds[c - 2]], [tA[k].d])
                TT("pool", yacc.ap[:, c - 2, :], tA[k].ap, zt[k].ap, ALU.mult, [tA[k].d, zt[k].d], [yacc.ds[c - 2]])
                ACT(junk2.ap, yacc.ap[:, c - 2, :], AF.Square, [yacc.ds[c - 2]], [junk2.d, ssg.ds[c - 2]],
                    accum=ssg.ap[:, c - 2:c - 1])

        def stageC(cl, k):
            STT("dve", ynb[k].ap, yacc.ap[:, cl, :], rsg.ap[:, cl:cl + 1], nwb.ap, ALU.mult, ALU.mult,
                [yacc.ds[cl], rsg.d, nwb.d], [ynb[k].d])
            pv = psbf(6 + k)
            for q in range(4):
                TR(pv[:, q * 128:(q + 1) * 128], ynb[k].ap[:, q * 128:(q + 1) * 128], identbf.ap,
                   [ynb[k].d, identbf.d], [PS[6 + k].d])
            CP("act", yTo[k].ap, pv[:, 0:512].rearrange("p (a b) -> p a b", b=128), [PS[6 + k].d], [yTo[k].d])
            DMA("sp", yT_d.ap[cl][:, g * 4:(g + 1) * 4, :], yTo[k].ap, [yTo[k].d], [yT_d.ds[cl]])

        items = []
        for d_ in range(SCAN):
            order = list(range(NT)) if d_ == 0 else [1, 0] + list(range(NT - 1, 1, -1))
            items += [(d_, c, ci == 0) for ci, c in enumerate(order)]
        if items:
            stageA(items[0][0], items[0][1], 0)
        for i_, (d_, c, first) in enumerate(items):
            if i_ + 1 < len(items):
                stageA(items[i_ + 1][0], items[i_ + 1][1], (i_ + 1) % 2)
            if first:
                MEMSET("dve", S32.ap, 0.0, [S32.d])
                MEMSET("pool", Sbf.ap, 0.0, [Sbf.d])
            stageB(d_, c, i_ % 2)
        if SCAN == 2:
            ACT(sdg.ap, ssg.ap, AF.Sqrt, ssg.ds, [sdg.d], bias=epsc.ap, scale=1.0 / 512.0)
            RECIP(rsg.ap, sdg.ap, [sdg.d], [rsg.d])
            for cl in range(NTL):
                stageC(cl, cl % 2)
        P.barrier()
        AR.top = p2_mark
    AR.top = persist_mark
    if LIMIT == 2:
        return finish()

    ss_sc = T(AR.alloc([128, NTL], F32))
    rs_sc = T(AR.alloc([128, NTL], F32))
    sd_sc = T(AR.alloc([128, NTL], F32))
    MEMSET("dve", ss_sc.ap, 0.0, [ss_sc.d])
    p3_mark = AR.top
    wscs = [T(AR.alloc([128, 16, 1536], BF16)) for _ in range(2)]
    cwcs = [T(AR.alloc([128, 4, 3], F32)) for _ in range(2)]
    scwts = [T(AR.alloc([128, 4], F32)) for _ in range(2)]

    def p3_wload(jb):
        DMA("sp", cwcs[jb % 2].ap, cw_sc[jb], [], [cwcs[jb % 2].d])
        DMA("sp", scwts[jb % 2].ap, scw[jb], [], [scwts[jb % 2].d])
        for k2 in range(8):
            DMA("pool", wscs[jb % 2].ap[:, 2 * k2:2 * k2 + 2, :], wg_sc[jb][:, 2 * k2:2 * k2 + 2, :], [], [wscs[jb % 2].d])
    aTl = [T(AR.alloc([128, 16, 512], BF16)) for _ in range(2)]
    cgs = [T(AR.alloc([128, 512], F32)) for _ in range(2)]
    vv = [T(AR.alloc([128, 512], F32)) for _ in range(2)]
    acc = [T(AR.alloc([128, 512], F32)) for _ in range(2)]
    q2 = [T(AR.alloc([128, 512], BF16)) for _ in range(4)]
    qw = [T(AR.alloc([128, 512], BF16)) for _ in range(4)]
    it = 0
    p3_wload(0)
    p3i = 0
    DMA("sp", aTl[0].ap, aT_d.ap[1], [aT_d.ds[1]], [aTl[0].d])
    for jb in range(4):
        wsc, cwc, scwt = wscs[jb % 2], cwcs[jb % 2], scwts[jb % 2]
        if jb + 1 < 4:
            p3_wload(jb + 1)
        for tb in range(1, 9):
            t0 = 4 * (tb - 1)
            al = aTl[p3i % 2]
            p3i += 1
            if p3i < 32:
                ntb = (p3i % 8) + 1
                DMA("sp", aTl[p3i % 2].ap, aT_d.ap[ntb], [aT_d.ds[ntb]], [aTl[p3i % 2].d])
            for cc in range(4):
                k = it % 2
                it += 1
                pbg, pcg, phv = PS[0 + 3 * k], PS[1 + 3 * k], PS[2 + 3 * k]
                for (pb, off) in ((pbg, 0), (pcg, 512), (phv, 1024)):
                    for kc in range(16):
                        MM(pb.ap, wsc.ap[:, kc, off + cc * 128:off + (cc + 1) * 128], al.ap[:, kc, :], kc == 0, kc == 15,
                           [wsc.d, al.d], [pb.d])
                CP("act", cgs[k].ap, pcg.ap, [pcg.d], [cgs[k].d])
                TT("dve", vv[k].ap, cgs[k].ap, phv.ap, ALU.mult, [cgs[k].d, phv.d], [vv[k].d])
                ACT(acc[k].ap, vv[k].ap, AF.Identity, [vv[k].d, cwc.d], [acc[k].d], scale=cwc.ap[:, cc, 1:2])
                v3 = vv[k].ap.rearrange("p (r w) -> p r w", w=64)
                a3 = acc[k].ap.rearrange("p (r w) -> p r w", w=64)
                STT("dve", a3[:, :, 1:64], v3[:, :, 0:63], cwc.ap[:, cc, 0:1], a3[:, :, 1:64], ALU.mult, ALU.add,
                    [vv[k].d, cwc.d, acc[k].d], [acc[k].d])
                STT("dve", a3[:, :, 0:63], v3[:, :, 1:64], cwc.ap[:, cc, 2:3], a3[:, :, 0:63], ALU.mult, ALU.add,
                    [vv[k].d, cwc.d, acc[k].d], [acc[k].d])
                TT("dve", acc[k].ap, acc[k].ap, pbg.ap, ALU.mult, [acc[k].d, pbg.d], [acc[k].d])
                ACT(q2[cc].ap, acc[k].ap, AF.Square, [acc[k].d], [q2[cc].d])
                TS("pool", qw[cc].ap, acc[k].ap, scwt.ap[:, cc:cc + 1], None, ALU.mult, None, [acc[k].d, scwt.d], [qw[cc].d])
                DMA("sp", yT_d.ap[t0:t0 + 4, :, 16 + jb * 4 + cc, :].rearrange("t p k -> p t k"),
                    qw[cc].ap.rearrange("p (t k) -> p t k", k=128), [qw[cc].d], [yT_d.ds[t0 + i] for i in range(4)])
            pss = PS[6 + (tb % 2)]
            for i in range(4):
                for cc in range(4):
                    MM(pss.ap[:, i:i + 1], q2[cc].ap[:, i * 128:(i + 1) * 128], onesbf.ap[:, 0:1], cc == 0, cc == 3,
                       [q2[cc].d, onesbf.d], [pss.d])
            TT("dve", ss_sc.ap[:, t0:t0 + 4], ss_sc.ap[:, t0:t0 + 4], pss.ap[:, 0:4], ALU.add, [ss_sc.d, pss.d], [ss_sc.d])
    rstd_from_ss(ss_sc.ap, sd_sc.ap, rs_sc.ap, 2048.0, ss_sc.d, sd_sc.d, rs_sc.d)
    if DEBUG:
        DMA("sp", dbg["rsc"], rs_sc.ap, [rs_sc.d], [])
    P.barrier()
    if LIMIT == 3:
        return finish()
    AR.top = p3_mark

    g1b = T(AR.alloc([128, 2048], F32))
    load_mod_bc(g1b, 0, 2)
    wos = [T(AR.alloc([128, 32, 1024], BF16)) for _ in range(2)]
    yt = [T(AR.alloc([128, 32, 128], BF16)) for _ in range(2)]
    xh = [T(AR.alloc([128, 1024], F32)) for _ in range(2)]
    t1 = [T(AR.alloc([128, 512], F32)) for _ in range(2)]
    ht = [T(AR.alloc([128, 1024], F32)) for _ in range(2)]
    it = 0
    for hf in range(2):
        for k4 in range(8):
            DMA("pool", wos[hf].ap[:, 4 * k4:4 * k4 + 4, :], w_out[hf][:, 4 * k4:4 * k4 + 4, :], [], [wos[hf].d])

    def p4_load(i4):
        hf_, t_ = i4 // NTL, i4 % NTL
        DMA("sp", yt[t_ % 2].ap, yT_d.ap[t_], [yT_d.ds[t_]], [yt[t_ % 2].d])
        DMA("sp", xh[t_ % 2].ap, xin[t_ + 2][:, hf_ * 1024:(hf_ + 1) * 1024], [], [xh[t_ % 2].d])

    p4_load(0)
    for hf in range(2):
        wo = wos[hf]
        for t in range(NTL):
            y_ = yt[t % 2]
            x_ = xh[t % 2]
            if hf * NTL + t + 1 < 2 * NTL:
                p4_load(hf * NTL + t + 1)
            h_ = ht[t % 2]
            for q in range(2):
                k = it % 2
                it += 1
                pm, pc = PS[0 + 2 * k], PS[1 + 2 * k]
                cols = slice(q * 512, (q + 1) * 512)
                for kc in range(16):
                    MM(pm.ap, y_.ap[:, kc, :], wo.ap[:, kc, cols], kc == 0, kc == 15, [y_.d, wo.d], [pm.d])
                for kc in range(16, 32):
                    MM(pc.ap, y_.ap[:, kc, :], wo.ap[:, kc, cols], kc == 16, kc == 31, [y_.d, wo.d], [pc.d])
                ACT(t1[k].ap, pc.ap, AF.Identity, [pc.d, rs_sc.d], [t1[k].d], scale=rs_sc.ap[:, t:t + 1])
                TT("dve", t1[k].ap, t1[k].ap, pm.ap, ALU.add, [t1[k].d, pm.d], [t1[k].d])
                gcols = slice(hf * 1024 + q * 512, hf * 1024 + (q + 1) * 512)
                TT("pool", t1[k].ap, t1[k].ap, g1b.ap[:, gcols], ALU.mult, [t1[k].d, g1b.d], [t1[k].d])
                TT("pool", h_.ap[:, cols], t1[k].ap, x_.ap[:, cols], ALU.add, [t1[k].d, x_.d], [h_.d])
            DMA("sp", hacc_h[hf].ap[t * 128:(t + 1) * 128, :], h_.ap, [h_.d], [hacc_h[hf].ds[t]])
    P.barrier()
    if LIMIT == 4:
        return finish()
    AR.top = persist_mark

    idxc = T(AR.alloc([128, 4, 16], I32))
    gatc = T(AR.alloc([128, 4, 16], F32))
    p5_mark = AR.top
    A2 = T(AR.alloc([128, 2048], F32))
    B2 = T(AR.alloc([128, 2048], F32))
    nfb = T(AR.alloc([128, 2048], F32))
    load_vec_bc(nfb, nffn)
    load_mod_bc(A2, 0, 4)
    load_mod_bc(B2, 0, 3)
    TT("pool", A2.ap, A2.ap, nfb.ap, ALU.mult, [A2.d, nfb.d], [A2.d])
    wr = T(AR.alloc([128, 16, 16], F32))
    DMA("sp", wr.ap, w_rt, [], [wr.d])
    affT = T(AR.alloc([16, 4096], F32))
    hx = [T(AR.alloc([128, 2048], F32)) for _ in range(2)]
    ff = [T(AR.alloc([128, 2048], F32)) for _ in range(2)]
    fb = [T(AR.alloc([128, 2048], BF16)) for _ in range(2)]
    fT = [T(AR.alloc([128, 16, 128], F32)) for _ in range(2)]
    st5 = [T(AR.alloc([128, 8], F32)) for _ in range(2)]
    ex = [T(AR.alloc([128, 16], F32)) for _ in range(2)]
    junk5 = T(AR.alloc([128, 2048], BF16))
    def p5_load(t_):
        for i2 in range(2):
            DMA("sp", hx[t_ % 2].ap[:, i2 * 1024:(i2 + 1) * 1024], hacc_h[i2].ap[t_ * 128:(t_ + 1) * 128, :],
                [hacc_h[i2].ds[t_]], [hx[t_ % 2].d])

    p5_load(0)
    for t in range(NTL):
        k = t % 2
        h_ = hx[k]
        if t + 1 < NTL:
            p5_load(t + 1)
        if DEBUG:
            DMA("sp", dbg["h1"][t * 128:(t + 1) * 128, :], h_.ap, [h_.d], [])
        s5 = st5[k]
        MEMSET("pool", s5.ap, 0.0, [s5.d])
        ACT(junk5.ap, h_.ap, AF.Square, [h_.d], [junk5.d, s5.d], accum=s5.ap[:, 0:1])
        rstd_from_ss(s5.ap[:, 0:1], s5.ap[:, 1:2], s5.ap[:, 2:3], 2048.0, s5.d, s5.d, s5.d)
        f_ = ff[k]
        STT("dve", f_.ap, h_.ap, s5.ap[:, 2:3], A2.ap, ALU.mult, ALU.mult, [h_.d, s5.d, A2.d], [f_.d])
        TT("pool", f_.ap, f_.ap, B2.ap, ALU.add, [f_.d, B2.d], [f_.d])
        ACT(fb[k].ap, f_.ap, AF.Copy, [f_.d], [fb[k].d])
        DMA("sp", f_d.ap[t * 128:(t + 1) * 128, :], fb[k].ap, [fb[k].d], [f_d.d])
        for q in range(4):
            pb = PS[q % 4]
            for i in range(4):
                kc = 4 * q + i
                TR(pb.ap[:, i * 128:(i + 1) * 128], f_.ap[:, kc * 128:(kc + 1) * 128], ident32.ap, [f_.d, ident32.d], [pb.d])
            CP("act" if q % 2 == 0 else "dve", fT[k].ap[:, 4 * q:4 * q + 4, :], pb.ap.rearrange("p (a b) -> p a b", b=128),
               [pb.d], [fT[k].d])
        pl = PS[4 + k]
        for kc in range(16):
            MM(pl.ap[:, 0:16], fT[k].ap[:, kc, :], wr.ap[:, kc, :], kc == 0, kc == 15, [fT[k].d, wr.d], [pl.d])
        P.emit("dve", (lambda o, i: (lambda e: e.reduce_max(out=o, in_=i, axis=mybir.AxisListType.X)))(s5.ap[:, 3:4], pl.ap[:, 0:16]),
               [pl.d], [s5.d])
        TS("dve", s5.ap[:, 4:5], s5.ap[:, 3:4], -1.0, None, ALU.mult, None, [s5.d], [s5.d])
        ACT(ex[k].ap, pl.ap[:, 0:16], AF.Exp, [pl.d, s5.d], [ex[k].d, s5.d], bias=s5.ap[:, 4:5], accum=s5.ap[:, 5:6])
        RECIP(s5.ap[:, 6:7], s5.ap[:, 5:6], [s5.d], [s5.d])
        TS("dve", ex[k].ap, ex[k].ap, s5.ap[:, 6:7], None, ALU.mult, None, [ex[k].d, s5.d], [ex[k].d])
        pa = PS[6 + k]
        TR(pa.ap[0:16, 0:128], ex[k].ap, ident32.ap, [ex[k].d, ident32.d], [pa.d])
        CP("act", affT.ap[:, t * 128:(t + 1) * 128], pa.ap[0:16, 0:128], [pa.d], [affT.d])
    if DEBUG:
        DMA("sp", dbg["aff"], affT.ap, [affT.d], [])
    vals = T(AR.alloc([16, 512], F32))
    idxu = T(AR.alloc([16, 512], U32))
    idxf = T(AR.alloc([16, 512], F32))
    for r in range(64):
        v8 = vals.ap[:, r * 8:(r + 1) * 8]
        i8 = idxu.ap[:, r * 8:(r + 1) * 8]
        P.emit("dve", (lambda o, i: (lambda e: e.max(out=o, in_=i)))(v8, affT.ap), [affT.d], [vals.d])
        P.emit("dve", (lambda o, m, i: (lambda e: e.max_index(out=o, in_max=m, in_values=i)))(i8, v8, affT.ap),
               [affT.d, vals.d], [idxu.d])
        P.emit("dve", (lambda o, m, i: (lambda e: e.match_replace(out=o, in_to_replace=m, in_values=i, imm_value=-1.0)))(affT.ap, v8, affT.ap),
               [vals.d, idxu.d], [affT.d])
    CP("dve", idxf.ap, idxu.ap, [idxu.d], [idxf.d])
    if DEBUG:
        DMA("sp", dbg["vals"], vals.ap, [vals.d], [])
        DMA("sp", dbg["idx"], idxu.ap, [idxu.d], [])
    for s in range(4):
        pa = PS[s % 2]
        TR(pa.ap[:, 0:16], idxf.ap[:, s * 128:(s + 1) * 128], ident32.ap[0:16, 0:16], [idxf.d, ident32.d], [pa.d])
        TR(pa.ap[:, 16:32], vals.ap[:, s * 128:(s + 1) * 128], ident32.ap[0:16, 0:16], [vals.d, ident32.d], [pa.d])
        CP("dve", idxc.ap[:, s, :], pa.ap[:, 0:16], [pa.d], [idxc.d])
        CP("act", gatc.ap[:, s, :], pa.ap[:, 16:32], [pa.d], [gatc.d])
    P.barrier()
    if LIMIT == 5:
        return finish()
    AR.top = p5_mark

    g2b = T(AR.alloc([128, 2048], F32))
    load_mod_bc(g2b, 0, 5)
    NW = 5
    wbuf = [T(AR.alloc([128, 8192], BF16)) for _ in range(NW)]
    xg = [T(AR.alloc([128, 2048], BF16)) for _ in range(8)]
    xeT = [T(AR.alloc([128, 16, 512], BF16)) for _ in range(2)]
    hidT = [T(AR.alloc([128, 8, 512], BF16), 8) for _ in range(2)]
    silt = [T(AR.alloc([128, 512], BF16)) for _ in range(2)]
    yet = [T(AR.alloc([128, 512], F32)) for _ in range(2)]
    yo = [T(AR.alloc([128, 1024], F32)) for _ in range(2)]
    hall = Dep()
    srcs_all = []
    for e_ in range(16):
        srcs_all += [(w_gate[e_, 0], 0), (w_up[e_, 0], 0), (w_gate[e_, 1], 0), (w_up[e_, 1], 0),
                     (w_down[e_, 0], 1), (w_down[e_, 1], 1)]
    piece_view = {}
    state6 = {"issued": 0}

    def ensure_issued(upto):
        while state6["issued"] < min(upto, len(srcs_all)):
            i = state6["issued"]
            src, kind = srcs_all[i]
            wb = wbuf[i % NW]
            if kind == 0:
                dst = wb.ap.rearrange("p (a b) -> p a b", b=512)
                DMA("pool", dst[:, 0:8, :], src[:, 0:8, :], [], [wb.d])
                DMA("pool", dst[:, 8:16, :], src[:, 8:16, :], [], [wb.d])
            else:
                dst = wb.ap.rearrange("p (a b) -> p a b", b=1024)
                DMA("pool", dst[:, 0:4, :], src[:, 0:4, :], [], [wb.d])
                DMA("pool", dst[:, 4:8, :], src[:, 4:8, :], [], [wb.d])
            piece_view[i] = (wb, dst)
            state6["issued"] = i + 1

    def gather_issue(e_):
        for s in range(4):
            xg_ = xg[(e_ % 2) * 4 + s]
            P.emit("pool", (lambda o, i, ix: (lambda e: e.indirect_dma_start(
                out=o, out_offset=None, in_=i, in_offset=bass.IndirectOffsetOnAxis(ap=ix, axis=0))))(
                    xg_.ap, f_d.ap, idxc.ap[:, s, e_:e_ + 1]), [f_d.d, idxc.d], [xg_.d], dma=True)

    yi = 0
    pgi = 0
    gather_issue(0)
    ensure_issued(NW - 1)
    for e_ in range(16):
        xe = xeT[e_ % 2]
        for s in range(4):
            xg_ = xg[(e_ % 2) * 4 + s]
            for q in range(4):
                pv = psbf(6 + (q % 2))
                pb = PS[6 + (q % 2)]
                for i in range(4):
                    kc = 4 * q + i
                    TR(pv[:, i * 128:(i + 1) * 128], xg_.ap[:, kc * 128:(kc + 1) * 128], identbf.ap, [xg_.d, identbf.d], [pb.d])
                CP("act" if q % 2 == 0 else "dve", xe.ap[:, 4 * q:4 * q + 4, s * 128:(s + 1) * 128],
                   pv[:, 0:512].rearrange("p (a b) -> p a b", b=128), [pb.d], [xe.d])
        if e_ + 1 < 16:
            gather_issue(e_ + 1)
        hT = hidT[e_ % 2]
        p0 = 6 * e_
        for fcu in range(8):
            if fcu % 4 == 0:
                ensure_issued(p0 + 2 * (fcu // 4) + NW)
            wgp, wgv = piece_view[p0 + 0 + 2 * (fcu // 4)]
            wup, wuv = piece_view[p0 + 1 + 2 * (fcu // 4)]
            off = (fcu % 4) * 128
            k = pgi % 2
            pgi += 1
            pg, pu = PS[0 + 2 * k], PS[1 + 2 * k]
            for kc in range(16):
                MM(pg.ap, wgv[:, kc, off:off + 128], xe.ap[:, kc, :], kc == 0, kc == 15, [wgp.d, xe.d], [pg.d])
            for kc in range(16):
                MM(pu.ap, wuv[:, kc, off:off + 128], xe.ap[:, kc, :], kc == 0, kc == 15, [wup.d, xe.d], [pu.d])
            ACT(silt[k].ap, pg.ap, AF.Silu, [pg.d], [silt[k].d])
            TT("dve", hT.ap[:, fcu, :], silt[k].ap, pu.ap, ALU.mult, [silt[k].d, pu.d], [hT.ds[fcu]])
        for dh in range(2):
            ensure_issued(p0 + 4 + dh + NW)
            wdp, wdv = piece_view[p0 + 4 + dh]
            for s in range(4):
                yo_ = yo[yi % 2]
                yi += 1
                for dq in range(2):
                    k = pgi % 2
                    pgi += 1
                    py = PS[4 + k]
                    for fc in range(8):
                        MM(py.ap, hT.ap[:, fc, s * 128:(s + 1) * 128], wdv[:, fc, dq * 512:(dq + 1) * 512], fc == 0, fc == 7,
                           [hT.ds[fc], wdp.d], [py.d])
                    ACT(yet[k].ap, py.ap, AF.Identity, [py.d, gatc.d], [yet[k].d], scale=gatc.ap[:, s, e_:e_ + 1])
                    gc = slice(dh * 1024 + dq * 512, dh * 1024 + (dq + 1) * 512)
                    TT("dve", yo_.ap[:, dq * 512:(dq + 1) * 512], yet[k].ap, g2b.ap[:, gc], ALU.mult,
                       [yet[k].d, g2b.d], [yo_.d])
                P.emit("pool", (lambda o, i, ix: (lambda e: e.indirect_dma_start(
                    out=o, out_offset=bass.IndirectOffsetOnAxis(ap=ix, axis=0), in_=i, in_offset=None, compute_op=ALU.add)))(
                        hacc_h[dh].ap, yo_.ap, idxc.ap[:, s, e_:e_ + 1]),
                    [yo_.d, idxc.d], [hall], dma=True)
    P.barrier()
    if LIMIT == 6:
        return finish()
    AR.top = persist_mark

    fnb = T(AR.alloc([128, 2048], F32))
    load_vec_bc(fnb, nfin)
    hx = [T(AR.alloc([128, 2048], F32)) for _ in range(2)]
    ob = [T(AR.alloc([128, 2048], F32)) for _ in range(2)]
    st7 = [T(AR.alloc([128, 4], F32)) for _ in range(2)]
    junk7 = T(AR.alloc([128, 2048], BF16))
    def p7_load(t_):
        for i2 in range(2):
            DMA("sp", hx[t_ % 2].ap[:, i2 * 1024:(i2 + 1) * 1024], hacc_h[i2].ap[t_ * 128:(t_ + 1) * 128, :], [hall], [hx[t_ % 2].d])

    p7_load(0)
    for t in range(NTL):
        k = t % 2
        if t + 1 < NTL:
            p7_load(t + 1)
        s7 = st7[k]
        MEMSET("pool", s7.ap, 0.0, [s7.d])
        ACT(junk7.ap, hx[k].ap, AF.Square, [hx[k].d], [junk7.d, s7.d], accum=s7.ap[:, 0:1])
        rstd_from_ss(s7.ap[:, 0:1], s7.ap[:, 1:2], s7.ap[:, 2:3], 2048.0, s7.d, s7.d, s7.d)
        STT("dve", ob[k].ap, hx[k].ap, s7.ap[:, 2:3], fnb.ap, ALU.mult, ALU.mult, [hx[k].d, s7.d, fnb.d], [ob[k].d])
        DMA("sp", out[t], ob[k].ap, [ob[k].d], [])
    P.barrier()
    P.finalize(st)
    st.close()
    return nc


def _kc_p(w):
    K, N = w.shape
    return np.ascontiguousarray(w.reshape(K // 128, 128, N).transpose(1, 0, 2))


def _prep_shared(c_ctx, w_mod, b_mod, norm_mix_w, w_in, ssm_conv_w, ssm_conv_b, ssm_a_log, ssm_dt_bias, ssm_d,
                 ssm_norm_w, sc_conv_w, sc_norm_w, w_out, norm_ffn_w, w_router, w_gate, w_up, w_down, final_norm_w):
    f = np.float32
    sh = {}
    sh["w_mod"] = np.ascontiguousarray(w_mod[0].reshape(128, 16, 12288))
    sh["b_mod"] = np.ascontiguousarray(b_mod[0].reshape(1, 12288))
    sh["nmix"] = norm_mix_w[0].reshape(1, 2048)
    sh["nffn"] = norm_ffn_w[0].reshape(1, 2048)
    sh["nfin"] = final_norm_w.reshape(1, 2048)
    win = w_in[0]
    wg, cw, cb = [], [], []
    for g in range(4):
        cols = np.concatenate([2048 + g * 512 + np.arange(512), 4096 + g * 128 + np.arange(128),
                               4608 + g * 128 + np.arange(128), g * 512 + np.arange(512),
                               5120 + g * 8 + np.arange(8), 5152 + g * 8 + np.arange(8)])
        wg.append(_kc_p(win[:, cols]))
        ch = np.concatenate([g * 512 + np.arange(512), 2048 + g * 128 + np.arange(128), 2560 + g * 128 + np.arange(128)])
        cw.append(ssm_conv_w[0][:, ch].T.reshape(6, 128, 3).transpose(1, 0, 2))
        cb.append(ssm_conv_b[0][ch].reshape(6, 128).T)
    sh["wg_ssm"] = np.ascontiguousarray(np.stack(wg)).astype(f)
    sh["cw_ssm"] = np.ascontiguousarray(np.stack(cw)).astype(f)
    sh["cb_ssm"] = np.ascontiguousarray(np.stack(cb)).astype(f)
    wsc, cws, scw = [], [], []
    for j in range(4):
        ch = j * 512 + np.arange(512)
        cols = np.concatenate([5184 + ch, 7232 + ch, 9280 + ch])
        wsc.append(_kc_p(win[:, cols]))
        cws.append(sc_conv_w[0][:, ch].T.reshape(4, 128, 3).transpose(1, 0, 2))
        scw.append(sc_norm_w[0][ch].reshape(4, 128).T)
    sh["wg_sc"] = np.ascontiguousarray(np.stack(wsc)).astype(f)
    sh["cw_sc"] = np.ascontiguousarray(np.stack(cws)).astype(f)
    sh["scw"] = np.ascontiguousarray(np.stack(scw)).astype(f)
    hsel = np.stack([np.concatenate([g * 8 + np.arange(8), 32 + g * 8 + np.arange(8)]) for g in range(4)])
    sh["dtb"] = np.ascontiguousarray(ssm_dt_bias[0][hsel].reshape(4, 1, 16))
    sh["alog"] = np.ascontiguousarray(ssm_a_log[0][hsel].reshape(4, 1, 16))
    sh["dsk"] = np.ascontiguousarray(np.repeat(ssm_d[0].reshape(4, 8), 64, axis=1).reshape(4, 1, 512))
    sh["ssm_nw"] = np.ascontiguousarray(ssm_norm_w[0].reshape(4, 1, 512))
    wo = _kc_p(w_out[0])
    sh["w_out"] = np.ascontiguousarray(np.stack([wo[:, :, 0:1024], wo[:, :, 1024:2048]]))
    sh["w_rt"] = _kc_p(w_router[0])
    wgt = w_gate[0].reshape(16, 16, 128, 2, 512).transpose(0, 3, 2, 1, 4)
    wup = w_up[0].reshape(16, 16, 128, 2, 512).transpose(0, 3, 2, 1, 4)
    wdn = w_down[0].reshape(16, 8, 128, 2, 1024).transpose(0, 3, 2, 1, 4)
    sh["w_gate"] = np.ascontiguousarray(wgt)
    sh["w_up"] = np.ascontiguousarray(wup)
    sh["w_down"] = np.ascontiguousarray(wdn)
    sh["c_ident"] = np.eye(128, dtype=f)
    k = np.arange(128)[:, None]
    i = np.arange(128)[None, :]
    sh["c_tri"] = np.stack([(k <= i), (k >= i)]).astype(f)
    mf = np.where(i < k, NEG, 0.0).astype(f)
    mb = np.where(i > k, NEG, 0.0).astype(f)
    sh["c_mask"] = np.stack([np.tile(mf, (1, 4)), np.tile(mb, (1, 4))]).astype(f)
    return sh


_CACHE = {}


def kernel(x, c, ctx, c_ctx, w_mod, b_mod, norm_mix_w, w_in, ssm_conv_w, ssm_conv_b, ssm_a_log, ssm_dt_bias, ssm_d,
           ssm_norm_w, sc_conv_w, sc_norm_w, w_out, norm_ffn_w, w_router, w_gate, w_up, w_down, final_norm_w):
    args = [np.asarray(a, dtype=np.float32) for a in (
        c_ctx, w_mod, b_mod, norm_mix_w, w_in, ssm_conv_w, ssm_conv_b, ssm_a_log, ssm_dt_bias, ssm_d, ssm_norm_w,
        sc_conv_w, sc_norm_w, w_out, norm_ffn_w, w_router, w_gate, w_up, w_down, final_norm_w)]
    x = np.asarray(x, dtype=np.float32)
    c = np.asarray(c, dtype=np.float32)
    ctx = np.asarray(ctx, dtype=np.float32)
    sh = _prep_shared(*args)
    if "nc" not in _CACHE:
        _CACHE["nc"] = build_program()
    nc = _CACHE["nc"]
    in_maps = []
    for core in range(NCORES):
        b = core % 4
        m = dict(sh)
        m["xin"] = np.ascontiguousarray(np.concatenate([ctx[b], x[b]], axis=0).reshape(NT, 128, 2048))
        m["cvec"] = np.ascontiguousarray(np.stack([c[b], args[0]], axis=-1).reshape(128, 16, 2))
        in_maps.append(m)
    res = run_bass_kernel_spmd(nc, in_maps, core_ids=list(range(NCORES)))
    _CACHE["res"] = res
    outs = [res.results[b % NCORES]["out"].reshape(4096, 2048) for b in range(4)]
    return np.stack(outs).astype(np.float32)
```

```python
import numpy as np
from contextlib import ExitStack
import concourse.bass as bass
import concourse.mybir as mybir
from concourse.bass_utils import run_bass_kernel_spmd

F32 = mybir.dt.float32
BF16 = mybir.dt.bfloat16
I32 = mybir.dt.int32
U32 = mybir.dt.uint32
U8 = mybir.dt.uint8
AF = mybir.ActivationFunctionType
ALU = mybir.AluOpType

ENGS = ("pe", "act", "dve", "pool", "sp")
DEBUG = False
LIMIT = 99
STOPAT = None
MARKS = {}
NG = 4
SCAN = 2
NCORES = 4
EPS = 1e-6
NT = 34
NTL = 32
NEG = -30000.0


class Dep:
    __slots__ = ("w", "r", "excl")

    def __init__(self, excl=False):
        self.w = None
        self.r = []
        self.excl = excl


class Instr:
    __slots__ = ("eng", "fn", "dma", "deps", "sig", "sem", "val")

    def __init__(self, eng, fn, dma):
        self.eng = eng
        self.fn = fn
        self.dma = dma
        self.deps = set()
        self.sig = False
        self.sem = None
        self.val = 0


def _nop_fn(eng):
    return eng.nop()


class Prog:
    def __init__(self, nc, n_dma_sems=12):
        self.nc = nc
        self.lists = {e: [] for e in ENGS}
        self.last = {e: None for e in ENGS}
        self.n_dma_sems = n_dma_sems
        self.dma_count = {e: 0 for e in ENGS}
        self.dma_ring = {e: [None] * n_dma_sems for e in ENGS}

    def emit(self, eng, fn, reads=(), writes=(), dma=False, extra=()):
        ins = Instr(eng, fn, dma)
        self.count = getattr(self, "count", 0) + 1
        if STOPAT is not None and self.count > STOPAT and not getattr(self, "in_finish", False):
            return ins
        for d in reads:
            if d.w is not None:
                ins.deps.add(d.w)
            if d.excl:
                for r in d.r:
                    if r.eng != eng:
                        ins.deps.add(r)
            if not dma:
                d.r = [r for r in d.r if r.dma or r.eng != eng]
            d.r.append(ins)
        for d in writes:
            if d.w is not None:
                ins.deps.add(d.w)
            for r in d.r:
                ins.deps.add(r)
            d.w = ins
            d.r = []
        for p in extra:
            if p is not None:
                ins.deps.add(p)
        ins.deps.discard(ins)
        if dma:
            k = self.dma_count[eng]
            self.dma_count[eng] = k + 1
            slot = k % self.n_dma_sems
            prev = self.dma_ring[eng][slot]
            if prev is not None:
                ins.deps.add(prev)
            self.dma_ring[eng][slot] = ins
            ins.sem = (eng, slot)
        self.lists[eng].append(ins)
        self.last[eng] = ins
        return ins

    def barrier(self):
        ext = [self.last[e] for e in ENGS if self.last[e] is not None]
        for e in ENGS:
            for p in self.dma_ring[e]:
                if p is not None:
                    ext.append(p)
        b = self.emit("dve", lambda eng: eng.engine_nop(), extra=ext)
        for e in ENGS:
            if e != "dve":
                self.emit(e, _nop_fn, extra=[b])
        return b

    def finalize(self, stack):
        nc = self.nc
        for e in ENGS:
            for ins in self.lists[e]:
                for p in ins.deps:
                    if p.eng == "pe" and ins.eng == "pe" and not p.dma and not ins.dma:
                        continue
                    p.sig = True
        esem = {e: stack.enter_context(nc.semaphore("s_" + e)) for e in ENGS}
        dsem = {}
        for e in ENGS:
            for s in range(min(self.n_dma_sems, self.dma_count[e])):
                dsem[(e, s)] = stack.enter_context(nc.semaphore("d_%s_%d" % (e, s)))
        dcount = {k: 0 for k in dsem}
        for e in ENGS:
            c = 0
            for ins in self.lists[e]:
                if ins.dma:
                    dcount[ins.sem] += 16
                    ins.val = dcount[ins.sem]
                    ins.sig = True
                elif ins.sig:
                    c += 1
                    ins.val = c
                    ins.sem = e
        block = stack.enter_context(nc.Block())
        engobj = {"pe": "tensor", "act": "scalar", "dve": "vector", "pool": "gpsimd", "sp": "sync"}

        def make_body(e):
            def body(eng):
                known = {}
                for ins in self.lists[e]:
                    for p in ins.deps:
                        if p.eng == "pe" and e == "pe" and not p.dma and not ins.dma:
                            continue
                        if p.dma:
                            sem = dsem[p.sem]
                            key = ("d",) + p.sem
                        else:
                            sem = esem[p.eng]
                            key = ("e", p.eng)
                        if known.get(key, 0) >= p.val:
                            continue
                        known[key] = p.val
                        eng.wait_ge(sem, p.val)
                    r = ins.fn(eng)
                    if ins.dma:
                        r.then_inc(dsem[ins.sem], 16)
                    elif ins.sig:
                        r.then_inc(esem[e], 1)
            return body

        for e in ENGS:
            if self.lists[e]:
                getattr(block, engobj[e])(make_body(e))


class Arena:
    def __init__(self, ap_u8, nbytes):
        self.a = ap_u8
        self.n = nbytes
        self.top = 0

    def alloc(self, shape, dt):
        esz = {F32: 4, BF16: 2, I32: 4, U32: 4}[dt]
        free = 1
        for s in shape[1:]:
            free *= s
        nb = (free * esz + 63) // 64 * 64
        assert self.top + nb <= self.n, ("SBUF arena overflow", self.top, nb, self.n)
        v = self.a[0:shape[0], self.top:self.top + free * esz].bitcast(dt)
        self.top += nb
        if len(shape) == 3:
            v = v.rearrange("p (a b) -> p a b", b=shape[2])
        return v


class T:
    def __init__(self, ap, nsub=0):
        self.ap = ap
        self.d = Dep()
        self.ds = [Dep() for _ in range(nsub)]


def build_program():
    nc = bass.Bass("TRN2", target_bir_lowering=False)
    st = ExitStack()
    P = Prog(nc)

    def finish():
        P.in_finish = True
        P.barrier()
        P.finalize(st)
        st.close()
        return nc

    def din(name, shape, dt=F32):
        return nc.dram_tensor(name, list(shape), dt, kind="ExternalInput").ap()

    def dscr(name, shape, dt):
        return nc.dram_tensor(name, list(shape), dt, kind="ExternalOutput" if DEBUG else "Internal").ap()

    xin = din("xin", [NT, 128, 2048])
    cvec = din("cvec", [128, 16, 2])
    w_mod = din("w_mod", [128, 16, 12288])
    b_mod = din("b_mod", [1, 12288])
    nmix = din("nmix", [1, 2048])
    nffn = din("nffn", [1, 2048])
    nfin = din("nfin", [1, 2048])
    wg_ssm = din("wg_ssm", [4, 128, 16, 1296]) if LIMIT >= 2 else None
    wg_sc = din("wg_sc", [4, 128, 16, 1536]) if LIMIT >= 3 else None
    cw_ssm = din("cw_ssm", [4, 128, 6, 3])
    cb_ssm = din("cb_ssm", [4, 128, 6])
    cw_sc = din("cw_sc", [4, 128, 4, 3])
    scw = din("scw", [4, 128, 4])
    dtb = din("dtb", [4, 1, 16])
    alog = din("alog", [4, 1, 16])
    dsk = din("dsk", [4, 1, 512])
    ssm_nw = din("ssm_nw", [4, 1, 512])
    w_out = din("w_out", [2, 128, 32, 1024]) if LIMIT >= 4 else None
    w_rt = din("w_rt", [128, 16, 16])
    if LIMIT >= 6:
        w_gate = din("w_gate", [16, 2, 128, 16, 512])
        w_up = din("w_up", [16, 2, 128, 16, 512])
        w_down = din("w_down", [16, 2, 128, 8, 1024])
    c_ident = din("c_ident", [128, 128])
    c_tri = din("c_tri", [2, 128, 128])
    c_mask = din("c_mask", [2, 128, 512])
    out = nc.dram_tensor("out", [NTL, 128, 2048], F32, kind="ExternalOutput").ap()

    mod_d = T(dscr("mod_d", [2, 12288], F32))
    aT_d = T(dscr("aT_d", [9, 128, 16, 512], BF16), 9)
    z_d = T(dscr("z_d", [4, NTL, 128, 512], BF16), 4 * NTL)
    yT_d = T(dscr("yT_d", [NTL, 128, 32, 128], BF16), NTL)
    hacc_h = [T(dscr("hacc_d%d" % i, [NTL * 128, 1024], F32), NTL) for i in range(2)]
    f_d = T(dscr("f_d", [NTL * 128, 2048], BF16))
    dbg = {}
    if DEBUG:
        dbg["h1"] = nc.dram_tensor("dbg_h1", [NTL * 128, 2048], F32, kind="ExternalOutput").ap()
        dbg["aff"] = nc.dram_tensor("dbg_aff", [16, 4096], F32, kind="ExternalOutput").ap()
        dbg["vals"] = nc.dram_tensor("dbg_vals", [16, 512], F32, kind="ExternalOutput").ap()
        dbg["idx"] = nc.dram_tensor("dbg_idx", [16, 512], U32, kind="ExternalOutput").ap()
        dbg["rsc"] = nc.dram_tensor("dbg_rsc", [128, NTL], F32, kind="ExternalOutput").ap()
        dbg["dt"] = nc.dram_tensor("dbg_dt", [4, 128, NT, 16], F32, kind="ExternalOutput").ap()
        dbg["xs"] = nc.dram_tensor("dbg_xs", [4, 128, NT, 512], BF16, kind="ExternalOutput").ap()

    sb_bytes = (int(nc.sbuf_bytes_remaining) - 2048) // 256 * 256
    arena_t = st.enter_context(nc.sbuf_tensor("arena", [128, sb_bytes], U8))
    AR = Arena(arena_t[:, :], sb_bytes)
    banks = [st.enter_context(nc.psum_tensor("bank%d" % i, [128, 512], F32)) for i in range(8)]
    PS = [T(b[:, :]) for b in banks]
    for p_ in PS:
        p_.d.excl = True

    def psbf(i):
        return banks[i][:, :].bitcast(BF16)

    def MM(out, lhsT, rhs, start, stop, rd, wr):
        return P.emit("pe", lambda e: e.matmul(out, lhsT=lhsT, rhs=rhs, start=start, stop=stop), rd, wr)

    def TR(out, in_, ident, rd, wr):
        return P.emit("pe", lambda e: e.transpose(out, in_, ident), rd, wr)

    def ACT(out, in_, func, rd, wr, bias=None, scale=None, accum=None):
        kw = {}
        if bias is not None:
            kw["bias"] = bias
        if scale is not None:
            kw["scale"] = scale
        if accum is not None:
            kw["accum_out"] = accum
        return P.emit("act", lambda e: e.activation(out=out, in_=in_, func=func, **kw), rd, wr)

    def TT(eng, out, in0, in1, op, rd, wr):
        return P.emit(eng, lambda e: e.tensor_tensor(out=out, in0=in0, in1=in1, op=op), rd, wr)

    def TS(eng, out, in0, s1, s2, op0, op1, rd, wr):
        if s2 is None:
            return P.emit(eng, lambda e: e.tensor_scalar(out=out, in0=in0, scalar1=s1, scalar2=None, op0=op0), rd, wr)
        return P.emit(eng, lambda e: e.tensor_scalar(out=out, in0=in0, scalar1=s1, scalar2=s2, op0=op0, op1=op1), rd, wr)

    def STT(eng, out, in0, scalar, in1, op0, op1, rd, wr):
        return P.emit(eng, lambda e: e.scalar_tensor_tensor(out=out, in0=in0, scalar=scalar, in1=in1, op0=op0, op1=op1), rd, wr)

    def CP(eng, out, in_, rd, wr):
        if eng == "act":
            return P.emit("act", lambda e: e.copy(out=out, in_=in_), rd, wr)
        return P.emit(eng, lambda e: e.tensor_copy(out=out, in_=in_), rd, wr)

    def MEMSET(eng, ap, val, wr):
        return P.emit(eng, lambda e: e.memset(ap, val), (), wr)

    def RECIP(out, in_, rd, wr):
        return P.emit("dve", lambda e: e.reciprocal(out=out, in_=in_), rd, wr)

    def DMA(q, out, in_, rd, wr):
        return P.emit(q, lambda e: e.dma_start(out=out, in_=in_), rd, wr, dma=True)

    def rstd_from_ss(ss_ap, std_ap, r_ap, n, dss, dstd, dr):
        ACT(std_ap, ss_ap, AF.Sqrt, [dss], [dstd], bias=epsc.ap[0:ss_ap.shape[0], :], scale=1.0 / n)
        RECIP(r_ap, std_ap, [dstd], [dr])

    ident32 = T(AR.alloc([128, 128], F32))
    identbf = T(AR.alloc([128, 128], BF16))
    ones32 = T(AR.alloc([128, 128], F32))
    onesbf = T(AR.alloc([128, 8], BF16))
    epsc = T(AR.alloc([128, 1], F32))
    DMA("sp", ident32.ap, c_ident, [], [ident32.d])
    DMA("pool", identbf.ap, c_ident, [], [identbf.d])
    MEMSET("dve", ones32.ap, 1.0, [ones32.d])
    MEMSET("dve", onesbf.ap, 1.0, [onesbf.d])
    MEMSET("dve", epsc.ap, EPS, [epsc.d])
    persist_mark = AR.top

    cv = T(AR.alloc([128, 16, 2], F32))
    cs_ = T(AR.alloc([128, 16, 2], F32))
    DMA("sp", cv.ap, cvec, [], [cv.d])
    ACT(cs_.ap, cv.ap, AF.Silu, [cv.d], [cs_.d])
    wch = [T(AR.alloc([128, 16, 512], F32)) for _ in range(3)]
    bch = [T(AR.alloc([2, 512], F32)) for _ in range(2)]
    mch = [T(AR.alloc([2, 512], F32)) for _ in range(2)]
    def p0_load(n_):
        w_ = wch[n_ % 3]
        cols_ = slice(n_ * 512, (n_ + 1) * 512)
        DMA("sp", w_.ap[:, 0:8, :], w_mod[:, 0:8, cols_], [], [w_.d])
        DMA("act", w_.ap[:, 8:16, :], w_mod[:, 8:16, cols_], [], [w_.d])

    p0_load(0)
    p0_load(1)
    for n in range(24):
        w = wch[n % 3]
        cols = slice(n * 512, (n + 1) * 512)
        if n + 2 < 24:
            p0_load(n + 2)
        bb = bch[n % 2]
        DMA("sp", bb.ap, b_mod[0:1, cols].partition_broadcast(2), [], [bb.d])
        pb = PS[n % 2]
        for kc in range(16):
            MM(pb.ap[0:2, :], cs_.ap[:, kc, :], w.ap[:, kc, :], kc == 0, kc == 15, [cs_.d, w.d], [pb.d])
        m = mch[n % 2]
        TT("dve", m.ap, pb.ap[0:2, :], bb.ap, ALU.add, [pb.d, bb.d], [m.d])
        if n // 4 in (1, 4):
            TS("dve", m.ap, m.ap, 1.0, None, ALU.add, None, [m.d], [m.d])
        DMA("sp", mod_d.ap[:, cols], m.ap, [m.d], [mod_d.d])
    P.barrier()
    if LIMIT == 0:
        return finish()
    AR.top = persist_mark

    def load_mod_bc(dst, row, seg, q="sp"):
        DMA(q, dst.ap, mod_d.ap[row:row + 1, seg * 2048:(seg + 1) * 2048].partition_broadcast(128), [mod_d.d], [dst.d])

    def load_vec_bc(dst, src, q="sp"):
        DMA(q, dst.ap, src.partition_broadcast(128), [], [dst.d])

    A1 = [T(AR.alloc([128, 2048], F32)) for _ in range(2)]
    B1 = [T(AR.alloc([128, 2048], F32)) for _ in range(2)]
    nmb = T(AR.alloc([128, 2048], F32))
    load_vec_bc(nmb, nmix)
    for r in range(2):
        load_mod_bc(A1[r], r, 1)
        load_mod_bc(B1[r], r, 0)
        TT("pool", A1[r].ap, A1[r].ap, nmb.ap, ALU.mult, [A1[r].d, nmb.d], [A1[r].d])
    xt = [T(AR.alloc([128, 2048], F32)) for _ in range(2)]
    tmp = [T(AR.alloc([128, 2048], F32)) for _ in range(2)]
    abf = [T(AR.alloc([128, 2048], BF16)) for _ in range(2)]
    junk = T(AR.alloc([128, 2048], BF16))
    ss1 = T(AR.alloc([128, NT], F32), NT)
    sd1 = T(AR.alloc([128, NT], F32), NT)
    rs1 = T(AR.alloc([128, NT], F32), NT)
    aTb = [T(AR.alloc([128, 16, 512], BF16)) for _ in range(2)]
    MEMSET("dve", ss1.ap, 0.0, ss1.ds)
    order1 = []
    for tb in range(9):
        tiles = [0, 1] if tb == 0 else [2 + 4 * (tb - 1) + i for i in range(4)]
        for j, t in enumerate(tiles):
            order1.append((tb, j, t, j == len(tiles) - 1, 128 * len(tiles)))

    def p1_load(t):
        DMA("sp", xt[t % 2].ap, xin[t], [], [xt[t % 2].d])

    def p1_stats(t):
        x_ = xt[t % 2]
        ACT(junk.ap, x_.ap, AF.Square, [x_.d], [junk.d, ss1.ds[t]], accum=ss1.ap[:, t:t + 1])
        rstd_from_ss(ss1.ap[:, t:t + 1], sd1.ap[:, t:t + 1], rs1.ap[:, t:t + 1], 2048.0, ss1.ds[t], sd1.ds[t], rs1.ds[t])

    def p1_stt(t_):
        r_ = 1 if t_ < 2 else 0
        STT("dve", tmp[t_ % 2].ap, xt[t_ % 2].ap, rs1.ap[:, t_:t_ + 1], A1[r_].ap, ALU.mult, ALU.mult,
            [xt[t_ % 2].d, rs1.ds[t_], A1[r_].d], [tmp[t_ % 2].d])

    p1_load(0)
    p1_stats(0)
    for i1, (tb, j, t, lastj, ntok) in enumerate(order1):
        ab = aTb[tb % 2]
        r = 1 if t < 2 else 0
        x_ = xt[t % 2]
        tm = tmp[t % 2]
        if i1 == 0:
            p1_stt(t)
        if i1 + 1 < len(order1):
            p1_load(order1[i1 + 1][2])
            p1_stats(order1[i1 + 1][2])
        a_ = abf[t % 2]
        TT("pool", a_.ap[:, 0:1024], tm.ap[:, 0:1024], B1[r].ap[:, 0:1024], ALU.add, [tm.d, B1[r].d], [a_.d])
        TT("dve", a_.ap[:, 1024:2048], tm.ap[:, 1024:2048], B1[r].ap[:, 1024:2048], ALU.add, [tm.d, B1[r].d], [a_.d])
        if i1 + 1 < len(order1):
            p1_stt(order1[i1 + 1][2])
        for q in range(4):
            pb = PS[2 + (q % 2)]
            pv = psbf(2 + (q % 2))
            for i in range(4):
                kc = 4 * q + i
                TR(pv[:, i * 128:(i + 1) * 128], a_.ap[:, kc * 128:(kc + 1) * 128], identbf.ap, [a_.d, identbf.d], [pb.d])
            CP("act" if q % 2 == 0 else "dve", ab.ap[:, 4 * q:4 * q + 4, j * 128:(j + 1) * 128],
               pv[:, 0:512].rearrange("p (a b) -> p a b", b=128), [pb.d], [ab.d])
        if lastj:
            DMA("sp", aT_d.ap[tb][:, :, 0:ntok], ab.ap[:, :, 0:ntok], [ab.d], [aT_d.ds[tb]])
    P.barrier()
    if LIMIT == 1:
        return finish()
    AR.top = persist_mark

    tri = [T(AR.alloc([128, 128], F32)) for _ in range(2)]
    maskn = [T(AR.alloc([128, 512], BF16)) for _ in range(2)]
    for d_ in range(2):
        DMA("sp", tri[d_].ap, c_tri[d_], [], [tri[d_].d])
        DMA("pool", maskn[d_].ap, c_mask[d_], [], [maskn[d_].d])
    xs = T(AR.alloc([128, NT, 512], BF16), NT)
    Btok = T(AR.alloc([128, NT, 128], BF16), NT)
    BT = T(AR.alloc([128, NT * 128], BF16), NT)
    CT = T(AR.alloc([128, NT * 128], BF16), NT)
    dtr = T(AR.alloc([128, NT, 16], F32))
    dtv = T(AR.alloc([128, NT, 16], F32))
    lav = T(AR.alloc([128, NT, 16], F32))
    cwt = T(AR.alloc([128, 6, 3], F32))
    cbt = T(AR.alloc([128, 6], F32))
    dtbb = T(AR.alloc([128, 16], F32))
    aneg = T(AR.alloc([128, 16], F32))
    Dbc = T(AR.alloc([128, 512], F32))
    nwb = T(AR.alloc([128, 512], F32))
    p2_mark = AR.top

    for g in range(NG):
        DMA("sp", cwt.ap, cw_ssm[g], [], [cwt.d])
        DMA("sp", cbt.ap, cb_ssm[g], [], [cbt.d])
        load_vec_bc(dtbb, dtb[g])
        load_vec_bc(aneg, alog[g])
        load_vec_bc(Dbc, dsk[g])
        load_vec_bc(nwb, ssm_nw[g])
        ACT(aneg.ap, aneg.ap, AF.Exp, [aneg.d], [aneg.d])
        TS("dve", aneg.ap, aneg.ap, -1.0, None, ALU.mult, None, [aneg.d], [aneg.d])
        wg = T(AR.alloc([128, 16, 1296], BF16))
        for k2 in range(8):
            DMA("pool", wg.ap[:, 2 * k2:2 * k2 + 2, :], wg_ssm[g][:, 2 * k2:2 * k2 + 2, :], [], [wg.d])
        aTl = [T(AR.alloc([128, 16, 512], BF16)) for _ in range(2)]
        acc = [T(AR.alloc([128, 512], F32)) for _ in range(2)]
        xTt = [[T(AR.alloc([128, 512], BF16)) for _ in range(4)] for _ in range(2)]
        zst = [T(AR.alloc([128, 512], BF16)) for _ in range(2)]
        fmi = 0
        zi = 0
        MARKS.setdefault('A', P.count)
        for tb in range(9):
            ntok = 256 if tb == 0 else 512
            W = 256 if tb == 0 else 64
            tiles = [0, 1] if tb == 0 else [2 + 4 * (tb - 1) + i for i in range(4)]
            tok0 = tiles[0] * 128
            if tb == 1:
                MARKS.setdefault('C', P.count)
            if tb == 2:
                MARKS.setdefault('D', P.count)
            al = aTl[tb % 2]
            if tb == 0:
                DMA("sp", al.ap[:, :, 0:ntok], aT_d.ap[tb][:, :, 0:ntok], [aT_d.ds[tb]], [al.d])
            if tb + 1 < 9:
                DMA("sp", aTl[(tb + 1) % 2].ap, aT_d.ap[tb + 1], [aT_d.ds[tb + 1]], [aTl[(tb + 1) % 2].d])
            xTb = xTt[tb % 2]
            for oc in range(6):
                pb = PS[fmi % 2]
                fmi += 1
                for kc in range(16):
                    MM(pb.ap[:, 0:ntok], wg.ap[:, kc, oc * 128:(oc + 1) * 128], al.ap[:, kc, 0:ntok], kc == 0, kc == 15,
                       [wg.d, al.d], [pb.d])
                ac = acc[oc % 2]
                ACT(ac.ap[:, 0:ntok], pb.ap[:, 0:ntok], AF.Identity, [pb.d, cwt.d, cbt.d], [ac.d],
                    bias=cbt.ap[:, oc:oc + 1], scale=cwt.ap[:, oc, 1:2])
                u3 = pb.ap[:, 0:ntok].rearrange("p (r w) -> p r w", w=W)
                a3 = ac.ap[:, 0:ntok].rearrange("p (r w) -> p r w", w=W)
                STT("dve", a3[:, :, 1:W], u3[:, :, 0:W - 1], cwt.ap[:, oc, 0:1], a3[:, :, 1:W], ALU.mult, ALU.add,
                    [pb.d, cwt.d, ac.d], [ac.d])
                STT("dve", a3[:, :, 0:W - 1], u3[:, :, 1:W], cwt.ap[:, oc, 2:3], a3[:, :, 0:W - 1], ALU.mult, ALU.add,
                    [pb.d, cwt.d, ac.d], [ac.d])
                if oc < 4:
                    ACT(xTb[oc].ap[:, 0:ntok], ac.ap[:, 0:ntok], AF.Silu, [ac.d], [xTb[oc].d])
                elif oc == 4:
                    ACT(BT.ap[:, tok0:tok0 + ntok], ac.ap[:, 0:ntok], AF.Silu, [ac.d], [BT.ds[t] for t in tiles])
                else:
                    ACT(CT.ap[:, tok0:tok0 + ntok], ac.ap[:, 0:ntok], AF.Silu, [ac.d], [CT.ds[t] for t in tiles])
            MARKS.setdefault('B', P.count)
            for j, t in enumerate(tiles):
                pb = PS[2 + (t % 2)]
                pv = psbf(2 + (t % 2))
                for oc in range(4):
                    TR(pv[:, oc * 128:(oc + 1) * 128], xTb[oc].ap[:, j * 128:(j + 1) * 128], identbf.ap,
                       [xTb[oc].d, identbf.d], [pb.d])
                TR(psbf(7)[:, 0:128], BT.ap[:, t * 128:(t + 1) * 128], identbf.ap, [BT.ds[t], identbf.d], [PS[7].d])
                CP("dve", xs.ap[:, t, :], pv[:, 0:512], [pb.d], [xs.ds[t]])
                CP("act", Btok.ap[:, t, :], psbf(7)[:, 0:128], [PS[7].d], [Btok.ds[t]])
                if t >= 2:
                    pz = PS[4 + (zi % 2)]
                    zz = zst[zi % 2]
                    zi += 1
                    for kc in range(16):
                        MM(pz.ap, al.ap[:, kc, j * 128:(j + 1) * 128], wg.ap[:, kc, 768:1280], kc == 0, kc == 15,
                           [al.d, wg.d], [pz.d])
                    ACT(zz.ap, pz.ap, AF.Silu, [pz.d], [zz.d])
                    DMA("sp", z_d.ap[g, t - 2], zz.ap, [zz.d], [z_d.ds[g * NTL + t - 2]])
                pd = PS[6]
                for kc in range(16):
                    MM(pd.ap[:, 0:16], al.ap[:, kc, j * 128:(j + 1) * 128], wg.ap[:, kc, 1280:1296], kc == 0, kc == 15,
                       [al.d, wg.d], [pd.d])
                TT("dve", dtr.ap[:, t, :], pd.ap[:, 0:16], dtbb.ap, ALU.add, [pd.d, dtbb.d], [dtr.d])
        MARKS.setdefault('E', P.count)
        ACT(dtv.ap, dtr.ap, AF.Exp, [dtr.d], [dtv.d])
        ACT(dtv.ap, dtv.ap, AF.Ln, [dtv.d], [dtv.d], bias=ones32.ap[:, 0:1])
        TT("dve", lav.ap, dtv.ap, aneg.ap.unsqueeze(1).to_broadcast([128, NT, 16]), ALU.mult, [dtv.d, aneg.d], [lav.d])
        if DEBUG:
            DMA("sp", dbg["dt"][g], dtv.ap, [dtv.d], [])
            DMA("sp", dbg["xs"][g], xs.ap, xs.ds, [])
        P.barrier()
        AR.top = p2_mark

        yacc = T(AR.alloc([128, NTL, 512], BF16), NTL)
        S32 = T(AR.alloc([128, 512], F32))
        Sbf = T(AR.alloc([128, 512], BF16))
        rc = [T(AR.alloc([128, 8, 128], F32)) for _ in range(2)]
        Lall = [T(AR.alloc([128, 8, 128], BF16), 8) for _ in range(2)]
        Mall = [T(AR.alloc([128, 8, 128], BF16), 8) for _ in range(2)]
        cbT = [T(AR.alloc([128, 128], BF16)) for _ in range(2)]
        smal = [T(AR.alloc([128, 32], F32)) for _ in range(2)]
        xw = [T(AR.alloc([128, 512], BF16)) for _ in range(2)]
        tA = [T(AR.alloc([128, 512], F32)) for _ in range(2)]
        tB = [T(AR.alloc([128, 512], F32)) for _ in range(2)]
        zt = [T(AR.alloc([128, 512], BF16)) for _ in range(2)]
        ynb = [T(AR.alloc([128, 512], BF16)) for _ in range(2)]
        yTo = [T(AR.alloc([128, 4, 128], BF16)) for _ in range(2)]
        gst = [T(AR.alloc([128, 4], F32)) for _ in range(2)]
        junk2 = T(AR.alloc([128, 512], BF16))
        ssg = T(AR.alloc([128, NTL], F32), NTL)
        sdg = T(AR.alloc([128, NTL], F32))
        rsg = T(AR.alloc([128, NTL], F32))
        MEMSET("pool", ssg.ap, 0.0, ssg.ds)
        def stageA(d_, c, k):
            lat = c >= 2
            last = 127 if d_ == 0 else 0
            la_c = lav.ap[:, c, d_ * 8:(d_ + 1) * 8]
            dt_c = dtv.ap[:, c, d_ * 8:(d_ + 1) * 8]
            sm = smal[k]
            pS = PS[7]
            MM(pS.ap[:, 0:8], tri[d_].ap, la_c, True, True, [tri[d_].d, lav.d], [pS.d])
            MM(pS.ap[:, 8:16], ones32.ap, la_c, True, True, [ones32.d, lav.d], [pS.d])
            TT("dve", rc[k].ap, tri[d_].ap.unsqueeze(1).to_broadcast([128, 8, 128]),
               la_c.unsqueeze(2).to_broadcast([128, 8, 128]), ALU.mult, [tri[d_].d, lav.d], [rc[k].d])
            Rb = [PS[0], PS[1]]
            for hf in range(2):
                MM(Rb[hf].ap, ones32.ap, rc[k].ap[:, 4 * hf:4 * hf + 4, :].rearrange("p a b -> p (a b)"), True, False,
                   [ones32.d, rc[k].d], [Rb[hf].d])
                MM(Rb[hf].ap, identbf.ap, maskn[d_].ap, False, True, [identbf.d, maskn[d_].d], [Rb[hf].d])
            ACT(sm.ap[:, 0:8], pS.ap[:, 0:8], AF.Identity, [pS.d], [sm.d], scale=-1.0)
            ACT(sm.ap[:, 8:24], pS.ap[:, 0:16], AF.Exp, [pS.d], [sm.d])
            if lat:
                MM(pS.ap[:, 128:256], BT.ap[:, c * 128:(c + 1) * 128], CT.ap[:, c * 128:(c + 1) * 128], True, True,
                   [BT.ds[c], CT.ds[c]], [pS.d])
                CP("act", cbT[k].ap, pS.ap[:, 128:256], [pS.d], [cbT[k].d])
            La = Lall[k]
            for h in range(8):
                ACT(La.ap[:, h, :], Rb[h // 4].ap[:, (h % 4) * 128:(h % 4 + 1) * 128], AF.Exp, [Rb[h // 4].d, sm.d],
                    [La.ds[h]], bias=sm.ap[:, h:h + 1])
            TT("dve", sm.ap[:, 24:32], La.ap[:, :, last], dt_c, ALU.mult, La.ds + [dtv.d], [sm.d])
            TT("pool", xw[k].ap.rearrange("p (h d) -> p h d", d=64), xs.ap[:, c, :].rearrange("p (h d) -> p h d", d=64),
               sm.ap[:, 24:32].unsqueeze(2).to_broadcast([128, 8, 64]), ALU.mult, [xs.ds[c], sm.d], [xw[k].d])
            if lat:
                Ma = Mall[k]
                for h in range(8):
                    STT("dve", Ma.ap[:, h, :], La.ap[:, h, :], dt_c[:, h:h + 1], cbT[k].ap, ALU.mult, ALU.mult,
                        [La.ds[h], dtv.d, cbT[k].d], [Ma.ds[h]])
                for h in range(8):
                    MM(PS[2 + k].ap[:, h * 64:(h + 1) * 64], Ma.ap[:, h, :], xs.ap[:, c, h * 64:(h + 1) * 64], True, True,
                       [Ma.ds[h], xs.ds[c]], [PS[2 + k].d])
            MM(PS[4 + k].ap, Btok.ap[:, c, :], xw[k].ap, True, True, [Btok.ds[c], xw[k].d], [PS[4 + k].d])

        def stageB(d_, c, k):
            lat = c >= 2
            sm = smal[k]
            Pd, Ps_, Po = PS[2 + k], PS[4 + k], PS[6]
            if lat:
                MM(Po.ap, CT.ap[:, c * 128:(c + 1) * 128], Sbf.ap, True, True, [CT.ds[c], Sbf.d], [Po.d])
                TT("dve", tB[k].ap.rearrange("p (h d) -> p h d", d=64), Po.ap.rearrange("p (h d) -> p h d", d=64),
                   sm.ap[:, 8:16].unsqueeze(2).to_broadcast([128, 8, 64]), ALU.mult, [Po.d, sm.d], [tB[k].d])
                TT("dve", tB[k].ap, tB[k].ap, Pd.ap, ALU.add, [tB[k].d, Pd.d], [tB[k].d])
            TT("dve", S32.ap.rearrange("p (h d) -> p h d", d=64), S32.ap.rearrange("p (h d) -> p h d", d=64),
               sm.ap[:, 16:24].unsqueeze(2).to_broadcast([128, 8, 64]), ALU.mult, [S32.d, sm.d], [S32.d])
            TT("dve", S32.ap, S32.ap, Ps_.ap, ALU.add, [S32.d, Ps_.d], [S32.d])
            CP("act", Sbf.ap, S32.ap, [S32.d], [Sbf.d])
            if lat and d_ == 0:
                TT("pool", tA[k].ap, xs.ap[:, c, :], Dbc.ap, ALU.mult, [xs.ds[c], Dbc.d], [tA[k].d])
                TT("pool", yacc.ap[:, c - 2, :], tA[k].ap, tB[k].ap, ALU.add, [tA[k].d, tB[k].d], [yacc.ds[c - 2]])
            if lat and d_ == 1:
                DMA("sp", zt[k].ap, z_d.ap[g, c - 2], [z_d.ds[g * NTL + c - 2]], [zt[k].d])
                TT("pool", tA[k].ap, tB[k].ap, yacc.ap[:, c - 2, :], ALU.add, [tB[k].d, yacc.ds[c - 2]], [tA[k].d])
                TT("pool", yacc.ap[:, c - 2, :], tA[k].ap, zt[k].ap, ALU.mult, [tA[k].d, zt[k].d], [yacc.ds[c - 2]])
                ACT(junk2.ap, yacc.ap[:, c - 2, :], AF.Square, [yacc.ds[c - 2]], [junk2.d, ssg.ds[c - 2]],
                    accum=ssg.ap[:, c - 2:c - 1])

        def stageC(cl, k):
            STT("dve", ynb[k].ap, yacc.ap[:, cl, :], rsg.ap[:, cl:cl + 1], nwb.ap, ALU.mult, ALU.mult,
                [yacc.ds[cl], rsg.d, nwb.d], [ynb[k].d])
            pv = psbf(6 + k)
            for q in range(4):
                TR(pv[:, q * 128:(q + 1) * 128], ynb[k].ap[:, q * 128:(q + 1) * 128], identbf.ap,
                   [ynb[k].d, identbf.d], [PS[6 + k].d])
            CP("act", yTo[k].ap, pv[:, 0:512].rearrange("p (a b) -> p a b", b=128), [PS[6 + k].d], [yTo[k].d])
            DMA("sp", yT_d.ap[cl][:, g * 4:(g + 1) * 4, :], yTo[k].ap, [yTo[k].d], [yT_d.ds[cl]])

        items = []
        for d_ in range(SCAN):
            order = list(range(NT)) if d_ == 0 else [1, 0] + list(range(NT - 1, 1, -1))
            items += [(d_, c, ci == 0) for ci, c in enumerate(order)]
        if items:
            stageA(items[0][0], items[0][1], 0)
        for i_, (d_, c, first) in enumerate(items):
            if i_ + 1 < len(items):
                stageA(items[i_ + 1][0], items[i_ + 1][1], (i_ + 1) % 2)
            if first:
                MEMSET("dve", S32.ap, 0.0, [S32.d])
                MEMSET("pool", Sbf.ap, 0.0, [Sbf.d])
            stageB(d_, c, i_ % 2)
        if SCAN == 2:
            ACT(sdg.ap, ssg.ap, AF.Sqrt, ssg.ds, [sdg.d], bias=epsc.ap, scale=1.0 / 512.0)
            RECIP(rsg.ap, sdg.ap, [sdg.d], [rsg.d])
            for cl in range(NTL):
                stageC(cl, cl % 2)
        P.barrier()
        AR.top = p2_mark
    AR.top = persist_mark
    if LIMIT == 2:
        return finish()

    ss_sc = T(AR.alloc([128, NTL], F32))
    rs_sc = T(AR.alloc([128, NTL], F32))
    sd_sc = T(AR.alloc([128, NTL], F32))
    MEMSET("dve", ss_sc.ap, 0.0, [ss_sc.d])
    p3_mark = AR.top
    wscs = [T(AR.alloc([128, 16, 1536], BF16)) for _ in range(2)]
    cwcs = [T(AR.alloc([128, 4, 3], F32)) for _ in range(2)]
    scwts = [T(AR.alloc([128, 4], F32)) for _ in range(2)]

    def p3_wload(jb):
        DMA("sp", cwcs[jb % 2].ap, cw_sc[jb], [], [cwcs[jb % 2].d])
        DMA("sp", scwts[jb % 2].ap, scw[jb], [], [scwts[jb % 2].d])
        for k2 in range(8):
            DMA("pool", wscs[jb % 2].ap[:, 2 * k2:2 * k2 + 2, :], wg_sc[jb][:, 2 * k2:2 * k2 + 2, :], [], [wscs[jb % 2].d])
    aTl = [T(AR.alloc([128, 16, 512], BF16)) for _ in range(2)]
    cgs = [T(AR.alloc([128, 512], F32)) for _ in range(2)]
    vv = [T(AR.alloc([128, 512], F32)) for _ in range(2)]
    acc = [T(AR.alloc([128, 512], F32)) for _ in range(2)]
    q2 = [T(AR.alloc([128, 512], BF16)) for _ in range(4)]
    qw = [T(AR.alloc([128, 512], BF16)) for _ in range(4)]
    it = 0
    p3_wload(0)
    p3i = 0
    DMA("sp", aTl[0].ap, aT_d.ap[1], [aT_d.ds[1]], [aTl[0].d])
    for jb in range(4):
        wsc, cwc, scwt = wscs[jb % 2], cwcs[jb % 2], scwts[jb % 2]
        if jb + 1 < 4:
            p3_wload(jb + 1)
        for tb in range(1, 9):
            t0 = 4 * (tb - 1)
            al = aTl[p3i % 2]
            p3i += 1
            if p3i < 32:
                ntb = (p3i % 8) + 1
                DMA("sp", aTl[p3i % 2].ap, aT_d.ap[ntb], [aT_d.ds[ntb]], [aTl[p3i % 2].d])
            for cc in range(4):
                k = it % 2
                it += 1
                pbg, pcg, phv = PS[0 + 3 * k], PS[1 + 3 * k], PS[2 + 3 * k]
                for (pb, off) in ((pbg, 0), (pcg, 512), (phv, 1024)):
                    for kc in range(16):
                        MM(pb.ap, wsc.ap[:, kc, off + cc * 128:off + (cc + 1) * 128], al.ap[:, kc, :], kc == 0, kc == 15,
                           [wsc.d, al.d], [pb.d])
                CP("act", cgs[k].ap, pcg.ap, [pcg.d], [cgs[k].d])
                TT("dve", vv[k].ap, cgs[k].ap, phv.ap, ALU.mult, [cgs[k].d, phv.d], [vv[k].d])
                ACT(acc[k].ap, vv[k].ap, AF.Identity, [vv[k].d, cwc.d], [acc[k].d], scale=cwc.ap[:, cc, 1:2])
                v3 = vv[k].ap.rearrange("p (r w) -> p r w", w=64)
                a3 = acc[k].ap.rearrange("p (r w) -> p r w", w=64)
                STT("dve", a3[:, :, 1:64], v3[:, :, 0:63], cwc.ap[:, cc, 0:1], a3[:, :, 1:64], ALU.mult, ALU.add,
                    [vv[k].d, cwc.d, acc[k].d], [acc[k].d])
                STT("dve", a3[:, :, 0:63], v3[:, :, 1:64], cwc.ap[:, cc, 2:3], a3[:, :, 0:63], ALU.mult, ALU.add,
                    [vv[k].d, cwc.d, acc[k].d], [acc[k].d])
                TT("dve", acc[k].ap, acc[k].ap, pbg.ap, ALU.mult, [acc[k].d, pbg.d], [acc[k].d])
                ACT(q2[cc].ap, acc[k].ap, AF.Square, [acc[k].d], [q2[cc].d])
                TS("pool", qw[cc].ap, acc[k].ap, scwt.ap[:, cc:cc + 1], None, ALU.mult, None, [acc[k].d, scwt.d], [qw[cc].d])
                DMA("sp", yT_d.ap[t0:t0 + 4, :, 16 + jb * 4 + cc, :].rearrange("t p k -> p t k"),
                    qw[cc].ap.rearrange("p (t k) -> p t k", k=128), [qw[cc].d], [yT_d.ds[t0 + i] for i in range(4)])
            pss = PS[6 + (tb % 2)]
            for i in range(4):
                for cc in range(4):
                    MM(pss.ap[:, i:i + 1], q2[cc].ap[:, i * 128:(i + 1) * 128], onesbf.ap[:, 0:1], cc == 0, cc == 3,
                       [q2[cc].d, onesbf.d], [pss.d])
            TT("dve", ss_sc.ap[:, t0:t0 + 4], ss_sc.ap[:, t0:t0 + 4], pss.ap[:, 0:4], ALU.add, [ss_sc.d, pss.d], [ss_sc.d])
    rstd_from_ss(ss_sc.ap, sd_sc.ap, rs_sc.ap, 2048.0, ss_sc.d, sd_sc.d, rs_sc.d)
    if DEBUG:
        DMA("sp", dbg["rsc"], rs_sc.ap, [rs_sc.d], [])
    P.barrier()
    if LIMIT == 3:
        return finish()
    AR.top = p3_mark

    g1b = T(AR.alloc([128, 2048], F32))
    load_mod_bc(g1b, 0, 2)
    wos = [T(AR.alloc([128, 32, 1024], BF16)) for _ in range(2)]
    yt = [T(AR.alloc([128, 32, 128], BF16)) for _ in range(2)]
    xh = [T(AR.alloc([128, 1024], F32)) for _ in range(2)]
    t1 = [T(AR.alloc([128, 512], F32)) for _ in range(2)]
    ht = [T(AR.alloc([128, 1024], F32)) for _ in range(2)]
    it = 0
    for hf in range(2):
        for k4 in range(8):
            DMA("pool", wos[hf].ap[:, 4 * k4:4 * k4 + 4, :], w_out[hf][:, 4 * k4:4 * k4 + 4, :], [], [wos[hf].d])

    def p4_load(i4):
        hf_, t_ = i4 // NTL, i4 % NTL
        DMA("sp", yt[t_ % 2].ap, yT_d.ap[t_], [yT_d.ds[t_]], [yt[t_ % 2].d])
        DMA("sp", xh[t_ % 2].ap, xin[t_ + 2][:, hf_ * 1024:(hf_ + 1) * 1024], [], [xh[t_ % 2].d])

    p4_load(0)
    for hf in range(2):
        wo = wos[hf]
        for t in range(NTL):
            y_ = yt[t % 2]
            x_ = xh[t % 2]
            if hf * NTL + t + 1 < 2 * NTL:
                p4_load(hf * NTL + t + 1)
            h_ = ht[t % 2]
            for q in range(2):
                k = it % 2
                it += 1
                pm, pc = PS[0 + 2 * k], PS[1 + 2 * k]
                cols = slice(q * 512, (q + 1) * 512)
                for kc in range(16):
                    MM(pm.ap, y_.ap[:, kc, :], wo.ap[:, kc, cols], kc == 0, kc == 15, [y_.d, wo.d], [pm.d])
                for kc in range(16, 32):
                    MM(pc.ap, y_.ap[:, kc, :], wo.ap[:, kc, cols], kc == 16, kc == 31, [y_.d, wo.d], [pc.d])
                ACT(t1[k].ap, pc.ap, AF.Identity, [pc.d, rs_sc.d], [t1[k].d], scale=rs_sc.ap[:, t:t + 1])
                TT("dve", t1[k].ap, t1[k].ap, pm.ap, ALU.add, [t1[k].d, pm.d], [t1[k].d])
                gcols = slice(hf * 1024 + q * 512, hf * 1024 + (q + 1) * 512)
                TT("pool", t1[k].ap, t1[k].ap, g1b.ap[:, gcols], ALU.mult, [t1[k].d, g1b.d], [t1[k].d])
                TT("pool", h_.ap[:, cols], t1[k].ap, x_.ap[:, cols], ALU.add, [t1[k].d, x_.d], [h_.d])
            DMA("sp", hacc_h[hf].ap[t * 128:(t + 1) * 128, :], h_.ap, [h_.d], [hacc_h[hf].ds[t]])
    P.barrier()
    if LIMIT == 4:
        return finish()
    AR.top = persist_mark

    idxc = T(AR.alloc([128, 4, 16], I32))
    gatc = T(AR.alloc([128, 4, 16], F32))
    p5_mark = AR.top
    A2 = T(AR.alloc([128, 2048], F32))
    B2 = T(AR.alloc([128, 2048], F32))
    nfb = T(AR.alloc([128, 2048], F32))
    load_vec_bc(nfb, nffn)
    load_mod_bc(A2, 0, 4)
    load_mod_bc(B2, 0, 3)
    TT("pool", A2.ap, A2.ap, nfb.ap, ALU.mult, [A2.d, nfb.d], [A2.d])
    wr = T(AR.alloc([128, 16, 16], F32))
    DMA("sp", wr.ap, w_rt, [], [wr.d])
    affT = T(AR.alloc([16, 4096], F32))
    hx = [T(AR.alloc([128, 2048], F32)) for _ in range(2)]
    ff = [T(AR.alloc([128, 2048], F32)) for _ in range(2)]
    fb = [T(AR.alloc([128, 2048], BF16)) for _ in range(2)]
    fT = [T(AR.alloc([128, 16, 128], F32)) for _ in range(2)]
    st5 = [T(AR.alloc([128, 8], F32)) for _ in range(2)]
    ex = [T(AR.alloc([128, 16], F32)) for _ in range(2)]
    junk5 = T(AR.alloc([128, 2048], BF16))
    def p5_load(t_):
        for i2 in range(2):
            DMA("sp", hx[t_ % 2].ap[:, i2 * 1024:(i2 + 1) * 1024], hacc_h[i2].ap[t_ * 128:(t_ + 1) * 128, :],
                [hacc_h[i2].ds[t_]], [hx[t_ % 2].d])

    p5_load(0)
    for t in range(NTL):
        k = t % 2
        h_ = hx[k]
        if t + 1 < NTL:
            p5_load(t + 1)
        if DEBUG:
            DMA("sp", dbg["h1"][t * 128:(t + 1) * 128, :], h_.ap, [h_.d], [])
        s5 = st5[k]
        MEMSET("pool", s5.ap, 0.0, [s5.d])
        ACT(junk5.ap, h_.ap, AF.Square, [h_.d], [junk5.d, s5.d], accum=s5.ap[:, 0:1])
        rstd_from_ss(s5.ap[:, 0:1], s5.ap[:, 1:2], s5.ap[:, 2:3], 2048.0, s5.d, s5.d, s5.d)
        f_ = ff[k]
        STT("dve", f_.ap, h_.ap, s5.ap[:, 2:3], A2.ap, ALU.mult, ALU.mult, [h_.d, s5.d, A2.d], [f_.d])
        TT("pool", f_.ap, f_.ap, B2.ap, ALU.add, [f_.d, B2.d], [f_.d])
        ACT(fb[k].ap, f_.ap, AF.Copy, [f_.d], [fb[k].d])
        DMA("sp", f_d.ap[t * 128:(t + 1) * 128, :], fb[k].ap, [fb[k].d], [f_d.d])
        for q in range(4):
            pb = PS[q % 4]
            for i in range(4):
                kc = 4 * q + i
                TR(pb.ap[:, i * 128:(i + 1) * 128], f_.ap[:, kc * 128:(kc + 1) * 128], ident32.ap, [f_.d, ident32.d], [pb.d])
            CP("act" if q % 2 == 0 else "dve", fT[k].ap[:, 4 * q:4 * q + 4, :], pb.ap.rearrange("p (a b) -> p a b", b=128),
               [pb.d], [fT[k].d])
        pl = PS[4 + k]
        for kc in range(16):
            MM(pl.ap[:, 0:16], fT[k].ap[:, kc, :], wr.ap[:, kc, :], kc == 0, kc == 15, [fT[k].d, wr.d], [pl.d])
        P.emit("dve", (lambda o, i: (lambda e: e.reduce_max(out=o, in_=i, axis=mybir.AxisListType.X)))(s5.ap[:, 3:4], pl.ap[:, 0:16]),
               [pl.d], [s5.d])
        TS("dve", s5.ap[:, 4:5], s5.ap[:, 3:4], -1.0, None, ALU.mult, None, [s5.d], [s5.d])
        ACT(ex[k].ap, pl.ap[:, 0:16], AF.Exp, [pl.d, s5.d], [ex[k].d, s5.d], bias=s5.ap[:, 4:5], accum=s5.ap[:, 5:6])
        RECIP(s5.ap[:, 6:7], s5.ap[:, 5:6], [s5.d], [s5.d])
        TS("dve", ex[k].ap, ex[k].ap, s5.ap[:, 6:7], None, ALU.mult, None, [ex[k].d, s5.d], [ex[k].d])
        pa = PS[6 + k]
        TR(pa.ap[0:16, 0:128], ex[k].ap, ident32.ap, [ex[k].d, ident32.d], [pa.d])
        CP("act", affT.ap[:, t * 128:(t + 1) * 128], pa.ap[0:16, 0:128], [pa.d], [affT.d])
    if DEBUG:
        DMA("sp", dbg["aff"], affT.ap, [affT.d], [])
    vals = T(AR.alloc([16, 512], F32))
    idxu = T(AR.alloc([16, 512], U32))
    idxf = T(AR.alloc([16, 512], F32))
    for r in range(64):
        v8 = vals.ap[:, r * 8:(r + 1) * 8]
        i8 = idxu.ap[:, r * 8:(r + 1) * 8]
        P.emit("dve", (lambda o, i: (lambda e: e.max(out=o, in_=i)))(v8, affT.ap), [affT.d], [vals.d])
        P.emit("dve", (lambda o, m, i: (lambda e: e.max_index(out=o, in_max=m, in_values=i)))(i8, v8, affT.ap),
               [affT.d, vals.d], [idxu.d])
        P.emit("dve", (lambda o, m, i: (lambda e: e.match_replace(out=o, in_to_replace=m, in_values=i, imm_value=-1.0)))(affT.ap, v8, affT.ap),
               [vals.d, idxu.d], [affT.d])
    CP("dve", idxf.ap, idxu.ap, [idxu.d], [idxf.d])
    if DEBUG:
        DMA("sp", dbg["vals"], vals.ap, [vals.d], [])
        DMA("sp", dbg["idx"], idxu.ap, [idxu.d], [])
    for s in range(4):
        pa = PS[s % 2]
        TR(pa.ap[:, 0:16], idxf.ap[:, s * 128:(s + 1) * 128], ident32.ap[0:16, 0:16], [idxf.d, ident32.d], [pa.d])
        TR(pa.ap[:, 16:32], vals.ap[:, s * 128:(s + 1) * 128], ident32.ap[0:16, 0:16], [vals.d, ident32.d], [pa.d])
        CP("dve", idxc.ap[:, s, :], pa.ap[:, 0:16], [pa.d], [idxc.d])
        CP("act", gatc.ap[:, s, :], pa.ap[:, 16:32], [pa.d], [gatc.d])
    P.barrier()
    if LIMIT == 5:
        return finish()
    AR.top = p5_mark

    g2b = T(AR.alloc([128, 2048], F32))
    load_mod_bc(g2b, 0, 5)
    NW = 5
    wbuf = [T(AR.alloc([128, 8192], BF16)) for _ in range(NW)]
    xg = [T(AR.alloc([128, 2048], BF16)) for _ in range(8)]
    xeT = [T(AR.alloc([128, 16, 512], BF16)) for _ in range(2)]
    hidT = [T(AR.alloc([128, 8, 512], BF16), 8) for _ in range(2)]
    silt = [T(AR.alloc([128, 512], BF16)) for _ in range(2)]
    yet = [T(AR.alloc([128, 512], F32)) for _ in range(2)]
    yo = [T(AR.alloc([128, 1024], F32)) for _ in range(2)]
    hall = Dep()
    srcs_all = []
    for e_ in range(16):
        srcs_all += [(w_gate[e_, 0], 0), (w_up[e_, 0], 0), (w_gate[e_, 1], 0), (w_up[e_, 1], 0),
                     (w_down[e_, 0], 1), (w_down[e_, 1], 1)]
    piece_view = {}
    state6 = {"issued": 0}

    def ensure_issued(upto):
        while state6["issued"] < min(upto, len(srcs_all)):
            i = state6["issued"]
            src, kind = srcs_all[i]
            wb = wbuf[i % NW]
            if kind == 0:
                dst = wb.ap.rearrange("p (a b) -> p a b", b=512)
                DMA("pool", dst[:, 0:8, :], src[:, 0:8, :], [], [wb.d])
                DMA("pool", dst[:, 8:16, :], src[:, 8:16, :], [], [wb.d])
            else:
                dst = wb.ap.rearrange("p (a b) -> p a b", b=1024)
                DMA("pool", dst[:, 0:4, :], src[:, 0:4, :], [], [wb.d])
                DMA("pool", dst[:, 4:8, :], src[:, 4:8, :], [], [wb.d])
            piece_view[i] = (wb, dst)
            state6["issued"] = i + 1

    def gather_issue(e_):
        for s in range(4):
            xg_ = xg[(e_ % 2) * 4 + s]
            P.emit("pool", (lambda o, i, ix: (lambda e: e.indirect_dma_start(
                out=o, out_offset=None, in_=i, in_offset=bass.IndirectOffsetOnAxis(ap=ix, axis=0))))(
                    xg_.ap, f_d.ap, idxc.ap[:, s, e_:e_ + 1]), [f_d.d, idxc.d], [xg_.d], dma=True)

    yi = 0
    pgi = 0
    gather_issue(0)
    ensure_issued(NW - 1)
    for e_ in range(16):
        xe = xeT[e_ % 2]
        for s in range(4):
            xg_ = xg[(e_ % 2) * 4 + s]
            for q in range(4):
                pv = psbf(6 + (q % 2))
                pb = PS[6 + (q % 2)]
                for i in range(4):
                    kc = 4 * q + i
                    TR(pv[:, i * 128:(i + 1) * 128], xg_.ap[:, kc * 128:(kc + 1) * 128], identbf.ap, [xg_.d, identbf.d], [pb.d])
                CP("act" if q % 2 == 0 else "dve", xe.ap[:, 4 * q:4 * q + 4, s * 128:(s + 1) * 128],
                   pv[:, 0:512].rearrange("p (a b) -> p a b", b=128), [pb.d], [xe.d])
        if e_ + 1 < 16:
            gather_issue(e_ + 1)
        hT = hidT[e_ % 2]
        p0 = 6 * e_
        for fcu in range(8):
            if fcu % 4 == 0:
                ensure_issued(p0 + 2 * (fcu // 4) + NW)
            wgp, wgv = piece_view[p0 + 0 + 2 * (fcu // 4)]
            wup, wuv = piece_view[p0 + 1 + 2 * (fcu // 4)]
            off = (fcu % 4) * 128
            k = pgi % 2
            pgi += 1
            pg, pu = PS[0 + 2 * k], PS[1 + 2 * k]
            for kc in range(16):
                MM(pg.ap, wgv[:, kc, off:off + 128], xe.ap[:, kc, :], kc == 0, kc == 15, [wgp.d, xe.d], [pg.d])
            for kc in range(16):
                MM(pu.ap, wuv[:, kc, off:off + 128], xe.ap[:, kc, :], kc == 0, kc == 15, [wup.d, xe.d], [pu.d])
            ACT(silt[k].ap, pg.ap, AF.Silu, [pg.d], [silt[k].d])
            TT("dve", hT.ap[:, fcu, :], silt[k].ap, pu.ap, ALU.mult, [silt[k].d, pu.d], [hT.ds[fcu]])
        for dh in range(2):
            ensure_issued(p0 + 4 + dh + NW)
            wdp, wdv = piece_view[p0 + 4 + dh]
            for s in range(4):
                yo_ = yo[yi % 2]
                yi += 1
                for dq in range(2):
                    k = pgi % 2
                    pgi += 1
                    py = PS[4 + k]
                    for fc in range(8):
                        MM(py.ap, hT.ap[:, fc, s * 128:(s + 1) * 128], wdv[:, fc, dq * 512:(dq + 1) * 512], fc == 0, fc == 7,
                           [hT.ds[fc], wdp.d], [py.d])
                    ACT(yet[k].ap, py.ap, AF.Identity, [py.d, gatc.d], [yet[k].d], scale=gatc.ap[:, s, e_:e_ + 1])
                    gc = slice(dh * 1024 + dq * 512, dh * 1024 + (dq + 1) * 512)
                    TT("dve", yo_.ap[:, dq * 512:(dq + 1) * 512], yet[k].ap, g2b.ap[:, gc], ALU.mult,
                       [yet[k].d, g2b.d], [yo_.d])
                P.emit("pool", (lambda o, i, ix: (lambda e: e.indirect_dma_start(
                    out=o, out_offset=bass.IndirectOffsetOnAxis(ap=ix, axis=0), in_=i, in_offset=None, compute_op=ALU.add)))(
                        hacc_h[dh].ap, yo_.ap, idxc.ap[:, s, e_:e_ + 1]),
                    [yo_.d, idxc.d], [hall], dma=True)
    P.barrier()
    if LIMIT == 6:
        return finish()
    AR.top = persist_mark

    fnb = T(AR.alloc([128, 2048], F32))
    load_vec_bc(fnb, nfin)
    hx = [T(AR.alloc([128, 2048], F32)) for _ in range(2)]
    ob = [T(AR.alloc([128, 2048], F32)) for _ in range(2)]
    st7 = [T(AR.alloc([128, 4], F32)) for _ in range(2)]
    junk7 = T(AR.alloc([128, 2048], BF16))
    def p7_load(t_):
        for i2 in range(2):
            DMA("sp", hx[t_ % 2].ap[:, i2 * 1024:(i2 + 1) * 1024], hacc_h[i2].ap[t_ * 128:(t_ + 1) * 128, :], [hall], [hx[t_ % 2].d])

    p7_load(0)
    for t in range(NTL):
        k = t % 2
        if t + 1 < NTL:
            p7_load(t + 1)
        s7 = st7[k]
        MEMSET("pool", s7.ap, 0.0, [s7.d])
        ACT(junk7.ap, hx[k].ap, AF.Square, [hx[k].d], [junk7.d, s7.d], accum=s7.ap[:, 0:1])
        rstd_from_ss(s7.ap[:, 0:1], s7.ap[:, 1:2], s7.ap[:, 2:3], 2048.0, s7.d, s7.d, s7.d)
        STT("dve", ob[k].ap, hx[k].ap, s7.ap[:, 2:3], fnb.ap, ALU.mult, ALU.mult, [hx[k].d, s7.d, fnb.d], [ob[k].d])
        DMA("sp", out[t], ob[k].ap, [ob[k].d], [])
    P.barrier()
    P.finalize(st)
    st.close()
    return nc


def _kc_p(w):
    K, N = w.shape
    return np.ascontiguousarray(w.reshape(K // 128, 128, N).transpose(1, 0, 2))


def _prep_shared(c_ctx, w_mod, b_mod, norm_mix_w, w_in, ssm_conv_w, ssm_conv_b, ssm_a_log, ssm_dt_bias, ssm_d,
                 ssm_norm_w, sc_conv_w, sc_norm_w, w_out, norm_ffn_w, w_router, w_gate, w_up, w_down, final_norm_w):
    f = np.float32
    sh = {}
    sh["w_mod"] = np.ascontiguousarray(w_mod[0].reshape(128, 16, 12288))
    sh["b_mod"] = np.ascontiguousarray(b_mod[0].reshape(1, 12288))
    sh["nmix"] = norm_mix_w[0].reshape(1, 2048)
    sh["nffn"] = norm_ffn_w[0].reshape(1, 2048)
    sh["nfin"] = final_norm_w.reshape(1, 2048)
    win = w_in[0]
    wg, cw, cb = [], [], []
    for g in range(4):
        cols = np.concatenate([2048 + g * 512 + np.arange(512), 4096 + g * 128 + np.arange(128),
                               4608 + g * 128 + np.arange(128), g * 512 + np.arange(512),
                               5120 + g * 8 + np.arange(8), 5152 + g * 8 + np.arange(8)])
        wg.append(_kc_p(win[:, cols]))
        ch = np.concatenate([g * 512 + np.arange(512), 2048 + g * 128 + np.arange(128), 2560 + g * 128 + np.arange(128)])
        cw.append(ssm_conv_w[0][:, ch].T.reshape(6, 128, 3).transpose(1, 0, 2))
        cb.append(ssm_conv_b[0][ch].reshape(6, 128).T)
    sh["wg_ssm"] = np.ascontiguousarray(np.stack(wg)).astype(f)
    sh["cw_ssm"] = np.ascontiguousarray(np.stack(cw)).astype(f)
    sh["cb_ssm"] = np.ascontiguousarray(np.stack(cb)).astype(f)
    wsc, cws, scw = [], [], []
    for j in range(4):
        ch = j * 512 + np.arange(512)
        cols = np.concatenate([5184 + ch, 7232 + ch, 9280 + ch])
        wsc.append(_kc_p(win[:, cols]))
        cws.append(sc_conv_w[0][:, ch].T.reshape(4, 128, 3).transpose(1, 0, 2))
        scw.append(sc_norm_w[0][ch].reshape(4, 128).T)
    sh["wg_sc"] = np.ascontiguousarray(np.stack(wsc)).astype(f)
    sh["cw_sc"] = np.ascontiguousarray(np.stack(cws)).astype(f)
    sh["scw"] = np.ascontiguousarray(np.stack(scw)).astype(f)
    hsel = np.stack([np.concatenate([g * 8 + np.arange(8), 32 + g * 8 + np.arange(8)]) for g in range(4)])
    sh["dtb"] = np.ascontiguousarray(ssm_dt_bias[0][hsel].reshape(4, 1, 16))
    sh["alog"] = np.ascontiguousarray(ssm_a_log[0][hsel].reshape(4, 1, 16))
    sh["dsk"] = np.ascontiguousarray(np.repeat(ssm_d[0].reshape(4, 8), 64, axis=1).reshape(4, 1, 512))
    sh["ssm_nw"] = np.ascontiguousarray(ssm_norm_w[0].reshape(4, 1, 512))
    wo = _kc_p(w_out[0])
    sh["w_out"] = np.ascontiguousarray(np.stack([wo[:, :, 0:1024], wo[:, :, 1024:2048]]))
    sh["w_rt"] = _kc_p(w_router[0])
    wgt = w_gate[0].reshape(16, 16, 128, 2, 512).transpose(0, 3, 2, 1, 4)
    wup = w_up[0].reshape(16, 16, 128, 2, 512).transpose(0, 3, 2, 1, 4)
    wdn = w_down[0].reshape(16, 8, 128, 2, 1024).transpose(0, 3, 2, 1, 4)
    sh["w_gate"] = np.ascontiguousarray(wgt)
    sh["w_up"] = np.ascontiguousarray(wup)
    sh["w_down"] = np.ascontiguousarray(wdn)
    sh["c_ident"] = np.eye(128, dtype=f)
    k = np.arange(128)[:, None]
    i = np.arange(128)[None, :]
    sh["c_tri"] = np.stack([(k <= i), (k >= i)]).astype(f)
    mf = np.where(i < k, NEG, 0.0).astype(f)
    mb = np.where(i > k, NEG, 0.0).astype(f)
    sh["c_mask"] = np.stack([np.tile(mf, (1, 4)), np.tile(mb, (1, 4))]).astype(f)
    return sh


_CACHE = {}


def kernel(x, c, ctx, c_ctx, w_mod, b_mod, norm_mix_w, w_in, ssm_conv_w, ssm_conv_b, ssm_a_log, ssm_dt_bias, ssm_d,
           ssm_norm_w, sc_conv_w, sc_norm_w, w_out, norm_ffn_w, w_router, w_gate, w_up, w_down, final_norm_w):
    args = [np.asarray(a, dtype=np.float32) for a in (
        c_ctx, w_mod, b_mod, norm_mix_w, w_in, ssm_conv_w, ssm_conv_b, ssm_a_log, ssm_dt_bias, ssm_d, ssm_norm_w,
        sc_conv_w, sc_norm_w, w_out, norm_ffn_w, w_router, w_gate, w_up, w_down, final_norm_w)]
    x = np.asarray(x, dtype=np.float32)
    c = np.asarray(c, dtype=np.float32)
    ctx = np.asarray(ctx, dtype=np.float32)
    sh = _prep_shared(*args)
    if "nc" not in _CACHE:
        _CACHE["nc"] = build_program()
    nc = _CACHE["nc"]
    in_maps = []
    for core in range(NCORES):
        b = core % 4
        m = dict(sh)
        m["xin"] = np.ascontiguousarray(np.concatenate([ctx[b], x[b]], axis=0).reshape(NT, 128, 2048))
        m["cvec"] = np.ascontiguousarray(np.stack([c[b], args[0]], axis=-1).reshape(128, 16, 2))
        in_maps.append(m)
    res = run_bass_kernel_spmd(nc, in_maps, core_ids=list(range(NCORES)))
    _CACHE["res"] = res
    outs = [res.results[b % NCORES]["out"].reshape(4096, 2048) for b in range(4)]
    return np.stack(outs).astype(np.float32)
```

```python
import numpy as np
from contextlib import ExitStack
import concourse.bass as bass
import concourse.mybir as mybir
from concourse.bass_utils import run_bass_kernel_spmd

F32 = mybir.dt.float32
BF16 = mybir.dt.bfloat16
I32 = mybir.dt.int32
U32 = mybir.dt.uint32
U8 = mybir.dt.uint8
AF = mybir.ActivationFunctionType
ALU = mybir.AluOpType

ENGS = ("pe", "act", "dve", "pool", "sp")
DEBUG = False
LIMIT = 99
STOPAT = None
MARKS = {}
NG = 4
SCAN = 2
NCORES = 4
EPS = 1e-6
NT = 34
NTL = 32
NEG = -30000.0


class Dep:
    __slots__ = ("w", "r", "excl")

    def __init__(self, excl=False):
        self.w = None
        self.r = []
        self.excl = excl


class Instr:
    __slots__ = ("eng", "fn", "dma", "deps", "sig", "sem", "val")

    def __init__(self, eng, fn, dma):
        self.eng = eng
        self.fn = fn
        self.dma = dma
        self.deps = set()
        self.sig = False
        self.sem = None
        self.val = 0


def _nop_fn(eng):
    return eng.nop()


class Prog:
    def __init__(self, nc, n_dma_sems=12):
        self.nc = nc
        self.lists = {e: [] for e in ENGS}
        self.last = {e: None for e in ENGS}
        self.n_dma_sems = n_dma_sems
        self.dma_count = {e: 0 for e in ENGS}
        self.dma_ring = {e: [None] * n_dma_sems for e in ENGS}

    def emit(self, eng, fn, reads=(), writes=(), dma=False, extra=()):
        ins = Instr(eng, fn, dma)
        self.count = getattr(self, "count", 0) + 1
        if STOPAT is not None and self.count > STOPAT and not getattr(self, "in_finish", False):
            return ins
        for d in reads:
            if d.w is not None:
                ins.deps.add(d.w)
            if d.excl:
                for r in d.r:
                    if r.eng != eng:
                        ins.deps.add(r)
            if not dma:
                d.r = [r for r in d.r if r.dma or r.eng != eng]
            d.r.append(ins)
        for d in writes:
            if d.w is not None:
                ins.deps.add(d.w)
            for r in d.r:
                ins.deps.add(r)
            d.w = ins
            d.r = []
        for p in extra:
            if p is not None:
                ins.deps.add(p)
        ins.deps.discard(ins)
        if dma:
            k = self.dma_count[eng]
            self.dma_count[eng] = k + 1
            slot = k % self.n_dma_sems
            prev = self.dma_ring[eng][slot]
            if prev is not None:
                ins.deps.add(prev)
            self.dma_ring[eng][slot] = ins
            ins.sem = (eng, slot)
        self.lists[eng].append(ins)
        self.last[eng] = ins
        return ins

    def barrier(self):
        ext = [self.last[e] for e in ENGS if self.last[e] is not None]
        for e in ENGS:
            for p in self.dma_ring[e]:
                if p is not None:
                    ext.append(p)
        b = self.emit("dve", lambda eng: eng.engine_nop(), extra=ext)
        for e in ENGS:
            if e != "dve":
                self.emit(e, _nop_fn, extra=[b])
        return b

    def finalize(self, stack):
        nc = self.nc
        for e in ENGS:
            for ins in self.lists[e]:
                for p in ins.deps:
                    if p.eng == "pe" and ins.eng == "pe" and not p.dma and not ins.dma:
                        continue
                    p.sig = True
        esem = {e: stack.enter_context(nc.semaphore("s_" + e)) for e in ENGS}
        dsem = {}
        for e in ENGS:
            for s in range(min(self.n_dma_sems, self.dma_count[e])):
                dsem[(e, s)] = stack.enter_context(nc.semaphore("d_%s_%d" % (e, s)))
        dcount = {k: 0 for k in dsem}
        for e in ENGS:
            c = 0
            for ins in self.lists[e]:
                if ins.dma:
                    dcount[ins.sem] += 16
                    ins.val = dcount[ins.sem]
                    ins.sig = True
                elif ins.sig:
                    c += 1
                    ins.val = c
                    ins.sem = e
        block = stack.enter_context(nc.Block())
        engobj = {"pe": "tensor", "act": "scalar", "dve": "vector", "pool": "gpsimd", "sp": "sync"}

        def make_body(e):
            def body(eng):
                known = {}
                for ins in self.lists[e]:
                    for p in ins.deps:
                        if p.eng == "pe" and e == "pe" and not p.dma and not ins.dma:
                            continue
                        if p.dma:
                            sem = dsem[p.sem]
                            key = ("d",) + p.sem
                        else:
                            sem = esem[p.eng]
                            key = ("e", p.eng)
                        if known.get(key, 0) >= p.val:
                            continue
                        known[key] = p.val
                        eng.wait_ge(sem, p.val)
                    r = ins.fn(eng)
                    if ins.dma:
                        r.then_inc(dsem[ins.sem], 16)
                    elif ins.sig:
                        r.then_inc(esem[e], 1)
            return body

        for e in ENGS:
            if self.lists[e]:
                getattr(block, engobj[e])(make_body(e))


class Arena:
    def __init__(self, ap_u8, nbytes):
        self.a = ap_u8
        self.n = nbytes
        self.top = 0

    def alloc(self, shape, dt):
        esz = {F32: 4, BF16: 2, I32: 4, U32: 4}[dt]
        free = 1
        for s in shape[1:]:
            free *= s
        nb = (free * esz + 63) // 64 * 64
        assert self.top + nb <= self.n, ("SBUF arena overflow", self.top, nb, self.n)
        v = self.a[0:shape[0], self.top:self.top + free * esz].bitcast(dt)
        self.top += nb
        if len(shape) == 3:
            v = v.rearrange("p (a b) -> p a b", b=shape[2])
        return v


class T:
    def __init__(self, ap, nsub=0):
        self.ap = ap
        self.d = Dep()
        self.ds = [Dep() for _ in range(nsub)]


def build_program():
    nc = bass.Bass("TRN2", target_bir_lowering=False)
    st = ExitStack()
    P = Prog(nc)

    def finish():
        P.in_finish = True
        P.barrier()
        P.finalize(st)
        st.close()
        return nc

    def din(name, shape, dt=F32):
        return nc.dram_tensor(name, list(shape), dt, kind="ExternalInput").ap()

    def dscr(name, shape, dt):
        return nc.dram_tensor(name, list(shape), dt, kind="ExternalOutput" if DEBUG else "Internal").ap()

    xin = din("xin", [NT, 128, 2048])
    cvec = din("cvec", [128, 16, 2])
    w_mod = din("w_mod", [128, 16, 12288])
    b_mod = din("b_mod", [1, 12288])
    nmix = din("nmix", [1, 2048])
    nffn = din("nffn", [1, 2048])
    nfin = din("nfin", [1, 2048])
    wg_ssm = din("wg_ssm", [4, 128, 16, 1296]) if LIMIT >= 2 else None
    wg_sc = din("wg_sc", [4, 128, 16, 1536]) if LIMIT >= 3 else None
    cw_ssm = din("cw_ssm", [4, 128, 6, 3])
    cb_ssm = din("cb_ssm", [4, 128, 6])
    cw_sc = din("cw_sc", [4, 128, 4, 3])
    scw = din("scw", [4, 128, 4])
    dtb = din("dtb", [4, 1, 16])
    alog = din("alog", [4, 1, 16])
    dsk = din("dsk", [4, 1, 512])
    ssm_nw = din("ssm_nw", [4, 1, 512])
    w_out = din("w_out", [2, 128, 32, 1024]) if LIMIT >= 4 else None
    w_rt = din("w_rt", [128, 16, 16])
    if LIMIT >= 6:
        w_gate = din("w_gate", [16, 2, 128, 16, 512])
        w_up = din("w_up", [16, 2, 128, 16, 512])
        w_down = din("w_down", [16, 2, 128, 8, 1024])
    c_ident = din("c_ident", [128, 128])
    c_tri = din("c_tri", [2, 128, 128])
    c_mask = din("c_mask", [2, 128, 512])
    out = nc.dram_tensor("out", [NTL, 128, 2048], F32, kind="ExternalOutput").ap()

    mod_d = T(dscr("mod_d", [2, 12288], F32))
    aT_d = T(dscr("aT_d", [9, 128, 16, 512], BF16), 9)
    z_d = T(dscr("z_d", [4, NTL, 128, 512], BF16), 4 * NTL)
    yT_d = T(dscr("yT_d", [NTL, 128, 32, 128], BF16), NTL)
    hacc_h = [T(dscr("hacc_d%d" % i, [NTL * 128, 1024], F32), NTL) for i in range(2)]
    f_d = T(dscr("f_d", [NTL * 128, 2048], BF16))
    dbg = {}
    if DEBUG:
        dbg["h1"] = nc.dram_tensor("dbg_h1", [NTL * 128, 2048], F32, kind="ExternalOutput").ap()
        dbg["aff"] = nc.dram_tensor("dbg_aff", [16, 4096], F32, kind="ExternalOutput").ap()
        dbg["vals"] = nc.dram_tensor("dbg_vals", [16, 512], F32, kind="ExternalOutput").ap()
        dbg["idx"] = nc.dram_tensor("dbg_idx", [16, 512], U32, kind="ExternalOutput").ap()
        dbg["rsc"] = nc.dram_tensor("dbg_rsc", [128, NTL], F32, kind="ExternalOutput").ap()
        dbg["dt"] = nc.dram_tensor("dbg_dt", [4, 128, NT, 16], F32, kind="ExternalOutput").ap()
        dbg["xs"] = nc.dram_tensor("dbg_xs", [4, 128, NT, 512], BF16, kind="ExternalOutput").ap()

    sb_bytes = (int(nc.sbuf_bytes_remaining) - 2048) // 256 * 256
    arena_t = st.enter_context(nc.sbuf_tensor("arena", [128, sb_bytes], U8))
    AR = Arena(arena_t[:, :], sb_bytes)
    banks = [st.enter_context(nc.psum_tensor("bank%d" % i, [128, 512], F32)) for i in range(8)]
    PS = [T(b[:, :]) for b in banks]
    for p_ in PS:
        p_.d.excl = True

    def psbf(i):
        return banks[i][:, :].bitcast(BF16)

    def MM(out, lhsT, rhs, start, stop, rd, wr):
        return P.emit("pe", lambda e: e.matmul(out, lhsT=lhsT, rhs=rhs, start=start, stop=stop), rd, wr)

    def TR(out, in_, ident, rd, wr):
        return P.emit("pe", lambda e: e.transpose(out, in_, ident), rd, wr)

    def ACT(out, in_, func, rd, wr, bias=None, scale=None, accum=None):
        kw = {}
        if bias is not None:
            kw["bias"] = bias
        if scale is not None:
            kw["scale"] = scale
        if accum is not None:
            kw["accum_out"] = accum
        return P.emit("act", lambda e: e.activation(out=out, in_=in_, func=func, **kw), rd, wr)

    def TT(eng, out, in0, in1, op, rd, wr):
        return P.emit(eng, lambda e: e.tensor_tensor(out=out, in0=in0, in1=in1, op=op), rd, wr)

    def TS(eng, out, in0, s1, s2, op0, op1, rd, wr):
        if s2 is None:
            return P.emit(eng, lambda e: e.tensor_scalar(out=out, in0=in0, scalar1=s1, scalar2=None, op0=op0), rd, wr)
        return P.emit(eng, lambda e: e.tensor_scalar(out=out, in0=in0, scalar1=s1, scalar2=s2, op0=op0, op1=op1), rd, wr)

    def STT(eng, out, in0, scalar, in1, op0, op1, rd, wr):
        return P.emit(eng, lambda e: e.scalar_tensor_tensor(out=out, in0=in0, scalar=scalar, in1=in1, op0=op0, op1=op1), rd, wr)

    def CP(eng, out, in_, rd, wr):
        if eng == "act":
            return P.emit("act", lambda e: e.copy(out=out, in_=in_), rd, wr)
        return P.emit(eng, lambda e: e.tensor_copy(out=out, in_=in_), rd, wr)

    def MEMSET(eng, ap, val, wr):
        return P.emit(eng, lambda e: e.memset(ap, val), (), wr)

    def RECIP(out, in_, rd, wr):
        return P.emit("dve", lambda e: e.reciprocal(out=out, in_=in_), rd, wr)

    def DMA(q, out, in_, rd, wr):
        return P.emit(q, lambda e: e.dma_start(out=out, in_=in_), rd, wr, dma=True)

    def rstd_from_ss(ss_ap, std_ap, r_ap, n, dss, dstd, dr):
        ACT(std_ap, ss_ap, AF.Sqrt, [dss], [dstd], bias=epsc.ap[0:ss_ap.shape[0], :], scale=1.0 / n)
        RECIP(r_ap, std_ap, [dstd], [dr])

    ident32 = T(AR.alloc([128, 128], F32))
    identbf = T(AR.alloc([128, 128], BF16))
    ones32 = T(AR.alloc([128, 128], F32))
    onesbf = T(AR.alloc([128, 8], BF16))
    epsc = T(AR.alloc([128, 1], F32))
    DMA("sp", ident32.ap, c_ident, [], [ident32.d])
    DMA("pool", identbf.ap, c_ident, [], [identbf.d])
    MEMSET("dve", ones32.ap, 1.0, [ones32.d])
    MEMSET("dve", onesbf.ap, 1.0, [onesbf.d])
    MEMSET("dve", epsc.ap, EPS, [epsc.d])
    persist_mark = AR.top

    cv = T(AR.alloc([128, 16, 2], F32))
    cs_ = T(AR.alloc([128, 16, 2], F32))
    DMA("sp", cv.ap, cvec, [], [cv.d])
    ACT(cs_.ap, cv.ap, AF.Silu, [cv.d], [cs_.d])
    wch = [T(AR.alloc([128, 16, 512], F32)) for _ in range(3)]
    bch = [T(AR.alloc([2, 512], F32)) for _ in range(2)]
    mch = [T(AR.alloc([2, 512], F32)) for _ in range(2)]
    def p0_load(n_):
        w_ = wch[n_ % 3]
        cols_ = slice(n_ * 512, (n_ + 1) * 512)
        DMA("sp", w_.ap[:, 0:8, :], w_mod[:, 0:8, cols_], [], [w_.d])
        DMA("act", w_.ap[:, 8:16, :], w_mod[:, 8:16, cols_], [], [w_.d])

    p0_load(0)
    p0_load(1)
    for n in range(24):
        w = wch[n % 3]
        cols = slice(n * 512, (n + 1) * 512)
        if n + 2 < 24:
            p0_load(n + 2)
        bb = bch[n % 2]
        DMA("sp", bb.ap, b_mod[0:1, cols].partition_broadcast(2), [], [bb.d])
        pb = PS[n % 2]
        for kc in range(16):
            MM(pb.ap[0:2, :], cs_.ap[:, kc, :], w.ap[:, kc, :], kc == 0, kc == 15, [cs_.d, w.d], [pb.d])
        m = mch[n % 2]
        TT("dve", m.ap, pb.ap[0:2, :], bb.ap, ALU.add, [pb.d, bb.d], [m.d])
        if n // 4 in (1, 4):
            TS("dve", m.ap, m.ap, 1.0, None, ALU.add, None, [m.d], [m.d])
        DMA("sp", mod_d.ap[:, cols], m.ap, [m.d], [mod_d.d])
    P.barrier()
    if LIMIT == 0:
        return finish()
    AR.top = persist_mark

    def load_mod_bc(dst, row, seg, q="sp"):
        DMA(q, dst.ap, mod_d.ap[row:row + 1, seg * 2048:(seg + 1) * 2048].partition_broadcast(128), [mod_d.d], [dst.d])

    def load_vec_bc(dst, src, q="sp"):
        DMA(q, dst.ap, src.partition_broadcast(128), [], [dst.d])

    A1 = [T(AR.alloc([128, 2048], F32)) for _ in range(2)]
    B1 = [T(AR.alloc([128, 2048], F32)) for _ in range(2)]
    nmb = T(AR.alloc([128, 2048], F32))
    load_vec_bc(nmb, nmix)
    for r in range(2):
        load_mod_bc(A1[r], r, 1)
        load_mod_bc(B1[r], r, 0)
        TT("pool", A1[r].ap, A1[r].ap, nmb.ap, ALU.mult, [A1[r].d, nmb.d], [A1[r].d])
    xt = [T(AR.alloc([128, 2048], F32)) for _ in range(2)]
    tmp = [T(AR.alloc([128, 2048], F32)) for _ in range(2)]
    abf = [T(AR.alloc([128, 2048], BF16)) for _ in range(2)]
    junk = T(AR.alloc([128, 2048], BF16))
    ss1 = T(AR.alloc([128, NT], F32), NT)
    sd1 = T(AR.alloc([128, NT], F32), NT)
    rs1 = T(AR.alloc([128, NT], F32), NT)
    aTb = [T(AR.alloc([128, 16, 512], BF16)) for _ in range(2)]
    MEMSET("dve", ss1.ap, 0.0, ss1.ds)
    order1 = []
    for tb in range(9):
        tiles = [0, 1] if tb == 0 else [2 + 4 * (tb - 1) + i for i in range(4)]
        for j, t in enumerate(tiles):
            order1.append((tb, j, t, j == len(tiles) - 1, 128 * len(tiles)))

    def p1_load(t):
        DMA("sp", xt[t % 2].ap, xin[t], [], [xt[t % 2].d])

    def p1_stats(t):
        x_ = xt[t % 2]
        ACT(junk.ap, x_.ap, AF.Square, [x_.d], [junk.d, ss1.ds[t]], accum=ss1.ap[:, t:t + 1])
        rstd_from_ss(ss1.ap[:, t:t + 1], sd1.ap[:, t:t + 1], rs1.ap[:, t:t + 1], 2048.0, ss1.ds[t], sd1.ds[t], rs1.ds[t])

    def p1_stt(t_):
        r_ = 1 if t_ < 2 else 0
        STT("dve", tmp[t_ % 2].ap, xt[t_ % 2].ap, rs1.ap[:, t_:t_ + 1], A1[r_].ap, ALU.mult, ALU.mult,
            [xt[t_ % 2].d, rs1.ds[t_], A1[r_].d], [tmp[t_ % 2].d])

    p1_load(0)
    p1_stats(0)
    for i1, (tb, j, t, lastj, ntok) in enumerate(order1):
        ab = aTb[tb % 2]
        r = 1 if t < 2 else 0
        x_ = xt[t % 2]
        tm = tmp[t % 2]
        if i1 == 0:
            p1_stt(t)
        if i1 + 1 < len(order1):
            p1_load(order1[i1 + 1][2])
            p1_stats(order1[i1 + 1][2])
        a_ = abf[t % 2]
        TT("pool", a_.ap[:, 0:1024], tm.ap[:, 0:1024], B1[r].ap[:, 0:1024], ALU.add, [tm.d, B1[r].d], [a_.d])
        TT("dve", a_.ap[:, 1024:2048], tm.ap[:, 1024:2048], B1[r].ap[:, 1024:2048], ALU.add, [tm.d, B1[r].d], [a_.d])
        if i1 + 1 < len(order1):
            p1_stt(order1[i1 + 1][2])
        for q in range(4):
            pb = PS[2 + (q % 2)]
            pv = psbf(2 + (q % 2))
            for i in range(4):
                kc = 4 * q + i
                TR(pv[:, i * 128:(i + 1) * 128], a_.ap[:, kc * 128:(kc + 1) * 128], identbf.ap, [a_.d, identbf.d], [pb.d])
            CP("act" if q % 2 == 0 else "dve", ab.ap[:, 4 * q:4 * q + 4, j * 128:(j + 1) * 128],
               pv[:, 0:512].rearrange("p (a b) -> p a b", b=128), [pb.d], [ab.d])
        if lastj:
            DMA("sp", aT_d.ap[tb][:, :, 0:ntok], ab.ap[:, :, 0:ntok], [ab.d], [aT_d.ds[tb]])
    P.barrier()
    if LIMIT == 1:
        return finish()
    AR.top = persist_mark

    tri = [T(AR.alloc([128, 128], F32)) for _ in range(2)]
    maskn = [T(AR.alloc([128, 512], BF16)) for _ in range(2)]
    for d_ in range(2):
        DMA("sp", tri[d_].ap, c_tri[d_], [], [tri[d_].d])
        DMA("pool", maskn[d_].ap, c_mask[d_], [], [maskn[d_].d])
    xs = T(AR.alloc([128, NT, 512], BF16), NT)
    Btok = T(AR.alloc([128, NT, 128], BF16), NT)
    BT = T(AR.alloc([128, NT * 128], BF16), NT)
    CT = T(AR.alloc([128, NT * 128], BF16), NT)
    dtr = T(AR.alloc([128, NT, 16], F32))
    dtv = T(AR.alloc([128, NT, 16], F32))
    lav = T(AR.alloc([128, NT, 16], F32))
    cwt = T(AR.alloc([128, 6, 3], F32))
    cbt = T(AR.alloc([128, 6], F32))
    dtbb = T(AR.alloc([128, 16], F32))
    aneg = T(AR.alloc([128, 16], F32))
    Dbc = T(AR.alloc([128, 512], F32))
    nwb = T(AR.alloc([128, 512], F32))
    p2_mark = AR.top

    for g in range(NG):
        DMA("sp", cwt.ap, cw_ssm[g], [], [cwt.d])
        DMA("sp", cbt.ap, cb_ssm[g], [], [cbt.d])
        load_vec_bc(dtbb, dtb[g])
        load_vec_bc(aneg, alog[g])
        load_vec_bc(Dbc, dsk[g])
        load_vec_bc(nwb, ssm_nw[g])
        ACT(aneg.ap, aneg.ap, AF.Exp, [aneg.d], [aneg.d])
        TS("dve", aneg.ap, aneg.ap, -1.0, None, ALU.mult, None, [aneg.d], [aneg.d])
        wg = T(AR.alloc([128, 16, 1296], BF16))
        for k2 in range(8):
            DMA("pool", wg.ap[:, 2 * k2:2 * k2 + 2, :], wg_ssm[g][:, 2 * k2:2 * k2 + 2, :], [], [wg.d])
        aTl = [T(AR.alloc([128, 16, 512], BF16)) for _ in range(2)]
        acc = [T(AR.alloc([128, 512], F32)) for _ in range(2)]
        xTt = [[T(AR.alloc([128, 512], BF16)) for _ in range(4)] for _ in range(2)]
        zst = [T(AR.alloc([128, 512], BF16)) for _ in range(2)]
        fmi = 0
        zi = 0
        MARKS.setdefault('A', P.count)
        for tb in range(9):
            ntok = 256 if tb == 0 else 512
            W = 256 if tb == 0 else 64
            tiles = [0, 1] if tb == 0 else [2 + 4 * (tb - 1) + i for i in range(4)]
            tok0 = tiles[0] * 128
            if tb == 1:
                MARKS.setdefault('C', P.count)
            if tb == 2:
                MARKS.setdefault('D', P.count)
            al = aTl[tb % 2]
            if tb == 0:
                DMA("sp", al.ap[:, :, 0:ntok], aT_d.ap[tb][:, :, 0:ntok], [aT_d.ds[tb]], [al.d])
            if tb + 1 < 9:
                DMA("sp", aTl[(tb + 1) % 2].ap, aT_d.ap[tb + 1], [aT_d.ds[tb + 1]], [aTl[(tb + 1) % 2].d])
            xTb = xTt[tb % 2]
            for oc in range(6):
                pb = PS[fmi % 2]
                fmi += 1
                for kc in range(16):
                    MM(pb.ap[:, 0:ntok], wg.ap[:, kc, oc * 128:(oc + 1) * 128], al.ap[:, kc, 0:ntok], kc == 0, kc == 15,
                       [wg.d, al.d], [pb.d])
                ac = acc[oc % 2]
                ACT(ac.ap[:, 0:ntok], pb.ap[:, 0:ntok], AF.Identity, [pb.d, cwt.d, cbt.d], [ac.d],
                    bias=cbt.ap[:, oc:oc + 1], scale=cwt.ap[:, oc, 1:2])
                u3 = pb.ap[:, 0:ntok].rearrange("p (r w) -> p r w", w=W)
                a3 = ac.ap[:, 0:ntok].rearrange("p (r w) -> p r w", w=W)
                STT("dve", a3[:, :, 1:W], u3[:, :, 0:W - 1], cwt.ap[:, oc, 0:1], a3[:, :, 1:W], ALU.mult, ALU.add,
                    [pb.d, cwt.d, ac.d], [ac.d])
                STT("dve", a3[:, :, 0:W - 1], u3[:, :, 1:W], cwt.ap[:, oc, 2:3], a3[:, :, 0:W - 1], ALU.mult, ALU.add,
                    [pb.d, cwt.d, ac.d], [ac.d])
                if oc < 4:
                    ACT(xTb[oc].ap[:, 0:ntok], ac.ap[:, 0:ntok], AF.Silu, [ac.d], [xTb[oc].d])
                elif oc == 4:
                    ACT(BT.ap[:, tok0:tok0 + ntok], ac.ap[:, 0:ntok], AF.Silu, [ac.d], [BT.ds[t] for t in tiles])
                else:
                    ACT(CT.ap[:, tok0:tok0 + ntok], ac.ap[:, 0:ntok], AF.Silu, [ac.d], [CT.ds[t] for t in tiles])
            MARKS.setdefault('B', P.count)
            for j, t in enumerate(tiles):
                pb = PS[2 + (t % 2)]
                pv = psbf(2 + (t % 2))
                for oc in range(4):
                    TR(pv[:, oc * 128:(oc + 1) * 128], xTb[oc].ap[:, j * 128:(j + 1) * 128], identbf.ap,
                       [xTb[oc].d, identbf.d], [pb.d])
                TR(psbf(7)[:, 0:128], BT.ap[:, t * 128:(t + 1) * 128], identbf.ap, [BT.ds[t], identbf.d], [PS[7].d])
                CP("dve", xs.ap[:, t, :], pv[:, 0:512], [pb.d], [xs.ds[t]])
                CP("act", Btok.ap[:, t, :], psbf(7)[:, 0:128], [PS[7].d], [Btok.ds[t]])
                if t >= 2:
                    pz = PS[4 + (zi % 2)]
                    zz = zst[zi % 2]
                    zi += 1
                    for kc in range(16):
                        MM(pz.ap, al.ap[:, kc, j * 128:(j + 1) * 128], wg.ap[:, kc, 768:1280], kc == 0, kc == 15,
                           [al.d, wg.d], [pz.d])
                    ACT(zz.ap, pz.ap, AF.Silu, [pz.d], [zz.d])
                    DMA("sp", z_d.ap[g, t - 2], zz.ap, [zz.d], [z_d.ds[g * NTL + t - 2]])
                pd = PS[6]
                for kc in range(16):
                    MM(pd.ap[:, 0:16], al.ap[:, kc, j * 128:(j + 1) * 128], wg.ap[:, kc, 1280:1296], kc == 0, kc == 15,
                       [al.d, wg.d], [pd.d])
                TT("dve", dtr.ap[:, t, :], pd.ap[:, 0:16], dtbb.ap, ALU.add, [pd.d, dtbb.d], [dtr.d])
        MARKS.setdefault('E', P.count)
        ACT(dtv.ap, dtr.ap, AF.Exp, [dtr.d], [dtv.d])
        ACT(dtv.ap, dtv.ap, AF.Ln, [dtv.d], [dtv.d], bias=ones32.ap[:, 0:1])
        TT("dve", lav.ap, dtv.ap, aneg.ap.unsqueeze(1).to_broadcast([128, NT, 16]), ALU.mult, [dtv.d, aneg.d], [lav.d])
        if DEBUG:
            DMA("sp", dbg["dt"][g], dtv.ap, [dtv.d], [])
            DMA("sp", dbg["xs"][g], xs.ap, xs.ds, [])
        P.barrier()
        AR.top = p2_mark

        yacc = T(AR.alloc([128, NTL, 512], BF16), NTL)
        S32 = T(AR.alloc([128, 512], F32))
        Sbf = T(AR.alloc([128, 512], BF16))
        rc = [T(AR.alloc([128, 8, 128], F32)) for _ in range(2)]
        Lall = [T(AR.alloc([128, 8, 128], BF16), 8) for _ in range(2)]
        Mall = [T(AR.alloc([128, 8, 128], BF16), 8) for _ in range(2)]
        cbT = [T(AR.alloc([128, 128], BF16)) for _ in range(2)]
        smal = [T(AR.alloc([128, 32], F32)) for _ in range(2)]
        xw = [T(AR.alloc([128, 512], BF16)) for _ in range(2)]
        tA = [T(AR.alloc([128, 512], F32)) for _ in range(2)]
        tB = [T(AR.alloc([128, 512], F32)) for _ in range(2)]
        zt = [T(AR.alloc([128, 512], BF16)) for _ in range(2)]
        ynb = [T(AR.alloc([128, 512], BF16)) for _ in range(2)]
        yTo = [T(AR.alloc([128, 4, 128], BF16)) for _ in range(2)]
        gst = [T(AR.alloc([128, 4], F32)) for _ in range(2)]
        junk2 = T(AR.alloc([128, 512], BF16))
        ssg = T(AR.alloc([128, NTL], F32), NTL)
        sdg = T(AR.alloc([128, NTL], F32))
        rsg = T(AR.alloc([128, NTL], F32))
        MEMSET("pool", ssg.ap, 0.0, ssg.ds)
        def stageA(d_, c, k):
            lat = c >= 2
            last = 127 if d_ == 0 else 0
            la_c = lav.ap[:, c, d_ * 8:(d_ + 1) * 8]
            dt_c = dtv.ap[:, c, d_ * 8:(d_ + 1) * 8]
            sm = smal[k]
            pS = PS[7]
            MM(pS.ap[:, 0:8], tri[d_].ap, la_c, True, True, [tri[d_].d, lav.d], [pS.d])
            MM(pS.ap[:, 8:16], ones32.ap, la_c, True, True, [ones32.d, lav.d], [pS.d])
            TT("dve", rc[k].ap, tri[d_].ap.unsqueeze(1).to_broadcast([128, 8, 128]),
               la_c.unsqueeze(2).to_broadcast([128, 8, 128]), ALU.mult, [tri[d_].d, lav.d], [rc[k].d])
            Rb = [PS[0], PS[1]]
            for hf in range(2):
                MM(Rb[hf].ap, ones32.ap, rc[k].ap[:, 4 * hf:4 * hf + 4, :].rearrange("p a b -> p (a b)"), True, False,
                   [ones32.d, rc[k].d], [Rb[hf].d])
                MM(Rb[hf].ap, identbf.ap, maskn[d_].ap, False, True, [identbf.d, maskn[d_].d], [Rb[hf].d])
            ACT(sm.ap[:, 0:8], pS.ap[:, 0:8], AF.Identity, [pS.d], [sm.d], scale=-1.0)
            ACT(sm.ap[:, 8:24], pS.ap[:, 0:16], AF.Exp, [pS.d], [sm.d])
            if lat:
                MM(pS.ap[:, 128:256], BT.ap[:, c * 128:(c + 1) * 128], CT.ap[:, c * 128:(c + 1) * 128], True, True,
                   [BT.ds[c], CT.ds[c]], [pS.d])
                CP("act", cbT[k].ap, pS.ap[:, 128:256], [pS.d], [cbT[k].d])
            La = Lall[k]
            for h in range(8):
                ACT(La.ap[:, h, :], Rb[h // 4].ap[:, (h % 4) * 128:(h % 4 + 1) * 128], AF.Exp, [Rb[h // 4].d, sm.d],
                    [La.ds[h]], bias=sm.ap[:, h:h + 1])
            TT("dve", sm.ap[:, 24:32], La.ap[:, :, last], dt_c, ALU.mult, La.ds + [dtv.d], [sm.d])
            TT("pool", xw[k].ap.rearrange("p (h d) -> p h d", d=64), xs.ap[:, c, :].rearrange("p (h d) -> p h d", d=64),
               sm.ap[:, 24:32].unsqueeze(2).to_broadcast([128, 8, 64]), ALU.mult, [xs.ds[c], sm.d], [xw[k].d])
            if lat:
                Ma = Mall[k]
                for h in range(8):
                    STT("dve", Ma.ap[:, h, :], La.ap[:, h, :], dt_c[:, h:h + 1], cbT[k].ap, ALU.mult, ALU.mult,
                        [La.ds[h], dtv.d, cbT[k].d], [Ma.ds[h]])
                for h in range(8):
                    MM(PS[2 + k].ap[:, h * 64:(h + 1) * 64], Ma.ap[:, h, :], xs.ap[:, c, h * 64:(h + 1) * 64], True, True,
                       [Ma.ds[h], xs.ds[c]], [PS[2 + k].d])
            MM(PS[4 + k].ap, Btok.ap[:, c, :], xw[k].ap, True, True, [Btok.ds[c], xw[k].d], [PS[4 + k].d])

        def stageB(d_, c, k):
            lat = c >= 2
            sm = smal[k]
            Pd, Ps_, Po = PS[2 + k], PS[4 + k], PS[6]
            if lat:
                MM(Po.ap, CT.ap[:, c * 128:(c + 1) * 128], Sbf.ap, True, True, [CT.ds[c], Sbf.d], [Po.d])
                TT("dve", tB[k].ap.rearrange("p (h d) -> p h d", d=64), Po.ap.rearrange("p (h d) -> p h d", d=64),
                   sm.ap[:, 8:16].unsqueeze(2).to_broadcast([128, 8, 64]), ALU.mult, [Po.d, sm.d], [tB[k].d])
                TT("dve", tB[k].ap, tB[k].ap, Pd.ap, ALU.add, [tB[k].d, Pd.d], [tB[k].d])
            TT("dve", S32.ap.rearrange("p (h d) -> p h d", d=64), S32.ap.rearrange("p (h d) -> p h d", d=64),
               sm.ap[:, 16:24].unsqueeze(2).to_broadcast([128, 8, 64]), ALU.mult, [S32.d, sm.d], [S32.d])
            TT("dve", S32.ap, S32.ap, Ps_.ap, ALU.add, [S32.d, Ps_.d], [S32.d])
            CP("act", Sbf.ap, S32.ap, [S32.d], [Sbf.d])
            if lat and d_ == 0:
                TT("pool", tA[k].ap, xs.ap[:, c, :], Dbc.ap, ALU.mult, [xs.ds[c], Dbc.d], [tA[k].d])
                TT("pool", yacc.ap[:, c - 2, :], tA[k].ap, tB[k].ap, ALU.add, [tA[k].d, tB[k].d], [yacc.ds[c - 2]])
            if lat and d_ == 1:
                DMA("sp", zt[k].ap, z_d.ap[g, c - 2], [z_d.ds[g * NTL + c - 2]], [zt[k].d])
                TT("pool", tA[k].ap, tB[k].ap, yacc.ap[:, c - 2, :], ALU.add, [tB[k].d, yacc.ds[c - 2]], [tA[k].d])
                TT("pool", yacc.ap[:, c - 2, :], tA[k].ap, zt[k].ap, ALU.mult, [tA[k].d, zt[k].d], [yacc.ds[c - 2]])
                ACT(junk2.ap, yacc.ap[:, c - 2, :], AF.Square, [yacc.ds[c - 2]], [junk2.d, ssg.ds[c - 2]],
                    accum=ssg.ap[:, c - 2:c - 1])

        def stageC(cl, k):
            STT("dve", ynb[k].ap, yacc.ap[:, cl, :], rsg.ap[:, cl:cl + 1], nwb.ap, ALU.mult, ALU.mult,
                [yacc.ds[cl], rsg.d, nwb.d], [ynb[k].d])
            pv = psbf(6 + k)
            for q in range(4):
                TR(pv[:, q * 128:(q + 1) * 128], ynb[k].ap[:, q * 128:(q + 1) * 128], identbf.ap,
                   [ynb[k].d, identbf.d], [PS[6 + k].d])
            CP("act", yTo[k].ap, pv[:, 0:512].rearrange("p (a b) -> p a b", b=128), [PS[6 + k].d], [yTo[k].d])
            DMA("sp", yT_d.ap[cl][:, g * 4:(g + 1) * 4, :], yTo[k].ap, [yTo[k].d], [yT_d.ds[cl]])

        items = []
        for d_ in range(SCAN):
            order = list(range(NT)) if d_ == 0 else [1, 0] + list(range(NT - 1, 1, -1))
            items += [(d_, c, ci == 0) for ci, c in enumerate(order)]
        if items:
            stageA(items[0][0], items[0][1], 0)
        for i_, (d_, c, first) in enumerate(items):
            if i_ + 1 < len(items):
                stageA(items[i_ + 1][0], items[i_ + 1][1], (i_ + 1) % 2)
            if first:
                MEMSET("dve", S32.ap, 0.0, [S32.d])
                MEMSET("pool", Sbf.ap, 0.0, [Sbf.d])
            stageB(d_, c, i_ % 2)
        if SCAN == 2:
            ACT(sdg.ap, ssg.ap, AF.Sqrt, ssg.ds, [sdg.d], bias=epsc.ap, scale=1.0 / 512.0)
            RECIP(rsg.ap, sdg.ap, [sdg.d], [rsg.d])
            for cl in range(NTL):
                stageC(cl, cl % 2)
        P.barrier()
        AR.top = p2_mark
    AR.top = persist_mark
    if LIMIT == 2:
        return finish()

    ss_sc = T(AR.alloc([128, NTL], F32))
    rs_sc = T(AR.alloc([128, NTL], F32))
    sd_sc = T(AR.alloc([128, NTL], F32))
    MEMSET("dve", ss_sc.ap, 0.0, [ss_sc.d])
    p3_mark = AR.top
    wscs = [T(AR.alloc([128, 16, 1536], BF16)) for _ in range(2)]
    cwcs = [T(AR.alloc([128, 4, 3], F32)) for _ in range(2)]
    scwts = [T(AR.alloc([128, 4], F32)) for _ in range(2)]

    def p3_wload(jb):
        DMA("sp", cwcs[jb % 2].ap, cw_sc[jb], [], [cwcs[jb % 2].d])
        DMA("sp", scwts[jb % 2].ap, scw[jb], [], [scwts[jb % 2].d])
        for k2 in range(8):
            DMA("pool", wscs[jb % 2].ap[:, 2 * k2:2 * k2 + 2, :], wg_sc[jb][:, 2 * k2:2 * k2 + 2, :], [], [wscs[jb % 2].d])
    aTl = [T(AR.alloc([128, 16, 512], BF16)) for _ in range(2)]
    cgs = [T(AR.alloc([128, 512], F32)) for _ in range(2)]
    vv = [T(AR.alloc([128, 512], F32)) for _ in range(2)]
    acc = [T(AR.alloc([128, 512], F32)) for _ in range(2)]
    q2 = [T(AR.alloc([128, 512], BF16)) for _ in range(4)]
    qw = [T(AR.alloc([128, 512], BF16)) for _ in range(4)]
    it = 0
    p3_wload(0)
    p3i = 0
    DMA("sp", aTl[0].ap, aT_d.ap[1], [aT_d.ds[1]], [aTl[0].d])
    for jb in range(4):
        wsc, cwc, scwt = wscs[jb % 2], cwcs[jb % 2], scwts[jb % 2]
        if jb + 1 < 4:
            p3_wload(jb + 1)
        for tb in range(1, 9):
            t0 = 4 * (tb - 1)
            al = aTl[p3i % 2]
            p3i += 1
            if p3i < 32:
                ntb = (p3i % 8) + 1
                DMA("sp", aTl[p3i % 2].ap, aT_d.ap[ntb], [aT_d.ds[ntb]], [aTl[p3i % 2].d])
            for cc in range(4):
                k = it % 2
                it += 1
                pbg, pcg, phv = PS[0 + 3 * k], PS[1 + 3 * k], PS[2 + 3 * k]
                for (pb, off) in ((pbg, 0), (pcg, 512), (phv, 1024)):
                    for kc in range(16):
                        MM(pb.ap, wsc.ap[:, kc, off + cc * 128:off + (cc + 1) * 128], al.ap[:, kc, :], kc == 0, kc == 15,
                           [wsc.d, al.d], [pb.d])
                CP("act", cgs[k].ap, pcg.ap, [pcg.d], [cgs[k].d])
                TT("dve", vv[k].ap, cgs[k].ap, phv.ap, ALU.mult, [cgs[k].d, phv.d], [vv[k].d])
                ACT(acc[k].ap, vv[k].ap, AF.Identity, [vv[k].d, cwc.d], [acc[k].d], scale=cwc.ap[:, cc, 1:2])
                v3 = vv[k].ap.rearrange("p (r w) -> p r w", w=64)
                a3 = acc[k].ap.rearrange("p (r w) -> p r w", w=64)
                STT("dve", a3[:, :, 1:64], v3[:, :, 0:63], cwc.ap[:, cc, 0:1], a3[:, :, 1:64], ALU.mult, ALU.add,
                    [vv[k].d, cwc.d, acc[k].d], [acc[k].d])
                STT("dve", a3[:, :, 0:63], v3[:, :, 1:64], cwc.ap[:, cc, 2:3], a3[:, :, 0:63], ALU.mult, ALU.add,
                    [vv[k].d, cwc.d, acc[k].d], [acc[k].d])
                TT("dve", acc[k].ap, acc[k].ap, pbg.ap, ALU.mult, [acc[k].d, pbg.d], [acc[k].d])
                ACT(q2[cc].ap, acc[k].ap, AF.Square, [acc[k].d], [q2[cc].d])
                TS("pool", qw[cc].ap, acc[k].ap, scwt.ap[:, cc:cc + 1], None, ALU.mult, None, [acc[k].d, scwt.d], [qw[cc].d])
                DMA("sp", yT_d.ap[t0:t0 + 4, :, 16 + jb * 4 + cc, :].rearrange("t p k -> p t k"),
                    qw[cc].ap.rearrange("p (t k) -> p t k", k=128), [qw[cc].d], [yT_d.ds[t0 + i] for i in range(4)])
            pss = PS[6 + (tb % 2)]
            for i in range(4):
                for cc in range(4):
                    MM(pss.ap[:, i:i + 1], q2[cc].ap[:, i * 128:(i + 1) * 128], onesbf.ap[:, 0:1], cc == 0, cc == 3,
                       [q2[cc].d, onesbf.d], [pss.d])
            TT("dve", ss_sc.ap[:, t0:t0 + 4], ss_sc.ap[:, t0:t0 + 4], pss.ap[:, 0:4], ALU.add, [ss_sc.d, pss.d], [ss_sc.d])
    rstd_from_ss(ss_sc.ap, sd_sc.ap, rs_sc.ap, 2048.0, ss_sc.d, sd_sc.d, rs_sc.d)
    if DEBUG:
        DMA("sp", dbg["rsc"], rs_sc.ap, [rs_sc.d], [])
    P.barrier()
    if LIMIT == 3:
        return finish()
    AR.top = p3_mark

    g1b = T(AR.alloc([128, 2048], F32))
    load_mod_bc(g1b, 0, 2)
    wos = [T(AR.alloc([128, 32, 1024], BF16)) for _ in range(2)]
    yt = [T(AR.alloc([128, 32, 128], BF16)) for _ in range(2)]
    xh = [T(AR.alloc([128, 1024], F32)) for _ in range(2)]
    t1 = [T(AR.alloc([128, 512], F32)) for _ in range(2)]
    ht = [T(AR.alloc([128, 1024], F32)) for _ in range(2)]
    it = 0
    for hf in range(2):
        for k4 in range(8):
            DMA("pool", wos[hf].ap[:, 4 * k4:4 * k4 + 4, :], w_out[hf][:, 4 * k4:4 * k4 + 4, :], [], [wos[hf].d])

    def p4_load(i4):
        hf_, t_ = i4 // NTL, i4 % NTL
        DMA("sp", yt[t_ % 2].ap, yT_d.ap[t_], [yT_d.ds[t_]], [yt[t_ % 2].d])
        DMA("sp", xh[t_ % 2].ap, xin[t_ + 2][:, hf_ * 1024:(hf_ + 1) * 1024], [], [xh[t_ % 2].d])

    p4_load(0)
    for hf in range(2):
        wo = wos[hf]
        for t in range(NTL):
            y_ = yt[t % 2]
            x_ = xh[t % 2]
            if hf * NTL + t + 1 < 2 * NTL:
                p4_load(hf * NTL + t + 1)
            h_ = ht[t % 2]
            for q in range(2):
                k = it % 2
                it += 1
                pm, pc = PS[0 + 2 * k], PS[1 + 2 * k]
                cols = slice(q * 512, (q + 1) * 512)
                for kc in range(16):
                    MM(pm.ap, y_.ap[:, kc, :], wo.ap[:, kc, cols], kc == 0, kc == 15, [y_.d, wo.d], [pm.d])
                for kc in range(16, 32):
                    MM(pc.ap, y_.ap[:, kc, :], wo.ap[:, kc, cols], kc == 16, kc == 31, [y_.d, wo.d], [pc.d])
                ACT(t1[k].ap, pc.ap, AF.Identity, [pc.d, rs_sc.d], [t1[k].d], scale=rs_sc.ap[:, t:t + 1])
                TT("dve", t1[k].ap, t1[k].ap, pm.ap, ALU.add, [t1[k].d, pm.d], [t1[k].d])
                gcols = slice(hf * 1024 + q * 512, hf * 1024 + (q + 1) * 512)
                TT("pool", t1[k].ap, t1[k].ap, g1b.ap[:, gcols], ALU.mult, [t1[k].d, g1b.d], [t1[k].d])
                TT("pool", h_.ap[:, cols], t1[k].ap, x_.ap[:, cols], ALU.add, [t1[k].d, x_.d], [h_.d])
            DMA("sp", hacc_h[hf].ap[t * 128:(t + 1) * 128, :], h_.ap, [h_.d], [hacc_h[hf].ds[t]])
    P.barrier()
    if LIMIT == 4:
        return finish()
    AR.top = persist_mark

    idxc = T(AR.alloc([128, 4, 16], I32))
    gatc = T(AR.alloc([128, 4, 16], F32))
    p5_mark = AR.top
    A2 = T(AR.alloc([128, 2048], F32))
    B2 = T(AR.alloc([128, 2048], F32))
    nfb = T(AR.alloc([128, 2048], F32))
    load_vec_bc(nfb, nffn)
    load_mod_bc(A2, 0, 4)
    load_mod_bc(B2, 0, 3)
    TT("pool", A2.ap, A2.ap, nfb.ap, ALU.mult, [A2.d, nfb.d], [A2.d])
    wr = T(AR.alloc([128, 16, 16], F32))
    DMA("sp", wr.ap, w_rt, [], [wr.d])
    affT = T(AR.alloc([16, 4096], F32))
    hx = [T(AR.alloc([128, 2048], F32)) for _ in range(2)]
    ff = [T(AR.alloc([128, 2048], F32)) for _ in range(2)]
    fb = [T(AR.alloc([128, 2048], BF16)) for _ in range(2)]
    fT = [T(AR.alloc([128, 16, 128], F32)) for _ in range(2)]
    st5 = [T(AR.alloc([128, 8], F32)) for _ in range(2)]
    ex = [T(AR.alloc([128, 16], F32)) for _ in range(2)]
    junk5 = T(AR.alloc([128, 2048], BF16))
    def p5_load(t_):
        for i2 in range(2):
            DMA("sp", hx[t_ % 2].ap[:, i2 * 1024:(i2 + 1) * 1024], hacc_h[i2].ap[t_ * 128:(t_ + 1) * 128, :],
                [hacc_h[i2].ds[t_]], [hx[t_ % 2].d])

    p5_load(0)
    for t in range(NTL):
        k = t % 2
        h_ = hx[k]
        if t + 1 < NTL:
            p5_load(t + 1)
        if DEBUG:
            DMA("sp", dbg["h1"][t * 128:(t + 1) * 128, :], h_.ap, [h_.d], [])
        s5 = st5[k]
        MEMSET("pool", s5.ap, 0.0, [s5.d])
        ACT(junk5.ap, h_.ap, AF.Square, [h_.d], [junk5.d, s5.d], accum=s5.ap[:, 0:1])
        rstd_from_ss(s5.ap[:, 0:1], s5.ap[:, 1:2], s5.ap[:, 2:3], 2048.0, s5.d, s5.d, s5.d)
        f_ = ff[k]
        STT("dve", f_.ap, h_.ap, s5.ap[:, 2:3], A2.ap, ALU.mult, ALU.mult, [h_.d, s5.d, A2.d], [f_.d])
        TT("pool", f_.ap, f_.ap, B2.ap, ALU.add, [f_.d, B2.d], [f_.d])
        ACT(fb[k].ap, f_.ap, AF.Copy, [f_.d], [fb[k].d])
        DMA("sp", f_d.ap[t * 128:(t + 1) * 128, :], fb[k].ap, [fb[k].d], [f_d.d])
        for q in range(4):
            pb = PS[q % 4]
            for i in range(4):
                kc = 4 * q + i
                TR(pb.ap[:, i * 128:(i + 1) * 128], f_.ap[:, kc * 128:(kc + 1) * 128], ident32.ap, [f_.d, ident32.d], [pb.d])
            CP("act" if q % 2 == 0 else "dve", fT[k].ap[:, 4 * q:4 * q + 4, :], pb.ap.rearrange("p (a b) -> p a b", b=128),
               [pb.d], [fT[k].d])
        pl = PS[4 + k]
        for kc in range(16):
            MM(pl.ap[:, 0:16], fT[k].ap[:, kc, :], wr.ap[:, kc, :], kc == 0, kc == 15, [fT[k].d, wr.d], [pl.d])
        P.emit("dve", (lambda o, i: (lambda e: e.reduce_max(out=o, in_=i, axis=mybir.AxisListType.X)))(s5.ap[:, 3:4], pl.ap[:, 0:16]),
               [pl.d], [s5.d])
        TS("dve", s5.ap[:, 4:5], s5.ap[:, 3:4], -1.0, None, ALU.mult, None, [s5.d], [s5.d])
        ACT(ex[k].ap, pl.ap[:, 0:16], AF.Exp, [pl.d, s5.d], [ex[k].d, s5.d], bias=s5.ap[:, 4:5], accum=s5.ap[:, 5:6])
        RECIP(s5.ap[:, 6:7], s5.ap[:, 5:6], [s5.d], [s5.d])
        TS("dve", ex[k].ap, ex[k].ap, s5.ap[:, 6:7], None, ALU.mult, None, [ex[k].d, s5.d], [ex[k].d])
        pa = PS[6 + k]
        TR(pa.ap[0:16, 0:128], ex[k].ap, ident32.ap, [ex[k].d, ident32.d], [pa.d])
        CP("act", affT.ap[:, t * 128:(t + 1) * 128], pa.ap[0:16, 0:128], [pa.d], [affT.d])
    if DEBUG:
        DMA("sp", dbg["aff"], affT.ap, [affT.d], [])
    NPRE = 36 if LIMIT >= 6 else 0
    srcs_all = []
    if LIMIT >= 6:
        for e_ in range(16):
            srcs_all += [(w_gate[e_, 0], 0), (w_up[e_, 0], 0), (w_gate[e_, 1], 0), (w_up[e_, 1], 0),
                         (w_down[e_, 0], 1), (w_down[e_, 1], 1)]
    wexp = T(nc.dram_tensor("wexp_bf", [max(NPRE, 1), 128, 8192], BF16, kind="Internal").ap(), max(NPRE, 1))
    for i in range(NPRE):
        src_, kind_ = srcs_all[i]
        if kind_ == 0:
            dv = wexp.ap[i].rearrange("p (a b) -> p a b", b=512)
            DMA("pool", dv[:, 0:8, :], src_[:, 0:8, :], [], [wexp.ds[i]])
            DMA("pool", dv[:, 8:16, :], src_[:, 8:16, :], [], [wexp.ds[i]])
        else:
            dv = wexp.ap[i].rearrange("p (a b) -> p a b", b=1024)
            DMA("pool", dv[:, 0:4, :], src_[:, 0:4, :], [], [wexp.ds[i]])
            DMA("pool", dv[:, 4:8, :], src_[:, 4:8, :], [], [wexp.ds[i]])
    vals = T(AR.alloc([16, 512], F32))
    idxu = T(AR.alloc([16, 512], U32))
    idxf = T(AR.alloc([16, 512], F32))
    for r in range(64):
        v8 = vals.ap[:, r * 8:(r + 1) * 8]
        i8 = idxu.ap[:, r * 8:(r + 1) * 8]
        P.emit("dve", (lambda o, i: (lambda e: e.max(out=o, in_=i)))(v8, affT.ap), [affT.d], [vals.d])
        P.emit("dve", (lambda o, m, i: (lambda e: e.max_index(out=o, in_max=m, in_values=i)))(i8, v8, affT.ap),
               [affT.d, vals.d], [idxu.d])
        P.emit("dve", (lambda o, m, i: (lambda e: e.match_replace(out=o, in_to_replace=m, in_values=i, imm_value=-1.0)))(affT.ap, v8, affT.ap),
               [vals.d, idxu.d], [affT.d])
    CP("dve", idxf.ap, idxu.ap, [idxu.d], [idxf.d])
    if DEBUG:
        DMA("sp", dbg["vals"], vals.ap, [vals.d], [])
        DMA("sp", dbg["idx"], idxu.ap, [idxu.d], [])
    for s in range(4):
        pa = PS[s % 2]
        TR(pa.ap[:, 0:16], idxf.ap[:, s * 128:(s + 1) * 128], ident32.ap[0:16, 0:16], [idxf.d, ident32.d], [pa.d])
        TR(pa.ap[:, 16:32], vals.ap[:, s * 128:(s + 1) * 128], ident32.ap[0:16, 0:16], [vals.d, ident32.d], [pa.d])
        CP("dve", idxc.ap[:, s, :], pa.ap[:, 0:16], [pa.d], [idxc.d])
        CP("act", gatc.ap[:, s, :], pa.ap[:, 16:32], [pa.d], [gatc.d])
    P.barrier()
    if LIMIT == 5:
        return finish()
    AR.top = p5_mark

    g2b = T(AR.alloc([128, 2048], F32))
    load_mod_bc(g2b, 0, 5)
    NW = 5
    wbuf = [T(AR.alloc([128, 8192], BF16)) for _ in range(NW)]
    xg = [T(AR.alloc([128, 2048], BF16)) for _ in range(8)]
    xeT = [T(AR.alloc([128, 16, 512], BF16)) for _ in range(2)]
    hidT = [T(AR.alloc([128, 8, 512], BF16), 8) for _ in range(2)]
    silt = [T(AR.alloc([128, 512], BF16)) for _ in range(2)]
    yet = [T(AR.alloc([128, 512], F32)) for _ in range(2)]
    yo = [T(AR.alloc([128, 1024], F32)) for _ in range(2)]
    hall = Dep()
    piece_view = {}
    state6 = {"issued": 0}

    def ensure_issued(upto):
        while state6["issued"] < min(upto, len(srcs_all)):
            i = state6["issued"]
            src, kind = srcs_all[i]
            wb = wbuf[i % NW]
            dst = wb.ap.rearrange("p (a b) -> p a b", b=512 if kind == 0 else 1024)
            if i < NPRE:
                DMA("sp", wb.ap[:, 0:4096], wexp.ap[i][:, 0:4096], [wexp.ds[i]], [wb.d])
                DMA("sp", wb.ap[:, 4096:8192], wexp.ap[i][:, 4096:8192], [wexp.ds[i]], [wb.d])
            elif kind == 0:
                DMA("pool", dst[:, 0:8, :], src[:, 0:8, :], [], [wb.d])
                DMA("pool", dst[:, 8:16, :], src[:, 8:16, :], [], [wb.d])
            else:
                DMA("pool", dst[:, 0:4, :], src[:, 0:4, :], [], [wb.d])
                DMA("pool", dst[:, 4:8, :], src[:, 4:8, :], [], [wb.d])
            piece_view[i] = (wb, dst)
            state6["issued"] = i + 1

    def gather_issue(e_):
        for s in range(4):
            xg_ = xg[(e_ % 2) * 4 + s]
            P.emit("pool", (lambda o, i, ix: (lambda e: e.indirect_dma_start(
                out=o, out_offset=None, in_=i, in_offset=bass.IndirectOffsetOnAxis(ap=ix, axis=0))))(
                    xg_.ap, f_d.ap, idxc.ap[:, s, e_:e_ + 1]), [f_d.d, idxc.d], [xg_.d], dma=True)

    yi = 0
    pgi = 0
    gather_issue(0)
    ensure_issued(NW - 1)
    for e_ in range(16):
        xe = xeT[e_ % 2]
        for s in range(4):
            xg_ = xg[(e_ % 2) * 4 + s]
            for q in range(4):
                pv = psbf(6 + (q % 2))
                pb = PS[6 + (q % 2)]
                for i in range(4):
                    kc = 4 * q + i
                    TR(pv[:, i * 128:(i + 1) * 128], xg_.ap[:, kc * 128:(kc + 1) * 128], identbf.ap, [xg_.d, identbf.d], [pb.d])
                CP("act" if q % 2 == 0 else "dve", xe.ap[:, 4 * q:4 * q + 4, s * 128:(s + 1) * 128],
                   pv[:, 0:512].rearrange("p (a b) -> p a b", b=128), [pb.d], [xe.d])
        if e_ + 1 < 16:
            gather_issue(e_ + 1)
        hT = hidT[e_ % 2]
        p0 = 6 * e_
        for fcu in range(8):
            if fcu % 4 == 0:
                ensure_issued(p0 + 2 * (fcu // 4) + NW)
            wgp, wgv = piece_view[p0 + 0 + 2 * (fcu // 4)]
            wup, wuv = piece_view[p0 + 1 + 2 * (fcu // 4)]
            off = (fcu % 4) * 128
            k = pgi % 2
            pgi += 1
            pg, pu = PS[0 + 2 * k], PS[1 + 2 * k]
            for kc in range(16):
                MM(pg.ap, wgv[:, kc, off:off + 128], xe.ap[:, kc, :], kc == 0, kc == 15, [wgp.d, xe.d], [pg.d])
            for kc in range(16):
                MM(pu.ap, wuv[:, kc, off:off + 128], xe.ap[:, kc, :], kc == 0, kc == 15, [wup.d, xe.d], [pu.d])
            ACT(silt[k].ap, pg.ap, AF.Silu, [pg.d], [silt[k].d])
            TT("dve", hT.ap[:, fcu, :], silt[k].ap, pu.ap, ALU.mult, [silt[k].d, pu.d], [hT.ds[fcu]])
        for dh in range(2):
            ensure_issued(p0 + 4 + dh + NW)
            wdp, wdv = piece_view[p0 + 4 + dh]
            for s in range(4):
                yo_ = yo[yi % 2]
                yi += 1
                for dq in range(2):
                    k = pgi % 2
                    pgi += 1
                    py = PS[4 + k]
                    for fc in range(8):
                        MM(py.ap, hT.ap[:, fc, s * 128:(s + 1) * 128], wdv[:, fc, dq * 512:(dq + 1) * 512], fc == 0, fc == 7,
                           [hT.ds[fc], wdp.d], [py.d])
                    ACT(yet[k].ap, py.ap, AF.Identity, [py.d, gatc.d], [yet[k].d], scale=gatc.ap[:, s, e_:e_ + 1])
                    gc = slice(dh * 1024 + dq * 512, dh * 1024 + (dq + 1) * 512)
                    TT("dve", yo_.ap[:, dq * 512:(dq + 1) * 512], yet[k].ap, g2b.ap[:, gc], ALU.mult,
                       [yet[k].d, g2b.d], [yo_.d])
                P.emit("pool", (lambda o, i, ix: (lambda e: e.indirect_dma_start(
                    out=o, out_offset=bass.IndirectOffsetOnAxis(ap=ix, axis=0), in_=i, in_offset=None, compute_op=ALU.add)))(
                        hacc_h[dh].ap, yo_.ap, idxc.ap[:, s, e_:e_ + 1]),
                    [yo_.d, idxc.d], [hall], dma=True)
    P.barrier()
    if LIMIT == 6:
        return finish()
    AR.top = persist_mark

    fnb = T(AR.alloc([128, 2048], F32))
    load_vec_bc(fnb, nfin)
    hx = [T(AR.alloc([128, 2048], F32)) for _ in range(2)]
    ob = [T(AR.alloc([128, 2048], F32)) for _ in range(2)]
    st7 = [T(AR.alloc([128, 4], F32)) for _ in range(2)]
    junk7 = T(AR.alloc([128, 2048], BF16))
    def p7_load(t_):
        for i2 in range(2):
            DMA("sp", hx[t_ % 2].ap[:, i2 * 1024:(i2 + 1) * 1024], hacc_h[i2].ap[t_ * 128:(t_ + 1) * 128, :], [hall], [hx[t_ % 2].d])

    p7_load(0)
    for t in range(NTL):
        k = t % 2
        if t + 1 < NTL:
            p7_load(t + 1)
        s7 = st7[k]
        MEMSET("pool", s7.ap, 0.0, [s7.d])
        ACT(junk7.ap, hx[k].ap, AF.Square, [hx[k].d], [junk7.d, s7.d], accum=s7.ap[:, 0:1])
        rstd_from_ss(s7.ap[:, 0:1], s7.ap[:, 1:2], s7.ap[:, 2:3], 2048.0, s7.d, s7.d, s7.d)
        STT("dve", ob[k].ap, hx[k].ap, s7.ap[:, 2:3], fnb.ap, ALU.mult, ALU.mult, [hx[k].d, s7.d, fnb.d], [ob[k].d])
        DMA("sp", out[t], ob[k].ap, [ob[k].d], [])
    P.barrier()
    P.finalize(st)
    st.close()
    return nc


def _kc_p(w):
    K, N = w.shape
    return np.ascontiguousarray(w.reshape(K // 128, 128, N).transpose(1, 0, 2))


def _prep_shared(c_ctx, w_mod, b_mod, norm_mix_w, w_in, ssm_conv_w, ssm_conv_b, ssm_a_log, ssm_dt_bias, ssm_d,
                 ssm_norm_w, sc_conv_w, sc_norm_w, w_out, norm_ffn_w, w_router, w_gate, w_up, w_down, final_norm_w):
    f = np.float32
    sh = {}
    sh["w_mod"] = np.ascontiguousarray(w_mod[0].reshape(128, 16, 12288))
    sh["b_mod"] = np.ascontiguousarray(b_mod[0].reshape(1, 12288))
    sh["nmix"] = norm_mix_w[0].reshape(1, 2048)
    sh["nffn"] = norm_ffn_w[0].reshape(1, 2048)
    sh["nfin"] = final_norm_w.reshape(1, 2048)
    win = w_in[0]
    wg, cw, cb = [], [], []
    for g in range(4):
        cols = np.concatenate([2048 + g * 512 + np.arange(512), 4096 + g * 128 + np.arange(128),
                               4608 + g * 128 + np.arange(128), g * 512 + np.arange(512),
                               5120 + g * 8 + np.arange(8), 5152 + g * 8 + np.arange(8)])
        wg.append(_kc_p(win[:, cols]))
        ch = np.concatenate([g * 512 + np.arange(512), 2048 + g * 128 + np.arange(128), 2560 + g * 128 + np.arange(128)])
        cw.append(ssm_conv_w[0][:, ch].T.reshape(6, 128, 3).transpose(1, 0, 2))
        cb.append(ssm_conv_b[0][ch].reshape(6, 128).T)
    sh["wg_ssm"] = np.ascontiguousarray(np.stack(wg)).astype(f)
    sh["cw_ssm"] = np.ascontiguousarray(np.stack(cw)).astype(f)
    sh["cb_ssm"] = np.ascontiguousarray(np.stack(cb)).astype(f)
    wsc, cws, scw = [], [], []
    for j in range(4):
        ch = j * 512 + np.arange(512)
        cols = np.concatenate([5184 + ch, 7232 + ch, 9280 + ch])
        wsc.append(_kc_p(win[:, cols]))
        cws.append(sc_conv_w[0][:, ch].T.reshape(4, 128, 3).transpose(1, 0, 2))
        scw.append(sc_norm_w[0][ch].reshape(4, 128).T)
    sh["wg_sc"] = np.ascontiguousarray(np.stack(wsc)).astype(f)
    sh["cw_sc"] = np.ascontiguousarray(np.stack(cws)).astype(f)
    sh["scw"] = np.ascontiguousarray(np.stack(scw)).astype(f)
    hsel = np.stack([np.concatenate([g * 8 + np.arange(8), 32 + g * 8 + np.arange(8)]) for g in range(4)])
    sh["dtb"] = np.ascontiguousarray(ssm_dt_bias[0][hsel].reshape(4, 1, 16))
    sh["alog"] = np.ascontiguousarray(ssm_a_log[0][hsel].reshape(4, 1, 16))
    sh["dsk"] = np.ascontiguousarray(np.repeat(ssm_d[0].reshape(4, 8), 64, axis=1).reshape(4, 1, 512))
    sh["ssm_nw"] = np.ascontiguousarray(ssm_norm_w[0].reshape(4, 1, 512))
    wo = _kc_p(w_out[0])
    sh["w_out"] = np.ascontiguousarray(np.stack([wo[:, :, 0:1024], wo[:, :, 1024:2048]]))
    sh["w_rt"] = _kc_p(w_router[0])
    wgt = w_gate[0].reshape(16, 16, 128, 2, 512).transpose(0, 3, 2, 1, 4)
    wup = w_up[0].reshape(16, 16, 128, 2, 512).transpose(0, 3, 2, 1, 4)
    wdn = w_down[0].reshape(16, 8, 128, 2, 1024).transpose(0, 3, 2, 1, 4)
    sh["w_gate"] = np.ascontiguousarray(wgt)
    sh["w_up"] = np.ascontiguousarray(wup)
    sh["w_down"] = np.ascontiguousarray(wdn)
    sh["c_ident"] = np.eye(128, dtype=f)
    k = np.arange(128)[:, None]
    i = np.arange(128)[None, :]
    sh["c_tri"] = np.stack([(k <= i), (k >= i)]).astype(f)
    mf = np.where(i < k, NEG, 0.0).astype(f)
    mb = np.where(i > k, NEG, 0.0).astype(f)
    sh["c_mask"] = np.stack([np.tile(mf, (1, 4)), np.tile(mb, (1, 4))]).astype(f)
    return sh


_CACHE = {}


def kernel(x, c, ctx, c_ctx, w_mod, b_mod, norm_mix_w, w_in, ssm_conv_w, ssm_conv_b, ssm_a_log, ssm_dt_bias, ssm_d,
           ssm_norm_w, sc_conv_w, sc_norm_w, w_out, norm_ffn_w, w_router, w_gate, w_up, w_down, final_norm_w):
    args = [np.asarray(a, dtype=np.float32) for a in (
        c_ctx, w_mod, b_mod, norm_mix_w, w_in, ssm_conv_w, ssm_conv_b, ssm_a_log, ssm_dt_bias, ssm_d, ssm_norm_w,
        sc_conv_w, sc_norm_w, w_out, norm_ffn_w, w_router, w_gate, w_up, w_down, final_norm_w)]
    x = np.asarray(x, dtype=np.float32)
    c = np.asarray(c, dtype=np.float32)
    ctx = np.asarray(ctx, dtype=np.float32)
    sh = _prep_shared(*args)
    if "nc" not in _CACHE:
        _CACHE["nc"] = build_program()
    nc = _CACHE["nc"]
    in_maps = []
    for core in range(NCORES):
        b = core % 4
        m = dict(sh)
        m["xin"] = np.ascontiguousarray(np.concatenate([ctx[b], x[b]], axis=0).reshape(NT, 128, 2048))
        m["cvec"] = np.ascontiguousarray(np.stack([c[b], args[0]], axis=-1).reshape(128, 16, 2))
        in_maps.append(m)
    res = run_bass_kernel_spmd(nc, in_maps, core_ids=list(range(NCORES)))
    _CACHE["res"] = res
    outs = [res.results[b % NCORES]["out"].reshape(4096, 2048) for b in range(4)]
    return np.stack(outs).astype(np.float32)
```

```python
import numpy as np
from contextlib import ExitStack
import concourse.bass as bass
import concourse.mybir as mybir
from concourse.bass_utils import run_bass_kernel_spmd

F32 = mybir.dt.float32
BF16 = mybir.dt.bfloat16
I32 = mybir.dt.int32
U32 = mybir.dt.uint32
U8 = mybir.dt.uint8
AF = mybir.ActivationFunctionType
ALU = mybir.AluOpType

ENGS = ("pe", "act", "dve", "pool", "sp")
DEBUG = False
LIMIT = 99
STOPAT = None
MARKS = {}
NG = 4
SCAN = 2
NCORES = 4
EPS = 1e-6
NT = 34
NTL = 32
NEG = -30000.0


class Dep:
    __slots__ = ("w", "r", "excl")

    def __init__(self, excl=False):
        self.w = None
        self.r = []
        self.excl = excl


class Instr:
    __slots__ = ("eng", "fn", "dma", "deps", "sig", "sem", "val")

    def __init__(self, eng, fn, dma):
        self.eng = eng
        self.fn = fn
        self.dma = dma
        self.deps = set()
        self.sig = False
        self.sem = None
        self.val = 0


def _nop_fn(eng):
    return eng.nop()


class Prog:
    def __init__(self, nc, n_dma_sems=12):
        self.nc = nc
        self.lists = {e: [] for e in ENGS}
        self.last = {e: None for e in ENGS}
        self.n_dma_sems = n_dma_sems
        self.dma_count = {e: 0 for e in ENGS}
        self.dma_ring = {e: [None] * n_dma_sems for e in ENGS}

    def emit(self, eng, fn, reads=(), writes=(), dma=False, extra=()):
        ins = Instr(eng, fn, dma)
        self.count = getattr(self, "count", 0) + 1
        if STOPAT is not None and self.count > STOPAT and not getattr(self, "in_finish", False):
            return ins
        for d in reads:
            if d.w is not None:
                ins.deps.add(d.w)
            if d.excl:
                for r in d.r:
                    if r.eng != eng:
                        ins.deps.add(r)
            if not dma:
                d.r = [r for r in d.r if r.dma or r.eng != eng]
            d.r.append(ins)
        for d in writes:
            if d.w is not None:
                ins.deps.add(d.w)
            for r in d.r:
                ins.deps.add(r)
            d.w = ins
            d.r = []
        for p in extra:
            if p is not None:
                ins.deps.add(p)
        ins.deps.discard(ins)
        if dma:
            k = self.dma_count[eng]
            self.dma_count[eng] = k + 1
            slot = k % self.n_dma_sems
            prev = self.dma_ring[eng][slot]
            if prev is not None:
                ins.deps.add(prev)
            self.dma_ring[eng][slot] = ins
            ins.sem = (eng, slot)
        self.lists[eng].append(ins)
        self.last[eng] = ins
        return ins

    def barrier(self):
        ext = [self.last[e] for e in ENGS if self.last[e] is not None]
        for e in ENGS:
            for p in self.dma_ring[e]:
                if p is not None:
                    ext.append(p)
        b = self.emit("dve", lambda eng: eng.engine_nop(), extra=ext)
        for e in ENGS:
            if e != "dve":
                self.emit(e, _nop_fn, extra=[b])
        return b

    def finalize(self, stack):
        nc = self.nc
        for e in ENGS:
            for ins in self.lists[e]:
                for p in ins.deps:
                    if p.eng == "pe" and ins.eng == "pe" and not p.dma and not ins.dma:
                        continue
                    p.sig = True
        esem = {e: stack.enter_context(nc.semaphore("s_" + e)) for e in ENGS}
        dsem = {}
        for e in ENGS:
            for s in range(min(self.n_dma_sems, self.dma_count[e])):
                dsem[(e, s)] = stack.enter_context(nc.semaphore("d_%s_%d" % (e, s)))
        dcount = {k: 0 for k in dsem}
        for e in ENGS:
            c = 0
            for ins in self.lists[e]:
                if ins.dma:
                    dcount[ins.sem] += 16
                    ins.val = dcount[ins.sem]
                    ins.sig = True
                elif ins.sig:
                    c += 1
                    ins.val = c
                    ins.sem = e
        block = stack.enter_context(nc.Block())
        engobj = {"pe": "tensor", "act": "scalar", "dve": "vector", "pool": "gpsimd", "sp": "sync"}

        def make_body(e):
            def body(eng):
                known = {}
                for ins in self.lists[e]:
                    for p in ins.deps:
                        if p.eng == "pe" and e == "pe" and not p.dma and not ins.dma:
                            continue
                        if p.dma:
                            sem = dsem[p.sem]
                            key = ("d",) + p.sem
                        else:
                            sem = esem[p.eng]
                            key = ("e", p.eng)
                        if known.get(key, 0) >= p.val:
                            continue
                        known[key] = p.val
                        eng.wait_ge(sem, p.val)
                    r = ins.fn(eng)
                    if ins.dma:
                        r.then_inc(dsem[ins.sem], 16)
                    elif ins.sig:
                        r.then_inc(esem[e], 1)
            return body

        for e in ENGS:
            if self.lists[e]:
                getattr(block, engobj[e])(make_body(e))


class Arena:
    def __init__(self, ap_u8, nbytes):
        self.a = ap_u8
        self.n = nbytes
        self.top = 0

    def alloc(self, shape, dt):
        esz = {F32: 4, BF16: 2, I32: 4, U32: 4}[dt]
        free = 1
        for s in shape[1:]:
            free *= s
        nb = (free * esz + 63) // 64 * 64
        assert self.top + nb <= self.n, ("SBUF arena overflow", self.top, nb, self.n)
        v = self.a[0:shape[0], self.top:self.top + free * esz].bitcast(dt)
        self.top += nb
        if len(shape) == 3:
            v = v.rearrange("p (a b) -> p a b", b=shape[2])
        return v


class T:
    def __init__(self, ap, nsub=0):
        self.ap = ap
        self.d = Dep()
        self.ds = [Dep() for _ in range(nsub)]


def build_program():
    nc = bass.Bass("TRN2", target_bir_lowering=False)
    st = ExitStack()
    P = Prog(nc)

    def finish():
        P.in_finish = True
        P.barrier()
        P.finalize(st)
        st.close()
        return nc

    def din(name, shape, dt=F32):
        return nc.dram_tensor(name, list(shape), dt, kind="ExternalInput").ap()

    def dscr(name, shape, dt):
        return nc.dram_tensor(name, list(shape), dt, kind="ExternalOutput" if DEBUG else "Internal").ap()

    xin = din("xin", [NT, 128, 2048])
    cvec = din("cvec", [128, 16, 2])
    w_mod = din("w_mod", [128, 16, 12288])
    b_mod = din("b_mod", [1, 12288])
    nmix = din("nmix", [1, 2048])
    nffn = din("nffn", [1, 2048])
    nfin = din("nfin", [1, 2048])
    wg_ssm = din("wg_ssm", [4, 128, 16, 1296]) if LIMIT >= 2 else None
    wg_sc = din("wg_sc", [4, 128, 16, 1536]) if LIMIT >= 3 else None
    cw_ssm = din("cw_ssm", [4, 128, 6, 3])
    cb_ssm = din("cb_ssm", [4, 128, 6])
    cw_sc = din("cw_sc", [4, 128, 4, 3])
    scw = din("scw", [4, 128, 4])
    dtb = din("dtb", [4, 1, 16])
    alog = din("alog", [4, 1, 16])
    dsk = din("dsk", [4, 1, 512])
    ssm_nw = din("ssm_nw", [4, 1, 512])
    w_out = din("w_out", [2, 128, 32, 1024]) if LIMIT >= 4 else None
    w_rt = din("w_rt", [128, 16, 16])
    if LIMIT >= 6:
        w_gate = din("w_gate", [16, 2, 128, 16, 512])
        w_up = din("w_up", [16, 2, 128, 16, 512])
        w_down = din("w_down", [16, 2, 128, 8, 1024])
    c_ident = din("c_ident", [128, 128])
    c_tri = din("c_tri", [2, 128, 128])
    c_mask = din("c_mask", [2, 128, 512])
    out = nc.dram_tensor("out", [NTL, 128, 2048], F32, kind="ExternalOutput").ap()

    mod_d = T(dscr("mod_d", [2, 12288], F32))
    aT_d = T(dscr("aT_d", [9, 128, 16, 512], BF16), 9)
    z_d = T(dscr("z_d", [4, NTL, 128, 512], BF16), 4 * NTL)
    yT_d = T(dscr("yT_d", [NTL, 128, 32, 128], BF16), NTL)
    hacc_h = [T(dscr("hacc_d%d" % i, [NTL * 128, 1024], F32), NTL) for i in range(2)]
    f_d = T(dscr("f_d", [NTL * 128, 2048], BF16))
    dbg = {}
    if DEBUG:
        dbg["h1"] = nc.dram_tensor("dbg_h1", [NTL * 128, 2048], F32, kind="ExternalOutput").ap()
        dbg["aff"] = nc.dram_tensor("dbg_aff", [16, 4096], F32, kind="ExternalOutput").ap()
        dbg["vals"] = nc.dram_tensor("dbg_vals", [16, 512], F32, kind="ExternalOutput").ap()
        dbg["idx"] = nc.dram_tensor("dbg_idx", [16, 512], U32, kind="ExternalOutput").ap()
        dbg["rsc"] = nc.dram_tensor("dbg_rsc", [128, NTL], F32, kind="ExternalOutput").ap()
        dbg["dt"] = nc.dram_tensor("dbg_dt", [4, 128, NT, 16], F32, kind="ExternalOutput").ap()
        dbg["xs"] = nc.dram_tensor("dbg_xs", [4, 128, NT, 512], BF16, kind="ExternalOutput").ap()

    sb_bytes = (int(nc.sbuf_bytes_remaining) - 2048) // 256 * 256
    arena_t = st.enter_context(nc.sbuf_tensor("arena", [128, sb_bytes], U8))
    AR = Arena(arena_t[:, :], sb_bytes)
    banks = [st.enter_context(nc.psum_tensor("bank%d" % i, [128, 512], F32)) for i in range(8)]
    PS = [T(b[:, :]) for b in banks]
    for p_ in PS:
        p_.d.excl = True

    def psbf(i):
        return banks[i][:, :].bitcast(BF16)

    def MM(out, lhsT, rhs, start, stop, rd, wr):
        return P.emit("pe", lambda e: e.matmul(out, lhsT=lhsT, rhs=rhs, start=start, stop=stop), rd, wr)

    def TR(out, in_, ident, rd, wr):
        return P.emit("pe", lambda e: e.transpose(out, in_, ident), rd, wr)

    def ACT(out, in_, func, rd, wr, bias=None, scale=None, accum=None):
        kw = {}
        if bias is not None:
            kw["bias"] = bias
        if scale is not None:
            kw["scale"] = scale
        if accum is not None:
            kw["accum_out"] = accum
        return P.emit("act", lambda e: e.activation(out=out, in_=in_, func=func, **kw), rd, wr)

    def TT(eng, out, in0, in1, op, rd, wr):
        return P.emit(eng, lambda e: e.tensor_tensor(out=out, in0=in0, in1=in1, op=op), rd, wr)

    def TS(eng, out, in0, s1, s2, op0, op1, rd, wr):
        if s2 is None:
            return P.emit(eng, lambda e: e.tensor_scalar(out=out, in0=in0, scalar1=s1, scalar2=None, op0=op0), rd, wr)
        return P.emit(eng, lambda e: e.tensor_scalar(out=out, in0=in0, scalar1=s1, scalar2=s2, op0=op0, op1=op1), rd, wr)

    def STT(eng, out, in0, scalar, in1, op0, op1, rd, wr):
        return P.emit(eng, lambda e: e.scalar_tensor_tensor(out=out, in0=in0, scalar=scalar, in1=in1, op0=op0, op1=op1), rd, wr)

    def CP(eng, out, in_, rd, wr):
        if eng == "act":
            return P.emit("act", lambda e: e.copy(out=out, in_=in_), rd, wr)
        return P.emit(eng, lambda e: e.tensor_copy(out=out, in_=in_), rd, wr)

    def MEMSET(eng, ap, val, wr):
        return P.emit(eng, lambda e: e.memset(ap, val), (), wr)

    def RECIP(out, in_, rd, wr):
        return P.emit("dve", lambda e: e.reciprocal(out=out, in_=in_), rd, wr)

    def DMA(q, out, in_, rd, wr):
        return P.emit(q, lambda e: e.dma_start(out=out, in_=in_), rd, wr, dma=True)

    def rstd_from_ss(ss_ap, std_ap, r_ap, n, dss, dstd, dr):
        ACT(std_ap, ss_ap, AF.Sqrt, [dss], [dstd], bias=epsc.ap[0:ss_ap.shape[0], :], scale=1.0 / n)
        RECIP(r_ap, std_ap, [dstd], [dr])

    ident32 = T(AR.alloc([128, 128], F32))
    identbf = T(AR.alloc([128, 128], BF16))
    ones32 = T(AR.alloc([128, 128], F32))
    onesbf = T(AR.alloc([128, 8], BF16))
    epsc = T(AR.alloc([128, 1], F32))
    DMA("sp", ident32.ap, c_ident, [], [ident32.d])
    DMA("pool", identbf.ap, c_ident, [], [identbf.d])
    MEMSET("dve", ones32.ap, 1.0, [ones32.d])
    MEMSET("dve", onesbf.ap, 1.0, [onesbf.d])
    MEMSET("dve", epsc.ap, EPS, [epsc.d])
    persist_mark = AR.top

    cv = T(AR.alloc([128, 16, 2], F32))
    cs_ = T(AR.alloc([128, 16, 2], F32))
    DMA("sp", cv.ap, cvec, [], [cv.d])
    ACT(cs_.ap, cv.ap, AF.Silu, [cv.d], [cs_.d])
    wch = [T(AR.alloc([128, 16, 512], F32)) for _ in range(3)]
    bch = [T(AR.alloc([2, 512], F32)) for _ in range(2)]
    mch = [T(AR.alloc([2, 512], F32)) for _ in range(2)]
    def p0_load(n_):
        w_ = wch[n_ % 3]
        cols_ = slice(n_ * 512, (n_ + 1) * 512)
        DMA("sp", w_.ap[:, 0:8, :], w_mod[:, 0:8, cols_], [], [w_.d])
        DMA("act", w_.ap[:, 8:16, :], w_mod[:, 8:16, cols_], [], [w_.d])

    p0_load(0)
    p0_load(1)
    for n in range(24):
        w = wch[n % 3]
        cols = slice(n * 512, (n + 1) * 512)
        if n + 2 < 24:
            p0_load(n + 2)
        bb = bch[n % 2]
        DMA("sp", bb.ap, b_mod[0:1, cols].partition_broadcast(2), [], [bb.d])
        pb = PS[n % 2]
        for kc in range(16):
            MM(pb.ap[0:2, :], cs_.ap[:, kc, :], w.ap[:, kc, :], kc == 0, kc == 15, [cs_.d, w.d], [pb.d])
        m = mch[n % 2]
        TT("dve", m.ap, pb.ap[0:2, :], bb.ap, ALU.add, [pb.d, bb.d], [m.d])
        if n // 4 in (1, 4):
            TS("dve", m.ap, m.ap, 1.0, None, ALU.add, None, [m.d], [m.d])
        DMA("sp", mod_d.ap[:, cols], m.ap, [m.d], [mod_d.d])
    P.barrier()
    if LIMIT == 0:
        return finish()
    AR.top = persist_mark

    def load_mod_bc(dst, row, seg, q="sp"):
        DMA(q, dst.ap, mod_d.ap[row:row + 1, seg * 2048:(seg + 1) * 2048].partition_broadcast(128), [mod_d.d], [dst.d])

    def load_vec_bc(dst, src, q="sp"):
        DMA(q, dst.ap, src.partition_broadcast(128), [], [dst.d])

    A1 = [T(AR.alloc([128, 2048], F32)) for _ in range(2)]
    B1 = [T(AR.alloc([128, 2048], F32)) for _ in range(2)]
    nmb = T(AR.alloc([128, 2048], F32))
    load_vec_bc(nmb, nmix)
    for r in range(2):
        load_mod_bc(A1[r], r, 1)
        load_mod_bc(B1[r], r, 0)
        TT("pool", A1[r].ap, A1[r].ap, nmb.ap, ALU.mult, [A1[r].d, nmb.d], [A1[r].d])
    xt = [T(AR.alloc([128, 2048], F32)) for _ in range(2)]
    tmp = [T(AR.alloc([128, 2048], F32)) for _ in range(2)]
    abf = [T(AR.alloc([128, 2048], BF16)) for _ in range(2)]
    junk = T(AR.alloc([128, 2048], BF16))
    ss1 = T(AR.alloc([128, NT], F32), NT)
    sd1 = T(AR.alloc([128, NT], F32), NT)
    rs1 = T(AR.alloc([128, NT], F32), NT)
    aTb = [T(AR.alloc([128, 16, 512], BF16)) for _ in range(2)]
    MEMSET("dve", ss1.ap, 0.0, ss1.ds)
    order1 = []
    for tb in range(9):
        tiles = [0, 1] if tb == 0 else [2 + 4 * (tb - 1) + i for i in range(4)]
        for j, t in enumerate(tiles):
            order1.append((tb, j, t, j == len(tiles) - 1, 128 * len(tiles)))

    def p1_load(t):
        DMA("sp", xt[t % 2].ap, xin[t], [], [xt[t % 2].d])

    def p1_stats(t):
        x_ = xt[t % 2]
        ACT(junk.ap, x_.ap, AF.Square, [x_.d], [junk.d, ss1.ds[t]], accum=ss1.ap[:, t:t + 1])
        rstd_from_ss(ss1.ap[:, t:t + 1], sd1.ap[:, t:t + 1], rs1.ap[:, t:t + 1], 2048.0, ss1.ds[t], sd1.ds[t], rs1.ds[t])

    def p1_stt(t_):
        r_ = 1 if t_ < 2 else 0
        STT("dve", tmp[t_ % 2].ap, xt[t_ % 2].ap, rs1.ap[:, t_:t_ + 1], A1[r_].ap, ALU.mult, ALU.mult,
            [xt[t_ % 2].d, rs1.ds[t_], A1[r_].d], [tmp[t_ % 2].d])

    p1_load(0)
    p1_stats(0)
    for i1, (tb, j, t, lastj, ntok) in enumerate(order1):
        ab = aTb[tb % 2]
        r = 1 if t < 2 else 0
        x_ = xt[t % 2]
        tm = tmp[t % 2]
        if i1 == 0:
            p1_stt(t)
        if i1 + 1 < len(order1):
            p1_load(order1[i1 + 1][2])
            p1_stats(order1[i1 + 1][2])
        a_ = abf[t % 2]
        TT("pool", a_.ap[:, 0:1024], tm.ap[:, 0:1024], B1[r].ap[:, 0:1024], ALU.add, [tm.d, B1[r].d], [a_.d])
        TT("dve", a_.ap[:, 1024:2048], tm.ap[:, 1024:2048], B1[r].ap[:, 1024:2048], ALU.add, [tm.d, B1[r].d], [a_.d])
        if i1 + 1 < len(order1):
            p1_stt(order1[i1 + 1][2])
        for q in range(4):
            pb = PS[2 + (q % 2)]
            pv = psbf(2 + (q % 2))
            for i in range(4):
                kc = 4 * q + i
                TR(pv[:, i * 128:(i + 1) * 128], a_.ap[:, kc * 128:(kc + 1) * 128], identbf.ap, [a_.d, identbf.d], [pb.d])
            CP("act" if q % 2 == 0 else "dve", ab.ap[:, 4 * q:4 * q + 4, j * 128:(j + 1) * 128],
               pv[:, 0:512].rearrange("p (a b) -> p a b", b=128), [pb.d], [ab.d])
        if lastj:
            DMA("sp", aT_d.ap[tb][:, :, 0:ntok], ab.ap[:, :, 0:ntok], [ab.d], [aT_d.ds[tb]])
    P.barrier()
    if LIMIT == 1:
        return finish()
    AR.top = persist_mark

    NPRE = 84 if LIMIT >= 6 else 0
    srcs_all = []
    if LIMIT >= 6:
        for e_ in range(16):
            srcs_all += [(w_gate[e_, 0], 0), (w_up[e_, 0], 0), (w_gate[e_, 1], 0), (w_up[e_, 1], 0),
                         (w_down[e_, 0], 1), (w_down[e_, 1], 1)]
    wexp = T(nc.dram_tensor("wexp_bf", [max(NPRE, 1), 128, 8192], BF16, kind="Internal").ap(), max(NPRE, 1))

    def precast(lo_, hi_):
        for i in range(lo_, min(hi_, NPRE)):
            src_, kind_ = srcs_all[i]
            if kind_ == 0:
                dv = wexp.ap[i].rearrange("p (a b) -> p a b", b=512)
                DMA("pool", dv[:, 0:8, :], src_[:, 0:8, :], [], [wexp.ds[i]])
                DMA("pool", dv[:, 8:16, :], src_[:, 8:16, :], [], [wexp.ds[i]])
            else:
                dv = wexp.ap[i].rearrange("p (a b) -> p a b", b=1024)
                DMA("pool", dv[:, 0:4, :], src_[:, 0:4, :], [], [wexp.ds[i]])
                DMA("pool", dv[:, 4:8, :], src_[:, 4:8, :], [], [wexp.ds[i]])

    tri = [T(AR.alloc([128, 128], F32)) for _ in range(2)]
    maskn = [T(AR.alloc([128, 512], BF16)) for _ in range(2)]
    for d_ in range(2):
        DMA("sp", tri[d_].ap, c_tri[d_], [], [tri[d_].d])
        DMA("pool", maskn[d_].ap, c_mask[d_], [], [maskn[d_].d])
    xs = T(AR.alloc([128, NT, 512], BF16), NT)
    Btok = T(AR.alloc([128, NT, 128], BF16), NT)
    BT = T(AR.alloc([128, NT * 128], BF16), NT)
    CT = T(AR.alloc([128, NT * 128], BF16), NT)
    dtr = T(AR.alloc([128, NT, 16], F32))
    dtv = T(AR.alloc([128, NT, 16], F32))
    lav = T(AR.alloc([128, NT, 16], F32))
    cwt = T(AR.alloc([128, 6, 3], F32))
    cbt = T(AR.alloc([128, 6], F32))
    dtbb = T(AR.alloc([128, 16], F32))
    aneg = T(AR.alloc([128, 16], F32))
    Dbc = T(AR.alloc([128, 512], F32))
    nwb = T(AR.alloc([128, 512], F32))
    p2_mark = AR.top

    for g in range(NG):
        DMA("sp", cwt.ap, cw_ssm[g], [], [cwt.d])
        DMA("sp", cbt.ap, cb_ssm[g], [], [cbt.d])
        load_vec_bc(dtbb, dtb[g])
        load_vec_bc(aneg, alog[g])
        load_vec_bc(Dbc, dsk[g])
        load_vec_bc(nwb, ssm_nw[g])
        ACT(aneg.ap, aneg.ap, AF.Exp, [aneg.d], [aneg.d])
        TS("dve", aneg.ap, aneg.ap, -1.0, None, ALU.mult, None, [aneg.d], [aneg.d])
        wg = T(AR.alloc([128, 16, 1296], BF16))
        for k2 in range(8):
            DMA("pool", wg.ap[:, 2 * k2:2 * k2 + 2, :], wg_ssm[g][:, 2 * k2:2 * k2 + 2, :], [], [wg.d])
        precast(36 + g * 12, 36 + (g + 1) * 12)
        aTl = [T(AR.alloc([128, 16, 512], BF16)) for _ in range(2)]
        acc = [T(AR.alloc([128, 512], F32)) for _ in range(2)]
        xTt = [[T(AR.alloc([128, 512], BF16)) for _ in range(4)] for _ in range(2)]
        zst = [T(AR.alloc([128, 512], BF16)) for _ in range(2)]
        fmi = 0
        zi = 0
        MARKS.setdefault('A', P.count)
        for tb in range(9):
            ntok = 256 if tb == 0 else 512
            W = 256 if tb == 0 else 64
            tiles = [0, 1] if tb == 0 else [2 + 4 * (tb - 1) + i for i in range(4)]
            tok0 = tiles[0] * 128
            if tb == 1:
                MARKS.setdefault('C', P.count)
            if tb == 2:
                MARKS.setdefault('D', P.count)
            al = aTl[tb % 2]
            if tb == 0:
                DMA("sp", al.ap[:, :, 0:ntok], aT_d.ap[tb][:, :, 0:ntok], [aT_d.ds[tb]], [al.d])
            if tb + 1 < 9:
                DMA("sp", aTl[(tb + 1) % 2].ap, aT_d.ap[tb + 1], [aT_d.ds[tb + 1]], [aTl[(tb + 1) % 2].d])
            xTb = xTt[tb % 2]
            for oc in range(6):
                pb = PS[fmi % 2]
                fmi += 1
                for kc in range(16):
                    MM(pb.ap[:, 0:ntok], wg.ap[:, kc, oc * 128:(oc + 1) * 128], al.ap[:, kc, 0:ntok], kc == 0, kc == 15,
                       [wg.d, al.d], [pb.d])
                ac = acc[oc % 2]
                ACT(ac.ap[:, 0:ntok], pb.ap[:, 0:ntok], AF.Identity, [pb.d, cwt.d, cbt.d], [ac.d],
                    bias=cbt.ap[:, oc:oc + 1], scale=cwt.ap[:, oc, 1:2])
                u3 = pb.ap[:, 0:ntok].rearrange("p (r w) -> p r w", w=W)
                a3 = ac.ap[:, 0:ntok].rearrange("p (r w) -> p r w", w=W)
                STT("dve", a3[:, :, 1:W], u3[:, :, 0:W - 1], cwt.ap[:, oc, 0:1], a3[:, :, 1:W], ALU.mult, ALU.add,
                    [pb.d, cwt.d, ac.d], [ac.d])
                STT("dve", a3[:, :, 0:W - 1], u3[:, :, 1:W], cwt.ap[:, oc, 2:3], a3[:, :, 0:W - 1], ALU.mult, ALU.add,
                    [pb.d, cwt.d, ac.d], [ac.d])
                if oc < 4:
                    ACT(xTb[oc].ap[:, 0:ntok], ac.ap[:, 0:ntok], AF.Silu, [ac.d], [xTb[oc].d])
                elif oc == 4:
                    ACT(BT.ap[:, tok0:tok0 + ntok], ac.ap[:, 0:ntok], AF.Silu, [ac.d], [BT.ds[t] for t in tiles])
                else:
                    ACT(CT.ap[:, tok0:tok0 + ntok], ac.ap[:, 0:ntok], AF.Silu, [ac.d], [CT.ds[t] for t in tiles])
            MARKS.setdefault('B', P.count)
            for j, t in enumerate(tiles):
                pb = PS[2 + (t % 2)]
                pv = psbf(2 + (t % 2))
                for oc in range(4):
                    TR(pv[:, oc * 128:(oc + 1) * 128], xTb[oc].ap[:, j * 128:(j + 1) * 128], identbf.ap,
                       [xTb[oc].d, identbf.d], [pb.d])
                TR(psbf(7)[:, 0:128], BT.ap[:, t * 128:(t + 1) * 128], identbf.ap, [BT.ds[t], identbf.d], [PS[7].d])
                CP("dve", xs.ap[:, t, :], pv[:, 0:512], [pb.d], [xs.ds[t]])
                CP("act", Btok.ap[:, t, :], psbf(7)[:, 0:128], [PS[7].d], [Btok.ds[t]])
                if t >= 2:
                    pz = PS[4 + (zi % 2)]
                    zz = zst[zi % 2]
                    zi += 1
                    for kc in range(16):
                        MM(pz.ap, al.ap[:, kc, j * 128:(j + 1) * 128], wg.ap[:, kc, 768:1280], kc == 0, kc == 15,
                           [al.d, wg.d], [pz.d])
                    ACT(zz.ap, pz.ap, AF.Silu, [pz.d], [zz.d])
                    DMA("sp", z_d.ap[g, t - 2], zz.ap, [zz.d], [z_d.ds[g * NTL + t - 2]])
                pd = PS[6]
                for kc in range(16):
                    MM(pd.ap[:, 0:16], al.ap[:, kc, j * 128:(j + 1) * 128], wg.ap[:, kc, 1280:1296], kc == 0, kc == 15,
                       [al.d, wg.d], [pd.d])
                TT("dve", dtr.ap[:, t, :], pd.ap[:, 0:16], dtbb.ap, ALU.add, [pd.d, dtbb.d], [dtr.d])
        MARKS.setdefault('E', P.count)
        ACT(dtv.ap, dtr.ap, AF.Exp, [dtr.d], [dtv.d])
        ACT(dtv.ap, dtv.ap, AF.Ln, [dtv.d], [dtv.d], bias=ones32.ap[:, 0:1])
        TT("dve", lav.ap, dtv.ap, aneg.ap.unsqueeze(1).to_broadcast([128, NT, 16]), ALU.mult, [dtv.d, aneg.d], [lav.d])
        if DEBUG:
            DMA("sp", dbg["dt"][g], dtv.ap, [dtv.d], [])
            DMA("sp", dbg["xs"][g], xs.ap, xs.ds, [])
        P.barrier()
        AR.top = p2_mark

        yacc = T(AR.alloc([128, NTL, 512], BF16), NTL)
        S32 = T(AR.alloc([128, 512], F32))
        Sbf = T(AR.alloc([128, 512], BF16))
        rc = [T(AR.alloc([128, 8, 128], F32)) for _ in range(2)]
        Lall = [T(AR.alloc([128, 8, 128], BF16), 8) for _ in range(2)]
        Mall = [T(AR.alloc([128, 8, 128], BF16), 8) for _ in range(2)]
        cbT = [T(AR.alloc([128, 128], BF16)) for _ in range(2)]
        smal = [T(AR.alloc([128, 32], F32)) for _ in range(2)]
        xw = [T(AR.alloc([128, 512], BF16)) for _ in range(2)]
        tA = [T(AR.alloc([128, 512], F32)) for _ in range(2)]
        tB = [T(AR.alloc([128, 512], F32)) for _ in range(2)]
        zt = [T(AR.alloc([128, 512], BF16)) for _ in range(2)]
        ynb = [T(AR.alloc([128, 512], BF16)) for _ in range(2)]
        yTo = [T(AR.alloc([128, 4, 128], BF16)) for _ in range(2)]
        gst = [T(AR.alloc([128, 4], F32)) for _ in range(2)]
        junk2 = T(AR.alloc([128, 512], BF16))
        ssg = T(AR.alloc([128, NTL], F32), NTL)
        sdg = T(AR.alloc([128, NTL], F32))
        rsg = T(AR.alloc([128, NTL], F32))
        MEMSET("pool", ssg.ap, 0.0, ssg.ds)
        def stageA(d_, c, k):
            lat = c >= 2
            last = 127 if d_ == 0 else 0
            la_c = lav.ap[:, c, d_ * 8:(d_ + 1) * 8]
            dt_c = dtv.ap[:, c, d_ * 8:(d_ + 1) * 8]
            sm = smal[k]
            pS = PS[7]
            MM(pS.ap[:, 0:8], tri[d_].ap, la_c, True, True, [tri[d_].d, lav.d], [pS.d])
            MM(pS.ap[:, 8:16], ones32.ap, la_c, True, True, [ones32.d, lav.d], [pS.d])
            TT("dve", rc[k].ap, tri[d_].ap.unsqueeze(1).to_broadcast([128, 8, 128]),
               la_c.unsqueeze(2).to_broadcast([128, 8, 128]), ALU.mult, [tri[d_].d, lav.d], [rc[k].d])
            Rb = [PS[0], PS[1]]
            for hf in range(2):
                MM(Rb[hf].ap, ones32.ap, rc[k].ap[:, 4 * hf:4 * hf + 4, :].rearrange("p a b -> p (a b)"), True, False,
                   [ones32.d, rc[k].d], [Rb[hf].d])
                MM(Rb[hf].ap, identbf.ap, maskn[d_].ap, False, True, [identbf.d, maskn[d_].d], [Rb[hf].d])
            ACT(sm.ap[:, 0:8], pS.ap[:, 0:8], AF.Identity, [pS.d], [sm.d], scale=-1.0)
            ACT(sm.ap[:, 8:24], pS.ap[:, 0:16], AF.Exp, [pS.d], [sm.d])
            if lat:
                MM(pS.ap[:, 128:256], BT.ap[:, c * 128:(c + 1) * 128], CT.ap[:, c * 128:(c + 1) * 128], True, True,
                   [BT.ds[c], CT.ds[c]], [pS.d])
                CP("act", cbT[k].ap, pS.ap[:, 128:256], [pS.d], [cbT[k].d])
            La = Lall[k]
            for h in range(8):
                ACT(La.ap[:, h, :], Rb[h // 4].ap[:, (h % 4) * 128:(h % 4 + 1) * 128], AF.Exp, [Rb[h // 4].d, sm.d],
                    [La.ds[h]], bias=sm.ap[:, h:h + 1])
            TT("dve", sm.ap[:, 24:32], La.ap[:, :, last], dt_c, ALU.mult, La.ds + [dtv.d], [sm.d])
            TT("pool", xw[k].ap.rearrange("p (h d) -> p h d", d=64), xs.ap[:, c, :].rearrange("p (h d) -> p h d", d=64),
               sm.ap[:, 24:32].unsqueeze(2).to_broadcast([128, 8, 64]), ALU.mult, [xs.ds[c], sm.d], [xw[k].d])
            if lat:
                Ma = Mall[k]
                for h in range(8):
                    STT("dve", Ma.ap[:, h, :], La.ap[:, h, :], dt_c[:, h:h + 1], cbT[k].ap, ALU.mult, ALU.mult,
                        [La.ds[h], dtv.d, cbT[k].d], [Ma.ds[h]])
                for h in range(8):
                    MM(PS[2 + k].ap[:, h * 64:(h + 1) * 64], Ma.ap[:, h, :], xs.ap[:, c, h * 64:(h + 1) * 64], True, True,
                       [Ma.ds[h], xs.ds[c]], [PS[2 + k].d])
            MM(PS[4 + k].ap, Btok.ap[:, c, :], xw[k].ap, True, True, [Btok.ds[c], xw[k].d], [PS[4 + k].d])

        def stageB(d_, c, k):
            lat = c >= 2
            sm = smal[k]
            Pd, Ps_, Po = PS[2 + k], PS[4 + k], PS[6]
            if lat:
                MM(Po.ap, CT.ap[:, c * 128:(c + 1) * 128], Sbf.ap, True, True, [CT.ds[c], Sbf.d], [Po.d])
                TT("dve", tB[k].ap.rearrange("p (h d) -> p h d", d=64), Po.ap.rearrange("p (h d) -> p h d", d=64),
                   sm.ap[:, 8:16].unsqueeze(2).to_broadcast([128, 8, 64]), ALU.mult, [Po.d, sm.d], [tB[k].d])
                TT("dve", tB[k].ap, tB[k].ap, Pd.ap, ALU.add, [tB[k].d, Pd.d], [tB[k].d])
            TT("dve", S32.ap.rearrange("p (h d) -> p h d", d=64), S32.ap.rearrange("p (h d) -> p h d", d=64),
               sm.ap[:, 16:24].unsqueeze(2).to_broadcast([128, 8, 64]), ALU.mult, [S32.d, sm.d], [S32.d])
            TT("dve", S32.ap, S32.ap, Ps_.ap, ALU.add, [S32.d, Ps_.d], [S32.d])
            CP("act", Sbf.ap, S32.ap, [S32.d], [Sbf.d])
            if lat and d_ == 0:
                TT("pool", tA[k].ap, xs.ap[:, c, :], Dbc.ap, ALU.mult, [xs.ds[c], Dbc.d], [tA[k].d])
                TT("pool", yacc.ap[:, c - 2, :], tA[k].ap, tB[k].ap, ALU.add, [tA[k].d, tB[k].d], [yacc.ds[c - 2]])
            if lat and d_ == 1:
                DMA("sp", zt[k].ap, z_d.ap[g, c - 2], [z_d.ds[g * NTL + c - 2]], [zt[k].d])
                TT("pool", tA[k].ap, tB[k].ap, yacc.ap[:, c - 2, :], ALU.add, [tB[k].d, yacc.ds[c - 2]], [tA[k].d])
                TT("pool", yacc.ap[:, c - 2, :], tA[k].ap, zt[k].ap, ALU.mult, [tA[k].d, zt[k].d], [yacc.ds[c - 2]])
                ACT(junk2.ap, yacc.ap[:, c - 2, :], AF.Square, [yacc.ds[c - 2]], [junk2.d, ssg.ds[c - 2]],
                    accum=ssg.ap[:, c - 2:c - 1])

        def stageC(cl, k):
            STT("dve", ynb[k].ap, yacc.ap[:, cl, :], rsg.ap[:, cl:cl + 1], nwb.ap, ALU.mult, ALU.mult,
                [yacc.ds[cl], rsg.d, nwb.d], [ynb[k].d])
            pv = psbf(6 + k)
            for q in range(4):
                TR(pv[:, q * 128:(q + 1) * 128], ynb[k].ap[:, q * 128:(q + 1) * 128], identbf.ap,
                   [ynb[k].d, identbf.d], [PS[6 + k].d])
            CP("act", yTo[k].ap, pv[:, 0:512].rearrange("p (a b) -> p a b", b=128), [PS[6 + k].d], [yTo[k].d])
            DMA("sp", yT_d.ap[cl][:, g * 4:(g + 1) * 4, :], yTo[k].ap, [yTo[k].d], [yT_d.ds[cl]])

        items = []
        for d_ in range(SCAN):
            order = list(range(NT)) if d_ == 0 else [1, 0] + list(range(NT - 1, 1, -1))
            items += [(d_, c, ci == 0) for ci, c in enumerate(order)]
        if items:
            stageA(items[0][0], items[0][1], 0)
        for i_, (d_, c, first) in enumerate(items):
            if i_ + 1 < len(items):
                stageA(items[i_ + 1][0], items[i_ + 1][1], (i_ + 1) % 2)
            if first:
                MEMSET("dve", S32.ap, 0.0, [S32.d])
                MEMSET("pool", Sbf.ap, 0.0, [Sbf.d])
            stageB(d_, c, i_ % 2)
        if SCAN == 2:
            ACT(sdg.ap, ssg.ap, AF.Sqrt, ssg.ds, [sdg.d], bias=epsc.ap, scale=1.0 / 512.0)
            RECIP(rsg.ap, sdg.ap, [sdg.d], [rsg.d])
            for cl in range(NTL):
                stageC(cl, cl % 2)
        P.barrier()
        AR.top = p2_mark
    AR.top = persist_mark
    if LIMIT == 2:
        return finish()

    ss_sc = T(AR.alloc([128, NTL], F32))
    rs_sc = T(AR.alloc([128, NTL], F32))
    sd_sc = T(AR.alloc([128, NTL], F32))
    MEMSET("dve", ss_sc.ap, 0.0, [ss_sc.d])
    p3_mark = AR.top
    wscs = [T(AR.alloc([128, 16, 1536], BF16)) for _ in range(2)]
    cwcs = [T(AR.alloc([128, 4, 3], F32)) for _ in range(2)]
    scwts = [T(AR.alloc([128, 4], F32)) for _ in range(2)]

    def p3_wload(jb):
        DMA("sp", cwcs[jb % 2].ap, cw_sc[jb], [], [cwcs[jb % 2].d])
        DMA("sp", scwts[jb % 2].ap, scw[jb], [], [scwts[jb % 2].d])
        for k2 in range(8):
            DMA("pool", wscs[jb % 2].ap[:, 2 * k2:2 * k2 + 2, :], wg_sc[jb][:, 2 * k2:2 * k2 + 2, :], [], [wscs[jb % 2].d])
    aTl = [T(AR.alloc([128, 16, 512], BF16)) for _ in range(2)]
    cgs = [T(AR.alloc([128, 512], F32)) for _ in range(2)]
    vv = [T(AR.alloc([128, 512], F32)) for _ in range(2)]
    acc = [T(AR.alloc([128, 512], F32)) for _ in range(2)]
    q2 = [T(AR.alloc([128, 512], BF16)) for _ in range(4)]
    qw = [T(AR.alloc([128, 512], BF16)) for _ in range(4)]
    it = 0
    p3_wload(0)
    p3i = 0
    DMA("sp", aTl[0].ap, aT_d.ap[1], [aT_d.ds[1]], [aTl[0].d])
    for jb in range(4):
        wsc, cwc, scwt = wscs[jb % 2], cwcs[jb % 2], scwts[jb % 2]
        if jb + 1 < 4:
            p3_wload(jb + 1)
        for tb in range(1, 9):
            t0 = 4 * (tb - 1)
            al = aTl[p3i % 2]
            p3i += 1
            if p3i < 32:
                ntb = (p3i % 8) + 1
                DMA("sp", aTl[p3i % 2].ap, aT_d.ap[ntb], [aT_d.ds[ntb]], [aTl[p3i % 2].d])
            for cc in range(4):
                k = it % 2
                it += 1
                pbg, pcg, phv = PS[0 + 3 * k], PS[1 + 3 * k], PS[2 + 3 * k]
                for (pb, off) in ((pbg, 0), (pcg, 512), (phv, 1024)):
                    for kc in range(16):
                        MM(pb.ap, wsc.ap[:, kc, off + cc * 128:off + (cc + 1) * 128], al.ap[:, kc, :], kc == 0, kc == 15,
                           [wsc.d, al.d], [pb.d])
                CP("act", cgs[k].ap, pcg.ap, [pcg.d], [cgs[k].d])
                TT("dve", vv[k].ap, cgs[k].ap, phv.ap, ALU.mult, [cgs[k].d, phv.d], [vv[k].d])
                ACT(acc[k].ap, vv[k].ap, AF.Identity, [vv[k].d, cwc.d], [acc[k].d], scale=cwc.ap[:, cc, 1:2])
                v3 = vv[k].ap.rearrange("p (r w) -> p r w", w=64)
                a3 = acc[k].ap.rearrange("p (r w) -> p r w", w=64)
                STT("dve", a3[:, :, 1:64], v3[:, :, 0:63], cwc.ap[:, cc, 0:1], a3[:, :, 1:64], ALU.mult, ALU.add,
                    [vv[k].d, cwc.d, acc[k].d], [acc[k].d])
                STT("dve", a3[:, :, 0:63], v3[:, :, 1:64], cwc.ap[:, cc, 2:3], a3[:, :, 0:63], ALU.mult, ALU.add,
                    [vv[k].d, cwc.d, acc[k].d], [acc[k].d])
                TT("dve", acc[k].ap, acc[k].ap, pbg.ap, ALU.mult, [acc[k].d, pbg.d], [acc[k].d])
                ACT(q2[cc].ap, acc[k].ap, AF.Square, [acc[k].d], [q2[cc].d])
                TS("pool", qw[cc].ap, acc[k].ap, scwt.ap[:, cc:cc + 1], None, ALU.mult, None, [acc[k].d, scwt.d], [qw[cc].d])
                DMA("sp", yT_d.ap[t0:t0 + 4, :, 16 + jb * 4 + cc, :].rearrange("t p k -> p t k"),
                    qw[cc].ap.rearrange("p (t k) -> p t k", k=128), [qw[cc].d], [yT_d.ds[t0 + i] for i in range(4)])
            pss = PS[6 + (tb % 2)]
            for i in range(4):
                for cc in range(4):
                    MM(pss.ap[:, i:i + 1], q2[cc].ap[:, i * 128:(i + 1) * 128], onesbf.ap[:, 0:1], cc == 0, cc == 3,
                       [q2[cc].d, onesbf.d], [pss.d])
            TT("dve", ss_sc.ap[:, t0:t0 + 4], ss_sc.ap[:, t0:t0 + 4], pss.ap[:, 0:4], ALU.add, [ss_sc.d, pss.d], [ss_sc.d])
    rstd_from_ss(ss_sc.ap, sd_sc.ap, rs_sc.ap, 2048.0, ss_sc.d, sd_sc.d, rs_sc.d)
    if DEBUG:
        DMA("sp", dbg["rsc"], rs_sc.ap, [rs_sc.d], [])
    P.barrier()
    if LIMIT == 3:
        return finish()
    AR.top = p3_mark

    g1b = T(AR.alloc([128, 2048], F32))
    load_mod_bc(g1b, 0, 2)
    wos = [T(AR.alloc([128, 32, 1024], BF16)) for _ in range(2)]
    yt = [T(AR.alloc([128, 32, 128], BF16)) for _ in range(2)]
    xh = [T(AR.alloc([128, 1024], F32)) for _ in range(2)]
    t1 = [T(AR.alloc([128, 512], F32)) for _ in range(2)]
    ht = [T(AR.alloc([128, 1024], F32)) for _ in range(2)]
    it = 0
    for hf in range(2):
        for k4 in range(8):
            DMA("pool", wos[hf].ap[:, 4 * k4:4 * k4 + 4, :], w_out[hf][:, 4 * k4:4 * k4 + 4, :], [], [wos[hf].d])

    def p4_load(i4):
        hf_, t_ = i4 // NTL, i4 % NTL
        DMA("sp", yt[t_ % 2].ap, yT_d.ap[t_], [yT_d.ds[t_]], [yt[t_ % 2].d])
        DMA("sp", xh[t_ % 2].ap, xin[t_ + 2][:, hf_ * 1024:(hf_ + 1) * 1024], [], [xh[t_ % 2].d])

    p4_load(0)
    for hf in range(2):
        wo = wos[hf]
        for t in range(NTL):
            y_ = yt[t % 2]
            x_ = xh[t % 2]
            if hf * NTL + t + 1 < 2 * NTL:
                p4_load(hf * NTL + t + 1)
            h_ = ht[t % 2]
            for q in range(2):
                k = it % 2
                it += 1
                pm, pc = PS[0 + 2 * k], PS[1 + 2 * k]
                cols = slice(q * 512, (q + 1) * 512)
                for kc in range(16):
                    MM(pm.ap, y_.ap[:, kc, :], wo.ap[:, kc, cols], kc == 0, kc == 15, [y_.d, wo.d], [pm.d])
                for kc in range(16, 32):
                    MM(pc.ap, y_.ap[:, kc, :], wo.ap[:, kc, cols], kc == 16, kc == 31, [y_.d, wo.d], [pc.d])
                ACT(t1[k].ap, pc.ap, AF.Identity, [pc.d, rs_sc.d], [t1[k].d], scale=rs_sc.ap[:, t:t + 1])
                TT("dve", t1[k].ap, t1[k].ap, pm.ap, ALU.add, [t1[k].d, pm.d], [t1[k].d])
                gcols = slice(hf * 1024 + q * 512, hf * 1024 + (q + 1) * 512)
                TT("pool", t1[k].ap, t1[k].ap, g1b.ap[:, gcols], ALU.mult, [t1[k].d, g1b.d], [t1[k].d])
                TT("pool", h_.ap[:, cols], t1[k].ap, x_.ap[:, cols], ALU.add, [t1[k].d, x_.d], [h_.d])
            DMA("sp", hacc_h[hf].ap[t * 128:(t + 1) * 128, :], h_.ap, [h_.d], [hacc_h[hf].ds[t]])
    P.barrier()
    if LIMIT == 4:
        return finish()
    AR.top = persist_mark

    idxc = T(AR.alloc([128, 4, 16], I32))
    gatc = T(AR.alloc([128, 4, 16], F32))
    p5_mark = AR.top
    A2 = T(AR.alloc([128, 2048], F32))
    B2 = T(AR.alloc([128, 2048], F32))
    nfb = T(AR.alloc([128, 2048], F32))
    load_vec_bc(nfb, nffn)
    load_mod_bc(A2, 0, 4)
    load_mod_bc(B2, 0, 3)
    TT("pool", A2.ap, A2.ap, nfb.ap, ALU.mult, [A2.d, nfb.d], [A2.d])
    wr = T(AR.alloc([128, 16, 16], F32))
    DMA("sp", wr.ap, w_rt, [], [wr.d])
    affT = T(AR.alloc([16, 4096], F32))
    hx = [T(AR.alloc([128, 2048], F32)) for _ in range(2)]
    ff = [T(AR.alloc([128, 2048], F32)) for _ in range(2)]
    fb = [T(AR.alloc([128, 2048], BF16)) for _ in range(2)]
    fT = [T(AR.alloc([128, 16, 128], F32)) for _ in range(2)]
    st5 = [T(AR.alloc([128, 8], F32)) for _ in range(2)]
    ex = [T(AR.alloc([128, 16], F32)) for _ in range(2)]
    junk5 = T(AR.alloc([128, 2048], BF16))
    def p5_load(t_):
        for i2 in range(2):
            DMA("sp", hx[t_ % 2].ap[:, i2 * 1024:(i2 + 1) * 1024], hacc_h[i2].ap[t_ * 128:(t_ + 1) * 128, :],
                [hacc_h[i2].ds[t_]], [hx[t_ % 2].d])

    p5_load(0)
    for t in range(NTL):
        k = t % 2
        h_ = hx[k]
        if t + 1 < NTL:
            p5_load(t + 1)
        if DEBUG:
            DMA("sp", dbg["h1"][t * 128:(t + 1) * 128, :], h_.ap, [h_.d], [])
        s5 = st5[k]
        MEMSET("pool", s5.ap, 0.0, [s5.d])
        ACT(junk5.ap, h_.ap, AF.Square, [h_.d], [junk5.d, s5.d], accum=s5.ap[:, 0:1])
        rstd_from_ss(s5.ap[:, 0:1], s5.ap[:, 1:2], s5.ap[:, 2:3], 2048.0, s5.d, s5.d, s5.d)
        f_ = ff[k]
        STT("dve", f_.ap, h_.ap, s5.ap[:, 2:3], A2.ap, ALU.mult, ALU.mult, [h_.d, s5.d, A2.d], [f_.d])
        TT("pool", f_.ap, f_.ap, B2.ap, ALU.add, [f_.d, B2.d], [f_.d])
        ACT(fb[k].ap, f_.ap, AF.Copy, [f_.d], [fb[k].d])
        DMA("sp", f_d.ap[t * 128:(t + 1) * 128, :], fb[k].ap, [fb[k].d], [f_d.d])
        for q in range(4):
            pb = PS[q % 4]
            for i in range(4):
                kc = 4 * q + i
                TR(pb.ap[:, i * 128:(i + 1) * 128], f_.ap[:, kc * 128:(kc + 1) * 128], ident32.ap, [f_.d, ident32.d], [pb.d])
            CP("act" if q % 2 == 0 else "dve", fT[k].ap[:, 4 * q:4 * q + 4, :], pb.ap.rearrange("p (a b) -> p a b", b=128),
               [pb.d], [fT[k].d])
        pl = PS[4 + k]
        for kc in range(16):
            MM(pl.ap[:, 0:16], fT[k].ap[:, kc, :], wr.ap[:, kc, :], kc == 0, kc == 15, [fT[k].d, wr.d], [pl.d])
        P.emit("dve", (lambda o, i: (lambda e: e.reduce_max(out=o, in_=i, axis=mybir.AxisListType.X)))(s5.ap[:, 3:4], pl.ap[:, 0:16]),
               [pl.d], [s5.d])
        TS("dve", s5.ap[:, 4:5], s5.ap[:, 3:4], -1.0, None, ALU.mult, None, [s5.d], [s5.d])
        ACT(ex[k].ap, pl.ap[:, 0:16], AF.Exp, [pl.d, s5.d], [ex[k].d, s5.d], bias=s5.ap[:, 4:5], accum=s5.ap[:, 5:6])
        RECIP(s5.ap[:, 6:7], s5.ap[:, 5:6], [s5.d], [s5.d])
        TS("dve", ex[k].ap, ex[k].ap, s5.ap[:, 6:7], None, ALU.mult, None, [ex[k].d, s5.d], [ex[k].d])
        pa = PS[6 + k]
        TR(pa.ap[0:16, 0:128], ex[k].ap, ident32.ap, [ex[k].d, ident32.d], [pa.d])
        CP("act", affT.ap[:, t * 128:(t + 1) * 128], pa.ap[0:16, 0:128], [pa.d], [affT.d])
    if DEBUG:
        DMA("sp", dbg["aff"], affT.ap, [affT.d], [])
    precast(0, 36)
    vals = T(AR.alloc([16, 512], F32))
    idxu = T(AR.alloc([16, 512], U32))
    idxf = T(AR.alloc([16, 512], F32))
    for r in range(64):
        v8 = vals.ap[:, r * 8:(r + 1) * 8]
        i8 = idxu.ap[:, r * 8:(r + 1) * 8]
        P.emit("dve", (lambda o, i: (lambda e: e.max(out=o, in_=i)))(v8, affT.ap), [affT.d], [vals.d])
        P.emit("dve", (lambda o, m, i: (lambda e: e.max_index(out=o, in_max=m, in_values=i)))(i8, v8, affT.ap),
               [affT.d, vals.d], [idxu.d])
        P.emit("dve", (lambda o, m, i: (lambda e: e.match_replace(out=o, in_to_replace=m, in_values=i, imm_value=-1.0)))(affT.ap, v8, affT.ap),
               [vals.d, idxu.d], [affT.d])
    CP("dve", idxf.ap, idxu.ap, [idxu.d], [idxf.d])
    if DEBUG:
        DMA("sp", dbg["vals"], vals.ap, [vals.d], [])
        DMA("sp", dbg["idx"], idxu.ap, [idxu.d], [])
    for s in range(4):
        pa = PS[s % 2]
        TR(pa.ap[:, 0:16], idxf.ap[:, s * 128:(s + 1) * 128], ident32.ap[0:16, 0:16], [idxf.d, ident32.d], [pa.d])
        TR(pa.ap[:, 16:32], vals.ap[:, s * 128:(s + 1) * 128], ident32.ap[0:16, 0:16], [vals.d, ident32.d], [pa.d])
        CP("dve", idxc.ap[:, s, :], pa.ap[:, 0:16], [pa.d], [idxc.d])
        CP("act", gatc.ap[:, s, :], pa.ap[:, 16:32], [pa.d], [gatc.d])
    P.barrier()
    if LIMIT == 5:
        return finish()
    AR.top = p5_mark

    g2b = T(AR.alloc([128, 2048], F32))
    load_mod_bc(g2b, 0, 5)
    NW = 5
    wbuf = [T(AR.alloc([128, 8192], BF16)) for _ in range(NW)]
    xg = [T(AR.alloc([128, 2048], BF16)) for _ in range(8)]
    xeT = [T(AR.alloc([128, 16, 512], BF16)) for _ in range(2)]
    hidT = [T(AR.alloc([128, 8, 512], BF16), 8) for _ in range(2)]
    silt = [T(AR.alloc([128, 512], BF16)) for _ in range(2)]
    yet = [T(AR.alloc([128, 512], F32)) for _ in range(2)]
    yo = [T(AR.alloc([128, 1024], F32)) for _ in range(2)]
    hall = Dep()
    piece_view = {}
    state6 = {"issued": 0}

    def ensure_issued(upto):
        while state6["issued"] < min(upto, len(srcs_all)):
            i = state6["issued"]
            src, kind = srcs_all[i]
            wb = wbuf[i % NW]
            dst = wb.ap.rearrange("p (a b) -> p a b", b=512 if kind == 0 else 1024)
            if i < NPRE:
                DMA("sp", wb.ap[:, 0:4096], wexp.ap[i][:, 0:4096], [wexp.ds[i]], [wb.d])
                DMA("sp", wb.ap[:, 4096:8192], wexp.ap[i][:, 4096:8192], [wexp.ds[i]], [wb.d])
            elif kind == 0:
                DMA("pool", dst[:, 0:8, :], src[:, 0:8, :], [], [wb.d])
                DMA("pool", dst[:, 8:16, :], src[:, 8:16, :], [], [wb.d])
            else:
                DMA("pool", dst[:, 0:4, :], src[:, 0:4, :], [], [wb.d])
                DMA("pool", dst[:, 4:8, :], src[:, 4:8, :], [], [wb.d])
            piece_view[i] = (wb, dst)
            state6["issued"] = i + 1

    def gather_issue(e_):
        for s in range(4):
            xg_ = xg[(e_ % 2) * 4 + s]
            P.emit("pool", (lambda o, i, ix: (lambda e: e.indirect_dma_start(
                out=o, out_offset=None, in_=i, in_offset=bass.IndirectOffsetOnAxis(ap=ix, axis=0))))(
                    xg_.ap, f_d.ap, idxc.ap[:, s, e_:e_ + 1]), [f_d.d, idxc.d], [xg_.d], dma=True)

    yi = 0
    pgi = 0
    gather_issue(0)
    ensure_issued(NW - 1)
    for e_ in range(16):
        xe = xeT[e_ % 2]
        for s in range(4):
            xg_ = xg[(e_ % 2) * 4 + s]
            for q in range(4):
                pv = psbf(6 + (q % 2))
                pb = PS[6 + (q % 2)]
                for i in range(4):
                    kc = 4 * q + i
                    TR(pv[:, i * 128:(i + 1) * 128], xg_.ap[:, kc * 128:(kc + 1) * 128], identbf.ap, [xg_.d, identbf.d], [pb.d])
                CP("act" if q % 2 == 0 else "dve", xe.ap[:, 4 * q:4 * q + 4, s * 128:(s + 1) * 128],
                   pv[:, 0:512].rearrange("p (a b) -> p a b", b=128), [pb.d], [xe.d])
        if e_ + 1 < 16:
            gather_issue(e_ + 1)
        hT = hidT[e_ % 2]
        p0 = 6 * e_
        for fcu in range(8):
            if fcu % 4 == 0:
                ensure_issued(p0 + 2 * (fcu // 4) + NW)
            wgp, wgv = piece_view[p0 + 0 + 2 * (fcu // 4)]
            wup, wuv = piece_view[p0 + 1 + 2 * (fcu // 4)]
            off = (fcu % 4) * 128
            k = pgi % 2
            pgi += 1
            pg, pu = PS[0 + 2 * k], PS[1 + 2 * k]
            for kc in range(16):
                MM(pg.ap, wgv[:, kc, off:off + 128], xe.ap[:, kc, :], kc == 0, kc == 15, [wgp.d, xe.d], [pg.d])
            for kc in range(16):
                MM(pu.ap, wuv[:, kc, off:off + 128], xe.ap[:, kc, :], kc == 0, kc == 15, [wup.d, xe.d], [pu.d])
            ACT(silt[k].ap, pg.ap, AF.Silu, [pg.d], [silt[k].d])
            TT("dve", hT.ap[:, fcu, :], silt[k].ap, pu.ap, ALU.mult, [silt[k].d, pu.d], [hT.ds[fcu]])
        for dh in range(2):
            ensure_issued(p0 + 4 + dh + NW)
            wdp, wdv = piece_view[p0 + 4 + dh]
            for s in range(4):
                yo_ = yo[yi % 2]
                yi += 1
                for dq in range(2):
                    k = pgi % 2
                    pgi += 1
                    py = PS[4 + k]
                    for fc in range(8):
                        MM(py.ap, hT.ap[:, fc, s * 128:(s + 1) * 128], wdv[:, fc, dq * 512:(dq + 1) * 512], fc == 0, fc == 7,
                           [hT.ds[fc], wdp.d], [py.d])
                    ACT(yet[k].ap, py.ap, AF.Identity, [py.d, gatc.d], [yet[k].d], scale=gatc.ap[:, s, e_:e_ + 1])
                    gc = slice(dh * 1024 + dq * 512, dh * 1024 + (dq + 1) * 512)
                    TT("dve", yo_.ap[:, dq * 512:(dq + 1) * 512], yet[k].ap, g2b.ap[:, gc], ALU.mult,
                       [yet[k].d, g2b.d], [yo_.d])
                P.emit("pool", (lambda o, i, ix: (lambda e: e.indirect_dma_start(
                    out=o, out_offset=bass.IndirectOffsetOnAxis(ap=ix, axis=0), in_=i, in_offset=None, compute_op=ALU.add)))(
                        hacc_h[dh].ap, yo_.ap, idxc.ap[:, s, e_:e_ + 1]),
                    [yo_.d, idxc.d], [hall], dma=True)
    P.barrier()
    if LIMIT == 6:
        return finish()
    AR.top = persist_mark

    fnb = T(AR.alloc([128, 2048], F32))
    load_vec_bc(fnb, nfin)
    hx = [T(AR.alloc([128, 2048], F32)) for _ in range(2)]
    ob = [T(AR.alloc([128, 2048], F32)) for _ in range(2)]
    st7 = [T(AR.alloc([128, 4], F32)) for _ in range(2)]
    junk7 = T(AR.alloc([128, 2048], BF16))
    def p7_load(t_):
        for i2 in range(2):
            DMA("sp", hx[t_ % 2].ap[:, i2 * 1024:(i2 + 1) * 1024], hacc_h[i2].ap[t_ * 128:(t_ + 1) * 128, :], [hall], [hx[t_ % 2].d])

    p7_load(0)
    for t in range(NTL):
        k = t % 2
        if t + 1 < NTL:
            p7_load(t + 1)
        s7 = st7[k]
        MEMSET("pool", s7.ap, 0.0, [s7.d])
        ACT(junk7.ap, hx[k].ap, AF.Square, [hx[k].d], [junk7.d, s7.d], accum=s7.ap[:, 0:1])
        rstd_from_ss(s7.ap[:, 0:1], s7.ap[:, 1:2], s7.ap[:, 2:3], 2048.0, s7.d, s7.d, s7.d)
        STT("dve", ob[k].ap, hx[k].ap, s7.ap[:, 2:3], fnb.ap, ALU.mult, ALU.mult, [hx[k].d, s7.d, fnb.d], [ob[k].d])
        DMA("sp", out[t], ob[k].ap, [ob[k].d], [])
    P.barrier()
    P.finalize(st)
    st.close()
    return nc


def _kc_p(w):
    K, N = w.shape
    return np.ascontiguousarray(w.reshape(K // 128, 128, N).transpose(1, 0, 2))


def _prep_shared(c_ctx, w_mod, b_mod, norm_mix_w, w_in, ssm_conv_w, ssm_conv_b, ssm_a_log, ssm_dt_bias, ssm_d,
                 ssm_norm_w, sc_conv_w, sc_norm_w, w_out, norm_ffn_w, w_router, w_gate, w_up, w_down, final_norm_w):
    f = np.float32
    sh = {}
    sh["w_mod"] = np.ascontiguousarray(w_mod[0].reshape(128, 16, 12288))
    sh["b_mod"] = np.ascontiguousarray(b_mod[0].reshape(1, 12288))
    sh["nmix"] = norm_mix_w[0].reshape(1, 2048)
    sh["nffn"] = norm_ffn_w[0].reshape(1, 2048)
    sh["nfin"] = final_norm_w.reshape(1, 2048)
    win = w_in[0]
    wg, cw, cb = [], [], []
    for g in range(4):
        cols = np.concatenate([2048 + g * 512 + np.arange(512), 4096 + g * 128 + np.arange(128),
                               4608 + g * 128 + np.arange(128), g * 512 + np.arange(512),
                               5120 + g * 8 + np.arange(8), 5152 + g * 8 + np.arange(8)])
        wg.append(_kc_p(win[:, cols]))
        ch = np.concatenate([g * 512 + np.arange(512), 2048 + g * 128 + np.arange(128), 2560 + g * 128 + np.arange(128)])
        cw.append(ssm_conv_w[0][:, ch].T.reshape(6, 128, 3).transpose(1, 0, 2))
        cb.append(ssm_conv_b[0][ch].reshape(6, 128).T)
    sh["wg_ssm"] = np.ascontiguousarray(np.stack(wg)).astype(f)
    sh["cw_ssm"] = np.ascontiguousarray(np.stack(cw)).astype(f)
    sh["cb_ssm"] = np.ascontiguousarray(np.stack(cb)).astype(f)
    wsc, cws, scw = [], [], []
    for j in range(4):
        ch = j * 512 + np.arange(512)
        cols = np.concatenate([5184 + ch, 7232 + ch, 9280 + ch])
        wsc.append(_kc_p(win[:, cols]))
        cws.append(sc_conv_w[0][:, ch].T.reshape(4, 128, 3).transpose(1, 0, 2))
        scw.append(sc_norm_w[0][ch].reshape(4, 128).T)
    sh["wg_sc"] = np.ascontiguousarray(np.stack(wsc)).astype(f)
    sh["cw_sc"] = np.ascontiguousarray(np.stack(cws)).astype(f)
    sh["scw"] = np.ascontiguousarray(np.stack(scw)).astype(f)
    hsel = np.stack([np.concatenate([g * 8 + np.arange(8), 32 + g * 8 + np.arange(8)]) for g in range(4)])
    sh["dtb"] = np.ascontiguousarray(ssm_dt_bias[0][hsel].reshape(4, 1, 16))
    sh["alog"] = np.ascontiguousarray(ssm_a_log[0][hsel].reshape(4, 1, 16))
    sh["dsk"] = np.ascontiguousarray(np.repeat(ssm_d[0].reshape(4, 8), 64, axis=1).reshape(4, 1, 512))
    sh["ssm_nw"] = np.ascontiguousarray(ssm_norm_w[0].reshape(4, 1, 512))
    wo = _kc_p(w_out[0])
    sh["w_out"] = np.ascontiguousarray(np.stack([wo[:, :, 0:1024], wo[:, :, 1024:2048]]))
    sh["w_rt"] = _kc_p(w_router[0])
    wgt = w_gate[0].reshape(16, 16, 128, 2, 512).transpose(0, 3, 2, 1, 4)
    wup = w_up[0].reshape(16, 16, 128, 2, 512).transpose(0, 3, 2, 1, 4)
    wdn = w_down[0].reshape(16, 8, 128, 2, 1024).transpose(0, 3, 2, 1, 4)
    sh["w_gate"] = np.ascontiguousarray(wgt)
    sh["w_up"] = np.ascontiguousarray(wup)
    sh["w_down"] = np.ascontiguousarray(wdn)
    sh["c_ident"] = np.eye(128, dtype=f)
    k = np.arange(128)[:, None]
    i = np.arange(128)[None, :]
    sh["c_tri"] = np.stack([(k <= i), (k >= i)]).astype(f)
    mf = np.where(i < k, NEG, 0.0).astype(f)
    mb = np.where(i > k, NEG, 0.0).astype(f)
    sh["c_mask"] = np.stack([np.tile(mf, (1, 4)), np.tile(mb, (1, 4))]).astype(f)
    return sh


_CACHE = {}


def kernel(x, c, ctx, c_ctx, w_mod, b_mod, norm_mix_w, w_in, ssm_conv_w, ssm_conv_b, ssm_a_log, ssm_dt_bias, ssm_d,
           ssm_norm_w, sc_conv_w, sc_norm_w, w_out, norm_ffn_w, w_router, w_gate, w_up, w_down, final_norm_w):
    args = [np.asarray(a, dtype=np.float32) for a in (
        c_ctx, w_mod, b_mod, norm_mix_w, w_in, ssm_conv_w, ssm_conv_b, ssm_a_log, ssm_dt_bias, ssm_d, ssm_norm_w,
        sc_conv_w, sc_norm_w, w_out, norm_ffn_w, w_router, w_gate, w_up, w_down, final_norm_w)]
    x = np.asarray(x, dtype=np.float32)
    c = np.asarray(c, dtype=np.float32)
    ctx = np.asarray(ctx, dtype=np.float32)
    sh = _prep_shared(*args)
    if "nc" not in _CACHE:
        _CACHE["nc"] = build_program()
    nc = _CACHE["nc"]
    in_maps = []
    for core in range(NCORES):
        b = core % 4
        m = dict(sh)
        m["xin"] = np.ascontiguousarray(np.concatenate([ctx[b], x[b]], axis=0).reshape(NT, 128, 2048))
        m["cvec"] = np.ascontiguousarray(np.stack([c[b], args[0]], axis=-1).reshape(128, 16, 2))
        in_maps.append(m)
    res = run_bass_kernel_spmd(nc, in_maps, core_ids=list(range(NCORES)))
    _CACHE["res"] = res
    outs = [res.results[b % NCORES]["out"].reshape(4096, 2048) for b in range(4)]
    return np.stack(outs).astype(np.float32)
```
